# Optimizing a Trainium2 kernel written in Bass

```python
import math
import jax
import jax.numpy as jnp
from jax import lax
import numpy as np

D_MODEL = 1024
BATCH = 8
SEQ = 2048
DEPTH = 2

F32 = jnp.float32
GRID_W = 64
CTX_LEN = 256
NORM_EPS = 1e-6

S5_WIDTH = D_MODEL // 4
S5_GROUP_CH = 16
S5_GROUPS = S5_WIDTH // S5_GROUP_CH
S5_STATE = 64

MLA_HEADS = D_MODEL // 128
MLA_NOPE = 64
MLA_ROPE = 32
MLA_V = 64
MLA_Q_LORA = D_MODEL // 4
MLA_KV_LORA = D_MODEL // 8
MLA_Q_BLOCK = 128
MLA_SCALE = 1.0 / math.sqrt(MLA_NOPE + MLA_ROPE)
ROPE_PAIRS = MLA_ROPE // 4
ROPE_BASE = 10000.0

HG_HEADS = D_MODEL // 256
HG_K = 128
HG_V = 64
HG_CHUNK = 64

MOE_GROUPS = 4
MOE_EXPERTS_PER_GROUP = 8
MOE_EXPERTS = MOE_GROUPS * MOE_EXPERTS_PER_GROUP
MOE_TOP_K = 2
MOE_HIDDEN = D_MODEL // 2
MOE_BLOCK = 128

IN_SPLITS = (S5_WIDTH, MLA_Q_LORA, MLA_KV_LORA, MLA_ROPE,
             HG_HEADS * HG_K, HG_HEADS * HG_K, HG_HEADS * HG_K,
             HG_HEADS * HG_V, HG_HEADS * HG_V,
             D_MODEL, D_MODEL, D_MODEL)
IN_COLS = sum(IN_SPLITS)

kernel_name = 'hybrid_s5_mla_hgrn2_hmoe_dit'


def rms_norm(x, g):
    xf = x.astype(F32)
    y = xf * lax.rsqrt(jnp.mean(xf * xf, axis=-1, keepdims=True) + NORM_EPS)
    return (y * g.astype(F32)).astype(x.dtype)


def modulate(h, shift, scale):
    return h * (1 + scale) + shift


def split_columns(z):
    cuts = [int(v) for v in np.cumsum(IN_SPLITS)[:-1]]
    return jnp.split(z, cuts, axis=-1)


def rev(a, direction, axis):
    return jnp.flip(a, axis=axis) if direction == 1 else a


def axial_rope_angles(n_tokens):
    rows = n_tokens // GRID_W
    row = jnp.repeat(jnp.arange(rows, dtype=F32), GRID_W)
    col = jnp.tile(jnp.arange(GRID_W, dtype=F32), rows)
    inv = ROPE_BASE ** (-jnp.arange(ROPE_PAIRS, dtype=F32) / ROPE_PAIRS)
    ang = jnp.stack([row[:, None] * inv, col[:, None] * inv], axis=1)
    return jnp.cos(ang), jnp.sin(ang)


def apply_axial_rope(x, cos, sin):
    shp = x.shape
    xr = x.astype(F32).reshape(shp[:-1] + (2, 2, ROPE_PAIRS))
    x1, x2 = xr[..., 0, :], xr[..., 1, :]
    out = jnp.stack([x1 * cos - x2 * sin, x1 * sin + x2 * cos], axis=-2)
    return out.reshape(shp).astype(x.dtype)


def s5_discretise(lam_re, lam_im, b_re, b_im, log_step):
    lam_re, lam_im = lam_re.astype(F32), lam_im.astype(F32)
    b_re, b_im = b_re.astype(F32), b_im.astype(F32)
    dt = jnp.exp(log_step.astype(F32))[:, None]
    mag = jnp.exp(lam_re * dt)
    lb_re, lb_im = mag * jnp.cos(lam_im * dt), mag * jnp.sin(lam_im * dt)
    den = lam_re * lam_re + lam_im * lam_im
    fr = ((lb_re - 1) * lam_re + lb_im * lam_im) / den
    fi = (lb_im * lam_re - (lb_re - 1) * lam_im) / den
    bb_re = fr[..., None] * b_re - fi[..., None] * b_im
    bb_im = fr[..., None] * b_im + fi[..., None] * b_re
    return lb_re, lb_im, bb_re, bb_im


def s5_states(u, lb_re, lb_im, bb_re, bb_im, h0_re, h0_im):
    bu_re = jnp.einsum('blgh,gph->blgp', u, bb_re)
    bu_im = jnp.einsum('blgh,gph->blgp', u, bb_im)
    bu_re = bu_re.at[:, 0].add(lb_re * h0_re - lb_im * h0_im)
    bu_im = bu_im.at[:, 0].add(lb_re * h0_im + lb_im * h0_re)
    a_re = jnp.broadcast_to(lb_re, bu_re.shape)
    a_im = jnp.broadcast_to(lb_im, bu_im.shape)

    def combine(e1, e2):
        a1r, a1i, b1r, b1i = e1
        a2r, a2i, b2r, b2i = e2
        return (a2r * a1r - a2i * a1i, a2r * a1i + a2i * a1r,
                a2r * b1r - a2i * b1i + b2r, a2r * b1i + a2i * b1r + b2i)

    _, _, h_re, h_im = lax.associative_scan(combine, (a_re, a_im, bu_re, bu_im), axis=1)
    return h_re, h_im


def s5_readout(h_re, h_im, c_re, c_im):
    return jnp.einsum('blgp,ghp->blgh', h_re, c_re) - jnp.einsum('blgp,ghp->blgh', h_im, c_im)


def s5_mixer(u_lat, u_ctx, lam_re, lam_im, b_re, b_im, c_re, c_im, log_step, d_skip,
             glu_w, glu_b, need_ctx):
    def grouped(u):
        return u.astype(F32).reshape(u.shape[0], u.shape[1], S5_GROUPS, S5_GROUP_CH)

    ul, uc = grouped(u_lat), grouped(u_ctx)
    zero = jnp.zeros((ul.shape[0], S5_GROUPS, S5_STATE), F32)
    y_lat, y_ctx = 0.0, 0.0
    for d in range(2):
        lb_re, lb_im, bb_re, bb_im = s5_discretise(lam_re[d], lam_im[d], b_re[d], b_im[d], log_step[d])
        cr, ci = c_re[d].astype(F32), c_im[d].astype(F32)
        hc_re, hc_im = s5_states(rev(uc, d, 1), lb_re, lb_im, bb_re, bb_im, zero, zero)
        hl_re, hl_im = s5_states(rev(ul, d, 1), lb_re, lb_im, bb_re, bb_im, hc_re[:, -1], hc_im[:, -1])
        y_lat = y_lat + rev(s5_readout(hl_re, hl_im, cr, ci), d, 1)
        if need_ctx:
            y_ctx = y_ctx + rev(s5_readout(hc_re, hc_im, cr, ci), d, 1)

    def finish(y, u, like):
        y = jax.nn.gelu(y + d_skip.astype(F32).reshape(S5_GROUPS, S5_GROUP_CH) * u).reshape(like.shape)
        y = y * jax.nn.sigmoid(y @ glu_w.astype(F32) + glu_b.astype(F32))
        return y.astype(like.dtype)

    return finish(y_lat, ul, u_lat), (finish(y_ctx, uc, u_ctx) if need_ctx else None)


def mla_queries(cq, qa_g, w_uq, cos, sin):
    n_b, n_t, _ = cq.shape
    q = (rms_norm(cq, qa_g) @ w_uq).reshape(n_b, n_t, MLA_HEADS, MLA_NOPE + MLA_ROPE)
    q_nope, q_pe = q[..., :MLA_NOPE], q[..., MLA_NOPE:]
    if cos is not None:
        q_pe = apply_axial_rope(q_pe, cos[:, None], sin[:, None])
    return q_nope, q_pe


def mla_keys_values(ckv, k_pe, kva_g, w_uk, w_uv, cos, sin):
    n_b, n_t, _ = ckv.shape
    ckv = rms_norm(ckv, kva_g)
    k_nope = (ckv @ w_uk).reshape(n_b, n_t, MLA_HEADS, MLA_NOPE)
    v = (ckv @ w_uv).reshape(n_b, n_t, MLA_HEADS, MLA_V)
    if cos is not None:
        k_pe = apply_axial_rope(k_pe, cos, sin)
    return k_nope, k_pe, v


def mla_attend(q_nope, q_pe, k_nope, k_pe, v):
    s = (jnp.einsum('bqhd,bkhd->bhqk', q_nope, k_nope)
         + jnp.einsum('bqhr,bkr->bhqk', q_pe, k_pe)).astype(F32)
    p = jax.nn.softmax(s * MLA_SCALE, axis=-1).astype(v.dtype)
    return jnp.einsum('bhqk,bkhd->bqhd', p, v)


def mla_attend_blocked(q_nope, q_pe, k_nope, k_pe, v):
    n_b, n_t = q_nope.shape[:2]
    n_blk = n_t // MLA_Q_BLOCK

    def blocks(a):
        return jnp.moveaxis(a.reshape((n_b, n_blk, MLA_Q_BLOCK) + a.shape[2:]), 1, 0)

    out = lax.map(lambda qb: mla_attend(qb[0], qb[1], k_nope, k_pe, v), (blocks(q_nope), blocks(q_pe)))
    return jnp.moveaxis(out, 0, 1).reshape(n_b, n_t, MLA_HEADS * MLA_V)


def hgrn2_chunk_scan(q, k, v, log_f, s0):
    n_b, n_h, n_t, _ = q.shape
    d_v = v.shape[-1]
    n_c = n_t // HG_CHUNK

    def chunks(a):
        return jnp.moveaxis(a.reshape(n_b, n_h, n_c, HG_CHUNK, a.shape[-1]), 2, 0)

    lower = jnp.tril(jnp.ones((HG_CHUNK, HG_CHUNK), bool))[:, :, None]

    def step(state, inp):
        qc, kc, vc, gc = inp
        b = jnp.cumsum(gc, axis=2)
        o_inter = jnp.einsum('bhtk,bhkv->bhtv', qc * jnp.exp(b), state)
        decay = jnp.exp(jnp.where(lower, b[:, :, :, None, :] - b[:, :, None, :, :], -jnp.inf))
        scores = jnp.einsum('bhtk,bhsk,bhtsk->bhts', qc, kc, decay)
        o = o_inter + jnp.einsum('bhts,bhsv->bhtv', scores, vc)
        b_last = b[:, :, -1:, :]
        state = (jnp.exp(b_last[:, :, 0, :, None]) * state
                 + jnp.einsum('bhsk,bhsv->bhkv', kc * jnp.exp(b_last - b), vc))
        return state, o

    state, o = lax.scan(step, s0, (chunks(q), chunks(k), chunks(v), chunks(log_f)))
    return jnp.moveaxis(o, 0, 2).reshape(n_b, n_h, n_t, d_v), state


def hgrn2_mixer(parts_lat, parts_ctx, lb, norm_g, need_ctx):
    def heads(a, dh):
        return a.astype(F32).reshape(a.shape[0], a.shape[1], HG_HEADS, dh).transpose(0, 2, 1, 3)

    def forget(z):
        log_f = jnp.logaddexp(jnp.log(lb), jnp.log1p(-lb) + jax.nn.log_sigmoid(heads(z, HG_K)))
        return -jnp.expm1(log_f), log_f

    q_l, i_l = heads(parts_lat[0], HG_K), heads(parts_lat[3], HG_V)
    q_c, i_c = heads(parts_ctx[0], HG_K), heads(parts_ctx[3], HG_V)
    state0 = jnp.zeros((q_l.shape[0], HG_HEADS, HG_K, HG_V), F32)
    o_lat, o_ctx = 0.0, 0.0
    for d in range(2):
        k_c, lf_c = forget(parts_ctx[1 + d])
        k_l, lf_l = forget(parts_lat[1 + d])
        oc, s_ctx = hgrn2_chunk_scan(rev(q_c, d, 2), rev(k_c, d, 2), rev(i_c, d, 2), rev(lf_c, d, 2), state0)
        ol, _ = hgrn2_chunk_scan(rev(q_l, d, 2), rev(k_l, d, 2), rev(i_l, d, 2), rev(lf_l, d, 2), s_ctx)
        o_lat = o_lat + rev(ol, d, 2)
        if need_ctx:
            o_ctx = o_ctx + rev(oc, d, 2)

    def finish(o, g):
        o = o.transpose(0, 2, 1, 3)
        o = o * lax.rsqrt(jnp.mean(o * o, axis=-1, keepdims=True) + NORM_EPS)
        o = o * norm_g.astype(F32).reshape(HG_HEADS, HG_V) * jax.nn.silu(g.astype(F32)).reshape(o.shape)
        return o.reshape(g.shape).astype(g.dtype)

    return finish(o_lat, parts_lat[4]), (finish(o_ctx, parts_ctx[4]) if need_ctx else None)


def merge_branches(gates, y_s5, y_mla, y_hg, w_pa, w_pb, w_pc, w_out):
    g_a, g_b, g_c = gates
    merged = (jax.nn.sigmoid(g_a) * (y_s5 @ w_pa)
              + jax.nn.sigmoid(g_b) * (y_mla @ w_pb)
              + jax.nn.sigmoid(g_c) * (y_hg @ w_pc))
    return merged @ w_out


def hier_moe(h, w_group, b_group, w_expert, b_expert, w1, w3, w2):
    n_tok = h.shape[0]
    hf = h.astype(F32)
    g_prob = jax.nn.softmax(hf @ w_group.astype(F32) + b_group.astype(F32), axis=-1)
    g_w, g_idx = lax.top_k(g_prob, 1)
    e_logits = (hf @ w_expert.astype(F32) + b_expert.astype(F32)).reshape(
        n_tok, MOE_GROUPS, MOE_EXPERTS_PER_GROUP)
    e_logits = jnp.take_along_axis(e_logits, g_idx[:, :, None], axis=1)[:, 0]
    top_v, top_i = lax.top_k(e_logits, MOE_TOP_K)
    gate = jax.nn.softmax(top_v, axis=-1) * g_w
    expert = g_idx * MOE_EXPERTS_PER_GROUP + top_i

    n_assign = n_tok * MOE_TOP_K
    flat_e = expert.reshape(n_assign)
    order = jnp.argsort(flat_e)
    s_exp = flat_e[order]
    s_tok = order // MOE_TOP_K
    s_gate = gate.reshape(n_assign)[order]
    counts = jnp.bincount(flat_e, length=MOE_EXPERTS)
    starts = jnp.cumsum(counts) - counts
    padded = (counts + MOE_BLOCK - 1) // MOE_BLOCK * MOE_BLOCK
    p_ends = jnp.cumsum(padded)
    dest = (p_ends - padded)[s_exp] + jnp.arange(n_assign) - starts[s_exp]
    n_blocks = (n_assign + MOE_EXPERTS * (MOE_BLOCK - 1) + MOE_BLOCK - 1) // MOE_BLOCK
    buf = jnp.zeros((n_blocks * MOE_BLOCK, h.shape[1]), h.dtype).at[dest].set(h[s_tok])
    block_expert = jnp.minimum(
        jnp.searchsorted(p_ends, jnp.arange(n_blocks) * MOE_BLOCK, side='right'), MOE_EXPERTS - 1)

    def expert_block(args):
        xb, e = args
        return (jax.nn.silu(xb @ w1[e]) * (xb @ w3[e])) @ w2[e]

    y = lax.map(expert_block, (buf.reshape(n_blocks, MOE_BLOCK, -1), block_expert))
    y = y.reshape(n_blocks * MOE_BLOCK, -1)[dest].astype(F32) * s_gate[:, None]
    return jnp.zeros(h.shape, F32).at[s_tok].add(y).astype(h.dtype)


def setup_inputs(seed: int = 0) -> dict:
    key = jax.random.key(seed)
    ks = iter(jax.random.split(key, 48))

    def nrm(shape, scale):
        return jax.random.normal(next(ks), shape, F32) * scale

    L, G, P, H5, E = DEPTH, S5_GROUPS, S5_STATE, S5_GROUP_CH, MOE_EXPERTS
    n_idx = jnp.arange(S5_STATE, dtype=F32)
    return {
        'x': nrm((BATCH, SEQ, D_MODEL), 1.0),
        'c': nrm((BATCH, D_MODEL), 1.0),
        'ctx': nrm((BATCH, CTX_LEN, D_MODEL), 1.0),
        'c_ctx': nrm((D_MODEL,), 1.0),
        'mod_w': nrm((L, D_MODEL, 6 * D_MODEL), 0.5 * D_MODEL ** -0.5),
        'mod_b': nrm((L, 6 * D_MODEL), 0.01),
        'norm1_g': 1.0 + nrm((L, D_MODEL), 0.05),
        'norm2_g': 1.0 + nrm((L, D_MODEL), 0.05),
        'w_in': nrm((L, D_MODEL, IN_COLS), D_MODEL ** -0.5),
        's5_lam_re': -0.5 + nrm((L, 2, G, P), 0.01),
        's5_lam_im': math.pi * n_idx + nrm((L, 2, G, P), 0.01),
        's5_b_re': nrm((L, 2, G, P, H5), (2 * H5) ** -0.5),
        's5_b_im': nrm((L, 2, G, P, H5), (2 * H5) ** -0.5),
        's5_c_re': nrm((L, 2, G, H5, P), 0.5),
        's5_c_im': nrm((L, 2, G, H5, P), 0.5),
        's5_log_step': jax.random.uniform(next(ks), (L, 2, G), F32, math.log(1e-3), math.log(1e-1)),
        's5_d': nrm((L, S5_WIDTH), 1.0),
        's5_glu_w': nrm((L, S5_WIDTH, S5_WIDTH), S5_WIDTH ** -0.5),
        's5_glu_b': nrm((L, S5_WIDTH), 0.01),
        'mla_qa_g': 1.0 + nrm((L, MLA_Q_LORA), 0.05),
        'mla_kva_g': 1.0 + nrm((L, MLA_KV_LORA), 0.05),
        'mla_w_uq': nrm((L, MLA_Q_LORA, MLA_HEADS * (MLA_NOPE + MLA_ROPE)), MLA_Q_LORA ** -0.5),
        'mla_w_uk': nrm((L, MLA_KV_LORA, MLA_HEADS * MLA_NOPE), MLA_KV_LORA ** -0.5),
        'mla_w_uv': nrm((L, MLA_KV_LORA, MLA_HEADS * MLA_V), MLA_KV_LORA ** -0.5),
        'hg_lb_logits': nrm((L,), 1.0),
        'hg_norm_g': 1.0 + nrm((L, HG_HEADS * HG_V), 0.05),
        'w_pa': nrm((L, S5_WIDTH, D_MODEL), S5_WIDTH ** -0.5),
        'w_pb': nrm((L, MLA_HEADS * MLA_V, D_MODEL), (MLA_HEADS * MLA_V) ** -0.5),
        'w_pc': nrm((L, HG_HEADS * HG_V, D_MODEL), (HG_HEADS * HG_V) ** -0.5),
        'w_out': nrm((L, D_MODEL, D_MODEL), D_MODEL ** -0.5),
        'moe_w_group': nrm((L, D_MODEL, MOE_GROUPS), D_MODEL ** -0.5),
        'moe_b_group': nrm((L, MOE_GROUPS), 0.01),
        'moe_w_expert': nrm((L, D_MODEL, E), D_MODEL ** -0.5),
        'moe_b_expert': nrm((L, E), 0.01),
        'moe_w1': nrm((L, E, D_MODEL, MOE_HIDDEN), D_MODEL ** -0.5),
        'moe_w3': nrm((L, E, D_MODEL, MOE_HIDDEN), D_MODEL ** -0.5),
        'moe_w2': nrm((L, E, MOE_HIDDEN, D_MODEL), MOE_HIDDEN ** -0.5),
        'final_norm_g': 1.0 + nrm((D_MODEL,), 0.05),
    }


def reference(x, c, ctx, c_ctx, mod_w, mod_b, norm1_g, norm2_g, w_in,
              s5_lam_re, s5_lam_im, s5_b_re, s5_b_im, s5_c_re, s5_c_im, s5_log_step, s5_d,
              s5_glu_w, s5_glu_b,
              mla_qa_g, mla_kva_g, mla_w_uq, mla_w_uk, mla_w_uv,
              hg_lb_logits, hg_norm_g,
              w_pa, w_pb, w_pc, w_out,
              moe_w_group, moe_b_group, moe_w_expert, moe_b_expert, moe_w1, moe_w3, moe_w2,
              final_norm_g):
    n_b, n_lat, _ = x.shape
    cos, sin = axial_rope_angles(n_lat)
    lb_all = jnp.cumsum(jax.nn.softmax(hg_lb_logits.astype(F32)))
    lb_all = lb_all - lb_all[0]
    silu_c = jax.nn.silu(c)
    silu_cc = jax.nn.silu(c_ctx)
    x_lat, x_ctx = x, ctx
    for layer in range(DEPTH):
        need_ctx = layer < DEPTH - 1
        mod_l = jnp.split((silu_c @ mod_w[layer] + mod_b[layer])[:, None, :], 6, axis=-1)
        mod_c = jnp.split(silu_cc @ mod_w[layer] + mod_b[layer], 6, axis=-1)

        h_lat = modulate(rms_norm(x_lat, norm1_g[layer]), mod_l[0], mod_l[1])
        h_ctx = modulate(rms_norm(x_ctx, norm1_g[layer]), mod_c[0], mod_c[1])
        z_lat = split_columns(h_lat @ w_in[layer])
        z_ctx = split_columns(h_ctx @ w_in[layer])

        s5_lat, s5_ctx = s5_mixer(z_lat[0], z_ctx[0], s5_lam_re[layer], s5_lam_im[layer],
                                  s5_b_re[layer], s5_b_im[layer], s5_c_re[layer], s5_c_im[layer],
                                  s5_log_step[layer], s5_d[layer], s5_glu_w[layer], s5_glu_b[layer],
                                  need_ctx)

        kn_l, kp_l, v_l = mla_keys_values(z_lat[2], z_lat[3], mla_kva_g[layer], mla_w_uk[layer],
                                          mla_w_uv[layer], cos, sin)
        kn_c, kp_c, v_c = mla_keys_values(z_ctx[2], z_ctx[3], mla_kva_g[layer], mla_w_uk[layer],
                                          mla_w_uv[layer], None, None)
        qn_l, qp_l = mla_queries(z_lat[1], mla_qa_g[layer], mla_w_uq[layer], cos, sin)
        mla_lat = mla_attend_blocked(qn_l, qp_l,
                                     jnp.concatenate([kn_l, kn_c], axis=1),
                                     jnp.concatenate([kp_l, kp_c], axis=1),
                                     jnp.concatenate([v_l, v_c], axis=1))

        hg_lat, hg_ctx = hgrn2_mixer(tuple(z_lat[4:9]), tuple(z_ctx[4:9]), lb_all[layer],
                                     hg_norm_g[layer], need_ctx)

        y_lat = merge_branches(z_lat[9:12], s5_lat, mla_lat, hg_lat,
                               w_pa[layer], w_pb[layer], w_pc[layer], w_out[layer])
        x_lat = x_lat + mod_l[2] * y_lat
        if need_ctx:
            qn_c, qp_c = mla_queries(z_ctx[1], mla_qa_g[layer], mla_w_uq[layer], None, None)
            mla_ctx = mla_attend(qn_c, qp_c, kn_c, kp_c, v_c).reshape(n_b, -1, MLA_HEADS * MLA_V)
            y_ctx = merge_branches(z_ctx[9:12], s5_ctx, mla_ctx, hg_ctx,
                                   w_pa[layer], w_pb[layer], w_pc[layer], w_out[layer])
            x_ctx = x_ctx + mod_c[2] * y_ctx

        h2_lat = modulate(rms_norm(x_lat, norm2_g[layer]), mod_l[3], mod_l[4]).reshape(-1, D_MODEL)
        n_lat_tok = h2_lat.shape[0]
        if need_ctx:
            h2_ctx = modulate(rms_norm(x_ctx, norm2_g[layer]), mod_c[3], mod_c[4]).reshape(-1, D_MODEL)
            tokens = jnp.concatenate([h2_lat, h2_ctx], axis=0)
        else:
            tokens = h2_lat
        f = hier_moe(tokens, moe_w_group[layer], moe_b_group[layer], moe_w_expert[layer],
                     moe_b_expert[layer], moe_w1[layer], moe_w3[layer], moe_w2[layer])
        x_lat = x_lat + mod_l[5] * f[:n_lat_tok].reshape(x_lat.shape)
        if need_ctx:
            x_ctx = x_ctx + mod_c[5] * f[n_lat_tok:].reshape(x_ctx.shape)
    return rms_norm(x_lat, final_norm_g)
```

```python
import math
from contextlib import ExitStack

import numpy as np
import concourse.bass as bass
import concourse.mybir as mybir
from concourse.bass_utils import run_bass_kernel_spmd

F32 = mybir.dt.float32
F32R = mybir.dt.float32r
I32 = mybir.dt.int32
AF = mybir.ActivationFunctionType
ALU = mybir.AluOpType
AX = mybir.AxisListType

D = 1024
NCTX = 256
NLAT = 2048
NT = NCTX + NLAT
DEPTH = 2
EPS = 1e-6
CHUNKS = [(0, 256)] + [(256 + 512 * i, 512) for i in range(4)]
SLOT = 256
NOV = 35
NROWS = 32 * SLOT + NOV * 128

COLT = []
def _ct(name, start, width):
    COLT.append((name, start, width))
for i in range(2): _ct(f"s5u{i}", 0 + 128 * i, 128)
for i in range(2): _ct(f"cq{i}", 256 + 128 * i, 128)
_ct("ckv", 512, 128)
_ct("kpeA", 5792, 96)
_ct("kpeB", 5792 + 96, 96)
for i in range(4): _ct(f"hq{i}", 672 + 128 * i, 128)
for i in range(4): _ct(f"hf{i}", 1184 + 128 * i, 128)
for i in range(4): _ct(f"hb{i}", 1696 + 128 * i, 128)
for i in range(4): _ct(f"hi{i}", 2208 + 64 * i, 64)
for i in range(4): _ct(f"hg{i}", 2464 + 64 * i, 64)
N_NONGATE = len(COLT)
for b in range(3):
    for i in range(8): _ct(f"gate{b}_{i}", 2720 + 1024 * b + 128 * i, 128)
COLIDX = {n: i for i, (n, _, _) in enumerate(COLT)}
WINX = 5792 + 192

VEC_ROWS = {}
def _vr(name, n):
    VEC_ROWS[name] = (sum(v[1] for v in VEC_ROWS.values()), n)
_vr("c", 8); _vr("c_ctx", 8); _vr("final_g", 8)
for l in range(DEPTH):
    _vr(f"norm1_g{l}", 8); _vr(f"norm2_g{l}", 8); _vr(f"mod_b{l}", 48)
    _vr(f"s5_d{l}", 2); _vr(f"glu_b{l}", 2); _vr(f"qa_g{l}", 2); _vr(f"kva_g{l}", 1)
    _vr(f"hgn_g{l}", 4)
    for d in range(2):
        _vr(f"lam_re{l}{d}", 8); _vr(f"lam_im{l}{d}", 8); _vr(f"lstep{l}{d}", 8)
NVROWS = sum(v[1] for v in VEC_ROWS.values())
NVBLK = (NVROWS + 127) // 128


_UQ = [0]


def uq(name):
    _UQ[0] += 1
    return f"{name}~{_UQ[0]}"


class Buf:
    __slots__ = ("name", "w", "r")

    def __init__(self, name):
        self.name = name
        self.w = None
        self.r = {}


class KB:
    RING = 12

    def __init__(self, nc, es):
        self.nc, self.es = nc, es
        self.eng = dict(pe=nc.tensor, dve=nc.vector, act=nc.scalar, pool=nc.gpsimd, sp=nc.sync)
        self.psem, self.pcnt, self.nsem = {}, {}, 0
        for e in self.eng:
            self._new_psem(e)
        self.waited = {}
        self.rings = {}
        self.rpos = {}
        for q in ("sp", "pool", "act"):
            self.rings[q] = [[self._sem(f"dq_{q}{i}"), 0] for i in range(self.RING)]
            self.rpos[q] = 0
        self.bufs = {}
        self.ninstr = 0

    def _sem(self, name):
        self.nsem += 1
        return self.es.enter_context(self.nc.semaphore(name))

    def _new_psem(self, e):
        self.psem[e] = self._sem(f"p_{e}_{self.nsem}")
        self.pcnt[e] = 0

    def buf(self, name):
        b = self.bufs.get(name)
        if b is None:
            b = self.bufs[name] = Buf(name)
        return b

    def _wait(self, e, tok):
        sem, val, src = tok
        key = (e, id(sem))
        if self.waited.get(key, 0) >= val:
            return
        self.eng[e].wait_ge(sem, val)
        self.waited[key] = val

    def _sync(self, e, reads, writes):
        for b in reads:
            if b.w is not None:
                self._dep(e, b.w)
        for b in writes:
            if b.w is not None:
                self._dep(e, b.w)
            for t in b.r.values():
                self._dep(e, t)

    def _dep(self, e, tok):
        if e == "pe" and tok[2] == "pe":
            return
        self._wait(e, tok)

    def _commit(self, tok, reads, writes):
        for b in writes:
            b.w = tok
            b.r = {}
        for b in reads:
            b.r[tok[2]] = tok

    def op(self, e, fn, reads=(), writes=()):
        reads = [b if isinstance(b, Buf) else self.buf(self._nm(b)) for b in reads]
        writes = [b if isinstance(b, Buf) else self.buf(self._nm(b)) for b in writes]
        self._sync(e, reads, writes)
        ins = fn(self.eng[e])
        if self.pcnt[e] >= 20000:
            self._new_psem(e)
        self.pcnt[e] += 1
        ins.then_inc(self.psem[e], 1)
        self.ninstr += 1
        self._commit((self.psem[e], self.pcnt[e], e), reads, writes)

    def dma(self, q, out, in_, reads=(), writes=()):
        reads = [b if isinstance(b, Buf) else self.buf(self._nm(b)) for b in reads]
        writes = [b if isinstance(b, Buf) else self.buf(self._nm(b)) for b in writes]
        self._sync(q, reads, writes)
        slot = self.rings[q][self.rpos[q] % self.RING]
        self.rpos[q] += 1
        sem, cnt = slot
        if cnt > 0:
            self._wait(q, (sem, cnt, "dma"))
        self.eng[q].dma_start(out=out, in_=in_).then_inc(sem, 16)
        slot[1] = cnt + 16
        self.ninstr += 1
        self._commit((sem, cnt + 16, f"dma_{q}{(self.rpos[q] - 1) % self.RING}"), reads, writes)

    @staticmethod
    def _nm(x):
        if isinstance(x, str):
            return x
        if isinstance(x, tuple):
            return KB._nm(x[0]) + x[1]
        if hasattr(x, "tensor"):
            return x.tensor.name.split("~")[0]
        return x.name.split("~")[0]

    def _names(self, xs):
        return [self._nm(x) for x in xs if not isinstance(x, (int, float)) and x is not None]

    def tt(self, e, out, a, b, op, rn=(), wn=()):
        self.op(e, lambda g: g.tensor_tensor(out, a, b, op), reads=self._names([a, b]) + list(rn), writes=self._names([out]) + list(wn))

    def ts(self, e, out, a, s1, op0, s2=None, op1=None, rn=(), wn=()):
        if op1 is None:
            self.op(e, lambda g: g.tensor_scalar(out, a, s1, None, op0), reads=self._names([a, s1]) + list(rn), writes=self._names([out]) + list(wn))
        else:
            self.op(e, lambda g: g.tensor_scalar(out, a, s1, s2, op0, op1), reads=self._names([a, s1, s2]) + list(rn), writes=self._names([out]) + list(wn))

    def stt(self, e, out, a, sc_, b, op0, op1, rn=(), wn=()):
        self.op(e, lambda g: g.scalar_tensor_tensor(out, a, sc_, b, op0, op1), reads=self._names([a, sc_, b]) + list(rn), writes=self._names([out]) + list(wn))

    def act(self, out, in_, func, bias=None, scale=1.0, rn=(), wn=()):
        kw = {}
        if bias is not None:
            kw["bias"] = bias
        self.op("act", lambda g: g.activation(out, in_, func, scale=scale, **kw), reads=self._names([in_, bias, scale]) + list(rn), writes=self._names([out]) + list(wn))

    def cp(self, e, out, in_, rn=(), wn=()):
        if e == "act":
            self.op(e, lambda g: g.copy(out, in_), reads=self._names([in_]) + list(rn), writes=self._names([out]) + list(wn))
        else:
            self.op(e, lambda g: g.tensor_copy(out, in_), reads=self._names([in_]) + list(rn), writes=self._names([out]) + list(wn))

    def mm(self, out, lhsT, rhs, start=True, stop=True, rn=(), wn=()):
        self.op("pe", lambda g: g.matmul(out, lhsT, rhs, start=start, stop=stop), reads=self._names([lhsT, rhs]) + list(rn), writes=self._names([out]) + list(wn))

    def tr(self, out, in_, ident, rn=(), wn=()):
        self.op("pe", lambda g: g.transpose(out, in_, ident), reads=self._names([in_, ident]) + list(rn), writes=self._names([out]) + list(wn))

    def scan(self, out, d0, d1, init, op0, op1, rn=(), wn=()):
        self.op("dve", lambda g: g.tensor_tensor_scan(out, d0, d1, init, op0, op1), reads=self._names([d0, d1, init]) + list(rn), writes=self._names([out]) + list(wn))

    def recip(self, out, in_):
        self.op("dve", lambda g: g.reciprocal(out, in_), reads=self._names([in_]), writes=self._names([out]))

    def memset(self, e, out, val):
        self.op(e, lambda g: g.memset(out, val), reads=[], writes=self._names([out]))

    def barrier(self):
        toks = [(self.psem[o], self.pcnt[o], o) for o in self.eng if self.pcnt[o] > 0]
        for q in self.rings:
            for sem, cnt in self.rings[q]:
                if cnt > 0:
                    toks.append((sem, cnt, "dma"))
        for e in self.eng:
            for t in toks:
                if t[2] != e:
                    self._wait(e, t)

    def idma(self, out, out_off, in_, in_off, reads=(), writes=(), bounds=None):
        q = "pool"
        reads = [b if isinstance(b, Buf) else self.buf(self._nm(b)) for b in reads]
        writes = [b if isinstance(b, Buf) else self.buf(self._nm(b)) for b in writes]
        self._sync(q, reads, writes)
        slot = self.rings[q][self.rpos[q] % self.RING]
        self.rpos[q] += 1
        sem, cnt = slot
        if cnt > 0:
            self._wait(q, (sem, cnt, "dma"))
        if bounds is None:
            self.eng[q].indirect_dma_start(out=out, out_offset=out_off, in_=in_, in_offset=in_off).then_inc(sem, 16)
        else:
            if not hasattr(self, "_breg") or self._breg[0] != bounds:
                self._breg = (bounds, self.eng[q].to_reg(bounds))
            self.eng[q].indirect_dma_start(out=out, out_offset=out_off, in_=in_, in_offset=in_off,
                                           bounds_check=self._breg[1], oob_is_err=False).then_inc(sem, 16)
        slot[1] = cnt + 16
        self.ninstr += 1
        self._commit((sem, cnt + 16, f"dma_{q}{(self.rpos[q] - 1) % self.RING}"), reads, writes)

    def finish(self, bufs):
        for b in bufs:
            b = self.buf(b) if isinstance(b, str) else b
            if b.w is not None:
                self._wait("sp", b.w)


def build_nc(stop_after=None, debug=False):
    nc = bass.Bass("TRN2", target_bir_lowering=False)
    okind = "ExternalOutput" if debug else "Internal"
    x_d = nc.dram_tensor("x", [NLAT, D], F32, kind="ExternalInput").ap()
    ctx_d = nc.dram_tensor("ctx", [NCTX, D], F32, kind="ExternalInput").ap()
    vecs_d = nc.dram_tensor("vecs", [NVBLK * 128, 128], F32, kind="ExternalInput").ap()
    consts_d = nc.dram_tensor("consts", [128, 256], F32, kind="ExternalInput").ap()
    modw_d = nc.dram_tensor("mod_w", [DEPTH, D, 6 * D], F32, kind="ExternalInput").ap()
    winx_d = nc.dram_tensor("w_in_x", [DEPTH, D, WINX], F32, kind="ExternalInput").ap()
    out_d = nc.dram_tensor("out", [NLAT, D], F32, kind="ExternalOutput").ap()
    zT_d = nc.dram_tensor("zT", [len(COLT) * 128, NT], F32, kind=okind).ap()
    ybT_d = nc.dram_tensor("ybT", [D, NT], F32, kind=okind).ap()
    s5B_d = nc.dram_tensor("s5B", [DEPTH, 2, 2, 8, 128, 128], F32, kind="ExternalInput").ap()
    s5C_d = nc.dram_tensor("s5C", [DEPTH, 2, 2, 8, 128, 128], F32, kind="ExternalInput").ap()
    gluw_d = nc.dram_tensor("s5_glu_w", [DEPTH, 256, 256], F32, kind="ExternalInput").ap()
    rope_d = nc.dram_tensor("rope", [128, 2, NLAT], F32, kind="ExternalInput").ap()
    wq_d = nc.dram_tensor("wq", [DEPTH, 256, 2, 768], F32, kind="ExternalInput").ap()
    wuk_d = nc.dram_tensor("mla_w_uk", [DEPTH, 128, 512], F32, kind="ExternalInput").ap()
    wuv_d = nc.dram_tensor("mla_w_uv", [DEPTH, 128, 512], F32, kind="ExternalInput").ap()
    lbl_d = nc.dram_tensor("hg_lb_logits", [1, DEPTH], F32, kind="ExternalInput").ap()
    wp_d = nc.dram_tensor("wp", [DEPTH, D, D], F32, kind="ExternalInput").ap()
    wo_d = nc.dram_tensor("w_out", [DEPTH, D, D], F32, kind="ExternalInput").ap()
    wr_d = nc.dram_tensor("wr", [DEPTH, D, 36], F32, kind="ExternalInput").ap()
    br_d = nc.dram_tensor("br", [DEPTH, 1, 36], F32, kind="ExternalInput").ap()
    w1_d = nc.dram_tensor("moe_w1", [DEPTH, 32 * 128, 8 * 512], F32, kind="ExternalInput").ap()
    w3_d = nc.dram_tensor("moe_w3", [DEPTH, 32 * 128, 8 * 512], F32, kind="ExternalInput").ap()
    w2_d = nc.dram_tensor("moe_w2", [DEPTH, 32 * 128, 4 * D], F32, kind="ExternalInput").ap()
    h2tok_d = nc.dram_tensor("h2tok", [NT, D], F32, kind=okind).ap()
    xs_d = nc.dram_tensor("xs", [NROWS, D], F32, kind=okind).ap()
    ys_d = nc.dram_tensor("ys", [NROWS, D], F32, kind=okind).ap()
    xdbg_d = nc.dram_tensor("xdbg", [128, 8 * NT], F32, kind=okind).ap()
    modT_d = nc.dram_tensor("modT", [DEPTH, 128, 96], F32, kind=okind).ap()

    with ExitStack() as es:
        kb = KB(nc, es)
        sb = lambda name, shape, dt=F32: es.enter_context(nc.sbuf_tensor(uq(name), shape, dt))

        xT = sb("xT", [128, 8, NT])
        cst = sb("cst", [128, 256])
        ident = cst[:, 0:128]
        onesr = sb("onesr", [128, 128], F32R)
        vecT = sb("vecT", [128, NVBLK * 128])
        modT = sb("modT_sb", [128, DEPTH, 48, 2])
        modA = sb("modA", [128, DEPTH, 2, 8, 2])
        sc = sb("sc", [128, 8, 2])

        def V(name, j=0):
            r0, n = VEC_ROWS[name]
            return vecT[:, r0 + j:r0 + j + 1]

        def Vn(name):
            r0, n = VEC_ROWS[name]
            return vecT[:, r0:r0 + n]

        kb.dma("sp", cst[:], consts_d, writes=["cst"])
        kb.dma("pool", onesr[:], consts_d[:, 128:256], writes=["onesr"])

        with ExitStack() as ph:
            psb = lambda name, shape, dt=F32: ph.enter_context(nc.sbuf_tensor(uq(name), shape, dt))
            pps = lambda name, shape, dt=F32: ph.enter_context(nc.psum_tensor(uq(name), shape, dt))
            stg = [psb(f"xstg{i}", [128, D]) for i in range(3)]
            tps = [pps(f"tps{i}", [128, 4, 128]) for i in range(4)]
            for blk in range(NVBLK):
                s = stg[blk % 3]
                kb.dma("sp", s[:, 0:128], vecs_d[blk * 128:(blk + 1) * 128, :], writes=[f"xstg{blk % 3}"])
                kb.op("pe", lambda e: e.transpose(tps[blk % 4][:, 0, :], s[:, 0:128], ident),
                      reads=[f"xstg{blk % 3}", "cst"], writes=[f"tps{blk % 4}"])
                kb.op("dve", lambda e: e.tensor_copy(vecT[:, blk * 128:(blk + 1) * 128], tps[blk % 4][:, 0, :]),
                      reads=[f"tps{blk % 4}"], writes=["vecT"])
            n_tt = NT // 128
            for tt in range(n_tt):
                s = stg[tt % 3]
                src = ctx_d[tt * 128:(tt + 1) * 128, :] if tt < 2 else x_d[(tt - 2) * 128:(tt - 1) * 128, :]
                kb.dma("sp", s[:], src, writes=[f"xstg{tt % 3}"])
                for half in range(2):
                    pi = (2 * tt + half) % 4
                    for j in range(4):
                        k = half * 4 + j
                        kb.op("pe", lambda e: e.transpose(tps[pi][:, j, :], s[:, k * 128:(k + 1) * 128], ident),
                              reads=[f"xstg{tt % 3}", "cst"], writes=[f"tps{pi}"])
                    eng = "dve" if half == 0 else "act"
                    dst = xT[:, half * 4:half * 4 + 4, tt * 128:(tt + 1) * 128]
                    if eng == "dve":
                        kb.op("dve", lambda e: e.tensor_copy(dst, tps[pi][:]), reads=[f"tps{pi}"], writes=["xT"])
                    else:
                        kb.op("act", lambda e: e.copy(dst, tps[pi][:]), reads=[f"tps{pi}"], writes=["xT"])
            kb.barrier()

        kb.op("act", lambda e: e.activation(sc[:, :, 0], Vn("c"), AF.Silu), reads=["vecT"], writes=["sc"])
        kb.op("act", lambda e: e.activation(sc[:, :, 1], Vn("c_ctx"), AF.Silu), reads=["vecT"], writes=["sc"])

        xTd_d = nc.dram_tensor("xTd", [128, 8 * NT], F32, kind=okind).ap()
        if debug and stop_after == "p0":
            kb.dma("sp", xTd_d, xT[:].rearrange("p a b -> p (a b)"), reads=["xT"], writes=["xTd"])
        lbv = sb("lbv", [128, 2, DEPTH])
        lbl = sb("lbl", [128, DEPTH])
        kb.dma("sp", lbl[:], lbl_d.to_broadcast([128, DEPTH]), writes=["lbl"])
        kb.memset("dve", lbv[:, 0, :], 0.0)
        kb.tt("dve", lbv[:, 0, 1:2], lbl[:, 1:2], lbl[:, 0:1], ALU.subtract)
        kb.act(lbv[:, 0, 1:2], lbv[:, 0, 1:2], AF.Sigmoid)
        kb.ts("dve", lbv[:, 1, :], lbv[:, 0, :], -1.0, ALU.mult, 1.0, ALU.add)
        for layer in range(DEPTH if stop_after != "p0" else 0):
            L = layer
            with ExitStack() as ph:
                psb = lambda name, shape, dt=F32: ph.enter_context(nc.sbuf_tensor(uq(name), shape, dt))
                pps = lambda name, shape, dt=F32: ph.enter_context(nc.psum_tensor(uq(name), shape, dt))
                mw = [psb(f"mw{i}", [128, 8, 128]) for i in range(3)]
                mps = pps("mps", [128, 48, 2])
                for j in range(48):
                    w = mw[j % 3]
                    kb.dma("sp", w[:], modw_d[L, :, j * 128:(j + 1) * 128].rearrange("(k p) c -> p k c", p=128),
                           writes=[f"mw{j % 3}"])
                    for k in range(8):
                        kb.op("pe", lambda e: e.matmul(mps[:, j, :], w[:, k, :], sc[:, k, :], start=(k == 0), stop=(k == 7)),
                              reads=[f"mw{j % 3}", "sc"], writes=["mps"])
                for s in range(2):
                    kb.op("dve", lambda e: e.tensor_tensor(modT[:, L, :, s], mps[:, :, s], Vn(f"mod_b{L}"), ALU.add),
                          reads=["mps", "vecT"], writes=["modT"])
                for ni, (gname, t0) in enumerate(((f"norm1_g{L}", 8), (f"norm2_g{L}", 32))):
                    for s in range(2):
                        kb.op("dve", lambda e: e.scalar_tensor_tensor(
                            modA[:, L, ni, :, s], modT[:, L, t0:t0 + 8, s], 1.0, Vn(gname), ALU.add, ALU.mult),
                            reads=["modT", "vecT"], writes=["modA"])
                if debug:
                    kb.dma("sp", modT_d[L], modT[:, L].rearrange("p a b -> p (a b)"), reads=["modT"], writes=["modT_d"])
                kb.barrier()
            if stop_after == f"mod{L}":
                break

            with ExitStack() as ph:
                psb = lambda name, shape, dt=F32: ph.enter_context(nc.sbuf_tensor(uq(name), shape, dt))
                pps = lambda name, shape, dt=F32: ph.enter_context(nc.psum_tensor(uq(name), shape, dt))
                hT = psb("hT", [128, 8, NT], F32R)
                emit_norm(nc, kb, xT, hT, [(t0, n, t0) for (t0, n) in CHUNKS],
                          lambda k, s_: modA[:, L, 0, k, s_:s_ + 1], lambda k, s_: modT[:, L, k, s_:s_ + 1], onesr, D)
                wb = [psb(f"wb{i}", [128, 8, 128], F32R) for i in range(3)]
                zs = [psb(f"zs{i}", [128, 512]) for i in range(4)]
                zp = [pps(f"zp{i}", [128, 512]) for i in range(4)]
                cnt = 0
                for ti, (name, c0, wd) in enumerate(COLT):
                    w = wb[ti % 3]
                    kb.dma("pool", w[:, :, 0:wd], winx_d[L, :, c0:c0 + wd].rearrange("(k p) c -> p k c", p=128),
                           writes=[f"wb{ti % 3}"])
                    for (t0, n) in CHUNKS:
                        pi = cnt % 4
                        cnt += 1
                        for k in range(8):
                            kb.op("pe", lambda e: e.matmul(zp[pi][0:wd, 0:n], w[:, k, 0:wd], hT[:, k, t0:t0 + n],
                                                           start=(k == 0), stop=(k == 7)),
                                  reads=[f"wb{ti % 3}", "hT"], writes=[f"zp{pi}"])
                        if cnt % 2 == 0:
                            kb.op("dve", lambda e: e.tensor_copy(zs[pi][0:wd, 0:n], zp[pi][0:wd, 0:n]),
                                  reads=[f"zp{pi}"], writes=[f"zs{pi}"])
                        else:
                            kb.op("act", lambda e: e.copy(zs[pi][0:wd, 0:n], zp[pi][0:wd, 0:n]),
                                  reads=[f"zp{pi}"], writes=[f"zs{pi}"])
                        kb.dma("sp", zT_d[ti * 128:ti * 128 + wd, t0:t0 + n], zs[pi][0:wd, 0:n],
                               reads=[f"zs{pi}"], writes=[f"zT_{ti}"])
                kb.barrier()
            if stop_after == f"A{L}":
                break
            G = dict(nc=nc, kb=kb, L=L, xT=xT, cst=cst, ident=ident, onesr=onesr, vecT=vecT, modT=modT, modA=modA,
                     V=V, Vn=Vn, zT_d=zT_d, ybT_d=ybT_d, s5B_d=s5B_d, s5C_d=s5C_d, gluw_d=gluw_d, debug=debug,
                     rope_d=rope_d, wq_d=wq_d, wuk_d=wuk_d, wuv_d=wuv_d)
            if not (debug and stop_after in (f"C{L}", f"D{L}")):
                phase_s5(G)
            if stop_after == f"B{L}":
                break
            G["lbv"] = lbv
            if not (debug and stop_after in (f"D{L}",)):
                phase_mla(G)
            if stop_after == f"C{L}":
                break
            G.update(wp_d=wp_d, wo_d=wo_d, wr_d=wr_d, br_d=br_d, w1_d=w1_d, w3_d=w3_d, w2_d=w2_d, out_d=out_d,
                     h2tok_d=h2tok_d, xs_d=xs_d, ys_d=ys_d)
            phase_hg(G)
            if stop_after == f"D{L}":
                break
            phase_merge(G)
            if stop_after == f"E{L}":
                kb.dma("sp", xdbg_d, xT[:].rearrange("p a b -> p (a b)"), reads=["xT"], writes=["xdbg"])
                break
            phase_moe_sparse(G)
            if stop_after == f"F{L}":
                kb.dma("sp", xdbg_d, xT[:].rearrange("p a b -> p (a b)"), reads=["xT"], writes=["xdbg"])
                break
        else:
            phase_final(G)

        kb.finish(list(kb.bufs.values()))
        print("instructions:", kb.ninstr, "sems:", kb.nsem, "sbuf left:", nc.sbuf_bytes_remaining)
    return nc


def emit_norm(nc, kb, xT, dst, chunks, A, Sh, onesr, dmodel):
    with ExitStack() as ns:
        psb = lambda name, shape, dt=F32: ns.enter_context(nc.sbuf_tensor(uq(name), shape, dt))
        pps = lambda name, shape, dt=F32: ns.enter_context(nc.psum_tensor(uq(name), shape, dt))
        sq = [psb(f"nsq{i}", [128, 8, 512], F32R) for i in range(2)]
        ms = [pps(f"nms{i}", [128, 512]) for i in range(2)]
        rs = [psb(f"nrs{i}", [128, 512]) for i in range(2)]
        tmp = [psb(f"ntmp{i}", [128, 512]) for i in range(2)]
        for ci, (t0, n, d0) in enumerate(chunks):
            s = 1 if t0 < NCTX else 0
            b = ci % 2
            for k in range(8):
                kb.act(sq[b][:, k, 0:n], xT[:, k, t0:t0 + n], AF.Square)
            for k in range(8):
                kb.mm(ms[b][:, 0:n], onesr[:], sq[b][:, k, 0:n], start=(k == 0), stop=(k == 7))
            kb.act(rs[b][:, 0:n], ms[b][:, 0:n], AF.Sqrt, scale=1.0 / dmodel, bias=EPS)
            kb.recip(rs[b][:, 0:n], rs[b][:, 0:n])
            for k in range(8):
                tb = k % 2
                sh = Sh(k, s) if Sh is not None else None
                if sh is None:
                    kb.stt("dve", dst[:, k, d0:d0 + n], xT[:, k, t0:t0 + n], A(k, s), rs[b][:, 0:n], ALU.mult, ALU.mult)
                else:
                    kb.stt("dve", tmp[tb][:, 0:n], xT[:, k, t0:t0 + n], A(k, s), rs[b][:, 0:n], ALU.mult, ALU.mult)
                    kb.act(dst[:, k, d0:d0 + n], tmp[tb][:, 0:n], AF.Identity, bias=sh, scale=1.0)
        kb.barrier()


TWO_PI = 2.0 * math.pi


def range_reduce(kb, r, x, tM, tI):
    kb.ts("dve", tM, x, 1.0 / TWO_PI, ALU.mult)
    kb.cp("dve", tI, tM)
    kb.cp("dve", tM, tI)
    kb.stt("dve", r, tM, -TWO_PI, x, ALU.mult, ALU.add)
    kb.ts("dve", tM, r, math.pi, ALU.is_gt)
    kb.stt("dve", r, tM, -TWO_PI, r, ALU.mult, ALU.add)
    kb.ts("dve", tM, r, -math.pi, ALU.is_lt)
    kb.stt("dve", r, tM, TWO_PI, r, ALU.mult, ALU.add)
    kb.ts("dve", r, r, 3.1415925, ALU.min, -3.1415925, ALU.max)


def sincos(kb, sn, cs, x, r, tM, tI):
    range_reduce(kb, r, x, tM, tI)
    kb.act(sn, r, AF.Sin)
    kb.ts("dve", tM, x, math.pi / 2, ALU.add)
    range_reduce(kb, r, tM, tM, tI) if False else None
    return


def phase_s5(G):
    nc, kb, L = G["nc"], G["kb"], G["L"]
    Vn = G["Vn"]
    zT_d, ybT_d = G["zT_d"], G["ybT_d"]
    T = 256
    NCH = NT // T
    with ExitStack() as ph:
        psb = lambda name, shape, dt=F32: ph.enter_context(nc.sbuf_tensor(uq(name), shape, dt))
        pps = lambda name, shape, dt=F32: ph.enter_context(nc.psum_tensor(uq(name), shape, dt))
        uT = psb("s5_uT", [128, 2, NT], F32R)
        yacc = psb("s5_yacc", [128, 2, NT])
        for t in range(2):
            ti = COLIDX[f"s5u{t}"]
            kb.dma("pool", uT[:, t, :], zT_d[ti * 128:(ti + 1) * 128, :], reads=[f"zT_{ti}"], writes=["s5_uT"])
        Bw = psb("s5_Bw", [128, 2, 8, 128], F32R)
        Cw = psb("s5_Cw", [128, 2, 8, 128], F32R)
        iota_i = psb("s5_iota_i", [128, T + 1], I32)
        iota_f = psb("s5_iota_f", [128, T + 1])
        kb.op("pool", lambda g: g.iota(iota_i[:], [[1, T + 1]], base=0, channel_multiplier=0), writes=["s5_iota_i"])
        kb.cp("dve", iota_f[:], iota_i[:])
        COS = psb("s5_COS", [128, 8, T + 1])
        SIN = psb("s5_SIN", [128, 8, T + 1])
        ang = psb("s5_ang", [128, 8, T + 1])
        rr = psb("s5_rr", [128, 8, T + 1])
        ERE = ang[:, :, 0:T]
        EIM = rr[:, :, 0:T]
        tM = psb("s5_tM", [128, 8, T + 1])
        tI = psb("s5_tI", [128, 8, T + 1], I32)
        sm = psb("s5_sm", [128, 24, 8])
        gin = psb("s5_gin", [128, 8, 2])
        tmp = [[psb(f"s5_t{b}_{i}", [128, T]) for i in range(8)] for b in range(2)]
        hh = [[psb(f"s5_h{b}_{i}", [128, T], F32R) for i in range(2)] for b in range(2)]
        Pp = [pps(f"s5_P{i}", [128, 2, T]) for i in range(3)]
        Yp = [[pps(f"s5_Y{b}_{ct}", [128, 512]) for ct in range(2)] for b in range(2)]
        flat = lambda t: t[:].rearrange("p a b -> p (a b)")
        for d in range(2):
            kb.dma("pool", Bw[:].rearrange("c r s q -> c (r s) q"),
                   G["s5B_d"][L, d].rearrange("r s c q -> c (r s) q"), writes=["s5_Bw"])
            kb.dma("pool", Cw[:].rearrange("c r s q -> c (r s) q"),
                   G["s5C_d"][L, d].rearrange("r s c q -> c (r s) q"), writes=["s5_Cw"])
            kb.ts("pool", Cw[:, 1], Cw[:, 1].bitcast(F32), -1.0, ALU.mult)
            lre, lim, lst = Vn(f"lam_re{L}{d}"), Vn(f"lam_im{L}{d}"), Vn(f"lstep{L}{d}")
            c_ = lambda i: sm[:, i, :]
            DT, MAG, TH, SN, CS, LBR, LBI, DEN, FR, FI, X1, X2, X3, MI = (c_(i) for i in range(14))
            kb.act(DT, lst, AF.Exp)
            kb.tt("dve", X1, lre, DT, ALU.mult)
            kb.act(MAG, X1, AF.Exp)
            kb.tt("dve", TH, lim, DT, ALU.mult)
            smI = tI[:, 0, 0:8]
            range_reduce(kb, X2, TH, X3, smI)
            kb.act(SN, X2, AF.Sin)
            kb.ts("dve", X1, TH, math.pi / 2, ALU.add)
            range_reduce(kb, X2, X1, X3, smI)
            kb.act(CS, X2, AF.Sin)
            kb.tt("dve", LBR, MAG, CS, ALU.mult)
            kb.tt("dve", LBI, MAG, SN, ALU.mult)
            kb.tt("dve", X1, lre, lre, ALU.mult)
            kb.tt("dve", X2, lim, lim, ALU.mult)
            kb.tt("dve", DEN, X1, X2, ALU.add)
            kb.recip(DEN, DEN)
            kb.ts("dve", X3, LBR, -1.0, ALU.add)
            kb.tt("dve", X1, X3, lre, ALU.mult)
            kb.tt("dve", X2, LBI, lim, ALU.mult)
            kb.tt("dve", X1, X1, X2, ALU.add)
            kb.tt("dve", FR, X1, DEN, ALU.mult)
            kb.tt("dve", X1, LBI, lre, ALU.mult)
            kb.tt("dve", X2, X3, lim, ALU.mult)
            kb.tt("dve", X1, X1, X2, ALU.subtract)
            kb.tt("dve", FI, X1, DEN, ALU.mult)
            kb.tt("dve", ang[:], TH.unsqueeze(2).to_broadcast([128, 8, T + 1]),
                  iota_f[:].unsqueeze(1).to_broadcast([128, 8, T + 1]), ALU.mult)
            range_reduce(kb, flat(rr), flat(ang), flat(tM), flat(tI))
            kb.act(flat(SIN), flat(rr), AF.Sin)
            kb.ts("dve", flat(ang), flat(ang), math.pi / 2, ALU.add)
            range_reduce(kb, flat(rr), flat(ang), flat(tM), flat(tI))
            kb.act(flat(COS), flat(rr), AF.Sin)
            frb = FR.unsqueeze(2).to_broadcast([128, 8, T])
            fib = FI.unsqueeze(2).to_broadcast([128, 8, T])
            tF = tI[:].bitcast(F32)
            kb.tt("dve", tM[:, :, 0:T], COS[:, :, 0:T], frb, ALU.mult)
            kb.tt("dve", tF[:, :, 0:T], SIN[:, :, 0:T], fib, ALU.mult)
            kb.tt("dve", ERE, tM[:, :, 0:T], tF[:, :, 0:T], ALU.add)
            kb.tt("dve", tM[:, :, 0:T], COS[:, :, 0:T], fib, ALU.mult)
            kb.tt("dve", tF[:, :, 0:T], SIN[:, :, 0:T], frb, ALU.mult)
            kb.tt("dve", EIM, tM[:, :, 0:T], tF[:, :, 0:T], ALU.subtract)
            kb.memset("dve", gin[:], 0.0)
            order = list(range(NCH)) if d == 0 else [0] + list(range(NCH - 1, 0, -1))
            units = [(oi, ci, s_) for oi, ci in enumerate(order) for s_ in range(8)]

            def stageA(u):
                oi, ci, s_ = units[u]
                t0 = ci * T
                ct = s_ // 4
                tq = tmp[u % 2]
                P = Pp[u % 3]
                kb.mm(P[:, 0, :], Bw[:, 0, s_, :], uT[:, ct, t0:t0 + T])
                kb.mm(P[:, 1, :], Bw[:, 1, s_, :], uT[:, ct, t0:t0 + T])
                Pre = P[:, 0, ::-1] if d == 1 else P[:, 0, :]
                Pim = P[:, 1, ::-1] if d == 1 else P[:, 1, :]
                kb.tt("dve", tq[0][:], ERE[:, s_, :], Pre, ALU.mult)
                kb.tt("dve", tq[1][:], EIM[:, s_, :], Pim, ALU.mult)
                kb.tt("pool", tq[4][:], tq[0][:], tq[1][:], ALU.subtract)
                kb.tt("dve", tq[2][:], ERE[:, s_, :], Pim, ALU.mult)
                kb.tt("dve", tq[3][:], EIM[:, s_, :], Pre, ALU.mult)
                kb.tt("pool", tq[5][:], tq[2][:], tq[3][:], ALU.add)

            def stageB(u):
                oi, ci, s_ = units[u]
                t0 = ci * T
                ct = s_ // 4
                yb_ = oi % 2
                tq = tmp[u % 2]
                hb = hh[u % 2]
                rb = MAG[:, s_:s_ + 1].to_broadcast([128, T])
                kb.scan(tq[6][:], rb, tq[4][:], gin[:, s_, 0:1], ALU.mult, ALU.add)
                kb.scan(tq[7][:], rb, tq[5][:], gin[:, s_, 1:2], ALU.mult, ALU.add)
                cT, sT = COS[:, s_, T:T + 1], SIN[:, s_, T:T + 1]
                lr, li = tq[6][:, T - 1:T], tq[7][:, T - 1:T]
                xa, xb = sm[:, 14 + (u % 2) * 2, 0:1], sm[:, 15 + (u % 2) * 2, 0:1]
                kb.ts("dve", xa, li, sT, ALU.mult)
                kb.stt("dve", gin[:, s_, 0:1], lr, cT, xa, ALU.mult, ALU.subtract)
                kb.ts("dve", xb, li, cT, ALU.mult)
                kb.stt("dve", gin[:, s_, 1:2], lr, sT, xb, ALU.mult, ALU.add)
                kb.tt("pool", tq[0][:], COS[:, s_, 0:T], tq[6][:], ALU.mult)
                kb.tt("pool", tq[1][:], SIN[:, s_, 0:T], tq[7][:], ALU.mult)
                kb.tt("pool", hb[0][:], tq[0][:], tq[1][:], ALU.subtract)
                kb.tt("dve", tq[2][:], SIN[:, s_, 0:T], tq[6][:], ALU.mult)
                kb.tt("dve", tq[3][:], COS[:, s_, 0:T], tq[7][:], ALU.mult)
                kb.tt("dve", hb[1][:], tq[2][:], tq[3][:], ALU.add)
                Y = Yp[yb_][ct]
                kb.mm(Y[:, 0:T], Cw[:, 0, s_, :], hb[0][:], start=(s_ % 4 == 0), stop=False)
                kb.mm(Y[:, 0:T], Cw[:, 1, s_, :], hb[1][:], start=False, stop=(s_ % 4 == 3))
                if s_ == 7:
                    for ct2 in range(2):
                        Y2 = Yp[yb_][ct2]
                        if d == 0:
                            kb.cp("act", yacc[:, ct2, t0:t0 + T], Y2[:, 0:T])
                        else:
                            rv = slice(t0 + T - 1, (t0 - 1 if t0 > 0 else None), -1)
                            kb.tt("dve", yacc[:, ct2, rv], yacc[:, ct2, rv], Y2[:, 0:T], ALU.add)

            stageA(0)
            for u in range(len(units)):
                if u + 1 < len(units):
                    stageA(u + 1)
                stageB(u)
        gw = psb("s5_gw", [128, 2, 256], F32R)
        kb.dma("pool", gw[:], G["gluw_d"][L].rearrange("(k p) c -> p k c", p=128), writes=["s5_gw"])
        y1 = psb("s5_y1", [128, 2, 512], F32R)
        for (t0, n) in CHUNKS:
            for ct in range(2):
                a, b2, c2 = tmp[ct][0], tmp[ct][1], tmp[ct][2]
                for h0 in range(0, n, T):
                    sl = slice(t0 + h0, t0 + h0 + T)
                    kb.stt("dve", a[:], uT[:, ct, sl].bitcast(F32), Vn(f"s5_d{L}")[:, ct:ct + 1], yacc[:, ct, sl], ALU.mult, ALU.add)
                    kb.tt("dve", b2[:], a[:], a[:], ALU.mult)
                    kb.ts("dve", b2[:], b2[:], 0.044715, ALU.mult, 1.0, ALU.add)
                    kb.tt("dve", b2[:], b2[:], a[:], ALU.mult)
                    kb.act(c2[:], b2[:], AF.Sigmoid, scale=1.5957691216057308)
                    kb.tt("dve", y1[:, ct, h0:h0 + T], a[:], c2[:], ALU.mult)
            for ct in range(2):
                Y = Yp[0][ct]
                for k in range(2):
                    kb.mm(Y[:, 0:n], gw[:, k, ct * 128:(ct + 1) * 128], y1[:, k, 0:n], start=(k == 0), stop=(k == 1))
                for h0 in range(0, n, T):
                    sg = tmp[ct][3]
                    o = tmp[ct][4]
                    kb.act(sg[:], Y[:, h0:h0 + T], AF.Sigmoid, bias=Vn(f"glu_b{L}")[:, ct:ct + 1])
                    kb.tt("dve", o[:], y1[:, ct, h0:h0 + T].bitcast(F32), sg[:], ALU.mult)
                    kb.dma("sp", ybT_d[ct * 128:(ct + 1) * 128, t0 + h0:t0 + h0 + T], o[:], reads=[o], writes=[f"ybT_{ct}"])
        kb.barrier()


MLA_SCALE = 1.0 / math.sqrt(96.0)


def phase_mla(G):
    nc, kb, L = G["nc"], G["kb"], G["L"]
    Vn, cst, onesr = G["Vn"], G["cst"], G["onesr"]
    zT_d, ybT_d = G["zT_d"], G["ybT_d"]
    need_ctx = L < DEPTH - 1
    with ExitStack() as ph:
        psb = lambda name, shape, dt=F32: ph.enter_context(nc.sbuf_tensor(uq(name), shape, dt))
        pps = lambda name, shape, dt=F32: ph.enter_context(nc.psum_tensor(uq(name), shape, dt))
        cqn = psb("ml_cqn", [128, 2, NT], F32R)
        ckvn = psb("ml_ckvn", [128, NT], F32R)
        KPE = psb("ml_KPE", [128, NT])
        ROPE = psb("ml_rope", [128, 2, NLAT])
        wq = psb("ml_wq", [128, 2, 2, 768], F32R)
        wuk = psb("ml_wuk", [128, 512], F32R)
        wuv = psb("ml_wuv", [128, 512], F32R)
        kb.dma("sp", ROPE[:], G["rope_d"], writes=["ml_rope"])
        kb.dma("pool", wq[:].rearrange("p k v c -> p k (v c)"),
               G["wq_d"][L].rearrange("(k p) v c -> p k (v c)", p=128), writes=["ml_wq"])
        kb.dma("pool", wuk[:], G["wuk_d"][L], writes=["ml_wuk"])
        kb.dma("pool", wuv[:], G["wuv_d"][L], writes=["ml_wuv"])
        ps = [pps(f"ml_ps{i}", [128, 512]) for i in range(7)]
        with ExitStack() as p1:
            qsb = lambda name, shape, dt=F32: p1.enter_context(nc.sbuf_tensor(uq(name), shape, dt))
            cqT = qsb("ml_cqT", [128, 2, NT])
            ckvT = qsb("ml_ckvT", [128, NT])
            kA = qsb("ml_kA", [128, NT])
            kB = qsb("ml_kB", [128, NT])
            for t in range(2):
                ti = COLIDX[f"cq{t}"]
                kb.dma("sp", cqT[:, t, :], zT_d[ti * 128:(ti + 1) * 128, :], reads=[f"zT_{ti}"], writes=["ml_cqT"])
            ti = COLIDX["ckv"]
            kb.dma("sp", ckvT[:], zT_d[ti * 128:(ti + 1) * 128, :], reads=[f"zT_{ti}"], writes=["ml_ckvT"])
            for nm_, tl in (("kpeA", kA), ("kpeB", kB)):
                ti = COLIDX[nm_]
                kb.dma("sp", tl[0:96, :], zT_d[ti * 128:ti * 128 + 96, :], reads=[f"zT_{ti}"], writes=[tl])
            sq = qsb("ml_sq", [128, 3, 512], F32R)
            rs = [qsb(f"ml_rs{i}", [128, 512]) for i in range(2)]
            tt1 = qsb("ml_tt1", [128, 512])
            tt2 = qsb("ml_tt2", [128, 512])
            for (t0, n) in CHUNKS:
                for t in range(2):
                    kb.act(sq[:, t, 0:n], cqT[:, t, t0:t0 + n], AF.Square)
                kb.act(sq[:, 2, 0:n], ckvT[:, t0:t0 + n], AF.Square)
                for t in range(2):
                    kb.mm(ps[0][:, 0:n], onesr[:], sq[:, t, 0:n], start=(t == 0), stop=(t == 1))
                kb.mm(ps[1][:, 0:n], onesr[:], sq[:, 2, 0:n])
                kb.act(rs[0][:, 0:n], ps[0][:, 0:n], AF.Sqrt, scale=1.0 / 256, bias=EPS)
                kb.recip(rs[0][:, 0:n], rs[0][:, 0:n])
                kb.act(rs[1][:, 0:n], ps[1][:, 0:n], AF.Sqrt, scale=1.0 / 128, bias=EPS)
                kb.recip(rs[1][:, 0:n], rs[1][:, 0:n])
                for t in range(2):
                    kb.stt("dve", cqn[:, t, t0:t0 + n], cqT[:, t, t0:t0 + n], Vn(f"qa_g{L}")[:, t:t + 1], rs[0][:, 0:n], ALU.mult, ALU.mult)
                kb.stt("dve", ckvn[:, t0:t0 + n], ckvT[:, t0:t0 + n], Vn(f"kva_g{L}")[:, 0:1], rs[1][:, 0:n], ALU.mult, ALU.mult)
                if t0 < NCTX:
                    kb.cp("dve", KPE[64:96, t0:t0 + n], kA[64:96, t0:t0 + n])
                else:
                    l0 = t0 - NCTX
                    kb.tt("dve", tt1[64:96, 0:n], kA[64:96, t0:t0 + n], ROPE[64:96, 0, l0:l0 + n], ALU.mult)
                    kb.tt("dve", tt2[64:96, 0:n], kB[64:96, t0:t0 + n], ROPE[64:96, 1, l0:l0 + n], ALU.mult)
                    kb.tt("dve", KPE[64:96, t0:t0 + n], tt1[64:96, 0:n], tt2[64:96, 0:n], ALU.add)
            kb.barrier()
        KT = psb("ml_KT", [128, NT], F32R)
        QT = psb("ml_QT", [128, NT], F32R)
        Vh = psb("ml_Vh", [128, 18, 65], F32R)
        PT = [psb(f"ml_PT{i}", [128, 512], F32R) for i in range(3)]
        Osb = [psb(f"ml_Osb{i}", [128, 512]) for i in range(2)]
        ys = [psb(f"ml_ys{i}", [128, 512]) for i in range(2)]
        u1 = psb("ml_u1", [128, 512])
        u2 = psb("ml_u2", [128, 512])
        onesf = cst[:, 128:256]
        kb.cp("dve", Vh[:, :, 64:65], onesf[:, 0:18].unsqueeze(2))
        cnt = 0
        for h in range(8):
            for (t0, n) in CHUNKS:
                kb.mm(ps[0][0:64, 0:n], wuk[:, h * 64:(h + 1) * 64], ckvn[:, t0:t0 + n])
                kb.cp("act", KT[0:64, t0:t0 + n], ps[0][0:64, 0:n])
            kb.cp("dve", KT[64:96, :], KPE[64:96, :])
            for g0 in range(0, 18, 8):
                gn = min(8, 18 - g0)
                for j in range(gn):
                    kt = g0 + j
                    kb.mm(ps[1][:, j * 64:(j + 1) * 64], ckvn[:, kt * 128:(kt + 1) * 128], wuv[:, h * 64:(h + 1) * 64])
                kb.cp("dve", Vh[:, g0:g0 + gn, 0:64], ps[1][:, 0:gn * 64].rearrange("p (a b) -> p a b", b=64))
            for (t0, n) in CHUNKS:
                lat = t0 >= NCTX
                if not lat and not need_ctx:
                    continue
                for k in range(2):
                    kb.mm(ps[0][0:96, 0:n], wq[:, k, 0, h * 96:(h + 1) * 96], cqn[:, k, t0:t0 + n], start=(k == 0), stop=(k == 1))
                if lat:
                    for k in range(2):
                        kb.mm(ps[1][0:96, 0:n], wq[:, k, 1, h * 96:(h + 1) * 96], cqn[:, k, t0:t0 + n], start=(k == 0), stop=(k == 1))
                kb.cp("act", QT[0:64, t0:t0 + n], ps[0][0:64, 0:n])
                if not lat:
                    kb.cp("act", QT[64:96, t0:t0 + n], ps[0][64:96, 0:n])
                else:
                    l0 = t0 - NCTX
                    kb.tt("dve", u1[64:96, 0:n], ps[0][64:96, 0:n], ROPE[64:96, 0, l0:l0 + n], ALU.mult)
                    kb.tt("dve", u2[64:96, 0:n], ps[1][64:96, 0:n], ROPE[64:96, 1, l0:l0 + n], ALU.mult)
                    kb.tt("dve", QT[64:96, t0:t0 + n], u1[64:96, 0:n], u2[64:96, 0:n], ALU.add)
            for (t0, n) in CHUNKS:
                lat = t0 >= NCTX
                if not lat and not need_ctx:
                    continue
                kts = list(range(18)) if lat else [0, 1]
                Op = ps[5 + cnt % 2]
                ob = Osb[cnt % 2]
                yo = ys[cnt % 2]
                bcp = ps[cnt % 2]
                cnt += 1
                def emitS(i):
                    kt = kts[i]
                    kb.mm(ps[2 + i % 3][:, 0:n], KT[0:96, kt * 128:(kt + 1) * 128], QT[0:96, t0:t0 + n])
                for i in range(min(2, len(kts))):
                    emitS(i)
                for i, kt in enumerate(kts):
                    Sp = ps[2 + i % 3]
                    pt = PT[i % 3]
                    kb.act(pt[:, 0:n], Sp[:, 0:n], AF.Exp, scale=MLA_SCALE)
                    if i + 2 < len(kts):
                        emitS(i + 2)
                    kb.mm(Op[0:65, 0:n], Vh[:, kt, :], pt[:, 0:n], start=(i == 0), stop=(i == len(kts) - 1))
                kb.cp("act", ob[0:65, 0:n], Op[0:65, 0:n])
                kb.recip(ob[64:65, 0:n], ob[64:65, 0:n])
                kb.mm(bcp[0:64, 0:n], onesf[64:65, 0:64], ob[64:65, 0:n])
                kb.tt("dve", yo[0:64, 0:n], ob[0:64, 0:n], bcp[0:64, 0:n], ALU.mult)
                kb.dma("sp", ybT_d[256 + h * 64:256 + (h + 1) * 64, t0:t0 + n], yo[0:64, 0:n], reads=[yo], writes=[f"ybT_m{h}"])
        kb.barrier()


def phase_hg(G):
    nc, kb, L = G["nc"], G["kb"], G["L"]
    Vn, cst, onesr, ident, lbv = G["Vn"], G["cst"], G["onesr"], G["ident"], G["lbv"]
    zT_d, ybT_d = G["zT_d"], G["ybT_d"]
    CH = 64
    NC_ = NT // CH
    onesf = cst[:, 128:256]
    with ExitStack() as ph:
        psb = lambda name, shape, dt=F32: ph.enter_context(nc.sbuf_tensor(uq(name), shape, dt))
        pps = lambda name, shape, dt=F32: ph.enter_context(nc.psum_tensor(uq(name), shape, dt))
        A = psb("hg_A", [128, NT])
        KK = psb("hg_KK", [128, NT], F32R)
        Bt = psb("hg_Bt", [128, NT])
        E1 = psb("hg_E1", [128, NT], F32R)
        qT = psb("hg_qT", [128, NT])
        ig = psb("hg_ig", [128, NT])
        itok = psb("hg_itok", [128, NC_, 64], F32R)
        oacc = psb("hg_oacc", [128, NT])
        U = psb("hg_U", [128, NC_, 64])
        PTall = psb("hg_PT", [128, NC_, 64], F32R)
        Sst = psb("hg_Sst", [128, NC_, 64], F32R)
        ktok = [psb(f"hg_ktok{i}", [128, 4, 128], F32R) for i in range(2)]
        sct = [psb(f"hg_sct{i}", [128, 512]) for i in range(2)]
        small = psb("hg_small", [128, 4, NC_])
        S = psb("hg_S", [128, 64])
        tU = psb("hg_tU", [128, 64])
        fin = [psb(f"hg_fin{i}", [128, 512]) for i in range(3)]
        ps = [pps(f"hg_ps{i}", [128, 512]) for i in range(7)]
        pcnt = [0]

        def nps():
            pcnt[0] += 1
            return ps[pcnt[0] % 7]

        b3 = lambda t: t[:].rearrange("p (n c) -> p n c", c=CH)
        for h in range(4):
            tq, ti_, tg = COLIDX[f"hq{h}"], COLIDX[f"hi{h}"], COLIDX[f"hg{h}"]
            kb.dma("sp", qT[:], zT_d[tq * 128:(tq + 1) * 128, :], reads=[f"zT_{tq}"], writes=[qT])
            kb.dma("sp", ig[0:64, :], zT_d[ti_ * 128:ti_ * 128 + 64, :], reads=[f"zT_{ti_}"], writes=[ig])
            for c0 in range(0, NC_, 8):
                gn = min(8, NC_ - c0)
                p = nps()
                for j in range(gn):
                    c = c0 + j
                    kb.tr(p[0:64, j * 64:(j + 1) * 64], ig[0:64, c * CH:(c + 1) * CH], ident[0:64, 0:64])
                kb.cp("act", itok[0:64, c0:c0 + gn, :], p[0:64, 0:gn * 64].rearrange("p (a b) -> p a b", b=64))
            for d in range(2):
                tf = COLIDX[f"hf{h}" if d == 0 else f"hb{h}"]
                kb.dma("sp", A[:], zT_d[tf * 128:(tf + 1) * 128, :], reads=[f"zT_{tf}"], writes=[A])
                kb.act(A[:], A[:], AF.Sigmoid)
                kb.ts("dve", A[:], A[:], lbv[:, 1, L:L + 1], ALU.mult, lbv[:, 0, L:L + 1], ALU.add)
                kb.ts("dve", KK[:], A[:], -1.0, ALU.mult, 1.0, ALU.add)
                kb.act(A[:], A[:], AF.Ln)
                for c in range(NC_):
                    sl = slice(c * CH, (c + 1) * CH)
                    if d == 0:
                        kb.scan(Bt[:, sl], onesf[:, 0:CH], A[:, sl], 0.0, ALU.mult, ALU.add)
                    else:
                        lo = c * CH
                        rv = slice(lo + CH - 1, (lo - 1) if lo > 0 else None, -1)
                        kb.scan(Bt[:, rv], onesf[:, 0:CH], A[:, rv], 0.0, ALU.mult, ALU.add)
                refpos = 31 if d == 0 else 32
                lastpos = 63 if d == 0 else 0
                refc, alpha, gamma, beta = (small[:, i, :] for i in range(4))
                kb.cp("dve", refc, b3(Bt)[:, :, refpos])
                kb.act(alpha, b3(Bt)[:, :, lastpos], AF.Exp)
                kb.act(gamma, refc, AF.Exp)
                kb.tt("dve", b3(Bt), b3(Bt), refc.unsqueeze(2).to_broadcast([128, NC_, CH]), ALU.subtract)
                kb.act(E1[:], Bt[:], AF.Exp)
                kb.cp("dve", beta, b3(E1)[:, :, lastpos].bitcast(F32))
                kb.act(Bt[:], Bt[:], AF.Exp, scale=-1.0)
                kb.tt("dve", E1[:], qT[:], E1[:].bitcast(F32), ALU.mult)
                kb.tt("dve", KK[:], KK[:].bitcast(F32), Bt[:], ALU.mult)
                for c0 in range(0, NC_, 4):
                    p = nps()
                    kt_ = ktok[(c0 // 4) % 2]
                    for j in range(4):
                        c = c0 + j
                        kb.tr(p[0:64, j * 128:(j + 1) * 128], KK[:, c * CH:(c + 1) * CH].bitcast(F32), ident)
                    kb.cp("act", kt_[0:64, :, :], p[0:64, :].rearrange("p (a b) -> p a b", b=128))
                    p2 = nps()
                    for j in range(4):
                        c = c0 + j
                        kb.mm(p2[:, j * 64:(j + 1) * 64], kt_[0:64, j, :], itok[0:64, c, :])
                    kb.cp("dve", U[:, c0:c0 + 4, :], p2[:, 0:256].rearrange("p (a b) -> p a b", b=64))
                for c0 in range(0, NC_, 8):
                    gn = min(8, NC_ - c0)
                    p = nps()
                    for j in range(gn):
                        c = c0 + j
                        kb.mm(p[0:64, j * 64:(j + 1) * 64], KK[:, c * CH:(c + 1) * CH], E1[:, c * CH:(c + 1) * CH])
                    st = sct[(c0 // 8) % 2]
                    kb.cp("act", st[0:64, 0:gn * 64], p[0:64, 0:gn * 64])
                    if d == 0:
                        kb.op("pool", lambda g: g.affine_select(PTall[0:64, c0:c0 + gn, :], st[0:64, 0:gn * 64].rearrange("p (a b) -> p a b", b=64),
                                                                [[0, gn], [1, 64]], ALU.is_ge, 0.0, base=0, channel_multiplier=-1),
                              reads=[st], writes=[PTall])
                    else:
                        kb.op("pool", lambda g: g.affine_select(PTall[0:64, c0:c0 + gn, :], st[0:64, 0:gn * 64].rearrange("p (a b) -> p a b", b=64),
                                                                [[0, gn], [-1, 64]], ALU.is_ge, 0.0, base=0, channel_multiplier=1),
                              reads=[st], writes=[PTall])
                kb.memset("dve", S[:], 0.0)
                order = list(range(NC_)) if d == 0 else [3, 2, 1, 0] + list(range(NC_ - 1, 3, -1))
                kb.tt("dve", U[:], U[:], beta.unsqueeze(2).to_broadcast([128, NC_, 64]), ALU.mult)
                for c in order:
                    kb.ts("dve", Sst[:, c, :], S[:], gamma[:, c:c + 1], ALU.mult)
                    kb.stt("dve", S[:], S[:], alpha[:, c:c + 1], U[:, c, :], ALU.mult, ALU.add)
                for c0 in range(0, NC_, 8):
                    gn = min(8, NC_ - c0)
                    p = nps()
                    for j in range(gn):
                        c = c0 + j
                        kb.mm(p[0:64, j * 64:(j + 1) * 64], itok[0:64, c, :], PTall[0:64, c, :], start=True, stop=False)
                        kb.mm(p[0:64, j * 64:(j + 1) * 64], Sst[:, c, :], E1[:, c * CH:(c + 1) * CH], start=False, stop=True)
                    if d == 0:
                        kb.cp("act", oacc[0:64, c0 * CH:(c0 + gn) * CH], p[0:64, 0:gn * 64])
                    else:
                        kb.tt("dve", oacc[0:64, c0 * CH:(c0 + gn) * CH], oacc[0:64, c0 * CH:(c0 + gn) * CH], p[0:64, 0:gn * 64], ALU.add)
            kb.dma("sp", ig[0:64, :], zT_d[tg * 128:tg * 128 + 64, :], reads=[f"zT_{tg}"], writes=[ig])
            kb.act(KK[0:64, :], oacc[0:64, :], AF.Square)
            kb.act(ig[0:64, :], ig[0:64, :], AF.Silu)
            for (t0, n) in CHUNKS:
                p = nps()
                kb.mm(p[0:64, 0:n], onesr[0:64, 0:64], KK[0:64, t0:t0 + n])
                kb.act(fin[0][0:64, 0:n], p[0:64, 0:n], AF.Sqrt, scale=1.0 / 64, bias=EPS)
                kb.recip(fin[0][0:64, 0:n], fin[0][0:64, 0:n])
                kb.stt("dve", fin[1][0:64, 0:n], oacc[0:64, t0:t0 + n], Vn(f"hgn_g{L}")[0:64, h:h + 1], fin[0][0:64, 0:n], ALU.mult, ALU.mult)
                kb.tt("dve", fin[2][0:64, 0:n], fin[1][0:64, 0:n], ig[0:64, t0:t0 + n], ALU.mult)
                kb.dma("sp", ybT_d[768 + h * 64:768 + (h + 1) * 64, t0:t0 + n], fin[2][0:64, 0:n], reads=[fin[2]], writes=[f"ybT_h{h}"])
        kb.barrier()


def phase_merge(G):
    nc, kb, L = G["nc"], G["kb"], G["L"]
    xT, modT = G["xT"], G["modT"]
    zT_d, ybT_d = G["zT_d"], G["ybT_d"]
    need_ctx = L < DEPTH - 1
    chs = CHUNKS if need_ctx else CHUNKS[1:]
    yb_names = ["ybT_0", "ybT_1"] + [f"ybT_m{h}" for h in range(8)] + [f"ybT_h{h}" for h in range(4)]
    with ExitStack() as ph:
        psb = lambda name, shape, dt=F32: ph.enter_context(nc.sbuf_tensor(uq(name), shape, dt))
        pps = lambda name, shape, dt=F32: ph.enter_context(nc.psum_tensor(uq(name), shape, dt))
        wp = psb("mg_wp", [128, 8, D], F32R)
        wo = psb("mg_wo", [128, 8, D], F32R)
        kb.dma("pool", wp[:], G["wp_d"][L].rearrange("(k p) c -> p k c", p=128), writes=[wp])
        kb.dma("pool", wo[:], G["wo_d"][L].rearrange("(k p) c -> p k c", p=128), writes=[wo])
        yb = psb("mg_yb", [128, 8, 512], F32R)
        mT = psb("mg_mT", [128, 8, 512], F32R)
        gt = [psb(f"mg_gt{i}", [128, 3, 512]) for i in range(2)]
        t3 = [psb(f"mg_t{i}", [128, 512]) for i in range(3)]
        ps = [pps(f"mg_ps{i}", [128, 512]) for i in range(8)]
        branches = ((0, 2), (2, 6), (6, 8))
        for (t0, n) in chs:
            s_ = 1 if t0 < NCTX else 0
            kb.dma("pool", yb[:, :, 0:n], ybT_d[:, t0:t0 + n].rearrange("(k p) t -> p k t", p=128), reads=yb_names, writes=[yb])
            for f in range(8):
                g = gt[f % 2]
                for b in range(3):
                    ti = COLIDX[f"gate{b}_{f}"]
                    kb.dma("sp", g[:, b, 0:n], zT_d[ti * 128:(ti + 1) * 128, t0:t0 + n], reads=[f"zT_{ti}"], writes=[g])
                kb.act(g[:, :, 0:n], g[:, :, 0:n], AF.Sigmoid)
                for b, (k0, k1) in enumerate(branches):
                    p = ps[(f % 2) * 3 + b]
                    for k in range(k0, k1):
                        kb.mm(p[:, 0:n], wp[:, k, f * 128:(f + 1) * 128], yb[:, k, 0:n], start=(k == k0), stop=(k == k1 - 1))
                    kb.tt("dve", t3[b][:, 0:n], p[:, 0:n], g[:, b, 0:n], ALU.mult)
                kb.tt("pool", t3[0][:, 0:n], t3[0][:, 0:n], t3[1][:, 0:n], ALU.add)
                kb.tt("pool", mT[:, f, 0:n], t3[0][:, 0:n], t3[2][:, 0:n], ALU.add)
            for f in range(8):
                p = ps[6 + f % 2]
                for k in range(8):
                    kb.mm(p[:, 0:n], wo[:, k, f * 128:(f + 1) * 128], mT[:, k, 0:n], start=(k == 0), stop=(k == 7))
                kb.stt("dve", xT[:, f, t0:t0 + n], p[:, 0:n], modT[:, L, 16 + f, s_:s_ + 1], xT[:, f, t0:t0 + n], ALU.mult, ALU.add)
        kb.barrier()


def phase_moe(G):
    nc, kb, L = G["nc"], G["kb"], G["L"]
    xT, modT, modA, onesr, ident, cst = G["xT"], G["modT"], G["modA"], G["onesr"], G["ident"], G["cst"]
    need_ctx = L < DEPTH - 1
    chs = CHUNKS if need_ctx else CHUNKS[1:]
    groups = [chs[:3], chs[3:]] if need_ctx else [chs[:2], chs[2:]]
    onesf = cst[:, 128:256]
    for grp in groups:
        GN = sum(n for _, n in grp)
        gch = []
        o = 0
        for (t0, n) in grp:
            gch.append((t0, n, o))
            o += n
        with ExitStack() as ph:
            psb = lambda name, shape, dt=F32: ph.enter_context(nc.sbuf_tensor(uq(name), shape, dt))
            pps = lambda name, shape, dt=F32: ph.enter_context(nc.psum_tensor(uq(name), shape, dt))
            h2T = psb("me_h2T", [128, 8, GN], F32R)
            gateT = psb("me_gateT", [128, GN], F32R)
            emit_norm(nc, kb, xT, h2T, gch, lambda k, s_: modA[:, L, 1, k, s_:s_ + 1],
                      lambda k, s_: modT[:, L, 24 + k, s_:s_ + 1], onesr, D)
            with ExitStack() as rp:
                rsb = lambda name, shape, dt=F32: rp.enter_context(nc.sbuf_tensor(uq(name), shape, dt))
                rps = lambda name, shape, dt=F32: rp.enter_context(nc.psum_tensor(uq(name), shape, dt))
                wr = rsb("me_wr", [128, 8, 36])
                br = rsb("me_br", [128, 36])
                kb.dma("sp", wr[:], G["wr_d"][L].rearrange("(k p) c -> p k c", p=128), writes=[wr])
                kb.dma("sp", br[:], G["br_d"][L].to_broadcast([128, 36]), writes=[br])
                lp = [rps(f"me_lp{i}", [128, 512]) for i in range(2)]
                gp = [rps(f"me_gp{i}", [128, 512]) for i in range(2)]
                R = [[rsb(f"me_r{b}_{i}", [128, 40]) for i in range(12)] for b in range(2)]
                for tt in range(GN // 128):
                    b = tt % 2
                    r = R[b]
                    for k in range(8):
                        kb.mm(lp[b][:, 0:36], h2T[:, k, tt * 128:(tt + 1) * 128].bitcast(F32), wr[:, k, :], start=(k == 0), stop=(k == 7))
                    lg = r[0]
                    kb.tt("dve", lg[:, 0:36], lp[b][:, 0:36], br[:], ALU.add)
                    gmax, ngmax, gsum, gw = r[1][:, 0:1], r[1][:, 1:2], r[1][:, 2:3], r[1][:, 3:4]
                    kb.op("dve", lambda g: g.tensor_reduce(gmax, lg[:, 0:4], AX.X, ALU.max), reads=[lg], writes=[r[1]])
                    kb.ts("dve", ngmax, gmax, -1.0, ALU.mult)
                    kb.act(r[2][:, 0:4], lg[:, 0:4], AF.Exp, bias=ngmax)
                    kb.op("dve", lambda g: g.tensor_reduce(gsum, r[2][:, 0:4], AX.X, ALU.add), reads=[r[2]], writes=[r[1]])
                    kb.recip(gw, gsum)
                    kb.ts("dve", r[3][:, 0:4], lg[:, 0:4], gmax, ALU.is_equal)
                    kb.ts("dve", r[3][:, 0:4], r[3][:, 0:4], -1.0, ALU.add, 1e30, ALU.mult)
                    kb.tt("dve", r[4][:, 0:32].rearrange("p (a b) -> p a b", b=8), lg[:, 4:36].rearrange("p (a b) -> p a b", b=8),
                          r[3][:, 0:4].unsqueeze(2).to_broadcast([128, 4, 8]), ALU.add)
                    kb.op("dve", lambda g: g.max(r[5][:, 0:8], r[4][:, 0:32]), reads=[r[4]], writes=[r[5]])
                    m1, m2 = r[5][:, 0:1], r[5][:, 1:2]
                    kb.ts("dve", r[6][:, 0:32], r[4][:, 0:32], m1, ALU.is_equal)
                    kb.ts("dve", r[7][:, 0:32], r[4][:, 0:32], m2, ALU.is_equal)
                    dm, ee, p1, p2 = r[8][:, 0:1], r[8][:, 1:2], r[8][:, 2:3], r[8][:, 3:4]
                    kb.tt("dve", dm, m2, m1, ALU.subtract)
                    kb.act(ee, dm, AF.Exp)
                    kb.ts("dve", p1, ee, 1.0, ALU.add)
                    kb.recip(p1, p1)
                    kb.tt("dve", p2, ee, p1, ALU.mult)
                    kb.tt("dve", p1, p1, gw, ALU.mult)
                    kb.tt("dve", p2, p2, gw, ALU.mult)
                    kb.ts("dve", r[9][:, 0:32], r[6][:, 0:32], p1, ALU.mult)
                    kb.stt("dve", r[9][:, 0:32], r[7][:, 0:32], p2, r[9][:, 0:32], ALU.mult, ALU.add)
                    kb.tr(gp[b][0:32, 0:128], r[9][:, 0:32], ident)
                    kb.cp("act", gateT[0:32, tt * 128:(tt + 1) * 128], gp[b][0:32, 0:128])
                kb.barrier()
            Gall = psb("me_G", [128, 4, GN], F32R)
            w13 = [[psb(f"me_w{a}_{i}", [128, 8, 128], F32R) for i in range(2)] for a in (1, 3)]
            w2 = [psb(f"me_w2_{i}", [128, 4, D], F32R) for i in range(2)]
            sel = [psb(f"me_sel{i}", [128, 128], F32R) for i in range(2)]
            sil = [psb(f"me_sil{i}", [128, 512]) for i in range(2)]
            hp = [pps(f"me_hp{i}", [128, 512]) for i in range(4)]
            bp = [pps(f"me_bp{i}", [128, 512]) for i in range(2)]
            yp = [pps(f"me_yp{i}", [128, 512]) for i in range(2)]
            cnt = 0
            for e in range(32):
                se = sel[e % 2]
                kb.op("pool", lambda g: g.affine_select(se[0:32, :], onesf[0:32, :], [[0, 128]], ALU.is_equal, 0.0,
                                                        base=-e, channel_multiplier=1), reads=[cst], writes=[se])
                kb.dma("pool", w2[e % 2][:], G["w2_d"][L, e].rearrange("(j p) c -> p j c", p=128), writes=[w2[e % 2]])
                for j in range(4):
                    wa, wb_ = w13[0][(e * 4 + j) % 2], w13[1][(e * 4 + j) % 2]
                    kb.dma("pool", wa[:], G["w1_d"][L, e, :, j * 128:(j + 1) * 128].rearrange("(k p) c -> p k c", p=128), writes=[wa])
                    kb.dma("pool", wb_[:], G["w3_d"][L, e, :, j * 128:(j + 1) * 128].rearrange("(k p) c -> p k c", p=128), writes=[wb_])
                    for (t0, n, o) in gch:
                        b = cnt % 2
                        cnt += 1
                        for k in range(8):
                            kb.mm(hp[b][:, 0:n], wa[:, k, :], h2T[:, k, o:o + n], start=(k == 0), stop=(k == 7))
                        for k in range(8):
                            kb.mm(hp[2 + b][:, 0:n], wb_[:, k, :], h2T[:, k, o:o + n], start=(k == 0), stop=(k == 7))
                        kb.mm(bp[b][:, 0:n], se[0:32, :], gateT[0:32, o:o + n])
                        kb.act(sil[b][:, 0:n], hp[b][:, 0:n], AF.Silu)
                        kb.tt("dve", sil[b][:, 0:n], sil[b][:, 0:n], hp[2 + b][:, 0:n], ALU.mult)
                        kb.tt("dve", Gall[:, j, o:o + n], sil[b][:, 0:n], bp[b][:, 0:n], ALU.mult)
                for (t0, n, o) in gch:
                    s_ = 1 if t0 < NCTX else 0
                    for f in range(8):
                        p = yp[f % 2]
                        for j in range(4):
                            kb.mm(p[:, 0:n], w2[e % 2][:, j, f * 128:(f + 1) * 128], Gall[:, j, o:o + n], start=(j == 0), stop=(j == 3))
                        kb.stt("dve", xT[:, f, t0:t0 + n], p[:, 0:n], modT[:, L, 40 + f, s_:s_ + 1], xT[:, f, t0:t0 + n], ALU.mult, ALU.add)
            kb.barrier()


def phase_moe_sparse(G):
    nc, kb, L = G["nc"], G["kb"], G["L"]
    xT, modT, modA, onesr, ident, cst = G["xT"], G["modT"], G["modA"], G["onesr"], G["ident"], G["cst"]
    h2tok_d, xs_d, ys_d = G["h2tok_d"], G["xs_d"], G["ys_d"]
    need_ctx = L < DEPTH - 1
    chs = CHUNKS if need_ctx else CHUNKS[1:]
    tiles = [t0 // 128 + j for (t0, n) in chs for j in range(n // 128)]
    NTL = len(tiles)
    onesf = cst[:, 128:256]
    with ExitStack() as ph:
        psb = lambda name, shape, dt=F32: ph.enter_context(nc.sbuf_tensor(uq(name), shape, dt))
        pps = lambda name, shape, dt=F32: ph.enter_context(nc.psum_tensor(uq(name), shape, dt))
        pr = ExitStack()
        rsb_ = lambda name, shape, dt=F32: pr.enter_context(nc.sbuf_tensor(uq(name), shape, dt))
        GA = psb("ms_GA", [128, 18])
        GB = psb("ms_GB", [128, 18])
        D1f = psb("ms_D1f", [128, 18])
        D2f = psb("ms_D2f", [128, 18])
        D1i = psb("ms_D1i", [128, 18], I32)
        D2i = psb("ms_D2i", [128, 18], I32)
        idxW = psb("ms_idxW", [128, 128], I32)
        p0 = ExitStack()
        h2T = p0.enter_context(nc.sbuf_tensor(uq("ms_h2T"), [128, 8, NT], F32R))
        OH1 = rsb_("ms_OH1", [128, 18, 32])
        OH2 = rsb_("ms_OH2", [128, 18, 32])
        AA = rsb_("ms_AA", [128, 18, 32])
        with ExitStack() as p1:
            qsb = lambda name, shape, dt=F32: p1.enter_context(nc.sbuf_tensor(uq(name), shape, dt))
            qps = lambda name, shape, dt=F32: p1.enter_context(nc.psum_tensor(uq(name), shape, dt))
            emit_norm(nc, kb, xT, h2T, [(t0, n, t0) for (t0, n) in chs], lambda k, s_: modA[:, L, 1, k, s_:s_ + 1],
                      lambda k, s_: modT[:, L, 24 + k, s_:s_ + 1], onesr, D)
            wr = qsb("ms_wr", [128, 8, 36])
            br = qsb("ms_br", [128, 36])
            kb.dma("sp", wr[:], G["wr_d"][L].rearrange("(k p) c -> p k c", p=128), writes=[wr])
            kb.dma("sp", br[:], G["br_d"][L].to_broadcast([128, 36]), writes=[br])
            lp = [qps(f"ms_lp{i}", [128, 512]) for i in range(2)]
            R = [[qsb(f"ms_r{b}_{i}", [128, 40]) for i in range(10)] for b in range(2)]
            for i, tt in enumerate(tiles):
                b = i % 2
                r = R[b]
                tsl = slice(tt * 128, (tt + 1) * 128)
                for k in range(8):
                    kb.mm(lp[b][:, 0:36], h2T[:, k, tsl].bitcast(F32), wr[:, k, :], start=(k == 0), stop=(k == 7))
                lg = r[0]
                kb.tt("dve", lg[:, 0:36], lp[b][:, 0:36], br[:], ALU.add)
                gmax, ngmax, gsum, gw = r[1][:, 0:1], r[1][:, 1:2], r[1][:, 2:3], r[1][:, 3:4]
                kb.op("dve", lambda g: g.tensor_reduce(gmax, lg[:, 0:4], AX.X, ALU.max), reads=[lg], writes=[r[1]])
                kb.ts("dve", ngmax, gmax, -1.0, ALU.mult)
                kb.act(r[2][:, 0:4], lg[:, 0:4], AF.Exp, bias=ngmax)
                kb.op("dve", lambda g: g.tensor_reduce(gsum, r[2][:, 0:4], AX.X, ALU.add), reads=[r[2]], writes=[r[1]])
                kb.recip(gw, gsum)
                kb.ts("dve", r[3][:, 0:4], lg[:, 0:4], gmax, ALU.is_equal)
                kb.ts("dve", r[3][:, 0:4], r[3][:, 0:4], -1.0, ALU.add, 1e30, ALU.mult)
                kb.tt("dve", r[4][:, 0:32].rearrange("p (a b) -> p a b", b=8), lg[:, 4:36].rearrange("p (a b) -> p a b", b=8),
                      r[3][:, 0:4].unsqueeze(2).to_broadcast([128, 4, 8]), ALU.add)
                kb.op("dve", lambda g: g.max(r[5][:, 0:8], r[4][:, 0:32]), reads=[r[4]], writes=[r[5]])
                m1, m2 = r[5][:, 0:1], r[5][:, 1:2]
                kb.ts("dve", OH1[:, i, :], r[4][:, 0:32], m1, ALU.is_equal)
                kb.ts("dve", OH2[:, i, :], r[4][:, 0:32], m2, ALU.is_equal)
                kb.tt("dve", AA[:, i, :], OH1[:, i, :], OH2[:, i, :], ALU.add)
                dm, ee, p1_, p2_ = r[8][:, 0:1], r[8][:, 1:2], r[8][:, 2:3], r[8][:, 3:4]
                kb.tt("dve", dm, m2, m1, ALU.subtract)
                kb.act(ee, dm, AF.Exp)
                kb.ts("dve", p1_, ee, 1.0, ALU.add)
                kb.recip(p1_, p1_)
                kb.tt("dve", p2_, ee, p1_, ALU.mult)
                kb.tt("dve", GA[:, i:i + 1], p1_, gw, ALU.mult)
                kb.tt("dve", GB[:, i:i + 1], p2_, gw, ALU.mult)
            kb.barrier()
        with ExitStack() as p2:
            qsb = lambda name, shape, dt=F32: p2.enter_context(nc.sbuf_tensor(uq(name), shape, dt))
            qps = lambda name, shape, dt=F32: p2.enter_context(nc.psum_tensor(uq(name), shape, dt))
            ltri = qsb("ms_ltri", [128, 128])
            kb.op("pool", lambda g: g.affine_select(ltri[:], onesf, [[1, 128]], ALU.is_gt, 0.0, base=0, channel_multiplier=-1),
                  reads=[cst], writes=[ltri])
            Rp = [qps(f"ms_Rp{i}", [128, 512]) for i in range(2)]
            Cp = qps("ms_Cp", [128, 512])
            for i in range(NTL):
                out = Rp[i // 16][:, (i % 16) * 32:(i % 16 + 1) * 32]
                kb.mm(out, ltri[:], AA[:, i, :], start=True, stop=(i == 0))
                for i2 in range(i):
                    kb.mm(out, onesf, AA[:, i2, :], start=False, stop=(i2 == i - 1))
            for i in range(NTL):
                kb.mm(Cp[:, 0:32], onesf, AA[:, i, :], start=(i == 0), stop=(i == NTL - 1))
            w_ = [qsb(f"ms_w{i}", [128, 32]) for i in range(8)]
            cnt, x_, kf, msk, nb, pend, pstart, tmp = w_
            kb.cp("dve", cnt[:], Cp[:, 0:32])
            kb.ts("dve", x_[:], cnt[:], -float(SLOT), ALU.add, 0.0, ALU.max)
            kb.ts("dve", x_[:], x_[:], 127.0, ALU.add, 1.0 / 128, ALU.mult)
            ki = qsb("ms_ki", [128, 32], I32)
            kb.cp("dve", ki[:], x_[:])
            kb.cp("dve", kf[:], ki[:])
            kb.tt("dve", msk[:], kf[:], x_[:], ALU.is_gt)
            kb.tt("dve", kf[:], kf[:], msk[:], ALU.subtract)
            kb.ts("dve", tmp[:], kf[:], 1.0, ALU.add)
            kb.tt("dve", msk[:], tmp[:], x_[:], ALU.is_le)
            kb.tt("dve", nb[:], kf[:], msk[:], ALU.add)
            kb.scan(pend[:], onesf[:, 0:32], nb[:], 0.0, ALU.mult, ALU.add)
            kb.tt("dve", pstart[:], pend[:], nb[:], ALU.subtract)
            base1_i = qsb("ms_b1i", [128, 32], I32)
            base1 = qsb("ms_b1", [128, 32])
            kb.op("pool", lambda g: g.iota(base1_i[:], [[SLOT, 32]], base=0, channel_multiplier=0), writes=[base1_i])
            kb.cp("dve", base1[:], base1_i[:])
            kb.ts("dve", pstart[:], pstart[:], 128.0, ALU.mult, float(32 * SLOT - SLOT), ALU.add)
            kb.tt("dve", pstart[:], pstart[:], base1[:], ALU.subtract)
            t3 = qsb("ms_t3", [128, 32])
            t4 = qsb("ms_t4", [128, 32])
            for i in range(NTL):
                Rv = Rp[i // 16][:, (i % 16) * 32:(i % 16 + 1) * 32]
                kb.ts("dve", t4[:], Rv, float(SLOT), ALU.is_ge)
                kb.tt("dve", t4[:], t4[:], pstart[:], ALU.mult)
                kb.tt("dve", t3[:], Rv, base1[:], ALU.add)
                kb.tt("dve", t3[:], t3[:], t4[:], ALU.add)
                kb.tt("dve", t4[:], t3[:], OH1[:, i, :], ALU.mult)
                kb.op("dve", lambda g: g.tensor_reduce(D1f[:, i:i + 1], t4[:], AX.X, ALU.add), reads=[t4], writes=[D1f])
                kb.tt("dve", t4[:], t3[:], OH2[:, i, :], ALU.mult)
                kb.op("dve", lambda g: g.tensor_reduce(D2f[:, i:i + 1], t4[:], AX.X, ALU.add), reads=[t4], writes=[D2f])
            kb.cp("dve", D1i[:, 0:NTL], D1f[:, 0:NTL])
            kb.cp("dve", D2i[:, 0:NTL], D2f[:, 0:NTL])
            pidx_i = qsb("ms_pidx_i", [128, 1], I32)
            pidx = qsb("ms_pidx", [128, 1])
            kb.op("pool", lambda g: g.iota(pidx_i[:], [[0, 1]], base=0, channel_multiplier=1), writes=[pidx_i])
            kb.cp("dve", pidx[:], pidx_i[:])
            be = qsb("ms_be", [128, 1])
            kb.ts("dve", tmp[:], pend[:], pidx[:, 0:1], ALU.is_le)
            kb.op("dve", lambda g: g.tensor_reduce(be[:], tmp[:], AX.X, ALU.add), reads=[tmp], writes=[be])
            kb.ts("dve", be[:], be[:], 31.0, ALU.min)
            vb = qsb("ms_vb", [128, 1])
            kb.ts("dve", vb[:], pidx[:], pend[:, 31:32], ALU.is_lt)
            kb.tt("dve", be[:], be[:], vb[:], ALU.mult)
            kb.ts("dve", vb[:], vb[:], -1.0, ALU.add, -1000.0, ALU.mult)
            kb.tt("dve", be[:], be[:], vb[:], ALU.add)
            dg = qsb("ms_dg", [128, 128])
            kb.ts("dve", dg[:], ident, be[:, 0:1], ALU.mult)
            kb.mm(Cp[:, 128:256], onesf, dg[:])
            bef = qsb("ms_bef", [128, 128])
            kb.ts("dve", bef[:], Cp[:, 128:256], 128.0, ALU.mult, pidx[:, 0:1], ALU.add)
            if L > 0:
                kb.ts("dve", bef[:], bef[:], float(L * 32 * 128), ALU.add)
            kb.cp("dve", idxW[:], bef[:])
            kb.barrier()
        pr.close()
        with ExitStack() as p3:
            qsb = lambda name, shape, dt=F32: p3.enter_context(nc.sbuf_tensor(uq(name), shape, dt))
            qps = lambda name, shape, dt=F32: p3.enter_context(nc.psum_tensor(uq(name), shape, dt))
            hk = [qsb(f"ms_hs{i}", [128, D]) for i in range(3)]
            tp = [qps(f"ms_tp{i}", [128, 512]) for i in range(4)]
            for i, tt in enumerate(tiles):
                h = hk[i % 3]
                tsl = slice(tt * 128, (tt + 1) * 128)
                for half in range(2):
                    p = tp[(2 * i + half) % 4]
                    for j in range(4):
                        k = half * 4 + j
                        kb.tr(p[:, j * 128:(j + 1) * 128], h2T[:, k, tsl].bitcast(F32), ident)
                    kb.cp("act" if half == 0 else "dve", h[:, half * 512:(half + 1) * 512], p[:])
                kb.idma(xs_d, bass.IndirectOffsetOnAxis(ap=D1i[:, i:i + 1], axis=0), h[:], None, reads=[h, D1i], writes=["xs"])
                kb.idma(xs_d, bass.IndirectOffsetOnAxis(ap=D2i[:, i:i + 1], axis=0), h[:], None, reads=[h, D2i], writes=["xs"])
            kb.barrier()
        p0.close()
        with ExitStack() as p4:
            qsb = lambda name, shape, dt=F32: p4.enter_context(nc.sbuf_tensor(uq(name), shape, dt))
            qps = lambda name, shape, dt=F32: p4.enter_context(nc.psum_tensor(uq(name), shape, dt))
            W1 = [qsb(f"ms_W1_{i}", [128, 8 * 512], F32R) for i in range(2)]
            W3 = [qsb(f"ms_W3_{i}", [128, 8 * 512], F32R) for i in range(2)]
            W2 = [qsb(f"ms_W2_{i}", [128, 4 * D], F32R) for i in range(2)]
            Xb = [qsb(f"ms_Xb{i}", [128, D]) for i in range(3)]
            XT = [qsb(f"ms_XT{i}", [128, 8, 128], F32R) for i in range(2)]
            SL = [qsb(f"ms_SL{i}", [128, 512]) for i in range(2)]
            Gt = [qsb(f"ms_Gt{i}", [128, 4, 128], F32R) for i in range(2)]
            Ys = [qsb("ms_Ys0", [128, D])] * 2
            tpp = [qps(f"ms_tq{i}", [128, 512]) for i in range(2)]
            hp1 = [qps("ms_h1", [128, 512])] * 2
            hp3 = [qps("ms_h3", [128, 512])] * 2
            ypp = [qps(f"ms_yp{i}", [128, 512]) for i in range(2)]
            xtp = [qps(f"ms_xq{i}", [128, 512]) for i in range(2)]
            def xload(i):
                if i < len(subs):
                    kb.dma("sp", Xb[i % 3][:], xs_d[subs[i][0]:subs[i][0] + 128, :], reads=["xs"], writes=[Xb[i % 3]])

            def stageA(row0, W1t, W3t, xq, xb3):
                for half in range(2):
                    p = xtp[half]
                    for j in range(4):
                        k = half * 4 + j
                        kb.tr(p[:, j * 128:(j + 1) * 128], Xb[xb3][:, k * 128:(k + 1) * 128], ident)
                    kb.cp("act" if half == 0 else "dve", XT[xq][:, half * 4:half * 4 + 4, :], p[:].rearrange("p (a b) -> p a b", b=128))
                w1v = W1t[:].rearrange("p (k c) -> p k c", c=512)
                w3v = W3t[:].rearrange("p (k c) -> p k c", c=512)
                for k in range(8):
                    kb.mm(hp1[xq][:], XT[xq][:, k, :], w1v[:, k, :], start=(k == 0), stop=(k == 7))
                    kb.mm(hp3[xq][:], XT[xq][:, k, :], w3v[:, k, :], start=(k == 0), stop=(k == 7))
                kb.act(SL[xq][:], hp1[xq][:], AF.Silu)
                kb.tt("dve", SL[xq][:], SL[xq][:], hp3[xq][:], ALU.mult)

            def stageB(row0, W2t, xq):
                w2v = W2t[:].rearrange("p (j c) -> p j c", c=D)
                gp_ = tpp[xq]
                for j in range(4):
                    kb.tr(gp_[:, j * 128:(j + 1) * 128], SL[xq][:, j * 128:(j + 1) * 128], ident)
                kb.cp("act", Gt[xq][:].rearrange("p a b -> p (a b)"), gp_[:])
                for half in range(2):
                    yp = ypp[half]
                    for j in range(4):
                        kb.mm(yp[:], Gt[xq][:, j, :], w2v[:, j, half * 512:(half + 1) * 512], start=(j == 0), stop=(j == 3))
                    kb.cp("act" if half == 0 else "dve", Ys[xq][:, half * 512:(half + 1) * 512], yp[:])
                kb.dma("sp", ys_d[row0:row0 + 128, :], Ys[xq][:], reads=[Ys[xq]], writes=["ys"])

            subs = []
            wcnt = 0
            for e in range(32):
                pb = wcnt % 2
                wcnt += 1

                def ld(e=e, pb=pb):
                    kb.dma("pool", W1[pb][:], G["w1_d"][L, e * 128:(e + 1) * 128, :], writes=[W1[pb]])
                    kb.dma("pool", W3[pb][:], G["w3_d"][L, e * 128:(e + 1) * 128, :], writes=[W3[pb]])
                    kb.dma("pool", W2[pb][:], G["w2_d"][L, e * 128:(e + 1) * 128, :], writes=[W2[pb]])
                for j in range(SLOT // 128):
                    subs.append((e * SLOT + j * 128, pb, ld if j == 0 else None))
            for b in range(NOV):
                pb = wcnt % 2
                wcnt += 1

                def ld(b=b, pb=pb):
                    off = bass.IndirectOffsetOnAxis(ap=idxW[:, b:b + 1], axis=0)
                    bnd = (L + 1) * 32 * 128 - 1
                    kb.idma(W1[pb][:], None, G["w1_d"].rearrange("l r c -> (l r) c"), off, reads=[idxW], writes=[W1[pb]], bounds=bnd)
                    kb.idma(W3[pb][:], None, G["w3_d"].rearrange("l r c -> (l r) c"), off, reads=[idxW], writes=[W3[pb]], bounds=bnd)
                    kb.idma(W2[pb][:], None, G["w2_d"].rearrange("l r c -> (l r) c"), off, reads=[idxW], writes=[W2[pb]], bounds=bnd)
                subs.append((32 * SLOT + b * 128, pb, ld))
            xload(0)
            xload(1)
            for i, (row0, pb, ld) in enumerate(subs):
                if ld is not None:
                    ld()
                xload(i + 2)
                stageA(row0, W1[pb], W3[pb], i % 2, i % 3)
                if i >= 1:
                    r1, pb1, _ = subs[i - 1]
                    stageB(r1, W2[pb1], (i - 1) % 2)
            r1, pb1, _ = subs[-1]
            stageB(r1, W2[pb1], (len(subs) - 1) % 2)
            kb.barrier()
        with ExitStack() as p5:
            qsb = lambda name, shape, dt=F32: p5.enter_context(nc.sbuf_tensor(uq(name), shape, dt))
            qps = lambda name, shape, dt=F32: p5.enter_context(nc.psum_tensor(uq(name), shape, dt))
            y1 = [qsb(f"ms_y1_{i}", [128, D]) for i in range(2)]
            y2 = [qsb(f"ms_y2_{i}", [128, D]) for i in range(2)]
            tq = [qps(f"ms_cq{i}", [128, 512]) for i in range(4)]
            for i, tt in enumerate(tiles):
                pb = i % 2
                s_ = 1 if tt * 128 < NCTX else 0
                kb.idma(y1[pb][:], None, ys_d, bass.IndirectOffsetOnAxis(ap=D1i[:, i:i + 1], axis=0), reads=["ys", D1i], writes=[y1[pb]])
                kb.idma(y2[pb][:], None, ys_d, bass.IndirectOffsetOnAxis(ap=D2i[:, i:i + 1], axis=0), reads=["ys", D2i], writes=[y2[pb]])
                kb.ts("dve", y1[pb][:], y1[pb][:], GA[:, i:i + 1], ALU.mult)
                kb.stt("dve", y1[pb][:], y2[pb][:], GB[:, i:i + 1], y1[pb][:], ALU.mult, ALU.add)
                for half in range(2):
                    p = tq[(2 * i + half) % 4]
                    for j in range(4):
                        k = half * 4 + j
                        kb.tr(p[:, j * 128:(j + 1) * 128], y1[pb][:, k * 128:(k + 1) * 128], ident)
                    for j in range(4):
                        k = half * 4 + j
                        kb.stt("dve", xT[:, k, tt * 128:(tt + 1) * 128], p[:, j * 128:(j + 1) * 128], modT[:, L, 40 + k, s_:s_ + 1],
                               xT[:, k, tt * 128:(tt + 1) * 128], ALU.mult, ALU.add)
            kb.barrier()


def phase_final(G):
    nc, kb = G["nc"], G["kb"]
    xT, onesr, ident, Vn = G["xT"], G["onesr"], G["ident"], G["Vn"]
    out_d = G["out_d"]
    with ExitStack() as ph:
        psb = lambda name, shape, dt=F32: ph.enter_context(nc.sbuf_tensor(uq(name), shape, dt))
        pps = lambda name, shape, dt=F32: ph.enter_context(nc.psum_tensor(uq(name), shape, dt))
        fo = psb("fn_fo", [128, 8, NLAT])
        emit_norm(nc, kb, xT, fo, [(t0, n, t0 - NCTX) for (t0, n) in CHUNKS[1:]],
                  lambda k, s_: Vn("final_g")[:, k:k + 1], None, onesr, D)
        ost = [psb(f"fn_ost{i}", [128, D]) for i in range(2)]
        tp = [pps(f"fn_tp{i}", [128, 512]) for i in range(4)]
        for tt in range(NLAT // 128):
            o = ost[tt % 2]
            for half in range(2):
                p = tp[(2 * tt + half) % 4]
                for j in range(4):
                    k = half * 4 + j
                    kb.tr(p[:, j * 128:(j + 1) * 128], fo[:, k, tt * 128:(tt + 1) * 128], ident)
                if half == 0:
                    kb.cp("dve", o[:, 0:512], p[:])
                else:
                    kb.cp("act", o[:, 512:1024], p[:])
            kb.dma("sp", out_d[tt * 128:(tt + 1) * 128, :], o[:], reads=[o], writes=["out"])
        kb.barrier()


def host_prep(inputs):
    f = lambda a: np.ascontiguousarray(np.asarray(a, dtype=np.float32))
    w_in = f(inputs["w_in"])
    perm = np.concatenate([np.arange(8, 16), np.arange(0, 8), np.arange(24, 32), np.arange(16, 24)]) + 640
    w_in_x = np.concatenate([w_in, w_in[:, :, 576:672], w_in[:, :, 576:640], w_in[:, :, perm]], axis=2)
    consts = np.concatenate([np.eye(128, dtype=np.float32), np.ones((128, 128), np.float32)], axis=1)
    shared = {"consts": consts, "mod_w": f(inputs["mod_w"]), "w_in_x": np.ascontiguousarray(w_in_x)}
    s5B = np.zeros((DEPTH, 2, 2, 8, 128, 128), np.float32)
    s5C = np.zeros((DEPTH, 2, 2, 8, 128, 128), np.float32)
    for ri, (bn, cn) in enumerate((("s5_b_re", "s5_c_re"), ("s5_b_im", "s5_c_im"))):
        bb = f(inputs[bn])
        cc = f(inputs[cn])
        for g in range(16):
            s_ = g // 2
            ct = s_ // 4
            q0 = (g - 2 * s_) * 64
            c0 = (g - 8 * ct) * 16
            s5B[:, :, ri, s_, c0:c0 + 16, q0:q0 + 64] = bb[:, :, g].transpose(0, 1, 3, 2)
            s5C[:, :, ri, s_, q0:q0 + 64, c0:c0 + 16] = cc[:, :, g].transpose(0, 1, 3, 2)
    shared["s5B"] = s5B
    shared["s5C"] = s5C
    shared["s5_glu_w"] = f(inputs["s5_glu_w"])
    rows = NLAT // 64
    row = np.repeat(np.arange(rows, dtype=np.float32), 64)
    col = np.tile(np.arange(64, dtype=np.float32), rows)
    inv = (np.float32(10000.0) ** (-np.arange(8, dtype=np.float32) / np.float32(8))).astype(np.float32)
    rope = np.zeros((128, 2, NLAT), np.float32)
    for r in range(32):
        i, axis, half = r % 8, r // 16, (r // 8) % 2
        ang = ((row if axis == 0 else col) * inv[i]).astype(np.float32)
        rope[64 + r, 0] = np.cos(ang)
        rope[64 + r, 1] = np.sin(ang) * (-1.0 if half == 0 else 1.0)
    shared["rope"] = rope
    wuq = f(inputs["mla_w_uq"]).reshape(DEPTH, 256, 8, 96)
    pperm = np.concatenate([np.arange(64), 64 + np.concatenate([np.arange(8, 16), np.arange(0, 8), np.arange(24, 32), np.arange(16, 24)])])
    shared["wq"] = np.ascontiguousarray(np.stack([wuq, wuq[:, :, :, pperm]], axis=2).reshape(DEPTH, 256, 2, 768))
    shared["mla_w_uk"] = f(inputs["mla_w_uk"])
    shared["hg_lb_logits"] = f(inputs["hg_lb_logits"]).reshape(1, DEPTH)
    shared["wp"] = np.ascontiguousarray(np.concatenate([f(inputs["w_pa"]), f(inputs["w_pb"]), f(inputs["w_pc"])], axis=1))
    shared["w_out"] = f(inputs["w_out"])
    shared["wr"] = np.ascontiguousarray(np.concatenate([f(inputs["moe_w_group"]), f(inputs["moe_w_expert"])], axis=2))
    shared["br"] = np.ascontiguousarray(np.concatenate([f(inputs["moe_b_group"]), f(inputs["moe_b_expert"])], axis=1).reshape(DEPTH, 1, 36))
    for nm_, kt in (("moe_w1", 8), ("moe_w3", 8), ("moe_w2", 4)):
        w_ = f(inputs[nm_])
        cols = w_.shape[-1]
        shared[nm_] = np.ascontiguousarray(w_.reshape(DEPTH, 32, kt, 128, cols).transpose(0, 1, 3, 2, 4)).reshape(DEPTH, 32 * 128, kt * cols)
    shared["mla_w_uv"] = f(inputs["mla_w_uv"])
    in_maps = []
    for b in range(8):
        vecs = np.zeros((NVBLK * 128, 128), np.float32)

        def put(name, arr):
            r0, n = VEC_ROWS[name]
            vecs[r0:r0 + n, :] = np.asarray(arr, np.float32).reshape(n, 128)
        put("c", inputs["c"][b]); put("c_ctx", inputs["c_ctx"]); put("final_g", inputs["final_norm_g"])
        for l in range(DEPTH):
            put(f"norm1_g{l}", inputs["norm1_g"][l]); put(f"norm2_g{l}", inputs["norm2_g"][l])
            put(f"mod_b{l}", inputs["mod_b"][l]); put(f"s5_d{l}", inputs["s5_d"][l])
            put(f"glu_b{l}", inputs["s5_glu_b"][l]); put(f"qa_g{l}", inputs["mla_qa_g"][l])
            put(f"kva_g{l}", inputs["mla_kva_g"][l])
            hg = np.zeros((4, 128), np.float32)
            hg[:, 0:64] = np.asarray(inputs["hg_norm_g"][l], np.float32).reshape(4, 64)
            put(f"hgn_g{l}", hg)
            for d in range(2):
                put(f"lam_re{l}{d}", inputs["s5_lam_re"][l, d]); put(f"lam_im{l}{d}", inputs["s5_lam_im"][l, d])
                put(f"lstep{l}{d}", np.repeat(np.asarray(inputs["s5_log_step"][l, d], np.float32), 64))
        m = dict(shared)
        m["x"] = f(inputs["x"][b]); m["ctx"] = f(inputs["ctx"][b]); m["vecs"] = vecs
        in_maps.append(m)
    return in_maps


def kernel(**inputs):
    in_maps = host_prep(inputs)
    nc = build_nc()
    res = run_bass_kernel_spmd(nc, in_maps, core_ids=list(range(8)))
    return np.stack([r["out"] for r in res.results], axis=0)
```

```python
import math
from contextlib import ExitStack

import numpy as np
import concourse.bass as bass
import concourse.mybir as mybir
from concourse.bass_utils import run_bass_kernel_spmd

F32 = mybir.dt.float32
F32R = mybir.dt.float32r
I32 = mybir.dt.int32
AF = mybir.ActivationFunctionType
ALU = mybir.AluOpType
AX = mybir.AxisListType

D = 1024
NCTX = 256
NLAT = 2048
NT = NCTX + NLAT
DEPTH = 2
EPS = 1e-6
CHUNKS = [(0, 256)] + [(256 + 512 * i, 512) for i in range(4)]
SLOT = 256
NOV = 35
NROWS = 32 * SLOT + NOV * 128

COLT = []
def _ct(name, start, width):
    COLT.append((name, start, width))
for i in range(2): _ct(f"s5u{i}", 0 + 128 * i, 128)
for i in range(2): _ct(f"cq{i}", 256 + 128 * i, 128)
_ct("ckv", 512, 128)
_ct("kpeA", 5792, 96)
_ct("kpeB", 5792 + 96, 96)
for i in range(4): _ct(f"hq{i}", 672 + 128 * i, 128)
for i in range(4): _ct(f"hf{i}", 1184 + 128 * i, 128)
for i in range(4): _ct(f"hb{i}", 1696 + 128 * i, 128)
for i in range(4): _ct(f"hi{i}", 2208 + 64 * i, 64)
for i in range(4): _ct(f"hg{i}", 2464 + 64 * i, 64)
N_NONGATE = len(COLT)
for b in range(3):
    for i in range(8): _ct(f"gate{b}_{i}", 2720 + 1024 * b + 128 * i, 128)
COLIDX = {n: i for i, (n, _, _) in enumerate(COLT)}
WINX = 5792 + 192

VEC_ROWS = {}
def _vr(name, n):
    VEC_ROWS[name] = (sum(v[1] for v in VEC_ROWS.values()), n)
_vr("c", 8); _vr("c_ctx", 8); _vr("final_g", 8)
for l in range(DEPTH):
    _vr(f"norm1_g{l}", 8); _vr(f"norm2_g{l}", 8); _vr(f"mod_b{l}", 48)
    _vr(f"s5_d{l}", 2); _vr(f"glu_b{l}", 2); _vr(f"qa_g{l}", 2); _vr(f"kva_g{l}", 1)
    _vr(f"hgn_g{l}", 4)
    for d in range(2):
        _vr(f"lam_re{l}{d}", 8); _vr(f"lam_im{l}{d}", 8); _vr(f"lstep{l}{d}", 8)
NVROWS = sum(v[1] for v in VEC_ROWS.values())
NVBLK = (NVROWS + 127) // 128


_UQ = [0]


def uq(name):
    _UQ[0] += 1
    return f"{name}~{_UQ[0]}"


class Buf:
    __slots__ = ("name", "w", "r")

    def __init__(self, name):
        self.name = name
        self.w = None
        self.r = {}


class KB:
    RING = 12

    def __init__(self, nc, es):
        self.nc, self.es = nc, es
        self.eng = dict(pe=nc.tensor, dve=nc.vector, act=nc.scalar, pool=nc.gpsimd, sp=nc.sync)
        self.psem, self.pcnt, self.nsem = {}, {}, 0
        for e in self.eng:
            self._new_psem(e)
        self.waited = {}
        self.rings = {}
        self.rpos = {}
        for q in ("sp", "pool", "act"):
            self.rings[q] = [[self._sem(f"dq_{q}{i}"), 0] for i in range(self.RING)]
            self.rpos[q] = 0
        self.bufs = {}
        self.ninstr = 0

    def _sem(self, name):
        self.nsem += 1
        return self.es.enter_context(self.nc.semaphore(name))

    def _new_psem(self, e):
        self.psem[e] = self._sem(f"p_{e}_{self.nsem}")
        self.pcnt[e] = 0

    def buf(self, name):
        b = self.bufs.get(name)
        if b is None:
            b = self.bufs[name] = Buf(name)
        return b

    def _wait(self, e, tok):
        sem, val, src = tok
        key = (e, id(sem))
        if self.waited.get(key, 0) >= val:
            return
        self.eng[e].wait_ge(sem, val)
        self.waited[key] = val

    def _sync(self, e, reads, writes):
        for b in reads:
            if b.w is not None:
                self._dep(e, b.w)
        for b in writes:
            if b.w is not None:
                self._dep(e, b.w)
            for t in b.r.values():
                self._dep(e, t)

    def _dep(self, e, tok):
        if e == "pe" and tok[2] == "pe":
            return
        self._wait(e, tok)

    def _commit(self, tok, reads, writes):
        for b in writes:
            b.w = tok
            b.r = {}
        for b in reads:
            b.r[tok[2]] = tok

    def op(self, e, fn, reads=(), writes=()):
        reads = [b if isinstance(b, Buf) else self.buf(self._nm(b)) for b in reads]
        writes = [b if isinstance(b, Buf) else self.buf(self._nm(b)) for b in writes]
        self._sync(e, reads, writes)
        ins = fn(self.eng[e])
        if self.pcnt[e] >= 20000:
            self._new_psem(e)
        self.pcnt[e] += 1
        ins.then_inc(self.psem[e], 1)
        self.ninstr += 1
        self._commit((self.psem[e], self.pcnt[e], e), reads, writes)

    def dma(self, q, out, in_, reads=(), writes=()):
        reads = [b if isinstance(b, Buf) else self.buf(self._nm(b)) for b in reads]
        writes = [b if isinstance(b, Buf) else self.buf(self._nm(b)) for b in writes]
        self._sync(q, reads, writes)
        slot = self.rings[q][self.rpos[q] % self.RING]
        self.rpos[q] += 1
        sem, cnt = slot
        if cnt > 0:
            self._wait(q, (sem, cnt, "dma"))
        self.eng[q].dma_start(out=out, in_=in_).then_inc(sem, 16)
        slot[1] = cnt + 16
        self.ninstr += 1
        self._commit((sem, cnt + 16, f"dma_{q}{(self.rpos[q] - 1) % self.RING}"), reads, writes)

    @staticmethod
    def _nm(x):
        if isinstance(x, str):
            return x
        if isinstance(x, tuple):
            return KB._nm(x[0]) + x[1]
        if hasattr(x, "tensor"):
            return x.tensor.name.split("~")[0]
        return x.name.split("~")[0]

    def _names(self, xs):
        return [self._nm(x) for x in xs if not isinstance(x, (int, float)) and x is not None]

    def tt(self, e, out, a, b, op, rn=(), wn=()):
        self.op(e, lambda g: g.tensor_tensor(out, a, b, op), reads=self._names([a, b]) + list(rn), writes=self._names([out]) + list(wn))

    def ts(self, e, out, a, s1, op0, s2=None, op1=None, rn=(), wn=()):
        if op1 is None:
            self.op(e, lambda g: g.tensor_scalar(out, a, s1, None, op0), reads=self._names([a, s1]) + list(rn), writes=self._names([out]) + list(wn))
        else:
            self.op(e, lambda g: g.tensor_scalar(out, a, s1, s2, op0, op1), reads=self._names([a, s1, s2]) + list(rn), writes=self._names([out]) + list(wn))

    def stt(self, e, out, a, sc_, b, op0, op1, rn=(), wn=()):
        self.op(e, lambda g: g.scalar_tensor_tensor(out, a, sc_, b, op0, op1), reads=self._names([a, sc_, b]) + list(rn), writes=self._names([out]) + list(wn))

    def act(self, out, in_, func, bias=None, scale=1.0, rn=(), wn=()):
        kw = {}
        if bias is not None:
            kw["bias"] = bias
        self.op("act", lambda g: g.activation(out, in_, func, scale=scale, **kw), reads=self._names([in_, bias, scale]) + list(rn), writes=self._names([out]) + list(wn))

    def cp(self, e, out, in_, rn=(), wn=()):
        if e == "act":
            self.op(e, lambda g: g.copy(out, in_), reads=self._names([in_]) + list(rn), writes=self._names([out]) + list(wn))
        else:
            self.op(e, lambda g: g.tensor_copy(out, in_), reads=self._names([in_]) + list(rn), writes=self._names([out]) + list(wn))

    def mm(self, out, lhsT, rhs, start=True, stop=True, rn=(), wn=()):
        self.op("pe", lambda g: g.matmul(out, lhsT, rhs, start=start, stop=stop), reads=self._names([lhsT, rhs]) + list(rn), writes=self._names([out]) + list(wn))

    def tr(self, out, in_, ident, rn=(), wn=()):
        self.op("pe", lambda g: g.transpose(out, in_, ident), reads=self._names([in_, ident]) + list(rn), writes=self._names([out]) + list(wn))

    def scan(self, out, d0, d1, init, op0, op1, rn=(), wn=()):
        self.op("dve", lambda g: g.tensor_tensor_scan(out, d0, d1, init, op0, op1), reads=self._names([d0, d1, init]) + list(rn), writes=self._names([out]) + list(wn))

    def recip(self, out, in_):
        self.op("dve", lambda g: g.reciprocal(out, in_), reads=self._names([in_]), writes=self._names([out]))

    def memset(self, e, out, val):
        self.op(e, lambda g: g.memset(out, val), reads=[], writes=self._names([out]))

    def barrier(self):
        toks = [(self.psem[o], self.pcnt[o], o) for o in self.eng if self.pcnt[o] > 0]
        for q in self.rings:
            for sem, cnt in self.rings[q]:
                if cnt > 0:
                    toks.append((sem, cnt, "dma"))
        for e in self.eng:
            for t in toks:
                if t[2] != e:
                    self._wait(e, t)

    def idma(self, out, out_off, in_, in_off, reads=(), writes=(), bounds=None):
        q = "pool"
        reads = [b if isinstance(b, Buf) else self.buf(self._nm(b)) for b in reads]
        writes = [b if isinstance(b, Buf) else self.buf(self._nm(b)) for b in writes]
        self._sync(q, reads, writes)
        slot = self.rings[q][self.rpos[q] % self.RING]
        self.rpos[q] += 1
        sem, cnt = slot
        if cnt > 0:
            self._wait(q, (sem, cnt, "dma"))
        if bounds is None:
            self.eng[q].indirect_dma_start(out=out, out_offset=out_off, in_=in_, in_offset=in_off).then_inc(sem, 16)
        else:
            if not hasattr(self, "_breg") or self._breg[0] != bounds:
                self._breg = (bounds, self.eng[q].to_reg(bounds))
            self.eng[q].indirect_dma_start(out=out, out_offset=out_off, in_=in_, in_offset=in_off,
                                           bounds_check=self._breg[1], oob_is_err=False).then_inc(sem, 16)
        slot[1] = cnt + 16
        self.ninstr += 1
        self._commit((sem, cnt + 16, f"dma_{q}{(self.rpos[q] - 1) % self.RING}"), reads, writes)

    def finish(self, bufs):
        for b in bufs:
            b = self.buf(b) if isinstance(b, str) else b
            if b.w is not None:
                self._wait("sp", b.w)


def build_nc(stop_after=None, debug=False):
    nc = bass.Bass("TRN2", target_bir_lowering=False)
    okind = "ExternalOutput" if debug else "Internal"
    x_d = nc.dram_tensor("x", [NLAT, D], F32, kind="ExternalInput").ap()
    ctx_d = nc.dram_tensor("ctx", [NCTX, D], F32, kind="ExternalInput").ap()
    vecs_d = nc.dram_tensor("vecs", [NVBLK * 128, 128], F32, kind="ExternalInput").ap()
    consts_d = nc.dram_tensor("consts", [128, 256], F32, kind="ExternalInput").ap()
    modw_d = nc.dram_tensor("mod_w", [DEPTH, D, 6 * D], F32, kind="ExternalInput").ap()
    winx_d = nc.dram_tensor("w_in_x", [DEPTH, D, WINX], F32, kind="ExternalInput").ap()
    out_d = nc.dram_tensor("out", [NLAT, D], F32, kind="ExternalOutput").ap()
    zT_d = nc.dram_tensor("zT", [len(COLT) * 128, NT], F32, kind=okind).ap()
    ybT_d = nc.dram_tensor("ybT", [D, NT], F32, kind=okind).ap()
    s5B_d = nc.dram_tensor("s5B", [DEPTH, 2, 2, 8, 128, 128], F32, kind="ExternalInput").ap()
    s5C_d = nc.dram_tensor("s5C", [DEPTH, 2, 2, 8, 128, 128], F32, kind="ExternalInput").ap()
    gluw_d = nc.dram_tensor("s5_glu_w", [DEPTH, 256, 256], F32, kind="ExternalInput").ap()
    rope_d = nc.dram_tensor("rope", [128, 2, NLAT], F32, kind="ExternalInput").ap()
    wq_d = nc.dram_tensor("wq", [DEPTH, 256, 2, 768], F32, kind="ExternalInput").ap()
    wuk_d = nc.dram_tensor("mla_w_uk", [DEPTH, 128, 512], F32, kind="ExternalInput").ap()
    wuv_d = nc.dram_tensor("mla_w_uv", [DEPTH, 128, 512], F32, kind="ExternalInput").ap()
    lbl_d = nc.dram_tensor("hg_lb_logits", [1, DEPTH], F32, kind="ExternalInput").ap()
    wp_d = nc.dram_tensor("wp", [DEPTH, D, D], F32, kind="ExternalInput").ap()
    wo_d = nc.dram_tensor("w_out", [DEPTH, D, D], F32, kind="ExternalInput").ap()
    wr_d = nc.dram_tensor("wr", [DEPTH, D, 36], F32, kind="ExternalInput").ap()
    br_d = nc.dram_tensor("br", [DEPTH, 1, 36], F32, kind="ExternalInput").ap()
    w1_d = nc.dram_tensor("moe_w1", [DEPTH, 32 * 128, 8 * 512], F32, kind="ExternalInput").ap()
    w3_d = nc.dram_tensor("moe_w3", [DEPTH, 32 * 128, 8 * 512], F32, kind="ExternalInput").ap()
    w2_d = nc.dram_tensor("moe_w2", [DEPTH, 32 * 128, 4 * D], F32, kind="ExternalInput").ap()
    h2tok_d = nc.dram_tensor("h2tok", [NT, D], F32, kind=okind).ap()
    xs_d = nc.dram_tensor("xs", [NROWS, D], F32, kind=okind).ap()
    ys_d = nc.dram_tensor("ys", [NROWS, D], F32, kind=okind).ap()
    xdbg_d = nc.dram_tensor("xdbg", [128, 8 * NT], F32, kind=okind).ap()
    modT_d = nc.dram_tensor("modT", [DEPTH, 128, 96], F32, kind=okind).ap()

    with ExitStack() as es:
        kb = KB(nc, es)
        sb = lambda name, shape, dt=F32: es.enter_context(nc.sbuf_tensor(uq(name), shape, dt))

        xT = sb("xT", [128, 8, NT])
        cst = sb("cst", [128, 256])
        ident = cst[:, 0:128]
        onesr = sb("onesr", [128, 128], F32R)
        vecT = sb("vecT", [128, NVBLK * 128])
        modT = sb("modT_sb", [128, DEPTH, 48, 2])
        modA = sb("modA", [128, DEPTH, 2, 8, 2])
        sc = sb("sc", [128, 8, 2], F32R)

        def V(name, j=0):
            r0, n = VEC_ROWS[name]
            return vecT[:, r0 + j:r0 + j + 1]

        def Vn(name):
            r0, n = VEC_ROWS[name]
            return vecT[:, r0:r0 + n]

        kb.dma("sp", cst[:], consts_d, writes=["cst"])
        kb.dma("pool", onesr[:], consts_d[:, 128:256], writes=["onesr"])

        with ExitStack() as ph:
            psb = lambda name, shape, dt=F32: ph.enter_context(nc.sbuf_tensor(uq(name), shape, dt))
            pps = lambda name, shape, dt=F32: ph.enter_context(nc.psum_tensor(uq(name), shape, dt))
            stg = [psb(f"xstg{i}", [128, D]) for i in range(3)]
            tps = [pps(f"tps{i}", [128, 4, 128]) for i in range(4)]
            for blk in range(NVBLK):
                s = stg[blk % 3]
                kb.dma("sp", s[:, 0:128], vecs_d[blk * 128:(blk + 1) * 128, :], writes=[f"xstg{blk % 3}"])
                kb.op("pe", lambda e: e.transpose(tps[blk % 4][:, 0, :], s[:, 0:128], ident),
                      reads=[f"xstg{blk % 3}", "cst"], writes=[f"tps{blk % 4}"])
                kb.op("dve", lambda e: e.tensor_copy(vecT[:, blk * 128:(blk + 1) * 128], tps[blk % 4][:, 0, :]),
                      reads=[f"tps{blk % 4}"], writes=["vecT"])
            n_tt = NT // 128
            for tt in range(n_tt):
                s = stg[tt % 3]
                src = ctx_d[tt * 128:(tt + 1) * 128, :] if tt < 2 else x_d[(tt - 2) * 128:(tt - 1) * 128, :]
                kb.dma("sp", s[:], src, writes=[f"xstg{tt % 3}"])
                for half in range(2):
                    pi = (2 * tt + half) % 4
                    for j in range(4):
                        k = half * 4 + j
                        kb.op("pe", lambda e: e.transpose(tps[pi][:, j, :], s[:, k * 128:(k + 1) * 128], ident),
                              reads=[f"xstg{tt % 3}", "cst"], writes=[f"tps{pi}"])
                    eng = "dve" if half == 0 else "act"
                    dst = xT[:, half * 4:half * 4 + 4, tt * 128:(tt + 1) * 128]
                    if eng == "dve":
                        kb.op("dve", lambda e: e.tensor_copy(dst, tps[pi][:]), reads=[f"tps{pi}"], writes=["xT"])
                    else:
                        kb.op("act", lambda e: e.copy(dst, tps[pi][:]), reads=[f"tps{pi}"], writes=["xT"])
            kb.barrier()

        kb.op("act", lambda e: e.activation(sc[:, :, 0], Vn("c"), AF.Silu), reads=["vecT"], writes=["sc"])
        kb.op("act", lambda e: e.activation(sc[:, :, 1], Vn("c_ctx"), AF.Silu), reads=["vecT"], writes=["sc"])

        xTd_d = nc.dram_tensor("xTd", [128, 8 * NT], F32, kind=okind).ap()
        if debug and stop_after == "p0":
            kb.dma("sp", xTd_d, xT[:].rearrange("p a b -> p (a b)"), reads=["xT"], writes=["xTd"])
        lbv = sb("lbv", [128, 2, DEPTH])
        lbl = sb("lbl", [128, DEPTH])
        kb.dma("sp", lbl[:], lbl_d.to_broadcast([128, DEPTH]), writes=["lbl"])
        kb.memset("dve", lbv[:, 0, :], 0.0)
        kb.tt("dve", lbv[:, 0, 1:2], lbl[:, 1:2], lbl[:, 0:1], ALU.subtract)
        kb.act(lbv[:, 0, 1:2], lbv[:, 0, 1:2], AF.Sigmoid)
        kb.ts("dve", lbv[:, 1, :], lbv[:, 0, :], -1.0, ALU.mult, 1.0, ALU.add)
        for layer in range(DEPTH if stop_after != "p0" else 0):
            L = layer
            with ExitStack() as ph:
                psb = lambda name, shape, dt=F32: ph.enter_context(nc.sbuf_tensor(uq(name), shape, dt))
                pps = lambda name, shape, dt=F32: ph.enter_context(nc.psum_tensor(uq(name), shape, dt))
                mw = [psb(f"mw{i}", [128, 8, 128], F32R) for i in range(3)]
                mps = pps("mps", [128, 48, 2])
                for j in range(48):
                    w = mw[j % 3]
                    kb.dma("pool", w[:], modw_d[L, :, j * 128:(j + 1) * 128].rearrange("(k p) c -> p k c", p=128),
                           writes=[f"mw{j % 3}"])
                    for k in range(8):
                        kb.op("pe", lambda e: e.matmul(mps[:, j, :], w[:, k, :], sc[:, k, :], start=(k == 0), stop=(k == 7)),
                              reads=[f"mw{j % 3}", "sc"], writes=["mps"])
                for s in range(2):
                    kb.op("dve", lambda e: e.tensor_tensor(modT[:, L, :, s], mps[:, :, s], Vn(f"mod_b{L}"), ALU.add),
                          reads=["mps", "vecT"], writes=["modT"])
                for ni, (gname, t0) in enumerate(((f"norm1_g{L}", 8), (f"norm2_g{L}", 32))):
                    for s in range(2):
                        kb.op("dve", lambda e: e.scalar_tensor_tensor(
                            modA[:, L, ni, :, s], modT[:, L, t0:t0 + 8, s], 1.0, Vn(gname), ALU.add, ALU.mult),
                            reads=["modT", "vecT"], writes=["modA"])
                if debug:
                    kb.dma("sp", modT_d[L], modT[:, L].rearrange("p a b -> p (a b)"), reads=["modT"], writes=["modT_d"])
                kb.barrier()
            if stop_after == f"mod{L}":
                break

            with ExitStack() as ph:
                psb = lambda name, shape, dt=F32: ph.enter_context(nc.sbuf_tensor(uq(name), shape, dt))
                pps = lambda name, shape, dt=F32: ph.enter_context(nc.psum_tensor(uq(name), shape, dt))
                hT = psb("hT", [128, 8, NT], F32R)
                emit_norm(nc, kb, xT, hT, [(t0, n, t0) for (t0, n) in CHUNKS],
                          lambda k, s_: modA[:, L, 0, k, s_:s_ + 1], lambda k, s_: modT[:, L, k, s_:s_ + 1], onesr, D)
                wb = [psb(f"wb{i}", [128, 8, 128], F32R) for i in range(3)]
                zs = [psb(f"zs{i}", [128, 512]) for i in range(4)]
                zp = [pps(f"zp{i}", [128, 512]) for i in range(4)]
                cnt = 0
                for ti, (name, c0, wd) in enumerate(COLT):
                    w = wb[ti % 3]
                    kb.dma("pool", w[:, :, 0:wd], winx_d[L, :, c0:c0 + wd].rearrange("(k p) c -> p k c", p=128),
                           writes=[f"wb{ti % 3}"])
                    for (t0, n) in CHUNKS:
                        pi = cnt % 4
                        cnt += 1
                        for k in range(8):
                            kb.op("pe", lambda e: e.matmul(zp[pi][0:wd, 0:n], w[:, k, 0:wd], hT[:, k, t0:t0 + n],
                                                           start=(k == 0), stop=(k == 7)),
                                  reads=[f"wb{ti % 3}", "hT"], writes=[f"zp{pi}"])
                        if cnt % 2 == 0:
                            kb.op("dve", lambda e: e.tensor_copy(zs[pi][0:wd, 0:n], zp[pi][0:wd, 0:n]),
                                  reads=[f"zp{pi}"], writes=[f"zs{pi}"])
                        else:
                            kb.op("act", lambda e: e.copy(zs[pi][0:wd, 0:n], zp[pi][0:wd, 0:n]),
                                  reads=[f"zp{pi}"], writes=[f"zs{pi}"])
                        kb.dma("sp", zT_d[ti * 128:ti * 128 + wd, t0:t0 + n], zs[pi][0:wd, 0:n],
                               reads=[f"zs{pi}"], writes=[f"zT_{ti}"])
                kb.barrier()
            if stop_after == f"A{L}":
                break
            G = dict(nc=nc, kb=kb, L=L, xT=xT, cst=cst, ident=ident, onesr=onesr, vecT=vecT, modT=modT, modA=modA,
                     V=V, Vn=Vn, zT_d=zT_d, ybT_d=ybT_d, s5B_d=s5B_d, s5C_d=s5C_d, gluw_d=gluw_d, debug=debug,
                     rope_d=rope_d, wq_d=wq_d, wuk_d=wuk_d, wuv_d=wuv_d)
            if not (debug and stop_after in (f"C{L}", f"D{L}")):
                phase_s5(G)
            if stop_after == f"B{L}":
                break
            G["lbv"] = lbv
            if not (debug and stop_after in (f"D{L}",)):
                phase_mla(G)
            if stop_after == f"C{L}":
                break
            G.update(wp_d=wp_d, wo_d=wo_d, wr_d=wr_d, br_d=br_d, w1_d=w1_d, w3_d=w3_d, w2_d=w2_d, out_d=out_d,
                     h2tok_d=h2tok_d, xs_d=xs_d, ys_d=ys_d)
            phase_hg(G)
            if stop_after == f"D{L}":
                break
            phase_merge(G)
            if stop_after == f"E{L}":
                kb.dma("sp", xdbg_d, xT[:].rearrange("p a b -> p (a b)"), reads=["xT"], writes=["xdbg"])
                break
            phase_moe_sparse(G)
            if stop_after == f"F{L}":
                kb.dma("sp", xdbg_d, xT[:].rearrange("p a b -> p (a b)"), reads=["xT"], writes=["xdbg"])
                break
        else:
            phase_final(G)

        kb.finish(list(kb.bufs.values()))
        print("instructions:", kb.ninstr, "sems:", kb.nsem, "sbuf left:", nc.sbuf_bytes_remaining)
    return nc


def emit_norm(nc, kb, xT, dst, chunks, A, Sh, onesr, dmodel):
    with ExitStack() as ns:
        psb = lambda name, shape, dt=F32: ns.enter_context(nc.sbuf_tensor(uq(name), shape, dt))
        pps = lambda name, shape, dt=F32: ns.enter_context(nc.psum_tensor(uq(name), shape, dt))
        sq = [psb(f"nsq{i}", [128, 8, 512], F32R) for i in range(2)]
        ms = [pps(f"nms{i}", [128, 512]) for i in range(2)]
        rs = [psb(f"nrs{i}", [128, 512]) for i in range(2)]
        tmp = [psb(f"ntmp{i}", [128, 512]) for i in range(2)]
        for ci, (t0, n, d0) in enumerate(chunks):
            s = 1 if t0 < NCTX else 0
            b = ci % 2
            for k in range(8):
                kb.act(sq[b][:, k, 0:n], xT[:, k, t0:t0 + n], AF.Square)
            for k in range(8):
                kb.mm(ms[b][:, 0:n], onesr[:], sq[b][:, k, 0:n], start=(k == 0), stop=(k == 7))
            kb.act(rs[b][:, 0:n], ms[b][:, 0:n], AF.Sqrt, scale=1.0 / dmodel, bias=EPS)
            kb.recip(rs[b][:, 0:n], rs[b][:, 0:n])
            for k in range(8):
                tb = k % 2
                sh = Sh(k, s) if Sh is not None else None
                if sh is None:
                    kb.stt("dve", dst[:, k, d0:d0 + n], xT[:, k, t0:t0 + n], A(k, s), rs[b][:, 0:n], ALU.mult, ALU.mult)
                else:
                    kb.stt("dve", tmp[tb][:, 0:n], xT[:, k, t0:t0 + n], A(k, s), rs[b][:, 0:n], ALU.mult, ALU.mult)
                    kb.act(dst[:, k, d0:d0 + n], tmp[tb][:, 0:n], AF.Identity, bias=sh, scale=1.0)
        kb.barrier()


TWO_PI = 2.0 * math.pi


def range_reduce(kb, r, x, tM, tI):
    kb.ts("dve", tM, x, 1.0 / TWO_PI, ALU.mult)
    kb.cp("dve", tI, tM)
    kb.cp("dve", tM, tI)
    kb.stt("dve", r, tM, -TWO_PI, x, ALU.mult, ALU.add)
    kb.ts("dve", tM, r, math.pi, ALU.is_gt)
    kb.stt("dve", r, tM, -TWO_PI, r, ALU.mult, ALU.add)
    kb.ts("dve", tM, r, -math.pi, ALU.is_lt)
    kb.stt("dve", r, tM, TWO_PI, r, ALU.mult, ALU.add)
    kb.ts("dve", r, r, 3.1415925, ALU.min, -3.1415925, ALU.max)


def sincos(kb, sn, cs, x, r, tM, tI):
    range_reduce(kb, r, x, tM, tI)
    kb.act(sn, r, AF.Sin)
    kb.ts("dve", tM, x, math.pi / 2, ALU.add)
    range_reduce(kb, r, tM, tM, tI) if False else None
    return


def phase_s5(G):
    nc, kb, L = G["nc"], G["kb"], G["L"]
    Vn = G["Vn"]
    zT_d, ybT_d = G["zT_d"], G["ybT_d"]
    T = 256
    NCH = NT // T
    with ExitStack() as ph:
        psb = lambda name, shape, dt=F32: ph.enter_context(nc.sbuf_tensor(uq(name), shape, dt))
        pps = lambda name, shape, dt=F32: ph.enter_context(nc.psum_tensor(uq(name), shape, dt))
        uT = psb("s5_uT", [128, 2, NT], F32R)
        yacc = psb("s5_yacc", [128, 2, NT])
        for t in range(2):
            ti = COLIDX[f"s5u{t}"]
            kb.dma("pool", uT[:, t, :], zT_d[ti * 128:(ti + 1) * 128, :], reads=[f"zT_{ti}"], writes=["s5_uT"])
        Bw = psb("s5_Bw", [128, 2, 8, 128], F32R)
        Cw = psb("s5_Cw", [128, 2, 8, 128], F32R)
        iota_i = psb("s5_iota_i", [128, T + 1], I32)
        iota_f = psb("s5_iota_f", [128, T + 1])
        kb.op("pool", lambda g: g.iota(iota_i[:], [[1, T + 1]], base=0, channel_multiplier=0), writes=["s5_iota_i"])
        kb.cp("dve", iota_f[:], iota_i[:])
        COS = psb("s5_COS", [128, 8, T + 1])
        SIN = psb("s5_SIN", [128, 8, T + 1])
        ang = psb("s5_ang", [128, 8, T + 1])
        rr = psb("s5_rr", [128, 8, T + 1])
        ERE = ang[:, :, 0:T]
        EIM = rr[:, :, 0:T]
        tM = psb("s5_tM", [128, 8, T + 1])
        tI = psb("s5_tI", [128, 8, T + 1], I32)
        sm = psb("s5_sm", [128, 24, 8])
        gin = psb("s5_gin", [128, 8, 2])
        tmp = [[psb(f"s5_t{b}_{i}", [128, T]) for i in range(8)] for b in range(2)]
        hh = [[psb(f"s5_h{b}_{i}", [128, T], F32R) for i in range(2)] for b in range(2)]
        Pp = [pps(f"s5_P{i}", [128, 2, T]) for i in range(3)]
        Yp = [[pps(f"s5_Y{b}_{ct}", [128, 512]) for ct in range(2)] for b in range(2)]
        flat = lambda t: t[:].rearrange("p a b -> p (a b)")
        for d in range(2):
            kb.dma("pool", Bw[:].rearrange("c r s q -> c (r s) q"),
                   G["s5B_d"][L, d].rearrange("r s c q -> c (r s) q"), writes=["s5_Bw"])
            kb.dma("pool", Cw[:].rearrange("c r s q -> c (r s) q"),
                   G["s5C_d"][L, d].rearrange("r s c q -> c (r s) q"), writes=["s5_Cw"])
            kb.ts("pool", Cw[:, 1], Cw[:, 1].bitcast(F32), -1.0, ALU.mult)
            lre, lim, lst = Vn(f"lam_re{L}{d}"), Vn(f"lam_im{L}{d}"), Vn(f"lstep{L}{d}")
            c_ = lambda i: sm[:, i, :]
            DT, MAG, TH, SN, CS, LBR, LBI, DEN, FR, FI, X1, X2, X3, MI = (c_(i) for i in range(14))
            kb.act(DT, lst, AF.Exp)
            kb.tt("dve", X1, lre, DT, ALU.mult)
            kb.act(MAG, X1, AF.Exp)
            kb.tt("dve", TH, lim, DT, ALU.mult)
            smI = tI[:, 0, 0:8]
            range_reduce(kb, X2, TH, X3, smI)
            kb.act(SN, X2, AF.Sin)
            kb.ts("dve", X1, TH, math.pi / 2, ALU.add)
            range_reduce(kb, X2, X1, X3, smI)
            kb.act(CS, X2, AF.Sin)
            kb.tt("dve", LBR, MAG, CS, ALU.mult)
            kb.tt("dve", LBI, MAG, SN, ALU.mult)
            kb.tt("dve", X1, lre, lre, ALU.mult)
            kb.tt("dve", X2, lim, lim, ALU.mult)
            kb.tt("dve", DEN, X1, X2, ALU.add)
            kb.recip(DEN, DEN)
            kb.ts("dve", X3, LBR, -1.0, ALU.add)
            kb.tt("dve", X1, X3, lre, ALU.mult)
            kb.tt("dve", X2, LBI, lim, ALU.mult)
            kb.tt("dve", X1, X1, X2, ALU.add)
            kb.tt("dve", FR, X1, DEN, ALU.mult)
            kb.tt("dve", X1, LBI, lre, ALU.mult)
            kb.tt("dve", X2, X3, lim, ALU.mult)
            kb.tt("dve", X1, X1, X2, ALU.subtract)
            kb.tt("dve", FI, X1, DEN, ALU.mult)
            kb.tt("dve", ang[:], TH.unsqueeze(2).to_broadcast([128, 8, T + 1]),
                  iota_f[:].unsqueeze(1).to_broadcast([128, 8, T + 1]), ALU.mult)
            range_reduce(kb, flat(rr), flat(ang), flat(tM), flat(tI))
            kb.act(flat(SIN), flat(rr), AF.Sin)
            kb.ts("dve", flat(ang), flat(ang), math.pi / 2, ALU.add)
            range_reduce(kb, flat(rr), flat(ang), flat(tM), flat(tI))
            kb.act(flat(COS), flat(rr), AF.Sin)
            frb = FR.unsqueeze(2).to_broadcast([128, 8, T])
            fib = FI.unsqueeze(2).to_broadcast([128, 8, T])
            tF = tI[:].bitcast(F32)
            kb.tt("dve", tM[:, :, 0:T], COS[:, :, 0:T], frb, ALU.mult)
            kb.tt("dve", tF[:, :, 0:T], SIN[:, :, 0:T], fib, ALU.mult)
            kb.tt("dve", ERE, tM[:, :, 0:T], tF[:, :, 0:T], ALU.add)
            kb.tt("dve", tM[:, :, 0:T], COS[:, :, 0:T], fib, ALU.mult)
            kb.tt("dve", tF[:, :, 0:T], SIN[:, :, 0:T], frb, ALU.mult)
            kb.tt("dve", EIM, tM[:, :, 0:T], tF[:, :, 0:T], ALU.subtract)
            kb.memset("dve", gin[:], 0.0)
            order = list(range(NCH)) if d == 0 else [0] + list(range(NCH - 1, 0, -1))
            units = [(oi, ci, s_) for oi, ci in enumerate(order) for s_ in range(8)]

            def stageA(u):
                oi, ci, s_ = units[u]
                t0 = ci * T
                ct = s_ // 4
                tq = tmp[u % 2]
                P = Pp[u % 3]
                kb.mm(P[:, 0, :], Bw[:, 0, s_, :], uT[:, ct, t0:t0 + T])
                kb.mm(P[:, 1, :], Bw[:, 1, s_, :], uT[:, ct, t0:t0 + T])
                Pre = P[:, 0, ::-1] if d == 1 else P[:, 0, :]
                Pim = P[:, 1, ::-1] if d == 1 else P[:, 1, :]
                kb.tt("dve", tq[0][:], ERE[:, s_, :], Pre, ALU.mult)
                kb.tt("dve", tq[1][:], EIM[:, s_, :], Pim, ALU.mult)
                kb.tt("pool", tq[4][:], tq[0][:], tq[1][:], ALU.subtract)
                kb.tt("dve", tq[2][:], ERE[:, s_, :], Pim, ALU.mult)
                kb.tt("dve", tq[3][:], EIM[:, s_, :], Pre, ALU.mult)
                kb.tt("pool", tq[5][:], tq[2][:], tq[3][:], ALU.add)

            def stageB(u):
                oi, ci, s_ = units[u]
                t0 = ci * T
                ct = s_ // 4
                yb_ = oi % 2
                tq = tmp[u % 2]
                hb = hh[u % 2]
                rb = MAG[:, s_:s_ + 1].to_broadcast([128, T])
                kb.scan(tq[6][:], rb, tq[4][:], gin[:, s_, 0:1], ALU.mult, ALU.add)
                kb.scan(tq[7][:], rb, tq[5][:], gin[:, s_, 1:2], ALU.mult, ALU.add)
                cT, sT = COS[:, s_, T:T + 1], SIN[:, s_, T:T + 1]
                lr, li = tq[6][:, T - 1:T], tq[7][:, T - 1:T]
                xa, xb = sm[:, 14 + (u % 2) * 2, 0:1], sm[:, 15 + (u % 2) * 2, 0:1]
                kb.ts("dve", xa, li, sT, ALU.mult)
                kb.ts("dve", xb, li, cT, ALU.mult)
                kb.stt("dve", gin[:, s_, 0:1], lr, cT, xa, ALU.mult, ALU.subtract)
                kb.stt("dve", gin[:, s_, 1:2], lr, sT, xb, ALU.mult, ALU.add)
                kb.tt("pool", tq[0][:], COS[:, s_, 0:T], tq[6][:], ALU.mult)
                kb.tt("pool", tq[1][:], SIN[:, s_, 0:T], tq[7][:], ALU.mult)
                kb.tt("pool", hb[0][:], tq[0][:], tq[1][:], ALU.subtract)
                kb.tt("dve", tq[2][:], SIN[:, s_, 0:T], tq[6][:], ALU.mult)
                kb.tt("dve", tq[3][:], COS[:, s_, 0:T], tq[7][:], ALU.mult)
                kb.tt("dve", hb[1][:], tq[2][:], tq[3][:], ALU.add)
                Y = Yp[yb_][ct]
                kb.mm(Y[:, 0:T], Cw[:, 0, s_, :], hb[0][:], start=(s_ % 4 == 0), stop=False)
                kb.mm(Y[:, 0:T], Cw[:, 1, s_, :], hb[1][:], start=False, stop=(s_ % 4 == 3))
                if s_ == 7:
                    for ct2 in range(2):
                        Y2 = Yp[yb_][ct2]
                        if d == 0:
                            kb.cp("act", yacc[:, ct2, t0:t0 + T], Y2[:, 0:T])
                        else:
                            rv = slice(t0 + T - 1, (t0 - 1 if t0 > 0 else None), -1)
                            kb.tt("dve", yacc[:, ct2, rv], yacc[:, ct2, rv], Y2[:, 0:T], ALU.add)

            stageA(0)
            for u in range(len(units)):
                if u + 1 < len(units):
                    stageA(u + 1)
                stageB(u)
        gw = psb("s5_gw", [128, 2, 256], F32R)
        kb.dma("pool", gw[:], G["gluw_d"][L].rearrange("(k p) c -> p k c", p=128), writes=["s5_gw"])
        y1 = psb("s5_y1", [128, 2, 512], F32R)
        for (t0, n) in CHUNKS:
            for ct in range(2):
                a, b2, c2 = tmp[ct][0], tmp[ct][1], tmp[ct][2]
                for h0 in range(0, n, T):
                    sl = slice(t0 + h0, t0 + h0 + T)
                    kb.stt("dve", a[:], uT[:, ct, sl].bitcast(F32), Vn(f"s5_d{L}")[:, ct:ct + 1], yacc[:, ct, sl], ALU.mult, ALU.add)
                    kb.tt("dve", b2[:], a[:], a[:], ALU.mult)
                    kb.ts("dve", b2[:], b2[:], 0.044715, ALU.mult, 1.0, ALU.add)
                    kb.tt("dve", b2[:], b2[:], a[:], ALU.mult)
                    kb.act(c2[:], b2[:], AF.Sigmoid, scale=1.5957691216057308)
                    kb.tt("dve", y1[:, ct, h0:h0 + T], a[:], c2[:], ALU.mult)
            for ct in range(2):
                Y = Yp[0][ct]
                for k in range(2):
                    kb.mm(Y[:, 0:n], gw[:, k, ct * 128:(ct + 1) * 128], y1[:, k, 0:n], start=(k == 0), stop=(k == 1))
                for h0 in range(0, n, T):
                    sg = tmp[ct][3]
                    o = tmp[ct][4]
                    kb.act(sg[:], Y[:, h0:h0 + T], AF.Sigmoid, bias=Vn(f"glu_b{L}")[:, ct:ct + 1])
                    kb.tt("dve", o[:], y1[:, ct, h0:h0 + T].bitcast(F32), sg[:], ALU.mult)
                    kb.dma("sp", ybT_d[ct * 128:(ct + 1) * 128, t0 + h0:t0 + h0 + T], o[:], reads=[o], writes=[f"ybT_{ct}"])
        kb.barrier()


MLA_SCALE = 1.0 / math.sqrt(96.0)


def phase_mla(G):
    nc, kb, L = G["nc"], G["kb"], G["L"]
    Vn, cst, onesr = G["Vn"], G["cst"], G["onesr"]
    zT_d, ybT_d = G["zT_d"], G["ybT_d"]
    need_ctx = L < DEPTH - 1
    with ExitStack() as ph:
        psb = lambda name, shape, dt=F32: ph.enter_context(nc.sbuf_tensor(uq(name), shape, dt))
        pps = lambda name, shape, dt=F32: ph.enter_context(nc.psum_tensor(uq(name), shape, dt))
        cqn = psb("ml_cqn", [128, 2, NT], F32R)
        ckvn = psb("ml_ckvn", [128, NT], F32R)
        KPE = psb("ml_KPE", [128, NT])
        ROPE = psb("ml_rope", [128, 2, NLAT])
        wq = psb("ml_wq", [128, 2, 2, 768], F32R)
        wuk = psb("ml_wuk", [128, 512], F32R)
        wuv = psb("ml_wuv", [128, 512], F32R)
        kb.dma("sp", ROPE[:], G["rope_d"], writes=["ml_rope"])
        kb.dma("pool", wq[:].rearrange("p k v c -> p k (v c)"),
               G["wq_d"][L].rearrange("(k p) v c -> p k (v c)", p=128), writes=["ml_wq"])
        kb.dma("pool", wuk[:], G["wuk_d"][L], writes=["ml_wuk"])
        kb.dma("pool", wuv[:], G["wuv_d"][L], writes=["ml_wuv"])
        ps = [pps(f"ml_ps{i}", [128, 512]) for i in range(7)]
        with ExitStack() as p1:
            qsb = lambda name, shape, dt=F32: p1.enter_context(nc.sbuf_tensor(uq(name), shape, dt))
            cqT = qsb("ml_cqT", [128, 2, NT])
            ckvT = qsb("ml_ckvT", [128, NT])
            kA = qsb("ml_kA", [128, NT])
            kB = qsb("ml_kB", [128, NT])
            for t in range(2):
                ti = COLIDX[f"cq{t}"]
                kb.dma("sp", cqT[:, t, :], zT_d[ti * 128:(ti + 1) * 128, :], reads=[f"zT_{ti}"], writes=["ml_cqT"])
            ti = COLIDX["ckv"]
            kb.dma("sp", ckvT[:], zT_d[ti * 128:(ti + 1) * 128, :], reads=[f"zT_{ti}"], writes=["ml_ckvT"])
            for nm_, tl in (("kpeA", kA), ("kpeB", kB)):
                ti = COLIDX[nm_]
                kb.dma("sp", tl[0:96, :], zT_d[ti * 128:ti * 128 + 96, :], reads=[f"zT_{ti}"], writes=[tl])
            sq = qsb("ml_sq", [128, 3, 512], F32R)
            rs = [qsb(f"ml_rs{i}", [128, 512]) for i in range(2)]
            tt1 = qsb("ml_tt1", [128, 512])
            tt2 = qsb("ml_tt2", [128, 512])
            for (t0, n) in CHUNKS:
                for t in range(2):
                    kb.act(sq[:, t, 0:n], cqT[:, t, t0:t0 + n], AF.Square)
                kb.act(sq[:, 2, 0:n], ckvT[:, t0:t0 + n], AF.Square)
                for t in range(2):
                    kb.mm(ps[0][:, 0:n], onesr[:], sq[:, t, 0:n], start=(t == 0), stop=(t == 1))
                kb.mm(ps[1][:, 0:n], onesr[:], sq[:, 2, 0:n])
                kb.act(rs[0][:, 0:n], ps[0][:, 0:n], AF.Sqrt, scale=1.0 / 256, bias=EPS)
                kb.recip(rs[0][:, 0:n], rs[0][:, 0:n])
                kb.act(rs[1][:, 0:n], ps[1][:, 0:n], AF.Sqrt, scale=1.0 / 128, bias=EPS)
                kb.recip(rs[1][:, 0:n], rs[1][:, 0:n])
                for t in range(2):
                    kb.stt("dve", cqn[:, t, t0:t0 + n], cqT[:, t, t0:t0 + n], Vn(f"qa_g{L}")[:, t:t + 1], rs[0][:, 0:n], ALU.mult, ALU.mult)
                kb.stt("dve", ckvn[:, t0:t0 + n], ckvT[:, t0:t0 + n], Vn(f"kva_g{L}")[:, 0:1], rs[1][:, 0:n], ALU.mult, ALU.mult)
                if t0 < NCTX:
                    kb.cp("dve", KPE[64:96, t0:t0 + n], kA[64:96, t0:t0 + n])
                else:
                    l0 = t0 - NCTX
                    kb.tt("dve", tt1[64:96, 0:n], kA[64:96, t0:t0 + n], ROPE[64:96, 0, l0:l0 + n], ALU.mult)
                    kb.tt("dve", tt2[64:96, 0:n], kB[64:96, t0:t0 + n], ROPE[64:96, 1, l0:l0 + n], ALU.mult)
                    kb.tt("dve", KPE[64:96, t0:t0 + n], tt1[64:96, 0:n], tt2[64:96, 0:n], ALU.add)
            kb.barrier()
        KT = psb("ml_KT", [128, NT], F32R)
        QT = psb("ml_QT", [128, NT], F32R)
        Vh = psb("ml_Vh", [128, 18, 65], F32R)
        PT = [psb(f"ml_PT{i}", [128, 512], F32R) for i in range(3)]
        Osb = [psb(f"ml_Osb{i}", [128, 512]) for i in range(2)]
        ys = [psb(f"ml_ys{i}", [128, 512]) for i in range(2)]
        u1 = psb("ml_u1", [128, 512])
        u2 = psb("ml_u2", [128, 512])
        onesf = cst[:, 128:256]
        kb.cp("dve", Vh[:, :, 64:65], onesf[:, 0:18].unsqueeze(2))
        cnt = 0
        for h in range(8):
            for (t0, n) in CHUNKS:
                kb.mm(ps[0][0:64, 0:n], wuk[:, h * 64:(h + 1) * 64], ckvn[:, t0:t0 + n])
                kb.cp("act", KT[0:64, t0:t0 + n], ps[0][0:64, 0:n])
            kb.cp("dve", KT[64:96, :], KPE[64:96, :])
            for g0 in range(0, 18, 8):
                gn = min(8, 18 - g0)
                for j in range(gn):
                    kt = g0 + j
                    kb.mm(ps[1][:, j * 64:(j + 1) * 64], ckvn[:, kt * 128:(kt + 1) * 128], wuv[:, h * 64:(h + 1) * 64])
                kb.cp("dve", Vh[:, g0:g0 + gn, 0:64], ps[1][:, 0:gn * 64].rearrange("p (a b) -> p a b", b=64))
            for (t0, n) in CHUNKS:
                lat = t0 >= NCTX
                if not lat and not need_ctx:
                    continue
                for k in range(2):
                    kb.mm(ps[0][0:96, 0:n], wq[:, k, 0, h * 96:(h + 1) * 96], cqn[:, k, t0:t0 + n], start=(k == 0), stop=(k == 1))
                if lat:
                    for k in range(2):
                        kb.mm(ps[1][0:96, 0:n], wq[:, k, 1, h * 96:(h + 1) * 96], cqn[:, k, t0:t0 + n], start=(k == 0), stop=(k == 1))
                kb.cp("act", QT[0:64, t0:t0 + n], ps[0][0:64, 0:n])
                if not lat:
                    kb.cp("act", QT[64:96, t0:t0 + n], ps[0][64:96, 0:n])
                else:
                    l0 = t0 - NCTX
                    kb.tt("dve", u1[64:96, 0:n], ps[0][64:96, 0:n], ROPE[64:96, 0, l0:l0 + n], ALU.mult)
                    kb.tt("dve", u2[64:96, 0:n], ps[1][64:96, 0:n], ROPE[64:96, 1, l0:l0 + n], ALU.mult)
                    kb.tt("dve", QT[64:96, t0:t0 + n], u1[64:96, 0:n], u2[64:96, 0:n], ALU.add)
            for (t0, n) in CHUNKS:
                lat = t0 >= NCTX
                if not lat and not need_ctx:
                    continue
                kts = list(range(18)) if lat else [0, 1]
                Op = ps[5 + cnt % 2]
                ob = Osb[cnt % 2]
                yo = ys[cnt % 2]
                bcp = ps[cnt % 2]
                cnt += 1
                def emitS(i):
                    kt = kts[i]
                    kb.mm(ps[2 + i % 3][:, 0:n], KT[0:96, kt * 128:(kt + 1) * 128], QT[0:96, t0:t0 + n])
                for i in range(min(2, len(kts))):
                    emitS(i)
                for i, kt in enumerate(kts):
                    Sp = ps[2 + i % 3]
                    pt = PT[i % 3]
                    kb.act(pt[:, 0:n], Sp[:, 0:n], AF.Exp, scale=MLA_SCALE)
                    if i + 2 < len(kts):
                        emitS(i + 2)
                    kb.mm(Op[0:65, 0:n], Vh[:, kt, :], pt[:, 0:n], start=(i == 0), stop=(i == len(kts) - 1))
                kb.cp("act", ob[0:65, 0:n], Op[0:65, 0:n])
                kb.recip(ob[64:65, 0:n], ob[64:65, 0:n])
                kb.mm(bcp[0:64, 0:n], onesf[64:65, 0:64], ob[64:65, 0:n])
                kb.tt("dve", yo[0:64, 0:n], ob[0:64, 0:n], bcp[0:64, 0:n], ALU.mult)
                kb.dma("sp", ybT_d[256 + h * 64:256 + (h + 1) * 64, t0:t0 + n], yo[0:64, 0:n], reads=[yo], writes=[f"ybT_m{h}"])
        kb.barrier()


def phase_hg(G):
    nc, kb, L = G["nc"], G["kb"], G["L"]
    Vn, cst, onesr, ident, lbv = G["Vn"], G["cst"], G["onesr"], G["ident"], G["lbv"]
    zT_d, ybT_d = G["zT_d"], G["ybT_d"]
    CH = 64
    NC_ = NT // CH
    onesf = cst[:, 128:256]
    with ExitStack() as ph:
        psb = lambda name, shape, dt=F32: ph.enter_context(nc.sbuf_tensor(uq(name), shape, dt))
        pps = lambda name, shape, dt=F32: ph.enter_context(nc.psum_tensor(uq(name), shape, dt))
        A = psb("hg_A", [128, NT])
        KK = psb("hg_KK", [128, NT], F32R)
        Bt = psb("hg_Bt", [128, NT])
        E1 = psb("hg_E1", [128, NT], F32R)
        qT = psb("hg_qT", [128, NT])
        ig = psb("hg_ig", [128, NT])
        itok = psb("hg_itok", [128, NC_, 64], F32R)
        oacc = psb("hg_oacc", [128, NT])
        U = psb("hg_U", [128, NC_, 64])
        PTall = psb("hg_PT", [128, NC_, 64], F32R)
        Sst = psb("hg_Sst", [128, NC_, 64], F32R)
        ktok = [psb(f"hg_ktok{i}", [128, 4, 128], F32R) for i in range(2)]
        sct = [psb(f"hg_sct{i}", [128, 512]) for i in range(2)]
        small = psb("hg_small", [128, 4, NC_])
        S = psb("hg_S", [128, 64])
        tU = psb("hg_tU", [128, 64])
        fin = [psb(f"hg_fin{i}", [128, 512]) for i in range(3)]
        ps = [pps(f"hg_ps{i}", [128, 512]) for i in range(7)]
        pcnt = [0]

        def nps():
            pcnt[0] += 1
            return ps[pcnt[0] % 7]

        b3 = lambda t: t[:].rearrange("p (n c) -> p n c", c=CH)
        for h in range(4):
            tq, ti_, tg = COLIDX[f"hq{h}"], COLIDX[f"hi{h}"], COLIDX[f"hg{h}"]
            kb.dma("sp", qT[:], zT_d[tq * 128:(tq + 1) * 128, :], reads=[f"zT_{tq}"], writes=[qT])
            kb.dma("sp", ig[0:64, :], zT_d[ti_ * 128:ti_ * 128 + 64, :], reads=[f"zT_{ti_}"], writes=[ig])
            for c0 in range(0, NC_, 8):
                gn = min(8, NC_ - c0)
                p = nps()
                for j in range(gn):
                    c = c0 + j
                    kb.tr(p[0:64, j * 64:(j + 1) * 64], ig[0:64, c * CH:(c + 1) * CH], ident[0:64, 0:64])
                kb.cp("act", itok[0:64, c0:c0 + gn, :], p[0:64, 0:gn * 64].rearrange("p (a b) -> p a b", b=64))
            for d in range(2):
                tf = COLIDX[f"hf{h}" if d == 0 else f"hb{h}"]
                kb.dma("sp", A[:], zT_d[tf * 128:(tf + 1) * 128, :], reads=[f"zT_{tf}"], writes=[A])
                kb.act(A[:], A[:], AF.Sigmoid)
                kb.ts("dve", A[:], A[:], lbv[:, 1, L:L + 1], ALU.mult, lbv[:, 0, L:L + 1], ALU.add)
                kb.ts("dve", KK[:], A[:], -1.0, ALU.mult, 1.0, ALU.add)
                kb.act(A[:], A[:], AF.Ln)
                for c in range(NC_):
                    sl = slice(c * CH, (c + 1) * CH)
                    if d == 0:
                        kb.scan(Bt[:, sl], onesf[:, 0:CH], A[:, sl], 0.0, ALU.mult, ALU.add)
                    else:
                        lo = c * CH
                        rv = slice(lo + CH - 1, (lo - 1) if lo > 0 else None, -1)
                        kb.scan(Bt[:, rv], onesf[:, 0:CH], A[:, rv], 0.0, ALU.mult, ALU.add)
                refpos = 31 if d == 0 else 32
                lastpos = 63 if d == 0 else 0
                refc, alpha, gamma, beta = (small[:, i, :] for i in range(4))
                kb.cp("dve", refc, b3(Bt)[:, :, refpos])
                kb.act(alpha, b3(Bt)[:, :, lastpos], AF.Exp)
                kb.act(gamma, refc, AF.Exp)
                kb.tt("dve", b3(Bt), b3(Bt), refc.unsqueeze(2).to_broadcast([128, NC_, CH]), ALU.subtract)
                kb.act(E1[:], Bt[:], AF.Exp)
                kb.cp("dve", beta, b3(E1)[:, :, lastpos].bitcast(F32))
                kb.act(Bt[:], Bt[:], AF.Exp, scale=-1.0)
                kb.tt("dve", E1[:], qT[:], E1[:].bitcast(F32), ALU.mult)
                kb.tt("dve", KK[:], KK[:].bitcast(F32), Bt[:], ALU.mult)
                for c0 in range(0, NC_, 4):
                    p = nps()
                    kt_ = ktok[(c0 // 4) % 2]
                    for j in range(4):
                        c = c0 + j
                        kb.tr(p[0:64, j * 128:(j + 1) * 128], KK[:, c * CH:(c + 1) * CH].bitcast(F32), ident)
                    kb.cp("act", kt_[0:64, :, :], p[0:64, :].rearrange("p (a b) -> p a b", b=128))
                    p2 = nps()
                    for j in range(4):
                        c = c0 + j
                        kb.mm(p2[:, j * 64:(j + 1) * 64], kt_[0:64, j, :], itok[0:64, c, :])
                    kb.cp("dve", U[:, c0:c0 + 4, :], p2[:, 0:256].rearrange("p (a b) -> p a b", b=64))
                for c0 in range(0, NC_, 8):
                    gn = min(8, NC_ - c0)
                    p = nps()
                    for j in range(gn):
                        c = c0 + j
                        kb.mm(p[0:64, j * 64:(j + 1) * 64], KK[:, c * CH:(c + 1) * CH], E1[:, c * CH:(c + 1) * CH])
                    st = sct[(c0 // 8) % 2]
                    kb.cp("act", st[0:64, 0:gn * 64], p[0:64, 0:gn * 64])
                    if d == 0:
                        kb.op("pool", lambda g: g.affine_select(PTall[0:64, c0:c0 + gn, :], st[0:64, 0:gn * 64].rearrange("p (a b) -> p a b", b=64),
                                                                [[0, gn], [1, 64]], ALU.is_ge, 0.0, base=0, channel_multiplier=-1),
                              reads=[st], writes=[PTall])
                    else:
                        kb.op("pool", lambda g: g.affine_select(PTall[0:64, c0:c0 + gn, :], st[0:64, 0:gn * 64].rearrange("p (a b) -> p a b", b=64),
                                                                [[0, gn], [-1, 64]], ALU.is_ge, 0.0, base=0, channel_multiplier=1),
                              reads=[st], writes=[PTall])
                kb.memset("dve", S[:], 0.0)
                order = list(range(NC_)) if d == 0 else [3, 2, 1, 0] + list(range(NC_ - 1, 3, -1))
                kb.tt("dve", U[:], U[:], beta.unsqueeze(2).to_broadcast([128, NC_, 64]), ALU.mult)
                for c in order:
                    kb.ts("dve", Sst[:, c, :], S[:], gamma[:, c:c + 1], ALU.mult)
                    kb.stt("dve", S[:], S[:], alpha[:, c:c + 1], U[:, c, :], ALU.mult, ALU.add)
                for c0 in range(0, NC_, 8):
                    gn = min(8, NC_ - c0)
                    p = nps()
                    for j in range(gn):
                        c = c0 + j
                        kb.mm(p[0:64, j * 64:(j + 1) * 64], itok[0:64, c, :], PTall[0:64, c, :], start=True, stop=False)
                        kb.mm(p[0:64, j * 64:(j + 1) * 64], Sst[:, c, :], E1[:, c * CH:(c + 1) * CH], start=False, stop=True)
                    if d == 0:
                        kb.cp("act", oacc[0:64, c0 * CH:(c0 + gn) * CH], p[0:64, 0:gn * 64])
                    else:
                        kb.tt("dve", oacc[0:64, c0 * CH:(c0 + gn) * CH], oacc[0:64, c0 * CH:(c0 + gn) * CH], p[0:64, 0:gn * 64], ALU.add)
            kb.dma("sp", ig[0:64, :], zT_d[tg * 128:tg * 128 + 64, :], reads=[f"zT_{tg}"], writes=[ig])
            kb.act(KK[0:64, :], oacc[0:64, :], AF.Square)
            kb.act(ig[0:64, :], ig[0:64, :], AF.Silu)
            for (t0, n) in CHUNKS:
                p = nps()
                kb.mm(p[0:64, 0:n], onesr[0:64, 0:64], KK[0:64, t0:t0 + n])
                kb.act(fin[0][0:64, 0:n], p[0:64, 0:n], AF.Sqrt, scale=1.0 / 64, bias=EPS)
                kb.recip(fin[0][0:64, 0:n], fin[0][0:64, 0:n])
                kb.stt("dve", fin[1][0:64, 0:n], oacc[0:64, t0:t0 + n], Vn(f"hgn_g{L}")[0:64, h:h + 1], fin[0][0:64, 0:n], ALU.mult, ALU.mult)
                kb.tt("dve", fin[2][0:64, 0:n], fin[1][0:64, 0:n], ig[0:64, t0:t0 + n], ALU.mult)
                kb.dma("sp", ybT_d[768 + h * 64:768 + (h + 1) * 64, t0:t0 + n], fin[2][0:64, 0:n], reads=[fin[2]], writes=[f"ybT_h{h}"])
        kb.barrier()


def phase_merge(G):
    nc, kb, L = G["nc"], G["kb"], G["L"]
    xT, modT = G["xT"], G["modT"]
    zT_d, ybT_d = G["zT_d"], G["ybT_d"]
    need_ctx = L < DEPTH - 1
    chs = CHUNKS if need_ctx else CHUNKS[1:]
    yb_names = ["ybT_0", "ybT_1"] + [f"ybT_m{h}" for h in range(8)] + [f"ybT_h{h}" for h in range(4)]
    with ExitStack() as ph:
        psb = lambda name, shape, dt=F32: ph.enter_context(nc.sbuf_tensor(uq(name), shape, dt))
        pps = lambda name, shape, dt=F32: ph.enter_context(nc.psum_tensor(uq(name), shape, dt))
        wp = psb("mg_wp", [128, 8, D], F32R)
        wo = psb("mg_wo", [128, 8, D], F32R)
        kb.dma("pool", wp[:], G["wp_d"][L].rearrange("(k p) c -> p k c", p=128), writes=[wp])
        kb.dma("pool", wo[:], G["wo_d"][L].rearrange("(k p) c -> p k c", p=128), writes=[wo])
        yb = psb("mg_yb", [128, 8, 512], F32R)
        mT = psb("mg_mT", [128, 8, 512], F32R)
        gt = [psb(f"mg_gt{i}", [128, 3, 512]) for i in range(2)]
        t3 = [psb(f"mg_t{i}", [128, 512]) for i in range(3)]
        ps = [pps(f"mg_ps{i}", [128, 512]) for i in range(8)]
        branches = ((0, 2), (2, 6), (6, 8))
        for (t0, n) in chs:
            s_ = 1 if t0 < NCTX else 0
            kb.dma("pool", yb[:, :, 0:n], ybT_d[:, t0:t0 + n].rearrange("(k p) t -> p k t", p=128), reads=yb_names, writes=[yb])
            for f in range(8):
                g = gt[f % 2]
                for b in range(3):
                    ti = COLIDX[f"gate{b}_{f}"]
                    kb.dma("sp", g[:, b, 0:n], zT_d[ti * 128:(ti + 1) * 128, t0:t0 + n], reads=[f"zT_{ti}"], writes=[g])
                kb.act(g[:, :, 0:n], g[:, :, 0:n], AF.Sigmoid)
                for b, (k0, k1) in enumerate(branches):
                    p = ps[(f % 2) * 3 + b]
                    for k in range(k0, k1):
                        kb.mm(p[:, 0:n], wp[:, k, f * 128:(f + 1) * 128], yb[:, k, 0:n], start=(k == k0), stop=(k == k1 - 1))
                    kb.tt("dve", t3[b][:, 0:n], p[:, 0:n], g[:, b, 0:n], ALU.mult)
                kb.tt("pool", t3[0][:, 0:n], t3[0][:, 0:n], t3[1][:, 0:n], ALU.add)
                kb.tt("pool", mT[:, f, 0:n], t3[0][:, 0:n], t3[2][:, 0:n], ALU.add)
            for f in range(8):
                p = ps[6 + f % 2]
                for k in range(8):
                    kb.mm(p[:, 0:n], wo[:, k, f * 128:(f + 1) * 128], mT[:, k, 0:n], start=(k == 0), stop=(k == 7))
                kb.stt("dve", xT[:, f, t0:t0 + n], p[:, 0:n], modT[:, L, 16 + f, s_:s_ + 1], xT[:, f, t0:t0 + n], ALU.mult, ALU.add)
        kb.barrier()


def phase_moe(G):
    nc, kb, L = G["nc"], G["kb"], G["L"]
    xT, modT, modA, onesr, ident, cst = G["xT"], G["modT"], G["modA"], G["onesr"], G["ident"], G["cst"]
    need_ctx = L < DEPTH - 1
    chs = CHUNKS if need_ctx else CHUNKS[1:]
    groups = [chs[:3], chs[3:]] if need_ctx else [chs[:2], chs[2:]]
    onesf = cst[:, 128:256]
    for grp in groups:
        GN = sum(n for _, n in grp)
        gch = []
        o = 0
        for (t0, n) in grp:
            gch.append((t0, n, o))
            o += n
        with ExitStack() as ph:
            psb = lambda name, shape, dt=F32: ph.enter_context(nc.sbuf_tensor(uq(name), shape, dt))
            pps = lambda name, shape, dt=F32: ph.enter_context(nc.psum_tensor(uq(name), shape, dt))
            h2T = psb("me_h2T", [128, 8, GN], F32R)
            gateT = psb("me_gateT", [128, GN], F32R)
            emit_norm(nc, kb, xT, h2T, gch, lambda k, s_: modA[:, L, 1, k, s_:s_ + 1],
                      lambda k, s_: modT[:, L, 24 + k, s_:s_ + 1], onesr, D)
            with ExitStack() as rp:
                rsb = lambda name, shape, dt=F32: rp.enter_context(nc.sbuf_tensor(uq(name), shape, dt))
                rps = lambda name, shape, dt=F32: rp.enter_context(nc.psum_tensor(uq(name), shape, dt))
                wr = rsb("me_wr", [128, 8, 36])
                br = rsb("me_br", [128, 36])
                kb.dma("sp", wr[:], G["wr_d"][L].rearrange("(k p) c -> p k c", p=128), writes=[wr])
                kb.dma("sp", br[:], G["br_d"][L].to_broadcast([128, 36]), writes=[br])
                lp = [rps(f"me_lp{i}", [128, 512]) for i in range(2)]
                gp = [rps(f"me_gp{i}", [128, 512]) for i in range(2)]
                R = [[rsb(f"me_r{b}_{i}", [128, 40]) for i in range(12)] for b in range(2)]
                for tt in range(GN // 128):
                    b = tt % 2
                    r = R[b]
                    for k in range(8):
                        kb.mm(lp[b][:, 0:36], h2T[:, k, tt * 128:(tt + 1) * 128].bitcast(F32), wr[:, k, :], start=(k == 0), stop=(k == 7))
                    lg = r[0]
                    kb.tt("dve", lg[:, 0:36], lp[b][:, 0:36], br[:], ALU.add)
                    gmax, ngmax, gsum, gw = r[1][:, 0:1], r[1][:, 1:2], r[1][:, 2:3], r[1][:, 3:4]
                    kb.op("dve", lambda g: g.tensor_reduce(gmax, lg[:, 0:4], AX.X, ALU.max), reads=[lg], writes=[r[1]])
                    kb.ts("dve", ngmax, gmax, -1.0, ALU.mult)
                    kb.act(r[2][:, 0:4], lg[:, 0:4], AF.Exp, bias=ngmax)
                    kb.op("dve", lambda g: g.tensor_reduce(gsum, r[2][:, 0:4], AX.X, ALU.add), reads=[r[2]], writes=[r[1]])
                    kb.recip(gw, gsum)
                    kb.ts("dve", r[3][:, 0:4], lg[:, 0:4], gmax, ALU.is_equal)
                    kb.ts("dve", r[3][:, 0:4], r[3][:, 0:4], -1.0, ALU.add, 1e30, ALU.mult)
                    kb.tt("dve", r[4][:, 0:32].rearrange("p (a b) -> p a b", b=8), lg[:, 4:36].rearrange("p (a b) -> p a b", b=8),
                          r[3][:, 0:4].unsqueeze(2).to_broadcast([128, 4, 8]), ALU.add)
                    kb.op("dve", lambda g: g.max(r[5][:, 0:8], r[4][:, 0:32]), reads=[r[4]], writes=[r[5]])
                    m1, m2 = r[5][:, 0:1], r[5][:, 1:2]
                    kb.ts("dve", r[6][:, 0:32], r[4][:, 0:32], m1, ALU.is_equal)
                    kb.ts("dve", r[7][:, 0:32], r[4][:, 0:32], m2, ALU.is_equal)
                    dm, ee, p1, p2 = r[8][:, 0:1], r[8][:, 1:2], r[8][:, 2:3], r[8][:, 3:4]
                    kb.tt("dve", dm, m2, m1, ALU.subtract)
                    kb.act(ee, dm, AF.Exp)
                    kb.ts("dve", p1, ee, 1.0, ALU.add)
                    kb.recip(p1, p1)
                    kb.tt("dve", p2, ee, p1, ALU.mult)
                    kb.tt("dve", p1, p1, gw, ALU.mult)
                    kb.tt("dve", p2, p2, gw, ALU.mult)
                    kb.ts("dve", r[9][:, 0:32], r[6][:, 0:32], p1, ALU.mult)
                    kb.stt("dve", r[9][:, 0:32], r[7][:, 0:32], p2, r[9][:, 0:32], ALU.mult, ALU.add)
                    kb.tr(gp[b][0:32, 0:128], r[9][:, 0:32], ident)
                    kb.cp("act", gateT[0:32, tt * 128:(tt + 1) * 128], gp[b][0:32, 0:128])
                kb.barrier()
            Gall = psb("me_G", [128, 4, GN], F32R)
            w13 = [[psb(f"me_w{a}_{i}", [128, 8, 128], F32R) for i in range(2)] for a in (1, 3)]
            w2 = [psb(f"me_w2_{i}", [128, 4, D], F32R) for i in range(2)]
            sel = [psb(f"me_sel{i}", [128, 128], F32R) for i in range(2)]
            sil = [psb(f"me_sil{i}", [128, 512]) for i in range(2)]
            hp = [pps(f"me_hp{i}", [128, 512]) for i in range(4)]
            bp = [pps(f"me_bp{i}", [128, 512]) for i in range(2)]
            yp = [pps(f"me_yp{i}", [128, 512]) for i in range(2)]
            cnt = 0
            for e in range(32):
                se = sel[e % 2]
                kb.op("pool", lambda g: g.affine_select(se[0:32, :], onesf[0:32, :], [[0, 128]], ALU.is_equal, 0.0,
                                                        base=-e, channel_multiplier=1), reads=[cst], writes=[se])
                kb.dma("pool", w2[e % 2][:], G["w2_d"][L, e].rearrange("(j p) c -> p j c", p=128), writes=[w2[e % 2]])
                for j in range(4):
                    wa, wb_ = w13[0][(e * 4 + j) % 2], w13[1][(e * 4 + j) % 2]
                    kb.dma("pool", wa[:], G["w1_d"][L, e, :, j * 128:(j + 1) * 128].rearrange("(k p) c -> p k c", p=128), writes=[wa])
                    kb.dma("pool", wb_[:], G["w3_d"][L, e, :, j * 128:(j + 1) * 128].rearrange("(k p) c -> p k c", p=128), writes=[wb_])
                    for (t0, n, o) in gch:
                        b = cnt % 2
                        cnt += 1
                        for k in range(8):
                            kb.mm(hp[b][:, 0:n], wa[:, k, :], h2T[:, k, o:o + n], start=(k == 0), stop=(k == 7))
                        for k in range(8):
                            kb.mm(hp[2 + b][:, 0:n], wb_[:, k, :], h2T[:, k, o:o + n], start=(k == 0), stop=(k == 7))
                        kb.mm(bp[b][:, 0:n], se[0:32, :], gateT[0:32, o:o + n])
                        kb.act(sil[b][:, 0:n], hp[b][:, 0:n], AF.Silu)
                        kb.tt("dve", sil[b][:, 0:n], sil[b][:, 0:n], hp[2 + b][:, 0:n], ALU.mult)
                        kb.tt("dve", Gall[:, j, o:o + n], sil[b][:, 0:n], bp[b][:, 0:n], ALU.mult)
                for (t0, n, o) in gch:
                    s_ = 1 if t0 < NCTX else 0
                    for f in range(8):
                        p = yp[f % 2]
                        for j in range(4):
                            kb.mm(p[:, 0:n], w2[e % 2][:, j, f * 128:(f + 1) * 128], Gall[:, j, o:o + n], start=(j == 0), stop=(j == 3))
                        kb.stt("dve", xT[:, f, t0:t0 + n], p[:, 0:n], modT[:, L, 40 + f, s_:s_ + 1], xT[:, f, t0:t0 + n], ALU.mult, ALU.add)
            kb.barrier()


def phase_moe_sparse(G):
    nc, kb, L = G["nc"], G["kb"], G["L"]
    xT, modT, modA, onesr, ident, cst = G["xT"], G["modT"], G["modA"], G["onesr"], G["ident"], G["cst"]
    h2tok_d, xs_d, ys_d = G["h2tok_d"], G["xs_d"], G["ys_d"]
    need_ctx = L < DEPTH - 1
    chs = CHUNKS if need_ctx else CHUNKS[1:]
    tiles = [t0 // 128 + j for (t0, n) in chs for j in range(n // 128)]
    NTL = len(tiles)
    onesf = cst[:, 128:256]
    with ExitStack() as ph:
        psb = lambda name, shape, dt=F32: ph.enter_context(nc.sbuf_tensor(uq(name), shape, dt))
        pps = lambda name, shape, dt=F32: ph.enter_context(nc.psum_tensor(uq(name), shape, dt))
        pr = ExitStack()
        rsb_ = lambda name, shape, dt=F32: pr.enter_context(nc.sbuf_tensor(uq(name), shape, dt))
        GA = psb("ms_GA", [128, 18])
        GB = psb("ms_GB", [128, 18])
        D1f = psb("ms_D1f", [128, 18])
        D2f = psb("ms_D2f", [128, 18])
        D1i = psb("ms_D1i", [128, 18], I32)
        D2i = psb("ms_D2i", [128, 18], I32)
        idxW = psb("ms_idxW", [128, 128], I32)
        p0 = ExitStack()
        h2T = p0.enter_context(nc.sbuf_tensor(uq("ms_h2T"), [128, 8, NT], F32R))
        OH1 = rsb_("ms_OH1", [128, 18, 32])
        OH2 = rsb_("ms_OH2", [128, 18, 32])
        AA = rsb_("ms_AA", [128, 18, 32])
        with ExitStack() as p1:
            qsb = lambda name, shape, dt=F32: p1.enter_context(nc.sbuf_tensor(uq(name), shape, dt))
            qps = lambda name, shape, dt=F32: p1.enter_context(nc.psum_tensor(uq(name), shape, dt))
            emit_norm(nc, kb, xT, h2T, [(t0, n, t0) for (t0, n) in chs], lambda k, s_: modA[:, L, 1, k, s_:s_ + 1],
                      lambda k, s_: modT[:, L, 24 + k, s_:s_ + 1], onesr, D)
            wr = qsb("ms_wr", [128, 8, 36])
            br = qsb("ms_br", [128, 36])
            kb.dma("sp", wr[:], G["wr_d"][L].rearrange("(k p) c -> p k c", p=128), writes=[wr])
            kb.dma("sp", br[:], G["br_d"][L].to_broadcast([128, 36]), writes=[br])
            lp = [qps(f"ms_lp{i}", [128, 512]) for i in range(2)]
            T_ = NTL
            LG = qsb("ms_LG", [128, 18, 36])
            for i, tt in enumerate(tiles):
                b = i % 2
                tsl = slice(tt * 128, (tt + 1) * 128)
                for k in range(8):
                    kb.mm(lp[b][:, 0:36], h2T[:, k, tsl].bitcast(F32), wr[:, k, :], start=(k == 0), stop=(k == 7))
                kb.tt("dve", LG[:, i, :], lp[b][:, 0:36], br[:], ALU.add)
            B4 = lambda t: t[:, 0:T_, :]
            g4 = qsb("ms_g4", [128, 18, 4])
            oh4 = qsb("ms_oh4", [128, 18, 4])
            ls = qsb("ms_ls", [128, 18, 32])
            l2 = qsb("ms_l2", [128, 18, 32])
            sm_ = qsb("ms_sm", [128, 8, 18])
            gmax, gsum, gw, m1, m2, ee, p1_, p2_ = (sm_[:, j, 0:T_] for j in range(8))
            bc4 = lambda v: v.unsqueeze(2).to_broadcast([128, T_, 4])
            bc32 = lambda v: v.unsqueeze(2).to_broadcast([128, T_, 32])
            kb.op("dve", lambda g: g.tensor_reduce(gmax, LG[:, 0:T_, 0:4], AX.X, ALU.max), reads=[LG], writes=[sm_])
            kb.tt("dve", g4[:, 0:T_, :], LG[:, 0:T_, 0:4], bc4(gmax), ALU.subtract)
            kb.tt("dve", oh4[:, 0:T_, :], LG[:, 0:T_, 0:4], bc4(gmax), ALU.is_equal)
            kb.act(g4[:, 0:T_, :], g4[:, 0:T_, :], AF.Exp)
            kb.op("dve", lambda g: g.tensor_reduce(gsum, g4[:, 0:T_, :], AX.X, ALU.add), reads=[g4], writes=[sm_])
            kb.recip(gw, gsum)
            kb.ts("dve", oh4[:, 0:T_, :], oh4[:, 0:T_, :], -1.0, ALU.add, 1e30, ALU.mult)
            kb.tt("dve", ls[:, 0:T_, :].rearrange("p t (a b) -> p t a b", b=8), LG[:, 0:T_, 4:36].rearrange("p t (a b) -> p t a b", b=8),
                  oh4[:, 0:T_, :].unsqueeze(3).to_broadcast([128, T_, 4, 8]), ALU.add)
            kb.op("dve", lambda g: g.tensor_reduce(m1, ls[:, 0:T_, :], AX.X, ALU.max), reads=[ls], writes=[sm_])
            kb.tt("dve", OH1[:, 0:T_, :], ls[:, 0:T_, :], bc32(m1), ALU.is_equal)
            kb.stt("dve", l2[:, 0:T_, :], OH1[:, 0:T_, :], -1e30, ls[:, 0:T_, :], ALU.mult, ALU.add)
            kb.op("dve", lambda g: g.tensor_reduce(m2, l2[:, 0:T_, :], AX.X, ALU.max), reads=[l2], writes=[sm_])
            kb.tt("dve", OH2[:, 0:T_, :], l2[:, 0:T_, :], bc32(m2), ALU.is_equal)
            kb.tt("dve", AA[:, 0:T_, :], OH1[:, 0:T_, :], OH2[:, 0:T_, :], ALU.add)
            kb.tt("dve", ee, m2, m1, ALU.subtract)
            kb.act(ee, ee, AF.Exp)
            kb.ts("dve", p1_, ee, 1.0, ALU.add)
            kb.recip(p1_, p1_)
            kb.tt("dve", p2_, ee, p1_, ALU.mult)
            kb.tt("dve", GA[:, 0:T_], p1_, gw, ALU.mult)
            kb.tt("dve", GB[:, 0:T_], p2_, gw, ALU.mult)
            kb.barrier()
        with ExitStack() as p2:
            qsb = lambda name, shape, dt=F32: p2.enter_context(nc.sbuf_tensor(uq(name), shape, dt))
            qps = lambda name, shape, dt=F32: p2.enter_context(nc.psum_tensor(uq(name), shape, dt))
            ltri = qsb("ms_ltri", [128, 128])
            kb.op("pool", lambda g: g.affine_select(ltri[:], onesf, [[1, 128]], ALU.is_gt, 0.0, base=0, channel_multiplier=-1),
                  reads=[cst], writes=[ltri])
            Rp = [qps(f"ms_Rp{i}", [128, 512]) for i in range(2)]
            Cp = qps("ms_Cp", [128, 512])
            for i in range(NTL):
                out = Rp[i // 16][:, (i % 16) * 32:(i % 16 + 1) * 32]
                kb.mm(out, ltri[:], AA[:, i, :], start=True, stop=(i == 0))
                for i2 in range(i):
                    kb.mm(out, onesf, AA[:, i2, :], start=False, stop=(i2 == i - 1))
            for i in range(NTL):
                kb.mm(Cp[:, 0:32], onesf, AA[:, i, :], start=(i == 0), stop=(i == NTL - 1))
            w_ = [qsb(f"ms_w{i}", [128, 32]) for i in range(8)]
            cnt, x_, kf, msk, nb, pend, pstart, tmp = w_
            kb.cp("dve", cnt[:], Cp[:, 0:32])
            kb.ts("dve", x_[:], cnt[:], -float(SLOT), ALU.add, 0.0, ALU.max)
            kb.ts("dve", x_[:], x_[:], 127.0, ALU.add, 1.0 / 128, ALU.mult)
            ki = qsb("ms_ki", [128, 32], I32)
            kb.cp("dve", ki[:], x_[:])
            kb.cp("dve", kf[:], ki[:])
            kb.tt("dve", msk[:], kf[:], x_[:], ALU.is_gt)
            kb.tt("dve", kf[:], kf[:], msk[:], ALU.subtract)
            kb.ts("dve", tmp[:], kf[:], 1.0, ALU.add)
            kb.tt("dve", msk[:], tmp[:], x_[:], ALU.is_le)
            kb.tt("dve", nb[:], kf[:], msk[:], ALU.add)
            kb.scan(pend[:], onesf[:, 0:32], nb[:], 0.0, ALU.mult, ALU.add)
            kb.tt("dve", pstart[:], pend[:], nb[:], ALU.subtract)
            base1_i = qsb("ms_b1i", [128, 32], I32)
            base1 = qsb("ms_b1", [128, 32])
            kb.op("pool", lambda g: g.iota(base1_i[:], [[SLOT, 32]], base=0, channel_multiplier=0), writes=[base1_i])
            kb.cp("dve", base1[:], base1_i[:])
            kb.ts("dve", pstart[:], pstart[:], 128.0, ALU.mult, float(32 * SLOT - SLOT), ALU.add)
            kb.tt("dve", pstart[:], pstart[:], base1[:], ALU.subtract)
            RR = qsb("ms_RR", [128, 18, 32])
            T3 = qsb("ms_T3", [128, 18, 32])
            T4 = qsb("ms_T4", [128, 18, 32])
            n0 = min(NTL, 16)
            kb.cp("dve", RR[:, 0:n0, :], Rp[0][:, 0:n0 * 32].rearrange("p (a b) -> p a b", b=32))
            if NTL > 16:
                kb.cp("dve", RR[:, 16:NTL, :], Rp[1][:, 0:(NTL - 16) * 32].rearrange("p (a b) -> p a b", b=32))
            bcT = lambda v: v.unsqueeze(1).to_broadcast([128, NTL, 32])
            kb.ts("dve", T4[:, 0:NTL, :], RR[:, 0:NTL, :], float(SLOT), ALU.is_ge)
            kb.tt("dve", T4[:, 0:NTL, :], T4[:, 0:NTL, :], bcT(pstart[:]), ALU.mult)
            kb.tt("dve", T3[:, 0:NTL, :], RR[:, 0:NTL, :], bcT(base1[:]), ALU.add)
            kb.tt("dve", T3[:, 0:NTL, :], T3[:, 0:NTL, :], T4[:, 0:NTL, :], ALU.add)
            kb.tt("dve", T4[:, 0:NTL, :], T3[:, 0:NTL, :], OH1[:, 0:NTL, :], ALU.mult)
            kb.op("dve", lambda g: g.tensor_reduce(D1f[:, 0:NTL], T4[:, 0:NTL, :], AX.X, ALU.add), reads=[T4], writes=[D1f])
            kb.tt("dve", T4[:, 0:NTL, :], T3[:, 0:NTL, :], OH2[:, 0:NTL, :], ALU.mult)
            kb.op("dve", lambda g: g.tensor_reduce(D2f[:, 0:NTL], T4[:, 0:NTL, :], AX.X, ALU.add), reads=[T4], writes=[D2f])
            kb.cp("dve", D1i[:, 0:NTL], D1f[:, 0:NTL])
            kb.cp("dve", D2i[:, 0:NTL], D2f[:, 0:NTL])
            pidx_i = qsb("ms_pidx_i", [128, 1], I32)
            pidx = qsb("ms_pidx", [128, 1])
            kb.op("pool", lambda g: g.iota(pidx_i[:], [[0, 1]], base=0, channel_multiplier=1), writes=[pidx_i])
            kb.cp("dve", pidx[:], pidx_i[:])
            be = qsb("ms_be", [128, 1])
            kb.ts("dve", tmp[:], pend[:], pidx[:, 0:1], ALU.is_le)
            kb.op("dve", lambda g: g.tensor_reduce(be[:], tmp[:], AX.X, ALU.add), reads=[tmp], writes=[be])
            kb.ts("dve", be[:], be[:], 31.0, ALU.min)
            vb = qsb("ms_vb", [128, 1])
            kb.ts("dve", vb[:], pidx[:], pend[:, 31:32], ALU.is_lt)
            kb.tt("dve", be[:], be[:], vb[:], ALU.mult)
            kb.ts("dve", vb[:], vb[:], -1.0, ALU.add, -1000.0, ALU.mult)
            kb.tt("dve", be[:], be[:], vb[:], ALU.add)
            dg = qsb("ms_dg", [128, 128])
            kb.ts("dve", dg[:], ident, be[:, 0:1], ALU.mult)
            kb.mm(Cp[:, 128:256], onesf, dg[:])
            bef = qsb("ms_bef", [128, 128])
            kb.ts("dve", bef[:], Cp[:, 128:256], 128.0, ALU.mult, pidx[:, 0:1], ALU.add)
            if L > 0:
                kb.ts("dve", bef[:], bef[:], float(L * 32 * 128), ALU.add)
            kb.cp("dve", idxW[:], bef[:])
            kb.barrier()
        pr.close()
        with ExitStack() as p3:
            qsb = lambda name, shape, dt=F32: p3.enter_context(nc.sbuf_tensor(uq(name), shape, dt))
            qps = lambda name, shape, dt=F32: p3.enter_context(nc.psum_tensor(uq(name), shape, dt))
            hk = [qsb(f"ms_hs{i}", [128, D]) for i in range(3)]
            tp = [qps(f"ms_tp{i}", [128, 512]) for i in range(4)]
            for i, tt in enumerate(tiles):
                h = hk[i % 3]
                tsl = slice(tt * 128, (tt + 1) * 128)
                for half in range(2):
                    p = tp[(2 * i + half) % 4]
                    for j in range(4):
                        k = half * 4 + j
                        kb.tr(p[:, j * 128:(j + 1) * 128], h2T[:, k, tsl].bitcast(F32), ident)
                    kb.cp("act" if half == 0 else "dve", h[:, half * 512:(half + 1) * 512], p[:])
                kb.idma(xs_d, bass.IndirectOffsetOnAxis(ap=D1i[:, i:i + 1], axis=0), h[:], None, reads=[h, D1i], writes=["xs"])
                kb.idma(xs_d, bass.IndirectOffsetOnAxis(ap=D2i[:, i:i + 1], axis=0), h[:], None, reads=[h, D2i], writes=["xs"])
            kb.barrier()
        p0.close()
        with ExitStack() as p4:
            qsb = lambda name, shape, dt=F32: p4.enter_context(nc.sbuf_tensor(uq(name), shape, dt))
            qps = lambda name, shape, dt=F32: p4.enter_context(nc.psum_tensor(uq(name), shape, dt))
            W1 = [qsb(f"ms_W1_{i}", [128, 8 * 512], F32R) for i in range(2)]
            W3 = [qsb(f"ms_W3_{i}", [128, 8 * 512], F32R) for i in range(2)]
            W2 = [qsb(f"ms_W2_{i}", [128, 4 * D], F32R) for i in range(2)]
            Xb = [qsb(f"ms_Xb{i}", [128, D]) for i in range(3)]
            XT = [qsb(f"ms_XT{i}", [128, 8, 128], F32R) for i in range(2)]
            SL = [qsb(f"ms_SL{i}", [128, 512]) for i in range(2)]
            Gt = [qsb(f"ms_Gt{i}", [128, 4, 128], F32R) for i in range(2)]
            Ys = [qsb("ms_Ys0", [128, D])] * 2
            tpp = [qps(f"ms_tq{i}", [128, 512]) for i in range(2)]
            hp1 = [qps("ms_h1", [128, 512])] * 2
            hp3 = [qps("ms_h3", [128, 512])] * 2
            ypp = [qps(f"ms_yp{i}", [128, 512]) for i in range(2)]
            xtp = [qps(f"ms_xq{i}", [128, 512]) for i in range(2)]
            def xload(i):
                if i < len(subs):
                    kb.dma("sp", Xb[i % 3][:], xs_d[subs[i][0]:subs[i][0] + 128, :], reads=["xs"], writes=[Xb[i % 3]])

            def stageA(row0, W1t, W3t, xq, xb3):
                for half in range(2):
                    p = xtp[half]
                    for j in range(4):
                        k = half * 4 + j
                        kb.tr(p[:, j * 128:(j + 1) * 128], Xb[xb3][:, k * 128:(k + 1) * 128], ident)
                    kb.cp("act" if half == 0 else "dve", XT[xq][:, half * 4:half * 4 + 4, :], p[:].rearrange("p (a b) -> p a b", b=128))
                w1v = W1t[:].rearrange("p (k c) -> p k c", c=512)
                w3v = W3t[:].rearrange("p (k c) -> p k c", c=512)
                for k in range(8):
                    kb.mm(hp1[xq][:], XT[xq][:, k, :], w1v[:, k, :], start=(k == 0), stop=(k == 7))
                    kb.mm(hp3[xq][:], XT[xq][:, k, :], w3v[:, k, :], start=(k == 0), stop=(k == 7))
                kb.act(SL[xq][:], hp1[xq][:], AF.Silu)
                kb.tt("dve", SL[xq][:], SL[xq][:], hp3[xq][:], ALU.mult)

            def stageB(row0, W2t, xq):
                w2v = W2t[:].rearrange("p (j c) -> p j c", c=D)
                gp_ = tpp[xq]
                for j in range(4):
                    kb.tr(gp_[:, j * 128:(j + 1) * 128], SL[xq][:, j * 128:(j + 1) * 128], ident)
                kb.cp("act", Gt[xq][:].rearrange("p a b -> p (a b)"), gp_[:])
                for half in range(2):
                    yp = ypp[half]
                    for j in range(4):
                        kb.mm(yp[:], Gt[xq][:, j, :], w2v[:, j, half * 512:(half + 1) * 512], start=(j == 0), stop=(j == 3))
                    kb.cp("act" if half == 0 else "dve", Ys[xq][:, half * 512:(half + 1) * 512], yp[:])
                kb.dma("sp", ys_d[row0:row0 + 128, :], Ys[xq][:], reads=[Ys[xq]], writes=["ys"])

            subs = []
            wcnt = 0
            for e in range(32):
                pb = wcnt % 2
                wcnt += 1

                def ld(e=e, pb=pb):
                    kb.dma("pool", W1[pb][:], G["w1_d"][L, e * 128:(e + 1) * 128, :], writes=[W1[pb]])
                    kb.dma("pool", W3[pb][:], G["w3_d"][L, e * 128:(e + 1) * 128, :], writes=[W3[pb]])
                    kb.dma("pool", W2[pb][:], G["w2_d"][L, e * 128:(e + 1) * 128, :], writes=[W2[pb]])
                for j in range(SLOT // 128):
                    subs.append((e * SLOT + j * 128, pb, ld if j == 0 else None))
            for b in range(NOV):
                pb = wcnt % 2
                wcnt += 1

                def ld(b=b, pb=pb):
                    off = bass.IndirectOffsetOnAxis(ap=idxW[:, b:b + 1], axis=0)
                    bnd = (L + 1) * 32 * 128 - 1
                    kb.idma(W1[pb][:], None, G["w1_d"].rearrange("l r c -> (l r) c"), off, reads=[idxW], writes=[W1[pb]], bounds=bnd)
                    kb.idma(W3[pb][:], None, G["w3_d"].rearrange("l r c -> (l r) c"), off, reads=[idxW], writes=[W3[pb]], bounds=bnd)
                    kb.idma(W2[pb][:], None, G["w2_d"].rearrange("l r c -> (l r) c"), off, reads=[idxW], writes=[W2[pb]], bounds=bnd)
                subs.append((32 * SLOT + b * 128, pb, ld))
            xload(0)
            xload(1)
            for i, (row0, pb, ld) in enumerate(subs):
                if ld is not None:
                    ld()
                xload(i + 2)
                stageA(row0, W1[pb], W3[pb], i % 2, i % 3)
                if i >= 1:
                    r1, pb1, _ = subs[i - 1]
                    stageB(r1, W2[pb1], (i - 1) % 2)
            r1, pb1, _ = subs[-1]
            stageB(r1, W2[pb1], (len(subs) - 1) % 2)
            kb.barrier()
        with ExitStack() as p5:
            qsb = lambda name, shape, dt=F32: p5.enter_context(nc.sbuf_tensor(uq(name), shape, dt))
            qps = lambda name, shape, dt=F32: p5.enter_context(nc.psum_tensor(uq(name), shape, dt))
            y1 = [qsb(f"ms_y1_{i}", [128, D]) for i in range(2)]
            y2 = [qsb(f"ms_y2_{i}", [128, D]) for i in range(2)]
            tq = [qps(f"ms_cq{i}", [128, 512]) for i in range(4)]
            for i, tt in enumerate(tiles):
                pb = i % 2
                s_ = 1 if tt * 128 < NCTX else 0
                kb.idma(y1[pb][:], None, ys_d, bass.IndirectOffsetOnAxis(ap=D1i[:, i:i + 1], axis=0), reads=["ys", D1i], writes=[y1[pb]])
                kb.idma(y2[pb][:], None, ys_d, bass.IndirectOffsetOnAxis(ap=D2i[:, i:i + 1], axis=0), reads=["ys", D2i], writes=[y2[pb]])
                kb.ts("dve", y1[pb][:], y1[pb][:], GA[:, i:i + 1], ALU.mult)
                kb.stt("dve", y1[pb][:], y2[pb][:], GB[:, i:i + 1], y1[pb][:], ALU.mult, ALU.add)
                for half in range(2):
                    p = tq[(2 * i + half) % 4]
                    for j in range(4):
                        k = half * 4 + j
                        kb.tr(p[:, j * 128:(j + 1) * 128], y1[pb][:, k * 128:(k + 1) * 128], ident)
                    for j in range(4):
                        k = half * 4 + j
                        kb.stt("dve", xT[:, k, tt * 128:(tt + 1) * 128], p[:, j * 128:(j + 1) * 128], modT[:, L, 40 + k, s_:s_ + 1],
                               xT[:, k, tt * 128:(tt + 1) * 128], ALU.mult, ALU.add)
            kb.barrier()


def phase_final(G):
    nc, kb = G["nc"], G["kb"]
    xT, onesr, ident, Vn = G["xT"], G["onesr"], G["ident"], G["Vn"]
    out_d = G["out_d"]
    with ExitStack() as ph:
        psb = lambda name, shape, dt=F32: ph.enter_context(nc.sbuf_tensor(uq(name), shape, dt))
        pps = lambda name, shape, dt=F32: ph.enter_context(nc.psum_tensor(uq(name), shape, dt))
        fo = psb("fn_fo", [128, 8, NLAT])
        emit_norm(nc, kb, xT, fo, [(t0, n, t0 - NCTX) for (t0, n) in CHUNKS[1:]],
                  lambda k, s_: Vn("final_g")[:, k:k + 1], None, onesr, D)
        ost = [psb(f"fn_ost{i}", [128, D]) for i in range(2)]
        tp = [pps(f"fn_tp{i}", [128, 512]) for i in range(4)]
        for tt in range(NLAT // 128):
            o = ost[tt % 2]
            for half in range(2):
                p = tp[(2 * tt + half) % 4]
                for j in range(4):
                    k = half * 4 + j
                    kb.tr(p[:, j * 128:(j + 1) * 128], fo[:, k, tt * 128:(tt + 1) * 128], ident)
                if half == 0:
                    kb.cp("dve", o[:, 0:512], p[:])
                else:
                    kb.cp("act", o[:, 512:1024], p[:])
            kb.dma("sp", out_d[tt * 128:(tt + 1) * 128, :], o[:], reads=[o], writes=["out"])
        kb.barrier()


def host_prep(inputs):
    f = lambda a: np.ascontiguousarray(np.asarray(a, dtype=np.float32))
    w_in = f(inputs["w_in"])
    perm = np.concatenate([np.arange(8, 16), np.arange(0, 8), np.arange(24, 32), np.arange(16, 24)]) + 640
    w_in_x = np.concatenate([w_in, w_in[:, :, 576:672], w_in[:, :, 576:640], w_in[:, :, perm]], axis=2)
    consts = np.concatenate([np.eye(128, dtype=np.float32), np.ones((128, 128), np.float32)], axis=1)
    shared = {"consts": consts, "mod_w": f(inputs["mod_w"]), "w_in_x": np.ascontiguousarray(w_in_x)}
    s5B = np.zeros((DEPTH, 2, 2, 8, 128, 128), np.float32)
    s5C = np.zeros((DEPTH, 2, 2, 8, 128, 128), np.float32)
    for ri, (bn, cn) in enumerate((("s5_b_re", "s5_c_re"), ("s5_b_im", "s5_c_im"))):
        bb = f(inputs[bn])
        cc = f(inputs[cn])
        for g in range(16):
            s_ = g // 2
            ct = s_ // 4
            q0 = (g - 2 * s_) * 64
            c0 = (g - 8 * ct) * 16
            s5B[:, :, ri, s_, c0:c0 + 16, q0:q0 + 64] = bb[:, :, g].transpose(0, 1, 3, 2)
            s5C[:, :, ri, s_, q0:q0 + 64, c0:c0 + 16] = cc[:, :, g].transpose(0, 1, 3, 2)
    shared["s5B"] = s5B
    shared["s5C"] = s5C
    shared["s5_glu_w"] = f(inputs["s5_glu_w"])
    rows = NLAT // 64
    row = np.repeat(np.arange(rows, dtype=np.float32), 64)
    col = np.tile(np.arange(64, dtype=np.float32), rows)
    inv = (np.float32(10000.0) ** (-np.arange(8, dtype=np.float32) / np.float32(8))).astype(np.float32)
    rope = np.zeros((128, 2, NLAT), np.float32)
    for r in range(32):
        i, axis, half = r % 8, r // 16, (r // 8) % 2
        ang = ((row if axis == 0 else col) * inv[i]).astype(np.float32)
        rope[64 + r, 0] = np.cos(ang)
        rope[64 + r, 1] = np.sin(ang) * (-1.0 if half == 0 else 1.0)
    shared["rope"] = rope
    wuq = f(inputs["mla_w_uq"]).reshape(DEPTH, 256, 8, 96)
    pperm = np.concatenate([np.arange(64), 64 + np.concatenate([np.arange(8, 16), np.arange(0, 8), np.arange(24, 32), np.arange(16, 24)])])
    shared["wq"] = np.ascontiguousarray(np.stack([wuq, wuq[:, :, :, pperm]], axis=2).reshape(DEPTH, 256, 2, 768))
    shared["mla_w_uk"] = f(inputs["mla_w_uk"])
    shared["hg_lb_logits"] = f(inputs["hg_lb_logits"]).reshape(1, DEPTH)
    shared["wp"] = np.ascontiguousarray(np.concatenate([f(inputs["w_pa"]), f(inputs["w_pb"]), f(inputs["w_pc"])], axis=1))
    shared["w_out"] = f(inputs["w_out"])
    shared["wr"] = np.ascontiguousarray(np.concatenate([f(inputs["moe_w_group"]), f(inputs["moe_w_expert"])], axis=2))
    shared["br"] = np.ascontiguousarray(np.concatenate([f(inputs["moe_b_group"]), f(inputs["moe_b_expert"])], axis=1).reshape(DEPTH, 1, 36))
    for nm_, kt in (("moe_w1", 8), ("moe_w3", 8), ("moe_w2", 4)):
        w_ = f(inputs[nm_])
        cols = w_.shape[-1]
        shared[nm_] = np.ascontiguousarray(w_.reshape(DEPTH, 32, kt, 128, cols).transpose(0, 1, 3, 2, 4)).reshape(DEPTH, 32 * 128, kt * cols)
    shared["mla_w_uv"] = f(inputs["mla_w_uv"])
    in_maps = []
    for b in range(8):
        vecs = np.zeros((NVBLK * 128, 128), np.float32)

        def put(name, arr):
            r0, n = VEC_ROWS[name]
            vecs[r0:r0 + n, :] = np.asarray(arr, np.float32).reshape(n, 128)
        put("c", inputs["c"][b]); put("c_ctx", inputs["c_ctx"]); put("final_g", inputs["final_norm_g"])
        for l in range(DEPTH):
            put(f"norm1_g{l}", inputs["norm1_g"][l]); put(f"norm2_g{l}", inputs["norm2_g"][l])
            put(f"mod_b{l}", inputs["mod_b"][l]); put(f"s5_d{l}", inputs["s5_d"][l])
            put(f"glu_b{l}", inputs["s5_glu_b"][l]); put(f"qa_g{l}", inputs["mla_qa_g"][l])
            put(f"kva_g{l}", inputs["mla_kva_g"][l])
            hg = np.zeros((4, 128), np.float32)
            hg[:, 0:64] = np.asarray(inputs["hg_norm_g"][l], np.float32).reshape(4, 64)
            put(f"hgn_g{l}", hg)
            for d in range(2):
                put(f"lam_re{l}{d}", inputs["s5_lam_re"][l, d]); put(f"lam_im{l}{d}", inputs["s5_lam_im"][l, d])
                put(f"lstep{l}{d}", np.repeat(np.asarray(inputs["s5_log_step"][l, d], np.float32), 64))
        m = dict(shared)
        m["x"] = f(inputs["x"][b]); m["ctx"] = f(inputs["ctx"][b]); m["vecs"] = vecs
        in_maps.append(m)
    return in_maps


def kernel(**inputs):
    in_maps = host_prep(inputs)
    nc = build_nc()
    res = run_bass_kernel_spmd(nc, in_maps, core_ids=list(range(8)))
    return np.stack([r["out"] for r in res.results], axis=0)
```

```python
import math
from contextlib import ExitStack

import numpy as np
import concourse.bass as bass
import concourse.mybir as mybir
from concourse.bass_utils import run_bass_kernel_spmd

F32 = mybir.dt.float32
F32R = mybir.dt.float32r
I32 = mybir.dt.int32
AF = mybir.ActivationFunctionType
ALU = mybir.AluOpType
AX = mybir.AxisListType

D = 1024
NCTX = 256
NLAT = 2048
NT = NCTX + NLAT
DEPTH = 2
EPS = 1e-6
CHUNKS = [(0, 256)] + [(256 + 512 * i, 512) for i in range(4)]
SLOT = 256
NOV = 35
NROWS = 32 * SLOT + NOV * 128

COLT = []
def _ct(name, start, width):
    COLT.append((name, start, width))
for i in range(2): _ct(f"s5u{i}", 0 + 128 * i, 128)
for i in range(2): _ct(f"cq{i}", 256 + 128 * i, 128)
_ct("ckv", 512, 128)
_ct("kpeA", 5792, 96)
_ct("kpeB", 5792 + 96, 96)
for i in range(4): _ct(f"hq{i}", 672 + 128 * i, 128)
for i in range(4): _ct(f"hf{i}", 1184 + 128 * i, 128)
for i in range(4): _ct(f"hb{i}", 1696 + 128 * i, 128)
for i in range(4): _ct(f"hi{i}", 2208 + 64 * i, 64)
for i in range(4): _ct(f"hg{i}", 2464 + 64 * i, 64)
N_NONGATE = len(COLT)
for b in range(3):
    for i in range(8): _ct(f"gate{b}_{i}", 2720 + 1024 * b + 128 * i, 128)
COLIDX = {n: i for i, (n, _, _) in enumerate(COLT)}
WINX = 5792 + 192

VEC_ROWS = {}
def _vr(name, n):
    VEC_ROWS[name] = (sum(v[1] for v in VEC_ROWS.values()), n)
_vr("c", 8); _vr("c_ctx", 8); _vr("final_g", 8)
for l in range(DEPTH):
    _vr(f"norm1_g{l}", 8); _vr(f"norm2_g{l}", 8); _vr(f"mod_b{l}", 48)
    _vr(f"s5_d{l}", 2); _vr(f"glu_b{l}", 2); _vr(f"qa_g{l}", 2); _vr(f"kva_g{l}", 1)
    _vr(f"hgn_g{l}", 4)
    for d in range(2):
        _vr(f"lam_re{l}{d}", 8); _vr(f"lam_im{l}{d}", 8); _vr(f"lstep{l}{d}", 8)
NVROWS = sum(v[1] for v in VEC_ROWS.values())
NVBLK = (NVROWS + 127) // 128


_UQ = [0]


def uq(name):
    _UQ[0] += 1
    return f"{name}~{_UQ[0]}"


class Buf:
    __slots__ = ("name", "w", "r")

    def __init__(self, name):
        self.name = name
        self.w = None
        self.r = {}


class KB:
    RING = 12

    def __init__(self, nc, es):
        self.nc, self.es = nc, es
        self.eng = dict(pe=nc.tensor, dve=nc.vector, act=nc.scalar, pool=nc.gpsimd, sp=nc.sync)
        self.psem, self.pcnt, self.nsem = {}, {}, 0
        for e in self.eng:
            self._new_psem(e)
        self.waited = {}
        self.rings = {}
        self.rpos = {}
        for q in ("sp", "pool", "act"):
            self.rings[q] = [[self._sem(f"dq_{q}{i}"), 0] for i in range(self.RING)]
            self.rpos[q] = 0
        self.bufs = {}
        self.ninstr = 0

    def _sem(self, name):
        self.nsem += 1
        return self.es.enter_context(self.nc.semaphore(name))

    def _new_psem(self, e):
        self.psem[e] = self._sem(f"p_{e}_{self.nsem}")
        self.pcnt[e] = 0

    def buf(self, name):
        b = self.bufs.get(name)
        if b is None:
            b = self.bufs[name] = Buf(name)
        return b

    def _wait(self, e, tok):
        sem, val, src = tok
        key = (e, id(sem))
        if self.waited.get(key, 0) >= val:
            return
        self.eng[e].wait_ge(sem, val)
        self.waited[key] = val

    def _sync(self, e, reads, writes):
        for b in reads:
            if b.w is not None:
                self._dep(e, b.w)
        for b in writes:
            if b.w is not None:
                self._dep(e, b.w)
            for t in b.r.values():
                self._dep(e, t)

    def _dep(self, e, tok):
        if e == "pe" and tok[2] == "pe":
            return
        self._wait(e, tok)

    def _commit(self, tok, reads, writes):
        for b in writes:
            b.w = tok
            b.r = {}
        for b in reads:
            b.r[tok[2]] = tok

    def op(self, e, fn, reads=(), writes=()):
        reads = [b if isinstance(b, Buf) else self.buf(self._nm(b)) for b in reads]
        writes = [b if isinstance(b, Buf) else self.buf(self._nm(b)) for b in writes]
        self._sync(e, reads, writes)
        ins = fn(self.eng[e])
        if self.pcnt[e] >= 20000:
            self._new_psem(e)
        self.pcnt[e] += 1
        ins.then_inc(self.psem[e], 1)
        self.ninstr += 1
        self._commit((self.psem[e], self.pcnt[e], e), reads, writes)

    def dma(self, q, out, in_, reads=(), writes=()):
        reads = [b if isinstance(b, Buf) else self.buf(self._nm(b)) for b in reads]
        writes = [b if isinstance(b, Buf) else self.buf(self._nm(b)) for b in writes]
        self._sync(q, reads, writes)
        slot = self.rings[q][self.rpos[q] % self.RING]
        self.rpos[q] += 1
        sem, cnt = slot
        if cnt > 0:
            self._wait(q, (sem, cnt, "dma"))
        self.eng[q].dma_start(out=out, in_=in_).then_inc(sem, 16)
        slot[1] = cnt + 16
        self.ninstr += 1
        self._commit((sem, cnt + 16, f"dma_{q}{(self.rpos[q] - 1) % self.RING}"), reads, writes)

    @staticmethod
    def _nm(x):
        if isinstance(x, str):
            return x
        if isinstance(x, tuple):
            return KB._nm(x[0]) + x[1]
        if hasattr(x, "tensor"):
            return x.tensor.name.split("~")[0]
        return x.name.split("~")[0]

    def _names(self, xs):
        return [self._nm(x) for x in xs if not isinstance(x, (int, float)) and x is not None]

    def tt(self, e, out, a, b, op, rn=(), wn=()):
        self.op(e, lambda g: g.tensor_tensor(out, a, b, op), reads=self._names([a, b]) + list(rn), writes=self._names([out]) + list(wn))

    def ts(self, e, out, a, s1, op0, s2=None, op1=None, rn=(), wn=()):
        if op1 is None:
            self.op(e, lambda g: g.tensor_scalar(out, a, s1, None, op0), reads=self._names([a, s1]) + list(rn), writes=self._names([out]) + list(wn))
        else:
            self.op(e, lambda g: g.tensor_scalar(out, a, s1, s2, op0, op1), reads=self._names([a, s1, s2]) + list(rn), writes=self._names([out]) + list(wn))

    def stt(self, e, out, a, sc_, b, op0, op1, rn=(), wn=()):
        self.op(e, lambda g: g.scalar_tensor_tensor(out, a, sc_, b, op0, op1), reads=self._names([a, sc_, b]) + list(rn), writes=self._names([out]) + list(wn))

    def act(self, out, in_, func, bias=None, scale=1.0, rn=(), wn=()):
        kw = {}
        if bias is not None:
            kw["bias"] = bias
        self.op("act", lambda g: g.activation(out, in_, func, scale=scale, **kw), reads=self._names([in_, bias, scale]) + list(rn), writes=self._names([out]) + list(wn))

    def cp(self, e, out, in_, rn=(), wn=()):
        if e == "act":
            self.op(e, lambda g: g.copy(out, in_), reads=self._names([in_]) + list(rn), writes=self._names([out]) + list(wn))
        else:
            self.op(e, lambda g: g.tensor_copy(out, in_), reads=self._names([in_]) + list(rn), writes=self._names([out]) + list(wn))

    def mm(self, out, lhsT, rhs, start=True, stop=True, rn=(), wn=()):
        self.op("pe", lambda g: g.matmul(out, lhsT, rhs, start=start, stop=stop), reads=self._names([lhsT, rhs]) + list(rn), writes=self._names([out]) + list(wn))

    def tr(self, out, in_, ident, rn=(), wn=()):
        self.op("pe", lambda g: g.transpose(out, in_, ident), reads=self._names([in_, ident]) + list(rn), writes=self._names([out]) + list(wn))

    def scan(self, out, d0, d1, init, op0, op1, rn=(), wn=()):
        self.op("dve", lambda g: g.tensor_tensor_scan(out, d0, d1, init, op0, op1), reads=self._names([d0, d1, init]) + list(rn), writes=self._names([out]) + list(wn))

    def recip(self, out, in_):
        self.op("dve", lambda g: g.reciprocal(out, in_), reads=self._names([in_]), writes=self._names([out]))

    def memset(self, e, out, val):
        self.op(e, lambda g: g.memset(out, val), reads=[], writes=self._names([out]))

    def barrier(self):
        toks = [(self.psem[o], self.pcnt[o], o) for o in self.eng if self.pcnt[o] > 0]
        for q in self.rings:
            for sem, cnt in self.rings[q]:
                if cnt > 0:
                    toks.append((sem, cnt, "dma"))
        for e in self.eng:
            for t in toks:
                if t[2] != e:
                    self._wait(e, t)

    def idma(self, out, out_off, in_, in_off, reads=(), writes=(), bounds=None):
        q = "pool"
        reads = [b if isinstance(b, Buf) else self.buf(self._nm(b)) for b in reads]
        writes = [b if isinstance(b, Buf) else self.buf(self._nm(b)) for b in writes]
        self._sync(q, reads, writes)
        slot = self.rings[q][self.rpos[q] % self.RING]
        self.rpos[q] += 1
        sem, cnt = slot
        if cnt > 0:
            self._wait(q, (sem, cnt, "dma"))
        if bounds is None:
            self.eng[q].indirect_dma_start(out=out, out_offset=out_off, in_=in_, in_offset=in_off).then_inc(sem, 16)
        else:
            if not hasattr(self, "_breg") or self._breg[0] != bounds:
                self._breg = (bounds, self.eng[q].to_reg(bounds))
            self.eng[q].indirect_dma_start(out=out, out_offset=out_off, in_=in_, in_offset=in_off,
                                           bounds_check=self._breg[1], oob_is_err=False).then_inc(sem, 16)
        slot[1] = cnt + 16
        self.ninstr += 1
        self._commit((sem, cnt + 16, f"dma_{q}{(self.rpos[q] - 1) % self.RING}"), reads, writes)

    def finish(self, bufs):
        for b in bufs:
            b = self.buf(b) if isinstance(b, str) else b
            if b.w is not None:
                self._wait("sp", b.w)


def build_nc(stop_after=None, debug=False):
    nc = bass.Bass("TRN2", target_bir_lowering=False)
    okind = "ExternalOutput" if debug else "Internal"
    x_d = nc.dram_tensor("x", [NLAT, D], F32, kind="ExternalInput").ap()
    ctx_d = nc.dram_tensor("ctx", [NCTX, D], F32, kind="ExternalInput").ap()
    vecs_d = nc.dram_tensor("vecs", [NVBLK * 128, 128], F32, kind="ExternalInput").ap()
    consts_d = nc.dram_tensor("consts", [128, 256], F32, kind="ExternalInput").ap()
    modw_d = nc.dram_tensor("mod_w", [DEPTH, D, 6 * D], F32, kind="ExternalInput").ap()
    winx_d = nc.dram_tensor("w_in_x", [DEPTH, D, WINX], F32, kind="ExternalInput").ap()
    out_d = nc.dram_tensor("out", [NLAT, D], F32, kind="ExternalOutput").ap()
    zT_d = nc.dram_tensor("zT", [len(COLT) * 128, NT], F32, kind=okind).ap()
    ybT_d = nc.dram_tensor("ybT", [D, NT], F32, kind=okind).ap()
    s5B_d = nc.dram_tensor("s5B", [DEPTH, 2, 2, 8, 128, 128], F32, kind="ExternalInput").ap()
    s5C_d = nc.dram_tensor("s5C", [DEPTH, 2, 2, 8, 128, 128], F32, kind="ExternalInput").ap()
    gluw_d = nc.dram_tensor("s5_glu_w", [DEPTH, 256, 256], F32, kind="ExternalInput").ap()
    rope_d = nc.dram_tensor("rope", [128, 2, NLAT], F32, kind="ExternalInput").ap()
    wq_d = nc.dram_tensor("wq", [DEPTH, 256, 2, 768], F32, kind="ExternalInput").ap()
    wuk_d = nc.dram_tensor("mla_w_uk", [DEPTH, 128, 512], F32, kind="ExternalInput").ap()
    wuv_d = nc.dram_tensor("mla_w_uv", [DEPTH, 128, 512], F32, kind="ExternalInput").ap()
    lbl_d = nc.dram_tensor("hg_lb_logits", [1, DEPTH], F32, kind="ExternalInput").ap()
    wp_d = nc.dram_tensor("wp", [DEPTH, D, D], F32, kind="ExternalInput").ap()
    wo_d = nc.dram_tensor("w_out", [DEPTH, D, D], F32, kind="ExternalInput").ap()
    wr_d = nc.dram_tensor("wr", [DEPTH, D, 36], F32, kind="ExternalInput").ap()
    br_d = nc.dram_tensor("br", [DEPTH, 1, 36], F32, kind="ExternalInput").ap()
    w1_d = nc.dram_tensor("moe_w1", [DEPTH, 32 * 128, 8 * 512], F32, kind="ExternalInput").ap()
    w3_d = nc.dram_tensor("moe_w3", [DEPTH, 32 * 128, 8 * 512], F32, kind="ExternalInput").ap()
    w2_d = nc.dram_tensor("moe_w2", [DEPTH, 32 * 128, 4 * D], F32, kind="ExternalInput").ap()
    h2tok_d = nc.dram_tensor("h2tok", [NT, D], F32, kind=okind).ap()
    xs_d = nc.dram_tensor("xs", [NROWS, D], F32, kind=okind).ap()
    ys_d = nc.dram_tensor("ys", [NROWS, D], F32, kind=okind).ap()
    xdbg_d = nc.dram_tensor("xdbg", [128, 8 * NT], F32, kind=okind).ap()
    modT_d = nc.dram_tensor("modT", [DEPTH, 128, 96], F32, kind=okind).ap()

    with ExitStack() as es:
        kb = KB(nc, es)
        sb = lambda name, shape, dt=F32: es.enter_context(nc.sbuf_tensor(uq(name), shape, dt))

        xT = sb("xT", [128, 8, NT])
        cst = sb("cst", [128, 256])
        ident = cst[:, 0:128]
        onesr = sb("onesr", [128, 128], F32R)
        vecT = sb("vecT", [128, NVBLK * 128])
        modT = sb("modT_sb", [128, DEPTH, 48, 2])
        modA = sb("modA", [128, DEPTH, 2, 8, 2])
        sc = sb("sc", [128, 8, 2], F32R)

        def V(name, j=0):
            r0, n = VEC_ROWS[name]
            return vecT[:, r0 + j:r0 + j + 1]

        def Vn(name):
            r0, n = VEC_ROWS[name]
            return vecT[:, r0:r0 + n]

        kb.dma("sp", cst[:], consts_d, writes=["cst"])
        kb.dma("pool", onesr[:], consts_d[:, 128:256], writes=["onesr"])

        with ExitStack() as ph:
            psb = lambda name, shape, dt=F32: ph.enter_context(nc.sbuf_tensor(uq(name), shape, dt))
            pps = lambda name, shape, dt=F32: ph.enter_context(nc.psum_tensor(uq(name), shape, dt))
            stg = [psb(f"xstg{i}", [128, D]) for i in range(3)]
            tps = [pps(f"tps{i}", [128, 4, 128]) for i in range(4)]
            for blk in range(NVBLK):
                s = stg[blk % 3]
                kb.dma("sp", s[:, 0:128], vecs_d[blk * 128:(blk + 1) * 128, :], writes=[f"xstg{blk % 3}"])
                kb.op("pe", lambda e: e.transpose(tps[blk % 4][:, 0, :], s[:, 0:128], ident),
                      reads=[f"xstg{blk % 3}", "cst"], writes=[f"tps{blk % 4}"])
                kb.op("dve", lambda e: e.tensor_copy(vecT[:, blk * 128:(blk + 1) * 128], tps[blk % 4][:, 0, :]),
                      reads=[f"tps{blk % 4}"], writes=["vecT"])
            n_tt = NT // 128
            for tt in range(n_tt):
                s = stg[tt % 3]
                src = ctx_d[tt * 128:(tt + 1) * 128, :] if tt < 2 else x_d[(tt - 2) * 128:(tt - 1) * 128, :]
                kb.dma("sp", s[:], src, writes=[f"xstg{tt % 3}"])
                for half in range(2):
                    pi = (2 * tt + half) % 4
                    for j in range(4):
                        k = half * 4 + j
                        kb.op("pe", lambda e: e.transpose(tps[pi][:, j, :], s[:, k * 128:(k + 1) * 128], ident),
                              reads=[f"xstg{tt % 3}", "cst"], writes=[f"tps{pi}"])
                    eng = "dve" if half == 0 else "act"
                    dst = xT[:, half * 4:half * 4 + 4, tt * 128:(tt + 1) * 128]
                    if eng == "dve":
                        kb.op("dve", lambda e: e.tensor_copy(dst, tps[pi][:]), reads=[f"tps{pi}"], writes=["xT"])
                    else:
                        kb.op("act", lambda e: e.copy(dst, tps[pi][:]), reads=[f"tps{pi}"], writes=["xT"])
            kb.barrier()

        kb.op("act", lambda e: e.activation(sc[:, :, 0], Vn("c"), AF.Silu), reads=["vecT"], writes=["sc"])
        kb.op("act", lambda e: e.activation(sc[:, :, 1], Vn("c_ctx"), AF.Silu), reads=["vecT"], writes=["sc"])

        xTd_d = nc.dram_tensor("xTd", [128, 8 * NT], F32, kind=okind).ap()
        if debug and stop_after == "p0":
            kb.dma("sp", xTd_d, xT[:].rearrange("p a b -> p (a b)"), reads=["xT"], writes=["xTd"])
        lbv = sb("lbv", [128, 2, DEPTH])
        lbl = sb("lbl", [128, DEPTH])
        kb.dma("sp", lbl[:], lbl_d.to_broadcast([128, DEPTH]), writes=["lbl"])
        kb.memset("dve", lbv[:, 0, :], 0.0)
        kb.tt("dve", lbv[:, 0, 1:2], lbl[:, 1:2], lbl[:, 0:1], ALU.subtract)
        kb.act(lbv[:, 0, 1:2], lbv[:, 0, 1:2], AF.Sigmoid)
        kb.ts("dve", lbv[:, 1, :], lbv[:, 0, :], -1.0, ALU.mult, 1.0, ALU.add)
        for layer in range(DEPTH if stop_after != "p0" else 0):
            L = layer
            with ExitStack() as ph:
                psb = lambda name, shape, dt=F32: ph.enter_context(nc.sbuf_tensor(uq(name), shape, dt))
                pps = lambda name, shape, dt=F32: ph.enter_context(nc.psum_tensor(uq(name), shape, dt))
                mw = [psb(f"mw{i}", [128, 8, 128], F32R) for i in range(3)]
                mps = pps("mps", [128, 48, 2])
                for j in range(48):
                    w = mw[j % 3]
                    kb.dma("pool", w[:], modw_d[L, :, j * 128:(j + 1) * 128].rearrange("(k p) c -> p k c", p=128),
                           writes=[f"mw{j % 3}"])
                    for k in range(8):
                        kb.op("pe", lambda e: e.matmul(mps[:, j, :], w[:, k, :], sc[:, k, :], start=(k == 0), stop=(k == 7)),
                              reads=[f"mw{j % 3}", "sc"], writes=["mps"])
                for s in range(2):
                    kb.op("dve", lambda e: e.tensor_tensor(modT[:, L, :, s], mps[:, :, s], Vn(f"mod_b{L}"), ALU.add),
                          reads=["mps", "vecT"], writes=["modT"])
                for ni, (gname, t0) in enumerate(((f"norm1_g{L}", 8), (f"norm2_g{L}", 32))):
                    for s in range(2):
                        kb.op("dve", lambda e: e.scalar_tensor_tensor(
                            modA[:, L, ni, :, s], modT[:, L, t0:t0 + 8, s], 1.0, Vn(gname), ALU.add, ALU.mult),
                            reads=["modT", "vecT"], writes=["modA"])
                if debug:
                    kb.dma("sp", modT_d[L], modT[:, L].rearrange("p a b -> p (a b)"), reads=["modT"], writes=["modT_d"])
                kb.barrier()
            if stop_after == f"mod{L}":
                break

            with ExitStack() as ph:
                psb = lambda name, shape, dt=F32: ph.enter_context(nc.sbuf_tensor(uq(name), shape, dt))
                pps = lambda name, shape, dt=F32: ph.enter_context(nc.psum_tensor(uq(name), shape, dt))
                hT = psb("hT", [128, 8, NT], F32R)
                emit_norm(nc, kb, xT, hT, [(t0, n, t0) for (t0, n) in CHUNKS],
                          lambda k, s_: modA[:, L, 0, k, s_:s_ + 1], lambda k, s_: modT[:, L, k, s_:s_ + 1], onesr, D)
                wb = [psb(f"wb{i}", [128, 8, 128], F32R) for i in range(3)]
                zs = [psb(f"zs{i}", [128, 512]) for i in range(4)]
                zp = [pps(f"zp{i}", [128, 512]) for i in range(4)]
                cnt = 0
                for ti, (name, c0, wd) in enumerate(COLT):
                    w = wb[ti % 3]
                    kb.dma("pool", w[:, :, 0:wd], winx_d[L, :, c0:c0 + wd].rearrange("(k p) c -> p k c", p=128),
                           writes=[f"wb{ti % 3}"])
                    for (t0, n) in CHUNKS:
                        pi = cnt % 4
                        cnt += 1
                        for k in range(8):
                            kb.op("pe", lambda e: e.matmul(zp[pi][0:wd, 0:n], w[:, k, 0:wd], hT[:, k, t0:t0 + n],
                                                           start=(k == 0), stop=(k == 7)),
                                  reads=[f"wb{ti % 3}", "hT"], writes=[f"zp{pi}"])
                        if cnt % 2 == 0:
                            kb.op("dve", lambda e: e.tensor_copy(zs[pi][0:wd, 0:n], zp[pi][0:wd, 0:n]),
                                  reads=[f"zp{pi}"], writes=[f"zs{pi}"])
                        else:
                            kb.op("act", lambda e: e.copy(zs[pi][0:wd, 0:n], zp[pi][0:wd, 0:n]),
                                  reads=[f"zp{pi}"], writes=[f"zs{pi}"])
                        kb.dma("sp", zT_d[ti * 128:ti * 128 + wd, t0:t0 + n], zs[pi][0:wd, 0:n],
                               reads=[f"zs{pi}"], writes=[f"zT_{ti}"])
                kb.barrier()
            if stop_after == f"A{L}":
                break
            G = dict(nc=nc, kb=kb, L=L, xT=xT, cst=cst, ident=ident, onesr=onesr, vecT=vecT, modT=modT, modA=modA,
                     V=V, Vn=Vn, zT_d=zT_d, ybT_d=ybT_d, s5B_d=s5B_d, s5C_d=s5C_d, gluw_d=gluw_d, debug=debug,
                     rope_d=rope_d, wq_d=wq_d, wuk_d=wuk_d, wuv_d=wuv_d)
            if not (debug and stop_after in (f"C{L}", f"D{L}")):
                phase_s5(G)
            if stop_after == f"B{L}":
                break
            G["lbv"] = lbv
            if not (debug and stop_after in (f"D{L}",)):
                phase_mla(G)
            if stop_after == f"C{L}":
                break
            G.update(wp_d=wp_d, wo_d=wo_d, wr_d=wr_d, br_d=br_d, w1_d=w1_d, w3_d=w3_d, w2_d=w2_d, out_d=out_d,
                     h2tok_d=h2tok_d, xs_d=xs_d, ys_d=ys_d)
            phase_hg(G)
            if stop_after == f"D{L}":
                break
            phase_merge(G)
            if stop_after == f"E{L}":
                kb.dma("sp", xdbg_d, xT[:].rearrange("p a b -> p (a b)"), reads=["xT"], writes=["xdbg"])
                break
            phase_moe_sparse(G)
            if stop_after == f"F{L}":
                kb.dma("sp", xdbg_d, xT[:].rearrange("p a b -> p (a b)"), reads=["xT"], writes=["xdbg"])
                break
        else:
            phase_final(G)

        kb.finish(list(kb.bufs.values()))
        print("instructions:", kb.ninstr, "sems:", kb.nsem, "sbuf left:", nc.sbuf_bytes_remaining)
    return nc


def emit_norm(nc, kb, xT, dst, chunks, A, Sh, onesr, dmodel):
    with ExitStack() as ns:
        psb = lambda name, shape, dt=F32: ns.enter_context(nc.sbuf_tensor(uq(name), shape, dt))
        pps = lambda name, shape, dt=F32: ns.enter_context(nc.psum_tensor(uq(name), shape, dt))
        sq = [psb(f"nsq{i}", [128, 8, 512], F32R) for i in range(2)]
        ms = [pps(f"nms{i}", [128, 512]) for i in range(2)]
        rs = [psb(f"nrs{i}", [128, 512]) for i in range(2)]
        tmp = [psb(f"ntmp{i}", [128, 512]) for i in range(2)]
        for ci, (t0, n, d0) in enumerate(chunks):
            s = 1 if t0 < NCTX else 0
            b = ci % 2
            for k in range(8):
                kb.act(sq[b][:, k, 0:n], xT[:, k, t0:t0 + n], AF.Square)
            for k in range(8):
                kb.mm(ms[b][:, 0:n], onesr[:], sq[b][:, k, 0:n], start=(k == 0), stop=(k == 7))
            kb.act(rs[b][:, 0:n], ms[b][:, 0:n], AF.Sqrt, scale=1.0 / dmodel, bias=EPS)
            kb.recip(rs[b][:, 0:n], rs[b][:, 0:n])
            for k in range(8):
                tb = k % 2
                sh = Sh(k, s) if Sh is not None else None
                if sh is None:
                    kb.stt("dve", dst[:, k, d0:d0 + n], xT[:, k, t0:t0 + n], A(k, s), rs[b][:, 0:n], ALU.mult, ALU.mult)
                else:
                    kb.stt("dve", tmp[tb][:, 0:n], xT[:, k, t0:t0 + n], A(k, s), rs[b][:, 0:n], ALU.mult, ALU.mult)
                    kb.act(dst[:, k, d0:d0 + n], tmp[tb][:, 0:n], AF.Identity, bias=sh, scale=1.0)
        kb.barrier()


TWO_PI = 2.0 * math.pi


def range_reduce(kb, r, x, tM, tI):
    kb.ts("dve", tM, x, 1.0 / TWO_PI, ALU.mult)
    kb.cp("dve", tI, tM)
    kb.cp("dve", tM, tI)
    kb.stt("dve", r, tM, -TWO_PI, x, ALU.mult, ALU.add)
    kb.ts("dve", tM, r, math.pi, ALU.is_gt)
    kb.stt("dve", r, tM, -TWO_PI, r, ALU.mult, ALU.add)
    kb.ts("dve", tM, r, -math.pi, ALU.is_lt)
    kb.stt("dve", r, tM, TWO_PI, r, ALU.mult, ALU.add)
    kb.ts("dve", r, r, 3.1415925, ALU.min, -3.1415925, ALU.max)


def sincos(kb, sn, cs, x, r, tM, tI):
    range_reduce(kb, r, x, tM, tI)
    kb.act(sn, r, AF.Sin)
    kb.ts("dve", tM, x, math.pi / 2, ALU.add)
    range_reduce(kb, r, tM, tM, tI) if False else None
    return


def phase_s5(G):
    nc, kb, L = G["nc"], G["kb"], G["L"]
    Vn = G["Vn"]
    zT_d, ybT_d = G["zT_d"], G["ybT_d"]
    T = 256
    NCH = NT // T
    with ExitStack() as ph:
        psb = lambda name, shape, dt=F32: ph.enter_context(nc.sbuf_tensor(uq(name), shape, dt))
        pps = lambda name, shape, dt=F32: ph.enter_context(nc.psum_tensor(uq(name), shape, dt))
        uT = psb("s5_uT", [128, 2, NT], F32R)
        yacc = psb("s5_yacc", [128, 2, NT])
        for t in range(2):
            ti = COLIDX[f"s5u{t}"]
            kb.dma("pool", uT[:, t, :], zT_d[ti * 128:(ti + 1) * 128, :], reads=[f"zT_{ti}"], writes=["s5_uT"])
        Bw = psb("s5_Bw", [128, 2, 8, 128], F32R)
        Cw = psb("s5_Cw", [128, 2, 8, 128], F32R)
        iota_i = psb("s5_iota_i", [128, T + 1], I32)
        iota_f = psb("s5_iota_f", [128, T + 1])
        kb.op("pool", lambda g: g.iota(iota_i[:], [[1, T + 1]], base=0, channel_multiplier=0), writes=["s5_iota_i"])
        kb.cp("dve", iota_f[:], iota_i[:])
        COS = psb("s5_COS", [128, 8, T + 1])
        SIN = psb("s5_SIN", [128, 8, T + 1])
        ang = psb("s5_ang", [128, 8, T + 1])
        rr = psb("s5_rr", [128, 8, T + 1])
        ERE = ang[:, :, 0:T]
        EIM = rr[:, :, 0:T]
        tM = psb("s5_tM", [128, 8, T + 1])
        tI = psb("s5_tI", [128, 8, T + 1], I32)
        sm = psb("s5_sm", [128, 24, 8])
        gin = psb("s5_gin", [128, 8, 2])
        tmp = [[psb(f"s5_t{b}_{i}", [128, T]) for i in range(6)] for b in range(3)]
        tmpB = [[psb(f"s5_g{b}_{i}", [128, T]) for i in range(2)] for b in range(2)]
        hh = [[psb(f"s5_h{b}_{i}", [128, T], F32R) for i in range(2)] for b in range(2)]
        Pp = [pps(f"s5_P{i}", [128, 2, T]) for i in range(3)]
        Yp = [[pps(f"s5_Y{b}_{ct}", [128, 512]) for ct in range(2)] for b in range(2)]
        flat = lambda t: t[:].rearrange("p a b -> p (a b)")
        for d in range(2):
            kb.dma("pool", Bw[:].rearrange("c r s q -> c (r s) q"),
                   G["s5B_d"][L, d].rearrange("r s c q -> c (r s) q"), writes=["s5_Bw"])
            kb.dma("pool", Cw[:].rearrange("c r s q -> c (r s) q"),
                   G["s5C_d"][L, d].rearrange("r s c q -> c (r s) q"), writes=["s5_Cw"])
            kb.ts("pool", Cw[:, 1], Cw[:, 1].bitcast(F32), -1.0, ALU.mult)
            lre, lim, lst = Vn(f"lam_re{L}{d}"), Vn(f"lam_im{L}{d}"), Vn(f"lstep{L}{d}")
            c_ = lambda i: sm[:, i, :]
            DT, MAG, TH, SN, CS, LBR, LBI, DEN, FR, FI, X1, X2, X3, MI = (c_(i) for i in range(14))
            kb.act(DT, lst, AF.Exp)
            kb.tt("dve", X1, lre, DT, ALU.mult)
            kb.act(MAG, X1, AF.Exp)
            kb.tt("dve", TH, lim, DT, ALU.mult)
            smI = tI[:, 0, 0:8]
            range_reduce(kb, X2, TH, X3, smI)
            kb.act(SN, X2, AF.Sin)
            kb.ts("dve", X1, TH, math.pi / 2, ALU.add)
            range_reduce(kb, X2, X1, X3, smI)
            kb.act(CS, X2, AF.Sin)
            kb.tt("dve", LBR, MAG, CS, ALU.mult)
            kb.tt("dve", LBI, MAG, SN, ALU.mult)
            kb.tt("dve", X1, lre, lre, ALU.mult)
            kb.tt("dve", X2, lim, lim, ALU.mult)
            kb.tt("dve", DEN, X1, X2, ALU.add)
            kb.recip(DEN, DEN)
            kb.ts("dve", X3, LBR, -1.0, ALU.add)
            kb.tt("dve", X1, X3, lre, ALU.mult)
            kb.tt("dve", X2, LBI, lim, ALU.mult)
            kb.tt("dve", X1, X1, X2, ALU.add)
            kb.tt("dve", FR, X1, DEN, ALU.mult)
            kb.tt("dve", X1, LBI, lre, ALU.mult)
            kb.tt("dve", X2, X3, lim, ALU.mult)
            kb.tt("dve", X1, X1, X2, ALU.subtract)
            kb.tt("dve", FI, X1, DEN, ALU.mult)
            kb.tt("dve", ang[:], TH.unsqueeze(2).to_broadcast([128, 8, T + 1]),
                  iota_f[:].unsqueeze(1).to_broadcast([128, 8, T + 1]), ALU.mult)
            range_reduce(kb, flat(rr), flat(ang), flat(tM), flat(tI))
            kb.act(flat(SIN), flat(rr), AF.Sin)
            kb.ts("dve", flat(ang), flat(ang), math.pi / 2, ALU.add)
            range_reduce(kb, flat(rr), flat(ang), flat(tM), flat(tI))
            kb.act(flat(COS), flat(rr), AF.Sin)
            frb = FR.unsqueeze(2).to_broadcast([128, 8, T])
            fib = FI.unsqueeze(2).to_broadcast([128, 8, T])
            tF = tI[:].bitcast(F32)
            kb.tt("dve", tM[:, :, 0:T], COS[:, :, 0:T], frb, ALU.mult)
            kb.tt("dve", tF[:, :, 0:T], SIN[:, :, 0:T], fib, ALU.mult)
            kb.tt("dve", ERE, tM[:, :, 0:T], tF[:, :, 0:T], ALU.add)
            kb.tt("dve", tM[:, :, 0:T], COS[:, :, 0:T], fib, ALU.mult)
            kb.tt("dve", tF[:, :, 0:T], SIN[:, :, 0:T], frb, ALU.mult)
            kb.tt("dve", EIM, tM[:, :, 0:T], tF[:, :, 0:T], ALU.subtract)
            kb.memset("dve", gin[:], 0.0)
            order = list(range(NCH)) if d == 0 else [0] + list(range(NCH - 1, 0, -1))
            units = [(oi, ci, s_) for oi, ci in enumerate(order) for s_ in range(8)]

            def stageA(u):
                oi, ci, s_ = units[u]
                t0 = ci * T
                ct = s_ // 4
                tq = tmp[u % 3]
                P = Pp[u % 3]
                kb.mm(P[:, 0, :], Bw[:, 0, s_, :], uT[:, ct, t0:t0 + T])
                kb.mm(P[:, 1, :], Bw[:, 1, s_, :], uT[:, ct, t0:t0 + T])
                Pre = P[:, 0, ::-1] if d == 1 else P[:, 0, :]
                Pim = P[:, 1, ::-1] if d == 1 else P[:, 1, :]
                kb.tt("dve", tq[0][:], ERE[:, s_, :], Pre, ALU.mult)
                kb.tt("dve", tq[1][:], EIM[:, s_, :], Pim, ALU.mult)
                kb.tt("pool", tq[4][:], tq[0][:], tq[1][:], ALU.subtract)
                kb.tt("dve", tq[2][:], ERE[:, s_, :], Pim, ALU.mult)
                kb.tt("dve", tq[3][:], EIM[:, s_, :], Pre, ALU.mult)
                kb.tt("pool", tq[5][:], tq[2][:], tq[3][:], ALU.add)

            def stageB(u):
                oi, ci, s_ = units[u]
                t0 = ci * T
                ct = s_ // 4
                yb_ = oi % 2
                tq = tmp[u % 3]
                gq = tmpB[u % 2]
                hb = hh[u % 2]
                rb = MAG[:, s_:s_ + 1].to_broadcast([128, T])
                kb.scan(gq[0][:], rb, tq[4][:], gin[:, s_, 0:1], ALU.mult, ALU.add)
                kb.scan(gq[1][:], rb, tq[5][:], gin[:, s_, 1:2], ALU.mult, ALU.add)
                cT, sT = COS[:, s_, T:T + 1], SIN[:, s_, T:T + 1]
                lr, li = gq[0][:, T - 1:T], gq[1][:, T - 1:T]
                xa, xb = sm[:, 14 + (u % 2) * 2, 0:1], sm[:, 15 + (u % 2) * 2, 0:1]
                kb.ts("dve", xa, li, sT, ALU.mult)
                kb.ts("dve", xb, li, cT, ALU.mult)
                kb.stt("dve", gin[:, s_, 0:1], lr, cT, xa, ALU.mult, ALU.subtract)
                kb.stt("dve", gin[:, s_, 1:2], lr, sT, xb, ALU.mult, ALU.add)
                kb.tt("pool", tq[0][:], COS[:, s_, 0:T], gq[0][:], ALU.mult)
                kb.tt("pool", tq[1][:], SIN[:, s_, 0:T], gq[1][:], ALU.mult)
                kb.tt("pool", hb[0][:], tq[0][:], tq[1][:], ALU.subtract)
                kb.tt("dve", tq[2][:], SIN[:, s_, 0:T], gq[0][:], ALU.mult)
                kb.tt("dve", tq[3][:], COS[:, s_, 0:T], gq[1][:], ALU.mult)
                kb.tt("pool", hb[1][:], tq[2][:], tq[3][:], ALU.add)
                Y = Yp[yb_][ct]
                kb.mm(Y[:, 0:T], Cw[:, 0, s_, :], hb[0][:], start=(s_ % 4 == 0), stop=False)
                kb.mm(Y[:, 0:T], Cw[:, 1, s_, :], hb[1][:], start=False, stop=(s_ % 4 == 3))
                if s_ == 7:
                    for ct2 in range(2):
                        Y2 = Yp[yb_][ct2]
                        if d == 0:
                            kb.cp("act", yacc[:, ct2, t0:t0 + T], Y2[:, 0:T])
                        else:
                            rv = slice(t0 + T - 1, (t0 - 1 if t0 > 0 else None), -1)
                            kb.tt("dve", yacc[:, ct2, rv], yacc[:, ct2, rv], Y2[:, 0:T], ALU.add)

            stageA(0)
            stageA(1)
            for u in range(len(units)):
                if u + 2 < len(units):
                    stageA(u + 2)
                stageB(u)
        gw = psb("s5_gw", [128, 2, 256], F32R)
        kb.dma("pool", gw[:], G["gluw_d"][L].rearrange("(k p) c -> p k c", p=128), writes=["s5_gw"])
        y1t = [hh[0][0], hh[0][1]]
        for ci in range(NCH):
            sl = slice(ci * T, (ci + 1) * T)
            for ct in range(2):
                a, b2, c2 = tmp[ct][0], tmp[ct][1], tmp[ct][2]
                kb.stt("dve", a[:], uT[:, ct, sl].bitcast(F32), Vn(f"s5_d{L}")[:, ct:ct + 1], yacc[:, ct, sl], ALU.mult, ALU.add)
                kb.tt("dve", b2[:], a[:], a[:], ALU.mult)
                kb.ts("dve", b2[:], b2[:], 0.044715, ALU.mult, 1.0, ALU.add)
                kb.tt("dve", b2[:], b2[:], a[:], ALU.mult)
                kb.act(c2[:], b2[:], AF.Sigmoid, scale=1.5957691216057308)
                kb.tt("dve", y1t[ct][:], a[:], c2[:], ALU.mult)
            for ct in range(2):
                Y = Yp[ci % 2][ct]
                for k in range(2):
                    kb.mm(Y[:, 0:T], gw[:, k, ct * 128:(ct + 1) * 128], y1t[k][:], start=(k == 0), stop=(k == 1))
                sg = tmp[ct][3]
                o = tmp[ct][4]
                kb.act(sg[:], Y[:, 0:T], AF.Sigmoid, bias=Vn(f"glu_b{L}")[:, ct:ct + 1])
                kb.tt("dve", o[:], y1t[ct][:].bitcast(F32), sg[:], ALU.mult)
                kb.dma("sp", ybT_d[ct * 128:(ct + 1) * 128, sl], o[:], reads=[o], writes=[f"ybT_{ct}"])
        kb.barrier()


MLA_SCALE = 1.0 / math.sqrt(96.0)


def phase_mla(G):
    nc, kb, L = G["nc"], G["kb"], G["L"]
    Vn, cst, onesr = G["Vn"], G["cst"], G["onesr"]
    zT_d, ybT_d = G["zT_d"], G["ybT_d"]
    need_ctx = L < DEPTH - 1
    with ExitStack() as ph:
        psb = lambda name, shape, dt=F32: ph.enter_context(nc.sbuf_tensor(uq(name), shape, dt))
        pps = lambda name, shape, dt=F32: ph.enter_context(nc.psum_tensor(uq(name), shape, dt))
        cqn = psb("ml_cqn", [128, 2, NT], F32R)
        ckvn = psb("ml_ckvn", [128, NT], F32R)
        KPE = psb("ml_KPE", [128, NT])
        ROPE = psb("ml_rope", [128, 2, NLAT])
        wq = psb("ml_wq", [128, 2, 2, 768], F32R)
        wuk = psb("ml_wuk", [128, 512], F32R)
        wuv = psb("ml_wuv", [128, 512], F32R)
        kb.dma("sp", ROPE[:], G["rope_d"], writes=["ml_rope"])
        kb.dma("pool", wq[:].rearrange("p k v c -> p k (v c)"),
               G["wq_d"][L].rearrange("(k p) v c -> p k (v c)", p=128), writes=["ml_wq"])
        kb.dma("pool", wuk[:], G["wuk_d"][L], writes=["ml_wuk"])
        kb.dma("pool", wuv[:], G["wuv_d"][L], writes=["ml_wuv"])
        ps = [pps(f"ml_ps{i}", [128, 512]) for i in range(8)]
        with ExitStack() as p1:
            qsb = lambda name, shape, dt=F32: p1.enter_context(nc.sbuf_tensor(uq(name), shape, dt))
            cqT = qsb("ml_cqT", [128, 2, NT])
            ckvT = qsb("ml_ckvT", [128, NT])
            kA = qsb("ml_kA", [128, NT])
            kB = qsb("ml_kB", [128, NT])
            for t in range(2):
                ti = COLIDX[f"cq{t}"]
                kb.dma("sp", cqT[:, t, :], zT_d[ti * 128:(ti + 1) * 128, :], reads=[f"zT_{ti}"], writes=["ml_cqT"])
            ti = COLIDX["ckv"]
            kb.dma("sp", ckvT[:], zT_d[ti * 128:(ti + 1) * 128, :], reads=[f"zT_{ti}"], writes=["ml_ckvT"])
            for nm_, tl in (("kpeA", kA), ("kpeB", kB)):
                ti = COLIDX[nm_]
                kb.dma("sp", tl[0:96, :], zT_d[ti * 128:ti * 128 + 96, :], reads=[f"zT_{ti}"], writes=[tl])
            sq = qsb("ml_sq", [128, 3, 512], F32R)
            rs = [qsb(f"ml_rs{i}", [128, 512]) for i in range(2)]
            tt1 = qsb("ml_tt1", [128, 512])
            tt2 = qsb("ml_tt2", [128, 512])
            for (t0, n) in CHUNKS:
                for t in range(2):
                    kb.act(sq[:, t, 0:n], cqT[:, t, t0:t0 + n], AF.Square)
                kb.act(sq[:, 2, 0:n], ckvT[:, t0:t0 + n], AF.Square)
                for t in range(2):
                    kb.mm(ps[0][:, 0:n], onesr[:], sq[:, t, 0:n], start=(t == 0), stop=(t == 1))
                kb.mm(ps[1][:, 0:n], onesr[:], sq[:, 2, 0:n])
                kb.act(rs[0][:, 0:n], ps[0][:, 0:n], AF.Sqrt, scale=1.0 / 256, bias=EPS)
                kb.recip(rs[0][:, 0:n], rs[0][:, 0:n])
                kb.act(rs[1][:, 0:n], ps[1][:, 0:n], AF.Sqrt, scale=1.0 / 128, bias=EPS)
                kb.recip(rs[1][:, 0:n], rs[1][:, 0:n])
                for t in range(2):
                    kb.stt("dve", cqn[:, t, t0:t0 + n], cqT[:, t, t0:t0 + n], Vn(f"qa_g{L}")[:, t:t + 1], rs[0][:, 0:n], ALU.mult, ALU.mult)
                kb.stt("dve", ckvn[:, t0:t0 + n], ckvT[:, t0:t0 + n], Vn(f"kva_g{L}")[:, 0:1], rs[1][:, 0:n], ALU.mult, ALU.mult)
                if t0 < NCTX:
                    kb.cp("dve", KPE[64:96, t0:t0 + n], kA[64:96, t0:t0 + n])
                else:
                    l0 = t0 - NCTX
                    kb.tt("dve", tt1[64:96, 0:n], kA[64:96, t0:t0 + n], ROPE[64:96, 0, l0:l0 + n], ALU.mult)
                    kb.tt("dve", tt2[64:96, 0:n], kB[64:96, t0:t0 + n], ROPE[64:96, 1, l0:l0 + n], ALU.mult)
                    kb.tt("dve", KPE[64:96, t0:t0 + n], tt1[64:96, 0:n], tt2[64:96, 0:n], ALU.add)
            kb.barrier()
        KT = psb("ml_KT", [128, NT], F32R)
        QT = psb("ml_QT", [128, NT], F32R)
        Vh = psb("ml_Vh", [128, 18, 65], F32R)
        PT = [psb(f"ml_PT{i}", [128, 512], F32R) for i in range(4)]
        Osb = [psb(f"ml_Osb{i}", [128, 512]) for i in range(2)]
        ys = [psb(f"ml_ys{i}", [128, 512]) for i in range(2)]
        u1 = psb("ml_u1", [128, 512])
        u2 = psb("ml_u2", [128, 512])
        onesf = cst[:, 128:256]
        kb.cp("dve", Vh[:, :, 64:65], onesf[:, 0:18].unsqueeze(2))
        cnt = 0
        for h in range(8):
            for (t0, n) in CHUNKS:
                kb.mm(ps[0][0:64, 0:n], wuk[:, h * 64:(h + 1) * 64], ckvn[:, t0:t0 + n])
                kb.cp("act", KT[0:64, t0:t0 + n], ps[0][0:64, 0:n])
            kb.cp("dve", KT[64:96, :], KPE[64:96, :])
            for g0 in range(0, 18, 8):
                gn = min(8, 18 - g0)
                for j in range(gn):
                    kt = g0 + j
                    kb.mm(ps[1][:, j * 64:(j + 1) * 64], ckvn[:, kt * 128:(kt + 1) * 128], wuv[:, h * 64:(h + 1) * 64])
                kb.cp("dve", Vh[:, g0:g0 + gn, 0:64], ps[1][:, 0:gn * 64].rearrange("p (a b) -> p a b", b=64))
            for (t0, n) in CHUNKS:
                lat = t0 >= NCTX
                if not lat and not need_ctx:
                    continue
                for k in range(2):
                    kb.mm(ps[0][0:96, 0:n], wq[:, k, 0, h * 96:(h + 1) * 96], cqn[:, k, t0:t0 + n], start=(k == 0), stop=(k == 1))
                if lat:
                    for k in range(2):
                        kb.mm(ps[1][0:96, 0:n], wq[:, k, 1, h * 96:(h + 1) * 96], cqn[:, k, t0:t0 + n], start=(k == 0), stop=(k == 1))
                kb.cp("act", QT[0:64, t0:t0 + n], ps[0][0:64, 0:n])
                if not lat:
                    kb.cp("act", QT[64:96, t0:t0 + n], ps[0][64:96, 0:n])
                else:
                    l0 = t0 - NCTX
                    kb.tt("dve", u1[64:96, 0:n], ps[0][64:96, 0:n], ROPE[64:96, 0, l0:l0 + n], ALU.mult)
                    kb.tt("dve", u2[64:96, 0:n], ps[1][64:96, 0:n], ROPE[64:96, 1, l0:l0 + n], ALU.mult)
                    kb.tt("dve", QT[64:96, t0:t0 + n], u1[64:96, 0:n], u2[64:96, 0:n], ALU.add)
            groups = ([[CHUNKS[0]]] if need_ctx else []) + [CHUNKS[1:3], CHUNKS[3:5]]
            for grp in groups:
                lat = grp[0][0] >= NCTX
                kts = list(range(18)) if lat else [0, 1]
                Sb = lambda a, i: ps[2 + 2 * a + i % 2]
                Pb = lambda a, i: PT[2 * a + i % 2]

                def emitS(i):
                    kt = kts[i]
                    for a, (t0, n) in enumerate(grp):
                        kb.mm(Sb(a, i)[:, 0:n], KT[0:96, kt * 128:(kt + 1) * 128], QT[0:96, t0:t0 + n])
                emitS(0)
                for i, kt in enumerate(kts):
                    for a, (t0, n) in enumerate(grp):
                        kb.act(Pb(a, i)[:, 0:n], Sb(a, i)[:, 0:n], AF.Exp, scale=MLA_SCALE)
                    if i + 1 < len(kts):
                        emitS(i + 1)
                    for a, (t0, n) in enumerate(grp):
                        kb.mm(ps[6 + a][0:65, 0:n], Vh[:, kt, :], Pb(a, i)[:, 0:n], start=(i == 0), stop=(i == len(kts) - 1))
                for a, (t0, n) in enumerate(grp):
                    ob = Osb[a]
                    yo = ys[a]
                    bcp = ps[a]
                    kb.cp("act", ob[0:65, 0:n], ps[6 + a][0:65, 0:n])
                    kb.recip(ob[64:65, 0:n], ob[64:65, 0:n])
                    kb.mm(bcp[0:64, 0:n], onesf[64:65, 0:64], ob[64:65, 0:n])
                    kb.tt("dve", yo[0:64, 0:n], ob[0:64, 0:n], bcp[0:64, 0:n], ALU.mult)
                    kb.dma("sp", ybT_d[256 + h * 64:256 + (h + 1) * 64, t0:t0 + n], yo[0:64, 0:n], reads=[yo], writes=[f"ybT_m{h}"])
        kb.barrier()


def phase_hg(G):
    nc, kb, L = G["nc"], G["kb"], G["L"]
    Vn, cst, onesr, ident, lbv = G["Vn"], G["cst"], G["onesr"], G["ident"], G["lbv"]
    zT_d, ybT_d = G["zT_d"], G["ybT_d"]
    CH = 64
    NC_ = NT // CH
    onesf = cst[:, 128:256]
    with ExitStack() as ph:
        psb = lambda name, shape, dt=F32: ph.enter_context(nc.sbuf_tensor(uq(name), shape, dt))
        pps = lambda name, shape, dt=F32: ph.enter_context(nc.psum_tensor(uq(name), shape, dt))
        A = psb("hg_A", [128, NT])
        KK = psb("hg_KK", [128, NT], F32R)
        Bt = psb("hg_Bt", [128, NT])
        E1 = psb("hg_E1", [128, NT], F32R)
        qT = psb("hg_qT", [128, NT])
        ig = psb("hg_ig", [128, NT])
        itok = psb("hg_itok", [128, NC_, 64], F32R)
        oacc = psb("hg_oacc", [128, NT])
        U = psb("hg_U", [128, NC_, 64])
        PTall = psb("hg_PT", [128, NC_, 64], F32R)
        Sst = psb("hg_Sst", [128, NC_, 64], F32R)
        ktok = [psb(f"hg_ktok{i}", [128, 4, 128], F32R) for i in range(2)]
        sct = [psb(f"hg_sct{i}", [128, 512]) for i in range(2)]
        small = psb("hg_small", [128, 4, NC_])
        S = psb("hg_S", [128, 64])
        tU = psb("hg_tU", [128, 64])
        fin = [psb(f"hg_fin{i}", [128, 512]) for i in range(3)]
        ps = [pps(f"hg_ps{i}", [128, 512]) for i in range(7)]
        MK = psb("hg_MK", [128, NT + 1])
        kb.memset("dve", MK[:], 1.0)
        kb.memset("dve", MK[:, 0:NT].rearrange("p (n c) -> p n c", c=CH)[:, :, 0:1], 0.0)
        pcnt = [0]

        def nps():
            pcnt[0] += 1
            return ps[pcnt[0] % 7]

        b3 = lambda t: t[:].rearrange("p (n c) -> p n c", c=CH)
        for h in range(4):
            tq, ti_, tg = COLIDX[f"hq{h}"], COLIDX[f"hi{h}"], COLIDX[f"hg{h}"]
            kb.dma("sp", qT[:], zT_d[tq * 128:(tq + 1) * 128, :], reads=[f"zT_{tq}"], writes=[qT])
            kb.dma("sp", ig[0:64, :], zT_d[ti_ * 128:ti_ * 128 + 64, :], reads=[f"zT_{ti_}"], writes=[ig])
            for c0 in range(0, NC_, 8):
                gn = min(8, NC_ - c0)
                p = nps()
                for j in range(gn):
                    c = c0 + j
                    kb.tr(p[0:64, j * 64:(j + 1) * 64], ig[0:64, c * CH:(c + 1) * CH], ident[0:64, 0:64])
                kb.cp("act", itok[0:64, c0:c0 + gn, :], p[0:64, 0:gn * 64].rearrange("p (a b) -> p a b", b=64))
            for d in range(2):
                tf = COLIDX[f"hf{h}" if d == 0 else f"hb{h}"]
                kb.dma("sp", A[:], zT_d[tf * 128:(tf + 1) * 128, :], reads=[f"zT_{tf}"], writes=[A])
                kb.act(A[:], A[:], AF.Sigmoid)
                kb.ts("dve", A[:], A[:], lbv[:, 1, L:L + 1], ALU.mult, lbv[:, 0, L:L + 1], ALU.add)
                kb.ts("dve", KK[:], A[:], -1.0, ALU.mult, 1.0, ALU.add)
                kb.act(A[:], A[:], AF.Ln)
                if d == 0:
                    kb.scan(Bt[:, :], MK[:, 0:NT], A[:, :], 0.0, ALU.mult, ALU.add)
                else:
                    kb.scan(Bt[:, ::-1], MK[:, 1:NT + 1][:, ::-1], A[:, ::-1], 0.0, ALU.mult, ALU.add)
                refpos = 31 if d == 0 else 32
                lastpos = 63 if d == 0 else 0
                refc, alpha, gamma, beta = (small[:, i, :] for i in range(4))
                kb.cp("dve", refc, b3(Bt)[:, :, refpos])
                kb.act(alpha, b3(Bt)[:, :, lastpos], AF.Exp)
                kb.act(gamma, refc, AF.Exp)
                kb.tt("dve", b3(Bt), b3(Bt), refc.unsqueeze(2).to_broadcast([128, NC_, CH]), ALU.subtract)
                kb.act(E1[:], Bt[:], AF.Exp)
                kb.cp("dve", beta, b3(E1)[:, :, lastpos].bitcast(F32))
                kb.act(Bt[:], Bt[:], AF.Exp, scale=-1.0)
                kb.tt("dve", E1[:], qT[:], E1[:].bitcast(F32), ALU.mult)
                kb.tt("dve", KK[:], KK[:].bitcast(F32), Bt[:], ALU.mult)
                for c0 in range(0, NC_, 4):
                    p = nps()
                    kt_ = ktok[(c0 // 4) % 2]
                    for j in range(4):
                        c = c0 + j
                        kb.tr(p[0:64, j * 128:(j + 1) * 128], KK[:, c * CH:(c + 1) * CH].bitcast(F32), ident)
                    kb.cp("act", kt_[0:64, :, :], p[0:64, :].rearrange("p (a b) -> p a b", b=128))
                    p2 = nps()
                    for j in range(4):
                        c = c0 + j
                        kb.mm(p2[:, j * 64:(j + 1) * 64], kt_[0:64, j, :], itok[0:64, c, :])
                    kb.cp("dve", U[:, c0:c0 + 4, :], p2[:, 0:256].rearrange("p (a b) -> p a b", b=64))
                for c0 in range(0, NC_, 8):
                    gn = min(8, NC_ - c0)
                    p = nps()
                    for j in range(gn):
                        c = c0 + j
                        kb.mm(p[0:64, j * 64:(j + 1) * 64], KK[:, c * CH:(c + 1) * CH], E1[:, c * CH:(c + 1) * CH])
                    st = sct[(c0 // 8) % 2]
                    kb.cp("act", st[0:64, 0:gn * 64], p[0:64, 0:gn * 64])
                    if d == 0:
                        kb.op("pool", lambda g: g.affine_select(PTall[0:64, c0:c0 + gn, :], st[0:64, 0:gn * 64].rearrange("p (a b) -> p a b", b=64),
                                                                [[0, gn], [1, 64]], ALU.is_ge, 0.0, base=0, channel_multiplier=-1),
                              reads=[st], writes=[PTall])
                    else:
                        kb.op("pool", lambda g: g.affine_select(PTall[0:64, c0:c0 + gn, :], st[0:64, 0:gn * 64].rearrange("p (a b) -> p a b", b=64),
                                                                [[0, gn], [-1, 64]], ALU.is_ge, 0.0, base=0, channel_multiplier=1),
                              reads=[st], writes=[PTall])
                AR = A[:].rearrange("p (v o) -> p v o", o=NC_)
                U2 = Bt[:].rearrange("p (v o) -> p v o", o=NC_)
                S2 = U[:].rearrange("p c v -> p (c v)").rearrange("p (v o) -> p v o", o=NC_)
                if d == 0:
                    segs = [(0, NC_, False)]
                    cof = lambda o: o
                else:
                    segs = [(0, 4, True), (4, NC_ - 4, True)]
                    cof = lambda o: (3 - o) if o < 4 else (NC_ + 3 - o)
                for (o0, n_, _) in segs:
                    c_hi, c_lo = cof(o0), cof(o0 + n_ - 1)
                    if d == 0:
                        csl = slice(c_hi, c_lo + 1)
                    else:
                        csl = slice(c_hi, (c_lo - 1) if c_lo > 0 else None, -1)
                    kb.tt("dve", U2[:, :, o0:o0 + n_].rearrange("p v o -> p o v"), U[:, csl, :],
                          beta[:, csl].unsqueeze(2).to_broadcast([128, n_, 64]), ALU.mult)
                    kb.cp("dve", AR[:, :, o0:o0 + n_].rearrange("p v o -> p o v"), alpha[:, csl].unsqueeze(2).to_broadcast([128, n_, 64]))
                kb.memset("dve", AR[:, :, 0:1], 0.0)
                kb.scan(U[:].rearrange("p c v -> p (c v)"), A[:, :], Bt[:, :], 0.0, ALU.mult, ALU.add)
                first_c = cof(0)
                kb.memset("dve", Sst[:, first_c, :].bitcast(F32), 0.0)
                for (o0, n_, _) in segs:
                    oa = max(o0, 1)
                    nn = o0 + n_ - oa
                    c_hi, c_lo = cof(oa), cof(oa + nn - 1)
                    if d == 0:
                        csl = slice(c_hi, c_lo + 1)
                    else:
                        csl = slice(c_hi, (c_lo - 1) if c_lo > 0 else None, -1)
                    kb.tt("dve", Sst[:, csl, :], S2[:, :, oa - 1:oa - 1 + nn].rearrange("p v o -> p o v"),
                          gamma[:, csl].unsqueeze(2).to_broadcast([128, nn, 64]), ALU.mult)
                for c0 in range(0, NC_, 8):
                    gn = min(8, NC_ - c0)
                    p = nps()
                    for j in range(gn):
                        c = c0 + j
                        kb.mm(p[0:64, j * 64:(j + 1) * 64], itok[0:64, c, :], PTall[0:64, c, :], start=True, stop=False)
                        kb.mm(p[0:64, j * 64:(j + 1) * 64], Sst[:, c, :], E1[:, c * CH:(c + 1) * CH], start=False, stop=True)
                    if d == 0:
                        kb.cp("act", oacc[0:64, c0 * CH:(c0 + gn) * CH], p[0:64, 0:gn * 64])
                    else:
                        kb.tt("dve", oacc[0:64, c0 * CH:(c0 + gn) * CH], oacc[0:64, c0 * CH:(c0 + gn) * CH], p[0:64, 0:gn * 64], ALU.add)
            kb.dma("sp", ig[0:64, :], zT_d[tg * 128:tg * 128 + 64, :], reads=[f"zT_{tg}"], writes=[ig])
            kb.act(KK[0:64, :], oacc[0:64, :], AF.Square)
            kb.act(ig[0:64, :], ig[0:64, :], AF.Silu)
            for (t0, n) in CHUNKS:
                p = nps()
                kb.mm(p[0:64, 0:n], onesr[0:64, 0:64], KK[0:64, t0:t0 + n])
                kb.act(fin[0][0:64, 0:n], p[0:64, 0:n], AF.Sqrt, scale=1.0 / 64, bias=EPS)
                kb.recip(fin[0][0:64, 0:n], fin[0][0:64, 0:n])
                kb.stt("dve", fin[1][0:64, 0:n], oacc[0:64, t0:t0 + n], Vn(f"hgn_g{L}")[0:64, h:h + 1], fin[0][0:64, 0:n], ALU.mult, ALU.mult)
                kb.tt("dve", fin[2][0:64, 0:n], fin[1][0:64, 0:n], ig[0:64, t0:t0 + n], ALU.mult)
                kb.dma("sp", ybT_d[768 + h * 64:768 + (h + 1) * 64, t0:t0 + n], fin[2][0:64, 0:n], reads=[fin[2]], writes=[f"ybT_h{h}"])
        kb.barrier()


def phase_merge(G):
    nc, kb, L = G["nc"], G["kb"], G["L"]
    xT, modT = G["xT"], G["modT"]
    zT_d, ybT_d = G["zT_d"], G["ybT_d"]
    need_ctx = L < DEPTH - 1
    chs = CHUNKS if need_ctx else CHUNKS[1:]
    yb_names = ["ybT_0", "ybT_1"] + [f"ybT_m{h}" for h in range(8)] + [f"ybT_h{h}" for h in range(4)]
    with ExitStack() as ph:
        psb = lambda name, shape, dt=F32: ph.enter_context(nc.sbuf_tensor(uq(name), shape, dt))
        pps = lambda name, shape, dt=F32: ph.enter_context(nc.psum_tensor(uq(name), shape, dt))
        wp = psb("mg_wp", [128, 8, D], F32R)
        wo = psb("mg_wo", [128, 8, D], F32R)
        kb.dma("pool", wp[:], G["wp_d"][L].rearrange("(k p) c -> p k c", p=128), writes=[wp])
        kb.dma("pool", wo[:], G["wo_d"][L].rearrange("(k p) c -> p k c", p=128), writes=[wo])
        yb = psb("mg_yb", [128, 8, 512], F32R)
        mT = psb("mg_mT", [128, 8, 512], F32R)
        gt = [psb(f"mg_gt{i}", [128, 3, 512]) for i in range(2)]
        t3 = [psb(f"mg_t{i}", [128, 512]) for i in range(3)]
        ps = [pps(f"mg_ps{i}", [128, 512]) for i in range(8)]
        branches = ((0, 2), (2, 6), (6, 8))
        for (t0, n) in chs:
            s_ = 1 if t0 < NCTX else 0
            kb.dma("pool", yb[:, :, 0:n], ybT_d[:, t0:t0 + n].rearrange("(k p) t -> p k t", p=128), reads=yb_names, writes=[yb])
            for f in range(8):
                g = gt[f % 2]
                for b in range(3):
                    ti = COLIDX[f"gate{b}_{f}"]
                    kb.dma("sp", g[:, b, 0:n], zT_d[ti * 128:(ti + 1) * 128, t0:t0 + n], reads=[f"zT_{ti}"], writes=[g])
                kb.act(g[:, :, 0:n], g[:, :, 0:n], AF.Sigmoid)
                for b, (k0, k1) in enumerate(branches):
                    p = ps[(f % 2) * 3 + b]
                    for k in range(k0, k1):
                        kb.mm(p[:, 0:n], wp[:, k, f * 128:(f + 1) * 128], yb[:, k, 0:n], start=(k == k0), stop=(k == k1 - 1))
                    kb.tt("dve", t3[b][:, 0:n], p[:, 0:n], g[:, b, 0:n], ALU.mult)
                kb.tt("pool", t3[0][:, 0:n], t3[0][:, 0:n], t3[1][:, 0:n], ALU.add)
                kb.tt("pool", mT[:, f, 0:n], t3[0][:, 0:n], t3[2][:, 0:n], ALU.add)
            for f in range(8):
                p = ps[6 + f % 2]
                for k in range(8):
                    kb.mm(p[:, 0:n], wo[:, k, f * 128:(f + 1) * 128], mT[:, k, 0:n], start=(k == 0), stop=(k == 7))
                kb.stt("dve", xT[:, f, t0:t0 + n], p[:, 0:n], modT[:, L, 16 + f, s_:s_ + 1], xT[:, f, t0:t0 + n], ALU.mult, ALU.add)
        kb.barrier()


def phase_moe(G):
    nc, kb, L = G["nc"], G["kb"], G["L"]
    xT, modT, modA, onesr, ident, cst = G["xT"], G["modT"], G["modA"], G["onesr"], G["ident"], G["cst"]
    need_ctx = L < DEPTH - 1
    chs = CHUNKS if need_ctx else CHUNKS[1:]
    groups = [chs[:3], chs[3:]] if need_ctx else [chs[:2], chs[2:]]
    onesf = cst[:, 128:256]
    for grp in groups:
        GN = sum(n for _, n in grp)
        gch = []
        o = 0
        for (t0, n) in grp:
            gch.append((t0, n, o))
            o += n
        with ExitStack() as ph:
            psb = lambda name, shape, dt=F32: ph.enter_context(nc.sbuf_tensor(uq(name), shape, dt))
            pps = lambda name, shape, dt=F32: ph.enter_context(nc.psum_tensor(uq(name), shape, dt))
            h2T = psb("me_h2T", [128, 8, GN], F32R)
            gateT = psb("me_gateT", [128, GN], F32R)
            emit_norm(nc, kb, xT, h2T, gch, lambda k, s_: modA[:, L, 1, k, s_:s_ + 1],
                      lambda k, s_: modT[:, L, 24 + k, s_:s_ + 1], onesr, D)
            with ExitStack() as rp:
                rsb = lambda name, shape, dt=F32: rp.enter_context(nc.sbuf_tensor(uq(name), shape, dt))
                rps = lambda name, shape, dt=F32: rp.enter_context(nc.psum_tensor(uq(name), shape, dt))
                wr = rsb("me_wr", [128, 8, 36])
                br = rsb("me_br", [128, 36])
                kb.dma("sp", wr[:], G["wr_d"][L].rearrange("(k p) c -> p k c", p=128), writes=[wr])
                kb.dma("sp", br[:], G["br_d"][L].to_broadcast([128, 36]), writes=[br])
                lp = [rps(f"me_lp{i}", [128, 512]) for i in range(2)]
                gp = [rps(f"me_gp{i}", [128, 512]) for i in range(2)]
                R = [[rsb(f"me_r{b}_{i}", [128, 40]) for i in range(12)] for b in range(2)]
                for tt in range(GN // 128):
                    b = tt % 2
                    r = R[b]
                    for k in range(8):
                        kb.mm(lp[b][:, 0:36], h2T[:, k, tt * 128:(tt + 1) * 128].bitcast(F32), wr[:, k, :], start=(k == 0), stop=(k == 7))
                    lg = r[0]
                    kb.tt("dve", lg[:, 0:36], lp[b][:, 0:36], br[:], ALU.add)
                    gmax, ngmax, gsum, gw = r[1][:, 0:1], r[1][:, 1:2], r[1][:, 2:3], r[1][:, 3:4]
                    kb.op("dve", lambda g: g.tensor_reduce(gmax, lg[:, 0:4], AX.X, ALU.max), reads=[lg], writes=[r[1]])
                    kb.ts("dve", ngmax, gmax, -1.0, ALU.mult)
                    kb.act(r[2][:, 0:4], lg[:, 0:4], AF.Exp, bias=ngmax)
                    kb.op("dve", lambda g: g.tensor_reduce(gsum, r[2][:, 0:4], AX.X, ALU.add), reads=[r[2]], writes=[r[1]])
                    kb.recip(gw, gsum)
                    kb.ts("dve", r[3][:, 0:4], lg[:, 0:4], gmax, ALU.is_equal)
                    kb.ts("dve", r[3][:, 0:4], r[3][:, 0:4], -1.0, ALU.add, 1e30, ALU.mult)
                    kb.tt("dve", r[4][:, 0:32].rearrange("p (a b) -> p a b", b=8), lg[:, 4:36].rearrange("p (a b) -> p a b", b=8),
                          r[3][:, 0:4].unsqueeze(2).to_broadcast([128, 4, 8]), ALU.add)
                    kb.op("dve", lambda g: g.max(r[5][:, 0:8], r[4][:, 0:32]), reads=[r[4]], writes=[r[5]])
                    m1, m2 = r[5][:, 0:1], r[5][:, 1:2]
                    kb.ts("dve", r[6][:, 0:32], r[4][:, 0:32], m1, ALU.is_equal)
                    kb.ts("dve", r[7][:, 0:32], r[4][:, 0:32], m2, ALU.is_equal)
                    dm, ee, p1, p2 = r[8][:, 0:1], r[8][:, 1:2], r[8][:, 2:3], r[8][:, 3:4]
                    kb.tt("dve", dm, m2, m1, ALU.subtract)
                    kb.act(ee, dm, AF.Exp)
                    kb.ts("dve", p1, ee, 1.0, ALU.add)
                    kb.recip(p1, p1)
                    kb.tt("dve", p2, ee, p1, ALU.mult)
                    kb.tt("dve", p1, p1, gw, ALU.mult)
                    kb.tt("dve", p2, p2, gw, ALU.mult)
                    kb.ts("dve", r[9][:, 0:32], r[6][:, 0:32], p1, ALU.mult)
                    kb.stt("dve", r[9][:, 0:32], r[7][:, 0:32], p2, r[9][:, 0:32], ALU.mult, ALU.add)
                    kb.tr(gp[b][0:32, 0:128], r[9][:, 0:32], ident)
                    kb.cp("act", gateT[0:32, tt * 128:(tt + 1) * 128], gp[b][0:32, 0:128])
                kb.barrier()
            Gall = psb("me_G", [128, 4, GN], F32R)
            w13 = [[psb(f"me_w{a}_{i}", [128, 8, 128], F32R) for i in range(2)] for a in (1, 3)]
            w2 = [psb(f"me_w2_{i}", [128, 4, D], F32R) for i in range(2)]
            sel = [psb(f"me_sel{i}", [128, 128], F32R) for i in range(2)]
            sil = [psb(f"me_sil{i}", [128, 512]) for i in range(2)]
            hp = [pps(f"me_hp{i}", [128, 512]) for i in range(4)]
            bp = [pps(f"me_bp{i}", [128, 512]) for i in range(2)]
            yp = [pps(f"me_yp{i}", [128, 512]) for i in range(2)]
            cnt = 0
            for e in range(32):
                se = sel[e % 2]
                kb.op("pool", lambda g: g.affine_select(se[0:32, :], onesf[0:32, :], [[0, 128]], ALU.is_equal, 0.0,
                                                        base=-e, channel_multiplier=1), reads=[cst], writes=[se])
                kb.dma("pool", w2[e % 2][:], G["w2_d"][L, e].rearrange("(j p) c -> p j c", p=128), writes=[w2[e % 2]])
                for j in range(4):
                    wa, wb_ = w13[0][(e * 4 + j) % 2], w13[1][(e * 4 + j) % 2]
                    kb.dma("pool", wa[:], G["w1_d"][L, e, :, j * 128:(j + 1) * 128].rearrange("(k p) c -> p k c", p=128), writes=[wa])
                    kb.dma("pool", wb_[:], G["w3_d"][L, e, :, j * 128:(j + 1) * 128].rearrange("(k p) c -> p k c", p=128), writes=[wb_])
                    for (t0, n, o) in gch:
                        b = cnt % 2
                        cnt += 1
                        for k in range(8):
                            kb.mm(hp[b][:, 0:n], wa[:, k, :], h2T[:, k, o:o + n], start=(k == 0), stop=(k == 7))
                        for k in range(8):
                            kb.mm(hp[2 + b][:, 0:n], wb_[:, k, :], h2T[:, k, o:o + n], start=(k == 0), stop=(k == 7))
                        kb.mm(bp[b][:, 0:n], se[0:32, :], gateT[0:32, o:o + n])
                        kb.act(sil[b][:, 0:n], hp[b][:, 0:n], AF.Silu)
                        kb.tt("dve", sil[b][:, 0:n], sil[b][:, 0:n], hp[2 + b][:, 0:n], ALU.mult)
                        kb.tt("dve", Gall[:, j, o:o + n], sil[b][:, 0:n], bp[b][:, 0:n], ALU.mult)
                for (t0, n, o) in gch:
                    s_ = 1 if t0 < NCTX else 0
                    for f in range(8):
                        p = yp[f % 2]
                        for j in range(4):
                            kb.mm(p[:, 0:n], w2[e % 2][:, j, f * 128:(f + 1) * 128], Gall[:, j, o:o + n], start=(j == 0), stop=(j == 3))
                        kb.stt("dve", xT[:, f, t0:t0 + n], p[:, 0:n], modT[:, L, 40 + f, s_:s_ + 1], xT[:, f, t0:t0 + n], ALU.mult, ALU.add)
            kb.barrier()


def phase_moe_sparse(G):
    nc, kb, L = G["nc"], G["kb"], G["L"]
    xT, modT, modA, onesr, ident, cst = G["xT"], G["modT"], G["modA"], G["onesr"], G["ident"], G["cst"]
    h2tok_d, xs_d, ys_d = G["h2tok_d"], G["xs_d"], G["ys_d"]
    need_ctx = L < DEPTH - 1
    chs = CHUNKS if need_ctx else CHUNKS[1:]
    tiles = [t0 // 128 + j for (t0, n) in chs for j in range(n // 128)]
    NTL = len(tiles)
    onesf = cst[:, 128:256]
    with ExitStack() as ph:
        psb = lambda name, shape, dt=F32: ph.enter_context(nc.sbuf_tensor(uq(name), shape, dt))
        pps = lambda name, shape, dt=F32: ph.enter_context(nc.psum_tensor(uq(name), shape, dt))
        pr = ExitStack()
        rsb_ = lambda name, shape, dt=F32: pr.enter_context(nc.sbuf_tensor(uq(name), shape, dt))
        GA = psb("ms_GA", [128, 18])
        GB = psb("ms_GB", [128, 18])
        D1f = psb("ms_D1f", [128, 18])
        D2f = psb("ms_D2f", [128, 18])
        D1i = psb("ms_D1i", [128, 18], I32)
        D2i = psb("ms_D2i", [128, 18], I32)
        idxW = psb("ms_idxW", [128, 128], I32)
        p0 = ExitStack()
        h2T = p0.enter_context(nc.sbuf_tensor(uq("ms_h2T"), [128, 8, NT], F32R))
        OH1 = rsb_("ms_OH1", [128, 18, 32])
        OH2 = rsb_("ms_OH2", [128, 18, 32])
        AA = rsb_("ms_AA", [128, 18, 32])
        with ExitStack() as p1:
            qsb = lambda name, shape, dt=F32: p1.enter_context(nc.sbuf_tensor(uq(name), shape, dt))
            qps = lambda name, shape, dt=F32: p1.enter_context(nc.psum_tensor(uq(name), shape, dt))
            emit_norm(nc, kb, xT, h2T, [(t0, n, t0) for (t0, n) in chs], lambda k, s_: modA[:, L, 1, k, s_:s_ + 1],
                      lambda k, s_: modT[:, L, 24 + k, s_:s_ + 1], onesr, D)
            wr = qsb("ms_wr", [128, 8, 36])
            br = qsb("ms_br", [128, 36])
            kb.dma("sp", wr[:], G["wr_d"][L].rearrange("(k p) c -> p k c", p=128), writes=[wr])
            kb.dma("sp", br[:], G["br_d"][L].to_broadcast([128, 36]), writes=[br])
            lp = [qps(f"ms_lp{i}", [128, 512]) for i in range(2)]
            T_ = NTL
            LG = qsb("ms_LG", [128, 18, 36])
            for i, tt in enumerate(tiles):
                b = i % 2
                tsl = slice(tt * 128, (tt + 1) * 128)
                for k in range(8):
                    kb.mm(lp[b][:, 0:36], h2T[:, k, tsl].bitcast(F32), wr[:, k, :], start=(k == 0), stop=(k == 7))
                kb.tt("dve", LG[:, i, :], lp[b][:, 0:36], br[:], ALU.add)
            B4 = lambda t: t[:, 0:T_, :]
            g4 = qsb("ms_g4", [128, 18, 4])
            oh4 = qsb("ms_oh4", [128, 18, 4])
            ls = qsb("ms_ls", [128, 18, 32])
            l2 = qsb("ms_l2", [128, 18, 32])
            sm_ = qsb("ms_sm", [128, 8, 18])
            gmax, gsum, gw, m1, m2, ee, p1_, p2_ = (sm_[:, j, 0:T_] for j in range(8))
            bc4 = lambda v: v.unsqueeze(2).to_broadcast([128, T_, 4])
            bc32 = lambda v: v.unsqueeze(2).to_broadcast([128, T_, 32])
            kb.op("dve", lambda g: g.tensor_reduce(gmax, LG[:, 0:T_, 0:4], AX.X, ALU.max), reads=[LG], writes=[sm_])
            kb.tt("dve", g4[:, 0:T_, :], LG[:, 0:T_, 0:4], bc4(gmax), ALU.subtract)
            kb.tt("dve", oh4[:, 0:T_, :], LG[:, 0:T_, 0:4], bc4(gmax), ALU.is_equal)
            kb.act(g4[:, 0:T_, :], g4[:, 0:T_, :], AF.Exp)
            kb.op("dve", lambda g: g.tensor_reduce(gsum, g4[:, 0:T_, :], AX.X, ALU.add), reads=[g4], writes=[sm_])
            kb.recip(gw, gsum)
            kb.ts("dve", oh4[:, 0:T_, :], oh4[:, 0:T_, :], -1.0, ALU.add, 1e30, ALU.mult)
            kb.tt("dve", ls[:, 0:T_, :].rearrange("p t (a b) -> p t a b", b=8), LG[:, 0:T_, 4:36].rearrange("p t (a b) -> p t a b", b=8),
                  oh4[:, 0:T_, :].unsqueeze(3).to_broadcast([128, T_, 4, 8]), ALU.add)
            kb.op("dve", lambda g: g.tensor_reduce(m1, ls[:, 0:T_, :], AX.X, ALU.max), reads=[ls], writes=[sm_])
            kb.tt("dve", OH1[:, 0:T_, :], ls[:, 0:T_, :], bc32(m1), ALU.is_equal)
            kb.stt("dve", l2[:, 0:T_, :], OH1[:, 0:T_, :], -1e30, ls[:, 0:T_, :], ALU.mult, ALU.add)
            kb.op("dve", lambda g: g.tensor_reduce(m2, l2[:, 0:T_, :], AX.X, ALU.max), reads=[l2], writes=[sm_])
            kb.tt("dve", OH2[:, 0:T_, :], l2[:, 0:T_, :], bc32(m2), ALU.is_equal)
            kb.tt("dve", AA[:, 0:T_, :], OH1[:, 0:T_, :], OH2[:, 0:T_, :], ALU.add)
            kb.tt("dve", ee, m2, m1, ALU.subtract)
            kb.act(ee, ee, AF.Exp)
            kb.ts("dve", p1_, ee, 1.0, ALU.add)
            kb.recip(p1_, p1_)
            kb.tt("dve", p2_, ee, p1_, ALU.mult)
            kb.tt("dve", GA[:, 0:T_], p1_, gw, ALU.mult)
            kb.tt("dve", GB[:, 0:T_], p2_, gw, ALU.mult)
            kb.barrier()
        with ExitStack() as p2:
            qsb = lambda name, shape, dt=F32: p2.enter_context(nc.sbuf_tensor(uq(name), shape, dt))
            qps = lambda name, shape, dt=F32: p2.enter_context(nc.psum_tensor(uq(name), shape, dt))
            ltri = qsb("ms_ltri", [128, 128])
            kb.op("pool", lambda g: g.affine_select(ltri[:], onesf, [[1, 128]], ALU.is_gt, 0.0, base=0, channel_multiplier=-1),
                  reads=[cst], writes=[ltri])
            Rp = [qps(f"ms_Rp{i}", [128, 512]) for i in range(2)]
            Cp = qps("ms_Cp", [128, 512])
            for i in range(NTL):
                out = Rp[i // 16][:, (i % 16) * 32:(i % 16 + 1) * 32]
                kb.mm(out, ltri[:], AA[:, i, :], start=True, stop=(i == 0))
                for i2 in range(i):
                    kb.mm(out, onesf, AA[:, i2, :], start=False, stop=(i2 == i - 1))
            for i in range(NTL):
                kb.mm(Cp[:, 0:32], onesf, AA[:, i, :], start=(i == 0), stop=(i == NTL - 1))
            w_ = [qsb(f"ms_w{i}", [128, 32]) for i in range(8)]
            cnt, x_, kf, msk, nb, pend, pstart, tmp = w_
            kb.cp("dve", cnt[:], Cp[:, 0:32])
            kb.ts("dve", x_[:], cnt[:], -float(SLOT), ALU.add, 0.0, ALU.max)
            kb.ts("dve", x_[:], x_[:], 127.0, ALU.add, 1.0 / 128, ALU.mult)
            ki = qsb("ms_ki", [128, 32], I32)
            kb.cp("dve", ki[:], x_[:])
            kb.cp("dve", kf[:], ki[:])
            kb.tt("dve", msk[:], kf[:], x_[:], ALU.is_gt)
            kb.tt("dve", kf[:], kf[:], msk[:], ALU.subtract)
            kb.ts("dve", tmp[:], kf[:], 1.0, ALU.add)
            kb.tt("dve", msk[:], tmp[:], x_[:], ALU.is_le)
            kb.tt("dve", nb[:], kf[:], msk[:], ALU.add)
            kb.scan(pend[:], onesf[:, 0:32], nb[:], 0.0, ALU.mult, ALU.add)
            kb.tt("dve", pstart[:], pend[:], nb[:], ALU.subtract)
            base1_i = qsb("ms_b1i", [128, 32], I32)
            base1 = qsb("ms_b1", [128, 32])
            kb.op("pool", lambda g: g.iota(base1_i[:], [[SLOT, 32]], base=0, channel_multiplier=0), writes=[base1_i])
            kb.cp("dve", base1[:], base1_i[:])
            kb.ts("dve", pstart[:], pstart[:], 128.0, ALU.mult, float(32 * SLOT - SLOT), ALU.add)
            kb.tt("dve", pstart[:], pstart[:], base1[:], ALU.subtract)
            RR = qsb("ms_RR", [128, 18, 32])
            T3 = qsb("ms_T3", [128, 18, 32])
            T4 = qsb("ms_T4", [128, 18, 32])
            n0 = min(NTL, 16)
            kb.cp("dve", RR[:, 0:n0, :], Rp[0][:, 0:n0 * 32].rearrange("p (a b) -> p a b", b=32))
            if NTL > 16:
                kb.cp("dve", RR[:, 16:NTL, :], Rp[1][:, 0:(NTL - 16) * 32].rearrange("p (a b) -> p a b", b=32))
            bcT = lambda v: v.unsqueeze(1).to_broadcast([128, NTL, 32])
            kb.ts("dve", T4[:, 0:NTL, :], RR[:, 0:NTL, :], float(SLOT), ALU.is_ge)
            kb.tt("dve", T4[:, 0:NTL, :], T4[:, 0:NTL, :], bcT(pstart[:]), ALU.mult)
            kb.tt("dve", T3[:, 0:NTL, :], RR[:, 0:NTL, :], bcT(base1[:]), ALU.add)
            kb.tt("dve", T3[:, 0:NTL, :], T3[:, 0:NTL, :], T4[:, 0:NTL, :], ALU.add)
            kb.tt("dve", T4[:, 0:NTL, :], T3[:, 0:NTL, :], OH1[:, 0:NTL, :], ALU.mult)
            kb.op("dve", lambda g: g.tensor_reduce(D1f[:, 0:NTL], T4[:, 0:NTL, :], AX.X, ALU.add), reads=[T4], writes=[D1f])
            kb.tt("dve", T4[:, 0:NTL, :], T3[:, 0:NTL, :], OH2[:, 0:NTL, :], ALU.mult)
            kb.op("dve", lambda g: g.tensor_reduce(D2f[:, 0:NTL], T4[:, 0:NTL, :], AX.X, ALU.add), reads=[T4], writes=[D2f])
            kb.cp("dve", D1i[:, 0:NTL], D1f[:, 0:NTL])
            kb.cp("dve", D2i[:, 0:NTL], D2f[:, 0:NTL])
            pidx_i = qsb("ms_pidx_i", [128, 1], I32)
            pidx = qsb("ms_pidx", [128, 1])
            kb.op("pool", lambda g: g.iota(pidx_i[:], [[0, 1]], base=0, channel_multiplier=1), writes=[pidx_i])
            kb.cp("dve", pidx[:], pidx_i[:])
            be = qsb("ms_be", [128, 1])
            kb.ts("dve", tmp[:], pend[:], pidx[:, 0:1], ALU.is_le)
            kb.op("dve", lambda g: g.tensor_reduce(be[:], tmp[:], AX.X, ALU.add), reads=[tmp], writes=[be])
            kb.ts("dve", be[:], be[:], 31.0, ALU.min)
            vb = qsb("ms_vb", [128, 1])
            kb.ts("dve", vb[:], pidx[:], pend[:, 31:32], ALU.is_lt)
            kb.tt("dve", be[:], be[:], vb[:], ALU.mult)
            kb.ts("dve", vb[:], vb[:], -1.0, ALU.add, -1000.0, ALU.mult)
            kb.tt("dve", be[:], be[:], vb[:], ALU.add)
            dg = qsb("ms_dg", [128, 128])
            kb.ts("dve", dg[:], ident, be[:, 0:1], ALU.mult)
            kb.mm(Cp[:, 128:256], onesf, dg[:])
            bef = qsb("ms_bef", [128, 128])
            kb.ts("dve", bef[:], Cp[:, 128:256], 128.0, ALU.mult, pidx[:, 0:1], ALU.add)
            if L > 0:
                kb.ts("dve", bef[:], bef[:], float(L * 32 * 128), ALU.add)
            kb.cp("dve", idxW[:], bef[:])
            kb.barrier()
        pr.close()
        with ExitStack() as p3:
            qsb = lambda name, shape, dt=F32: p3.enter_context(nc.sbuf_tensor(uq(name), shape, dt))
            qps = lambda name, shape, dt=F32: p3.enter_context(nc.psum_tensor(uq(name), shape, dt))
            hk = [qsb(f"ms_hs{i}", [128, D]) for i in range(3)]
            tp = [qps(f"ms_tp{i}", [128, 512]) for i in range(4)]
            for i, tt in enumerate(tiles):
                h = hk[i % 3]
                tsl = slice(tt * 128, (tt + 1) * 128)
                for half in range(2):
                    p = tp[(2 * i + half) % 4]
                    for j in range(4):
                        k = half * 4 + j
                        kb.tr(p[:, j * 128:(j + 1) * 128], h2T[:, k, tsl].bitcast(F32), ident)
                    kb.cp("act" if half == 0 else "dve", h[:, half * 512:(half + 1) * 512], p[:])
                kb.idma(xs_d, bass.IndirectOffsetOnAxis(ap=D1i[:, i:i + 1], axis=0), h[:], None, reads=[h, D1i], writes=["xs"])
                kb.idma(xs_d, bass.IndirectOffsetOnAxis(ap=D2i[:, i:i + 1], axis=0), h[:], None, reads=[h, D2i], writes=["xs"])
            kb.barrier()
        p0.close()
        with ExitStack() as p4:
            qsb = lambda name, shape, dt=F32: p4.enter_context(nc.sbuf_tensor(uq(name), shape, dt))
            qps = lambda name, shape, dt=F32: p4.enter_context(nc.psum_tensor(uq(name), shape, dt))
            W1 = [qsb(f"ms_W1_{i}", [128, 8 * 512], F32R) for i in range(2)]
            W3 = [qsb(f"ms_W3_{i}", [128, 8 * 512], F32R) for i in range(2)]
            W2 = [qsb(f"ms_W2_{i}", [128, 4 * D], F32R) for i in range(2)]
            Xb = [qsb(f"ms_Xb{i}", [128, D]) for i in range(3)]
            XT = [qsb(f"ms_XT{i}", [128, 8, 128], F32R) for i in range(2)]
            SL = [qsb(f"ms_SL{i}", [128, 512]) for i in range(2)]
            Gt = [qsb(f"ms_Gt{i}", [128, 4, 128], F32R) for i in range(2)]
            Ys = [qsb("ms_Ys0", [128, D])] * 2
            tpp = [qps(f"ms_tq{i}", [128, 512]) for i in range(2)]
            hp1 = [qps("ms_h1", [128, 512])] * 2
            hp3 = [qps("ms_h3", [128, 512])] * 2
            ypp = [qps(f"ms_yp{i}", [128, 512]) for i in range(2)]
            xtp = [qps(f"ms_xq{i}", [128, 512]) for i in range(2)]
            def xload(i):
                if i < len(subs):
                    kb.dma("sp", Xb[i % 3][:], xs_d[subs[i][0]:subs[i][0] + 128, :], reads=["xs"], writes=[Xb[i % 3]])

            def stageA(row0, W1t, W3t, xq, xb3):
                for half in range(2):
                    p = xtp[half]
                    for j in range(4):
                        k = half * 4 + j
                        kb.tr(p[:, j * 128:(j + 1) * 128], Xb[xb3][:, k * 128:(k + 1) * 128], ident)
                    kb.cp("act" if half == 0 else "dve", XT[xq][:, half * 4:half * 4 + 4, :], p[:].rearrange("p (a b) -> p a b", b=128))
                w1v = W1t[:].rearrange("p (k c) -> p k c", c=512)
                w3v = W3t[:].rearrange("p (k c) -> p k c", c=512)
                for k in range(8):
                    kb.mm(hp1[xq][:], XT[xq][:, k, :], w1v[:, k, :], start=(k == 0), stop=(k == 7))
                    kb.mm(hp3[xq][:], XT[xq][:, k, :], w3v[:, k, :], start=(k == 0), stop=(k == 7))
                kb.act(SL[xq][:], hp1[xq][:], AF.Silu)
                kb.tt("dve", SL[xq][:], SL[xq][:], hp3[xq][:], ALU.mult)

            def stageB(row0, W2t, xq):
                w2v = W2t[:].rearrange("p (j c) -> p j c", c=D)
                gp_ = tpp[xq]
                for j in range(4):
                    kb.tr(gp_[:, j * 128:(j + 1) * 128], SL[xq][:, j * 128:(j + 1) * 128], ident)
                kb.cp("act", Gt[xq][:].rearrange("p a b -> p (a b)"), gp_[:])
                for half in range(2):
                    yp = ypp[half]
                    for j in range(4):
                        kb.mm(yp[:], Gt[xq][:, j, :], w2v[:, j, half * 512:(half + 1) * 512], start=(j == 0), stop=(j == 3))
                    kb.cp("act" if half == 0 else "dve", Ys[xq][:, half * 512:(half + 1) * 512], yp[:])
                kb.dma("sp", ys_d[row0:row0 + 128, :], Ys[xq][:], reads=[Ys[xq]], writes=["ys"])

            subs = []
            wcnt = 0
            for e in range(32):
                pb = wcnt % 2
                wcnt += 1

                def ld(e=e, pb=pb):
                    kb.dma("pool", W1[pb][:], G["w1_d"][L, e * 128:(e + 1) * 128, :], writes=[W1[pb]])
                    kb.dma("pool", W3[pb][:], G["w3_d"][L, e * 128:(e + 1) * 128, :], writes=[W3[pb]])
                    kb.dma("pool", W2[pb][:], G["w2_d"][L, e * 128:(e + 1) * 128, :], writes=[W2[pb]])
                for j in range(SLOT // 128):
                    subs.append((e * SLOT + j * 128, pb, ld if j == 0 else None))
            for b in range(NOV):
                pb = wcnt % 2
                wcnt += 1

                def ld(b=b, pb=pb):
                    off = bass.IndirectOffsetOnAxis(ap=idxW[:, b:b + 1], axis=0)
                    bnd = (L + 1) * 32 * 128 - 1
                    kb.idma(W1[pb][:], None, G["w1_d"].rearrange("l r c -> (l r) c"), off, reads=[idxW], writes=[W1[pb]], bounds=bnd)
                    kb.idma(W3[pb][:], None, G["w3_d"].rearrange("l r c -> (l r) c"), off, reads=[idxW], writes=[W3[pb]], bounds=bnd)
                    kb.idma(W2[pb][:], None, G["w2_d"].rearrange("l r c -> (l r) c"), off, reads=[idxW], writes=[W2[pb]], bounds=bnd)
                subs.append((32 * SLOT + b * 128, pb, ld))
            xload(0)
            xload(1)
            for i, (row0, pb, ld) in enumerate(subs):
                if ld is not None:
                    ld()
                xload(i + 2)
                stageA(row0, W1[pb], W3[pb], i % 2, i % 3)
                if i >= 1:
                    r1, pb1, _ = subs[i - 1]
                    stageB(r1, W2[pb1], (i - 1) % 2)
            r1, pb1, _ = subs[-1]
            stageB(r1, W2[pb1], (len(subs) - 1) % 2)
            kb.barrier()
        with ExitStack() as p5:
            qsb = lambda name, shape, dt=F32: p5.enter_context(nc.sbuf_tensor(uq(name), shape, dt))
            qps = lambda name, shape, dt=F32: p5.enter_context(nc.psum_tensor(uq(name), shape, dt))
            y1 = [qsb(f"ms_y1_{i}", [128, D]) for i in range(2)]
            y2 = [qsb(f"ms_y2_{i}", [128, D]) for i in range(2)]
            tq = [qps(f"ms_cq{i}", [128, 512]) for i in range(4)]
            for i, tt in enumerate(tiles):
                pb = i % 2
                s_ = 1 if tt * 128 < NCTX else 0
                kb.idma(y1[pb][:], None, ys_d, bass.IndirectOffsetOnAxis(ap=D1i[:, i:i + 1], axis=0), reads=["ys", D1i], writes=[y1[pb]])
                kb.idma(y2[pb][:], None, ys_d, bass.IndirectOffsetOnAxis(ap=D2i[:, i:i + 1], axis=0), reads=["ys", D2i], writes=[y2[pb]])
                kb.ts("dve", y1[pb][:], y1[pb][:], GA[:, i:i + 1], ALU.mult)
                kb.stt("dve", y1[pb][:], y2[pb][:], GB[:, i:i + 1], y1[pb][:], ALU.mult, ALU.add)
                for half in range(2):
                    p = tq[(2 * i + half) % 4]
                    for j in range(4):
                        k = half * 4 + j
                        kb.tr(p[:, j * 128:(j + 1) * 128], y1[pb][:, k * 128:(k + 1) * 128], ident)
                    for j in range(4):
                        k = half * 4 + j
                        kb.stt("dve", xT[:, k, tt * 128:(tt + 1) * 128], p[:, j * 128:(j + 1) * 128], modT[:, L, 40 + k, s_:s_ + 1],
                               xT[:, k, tt * 128:(tt + 1) * 128], ALU.mult, ALU.add)
            kb.barrier()


def phase_final(G):
    nc, kb = G["nc"], G["kb"]
    xT, onesr, ident, Vn = G["xT"], G["onesr"], G["ident"], G["Vn"]
    out_d = G["out_d"]
    with ExitStack() as ph:
        psb = lambda name, shape, dt=F32: ph.enter_context(nc.sbuf_tensor(uq(name), shape, dt))
        pps = lambda name, shape, dt=F32: ph.enter_context(nc.psum_tensor(uq(name), shape, dt))
        fo = psb("fn_fo", [128, 8, NLAT])
        emit_norm(nc, kb, xT, fo, [(t0, n, t0 - NCTX) for (t0, n) in CHUNKS[1:]],
                  lambda k, s_: Vn("final_g")[:, k:k + 1], None, onesr, D)
        ost = [psb(f"fn_ost{i}", [128, D]) for i in range(2)]
        tp = [pps(f"fn_tp{i}", [128, 512]) for i in range(4)]
        for tt in range(NLAT // 128):
            o = ost[tt % 2]
            for half in range(2):
                p = tp[(2 * tt + half) % 4]
                for j in range(4):
                    k = half * 4 + j
                    kb.tr(p[:, j * 128:(j + 1) * 128], fo[:, k, tt * 128:(tt + 1) * 128], ident)
                if half == 0:
                    kb.cp("dve", o[:, 0:512], p[:])
                else:
                    kb.cp("act", o[:, 512:1024], p[:])
            kb.dma("sp", out_d[tt * 128:(tt + 1) * 128, :], o[:], reads=[o], writes=["out"])
        kb.barrier()


def host_prep(inputs):
    f = lambda a: np.ascontiguousarray(np.asarray(a, dtype=np.float32))
    w_in = f(inputs["w_in"])
    perm = np.concatenate([np.arange(8, 16), np.arange(0, 8), np.arange(24, 32), np.arange(16, 24)]) + 640
    w_in_x = np.concatenate([w_in, w_in[:, :, 576:672], w_in[:, :, 576:640], w_in[:, :, perm]], axis=2)
    consts = np.concatenate([np.eye(128, dtype=np.float32), np.ones((128, 128), np.float32)], axis=1)
    shared = {"consts": consts, "mod_w": f(inputs["mod_w"]), "w_in_x": np.ascontiguousarray(w_in_x)}
    s5B = np.zeros((DEPTH, 2, 2, 8, 128, 128), np.float32)
    s5C = np.zeros((DEPTH, 2, 2, 8, 128, 128), np.float32)
    for ri, (bn, cn) in enumerate((("s5_b_re", "s5_c_re"), ("s5_b_im", "s5_c_im"))):
        bb = f(inputs[bn])
        cc = f(inputs[cn])
        for g in range(16):
            s_ = g // 2
            ct = s_ // 4
            q0 = (g - 2 * s_) * 64
            c0 = (g - 8 * ct) * 16
            s5B[:, :, ri, s_, c0:c0 + 16, q0:q0 + 64] = bb[:, :, g].transpose(0, 1, 3, 2)
            s5C[:, :, ri, s_, q0:q0 + 64, c0:c0 + 16] = cc[:, :, g].transpose(0, 1, 3, 2)
    shared["s5B"] = s5B
    shared["s5C"] = s5C
    shared["s5_glu_w"] = f(inputs["s5_glu_w"])
    rows = NLAT // 64
    row = np.repeat(np.arange(rows, dtype=np.float32), 64)
    col = np.tile(np.arange(64, dtype=np.float32), rows)
    inv = (np.float32(10000.0) ** (-np.arange(8, dtype=np.float32) / np.float32(8))).astype(np.float32)
    rope = np.zeros((128, 2, NLAT), np.float32)
    for r in range(32):
        i, axis, half = r % 8, r // 16, (r // 8) % 2
        ang = ((row if axis == 0 else col) * inv[i]).astype(np.float32)
        rope[64 + r, 0] = np.cos(ang)
        rope[64 + r, 1] = np.sin(ang) * (-1.0 if half == 0 else 1.0)
    shared["rope"] = rope
    wuq = f(inputs["mla_w_uq"]).reshape(DEPTH, 256, 8, 96)
    pperm = np.concatenate([np.arange(64), 64 + np.concatenate([np.arange(8, 16), np.arange(0, 8), np.arange(24, 32), np.arange(16, 24)])])
    shared["wq"] = np.ascontiguousarray(np.stack([wuq, wuq[:, :, :, pperm]], axis=2).reshape(DEPTH, 256, 2, 768))
    shared["mla_w_uk"] = f(inputs["mla_w_uk"])
    shared["hg_lb_logits"] = f(inputs["hg_lb_logits"]).reshape(1, DEPTH)
    shared["wp"] = np.ascontiguousarray(np.concatenate([f(inputs["w_pa"]), f(inputs["w_pb"]), f(inputs["w_pc"])], axis=1))
    shared["w_out"] = f(inputs["w_out"])
    shared["wr"] = np.ascontiguousarray(np.concatenate([f(inputs["moe_w_group"]), f(inputs["moe_w_expert"])], axis=2))
    shared["br"] = np.ascontiguousarray(np.concatenate([f(inputs["moe_b_group"]), f(inputs["moe_b_expert"])], axis=1).reshape(DEPTH, 1, 36))
    for nm_, kt in (("moe_w1", 8), ("moe_w3", 8), ("moe_w2", 4)):
        w_ = f(inputs[nm_])
        cols = w_.shape[-1]
        shared[nm_] = np.ascontiguousarray(w_.reshape(DEPTH, 32, kt, 128, cols).transpose(0, 1, 3, 2, 4)).reshape(DEPTH, 32 * 128, kt * cols)
    shared["mla_w_uv"] = f(inputs["mla_w_uv"])
    in_maps = []
    for b in range(8):
        vecs = np.zeros((NVBLK * 128, 128), np.float32)

        def put(name, arr):
            r0, n = VEC_ROWS[name]
            vecs[r0:r0 + n, :] = np.asarray(arr, np.float32).reshape(n, 128)
        put("c", inputs["c"][b]); put("c_ctx", inputs["c_ctx"]); put("final_g", inputs["final_norm_g"])
        for l in range(DEPTH):
            put(f"norm1_g{l}", inputs["norm1_g"][l]); put(f"norm2_g{l}", inputs["norm2_g"][l])
            put(f"mod_b{l}", inputs["mod_b"][l]); put(f"s5_d{l}", inputs["s5_d"][l])
            put(f"glu_b{l}", inputs["s5_glu_b"][l]); put(f"qa_g{l}", inputs["mla_qa_g"][l])
            put(f"kva_g{l}", inputs["mla_kva_g"][l])
            hg = np.zeros((4, 128), np.float32)
            hg[:, 0:64] = np.asarray(inputs["hg_norm_g"][l], np.float32).reshape(4, 64)
            put(f"hgn_g{l}", hg)
            for d in range(2):
                put(f"lam_re{l}{d}", inputs["s5_lam_re"][l, d]); put(f"lam_im{l}{d}", inputs["s5_lam_im"][l, d])
                put(f"lstep{l}{d}", np.repeat(np.asarray(inputs["s5_log_step"][l, d], np.float32), 64))
        m = dict(shared)
        m["x"] = f(inputs["x"][b]); m["ctx"] = f(inputs["ctx"][b]); m["vecs"] = vecs
        in_maps.append(m)
    return in_maps


def kernel(**inputs):
    in_maps = host_prep(inputs)
    nc = build_nc()
    res = run_bass_kernel_spmd(nc, in_maps, core_ids=list(range(8)))
    return np.stack([r["out"] for r in res.results], axis=0)
```

```python
import math
from contextlib import ExitStack

import numpy as np
import concourse.bass as bass
import concourse.mybir as mybir
from concourse.bass_utils import run_bass_kernel_spmd

F32 = mybir.dt.float32
F32R = mybir.dt.float32r
I32 = mybir.dt.int32
AF = mybir.ActivationFunctionType
ALU = mybir.AluOpType
AX = mybir.AxisListType

D = 1024
NCTX = 256
NLAT = 2048
NT = NCTX + NLAT
DEPTH = 2
EPS = 1e-6
CHUNKS = [(0, 256)] + [(256 + 512 * i, 512) for i in range(4)]
SLOT = 256
NOV = 35
NROWS = 32 * SLOT + NOV * 128

COLT = []
def _ct(name, start, width):
    COLT.append((name, start, width))
for i in range(2): _ct(f"s5u{i}", 0 + 128 * i, 128)
for i in range(2): _ct(f"cq{i}", 256 + 128 * i, 128)
_ct("ckv", 512, 128)
_ct("kpeA", 5792, 96)
_ct("kpeB", 5792 + 96, 96)
for i in range(4): _ct(f"hq{i}", 672 + 128 * i, 128)
for i in range(4): _ct(f"hf{i}", 1184 + 128 * i, 128)
for i in range(4): _ct(f"hb{i}", 1696 + 128 * i, 128)
for i in range(4): _ct(f"hi{i}", 2208 + 64 * i, 64)
for i in range(4): _ct(f"hg{i}", 2464 + 64 * i, 64)
N_NONGATE = len(COLT)
for b in range(3):
    for i in range(8): _ct(f"gate{b}_{i}", 2720 + 1024 * b + 128 * i, 128)
COLIDX = {n: i for i, (n, _, _) in enumerate(COLT)}
WINX = 5792 + 192

VEC_ROWS = {}
def _vr(name, n):
    VEC_ROWS[name] = (sum(v[1] for v in VEC_ROWS.values()), n)
_vr("c", 8); _vr("c_ctx", 8); _vr("final_g", 8)
for l in range(DEPTH):
    _vr(f"norm1_g{l}", 8); _vr(f"norm2_g{l}", 8); _vr(f"mod_b{l}", 48)
    _vr(f"s5_d{l}", 2); _vr(f"glu_b{l}", 2); _vr(f"qa_g{l}", 2); _vr(f"kva_g{l}", 1)
    _vr(f"hgn_g{l}", 4)
    for d in range(2):
        _vr(f"lam_re{l}{d}", 8); _vr(f"lam_im{l}{d}", 8); _vr(f"lstep{l}{d}", 8)
NVROWS = sum(v[1] for v in VEC_ROWS.values())
NVBLK = (NVROWS + 127) // 128


_UQ = [0]


def uq(name):
    _UQ[0] += 1
    return f"{name}~{_UQ[0]}"


class Buf:
    __slots__ = ("name", "w", "r")

    def __init__(self, name):
        self.name = name
        self.w = None
        self.r = {}


class KB:
    RING = 12

    def __init__(self, nc, es):
        self.nc, self.es = nc, es
        self.eng = dict(pe=nc.tensor, dve=nc.vector, act=nc.scalar, pool=nc.gpsimd, sp=nc.sync)
        self.psem, self.pcnt, self.nsem = {}, {}, 0
        for e in self.eng:
            self._new_psem(e)
        self.waited = {}
        self.rings = {}
        self.rpos = {}
        for q in ("sp", "pool", "act"):
            self.rings[q] = [[self._sem(f"dq_{q}{i}"), 0] for i in range(self.RING)]
            self.rpos[q] = 0
        self.bufs = {}
        self.ninstr = 0

    def _sem(self, name):
        self.nsem += 1
        return self.es.enter_context(self.nc.semaphore(name))

    def _new_psem(self, e):
        self.psem[e] = self._sem(f"p_{e}_{self.nsem}")
        self.pcnt[e] = 0

    def buf(self, name):
        b = self.bufs.get(name)
        if b is None:
            b = self.bufs[name] = Buf(name)
        return b

    def _wait(self, e, tok):
        sem, val, src = tok
        key = (e, id(sem))
        if self.waited.get(key, 0) >= val:
            return
        self.eng[e].wait_ge(sem, val)
        self.waited[key] = val

    def _sync(self, e, reads, writes):
        for b in reads:
            if b.w is not None:
                self._dep(e, b.w)
        for b in writes:
            if b.w is not None:
                self._dep(e, b.w)
            for t in b.r.values():
                self._dep(e, t)

    def _dep(self, e, tok):
        if e == "pe" and tok[2] == "pe":
            return
        self._wait(e, tok)

    def _commit(self, tok, reads, writes):
        for b in writes:
            b.w = tok
            b.r = {}
        for b in reads:
            b.r[tok[2]] = tok

    def op(self, e, fn, reads=(), writes=()):
        reads = [b if isinstance(b, Buf) else self.buf(self._nm(b)) for b in reads]
        writes = [b if isinstance(b, Buf) else self.buf(self._nm(b)) for b in writes]
        self._sync(e, reads, writes)
        ins = fn(self.eng[e])
        if self.pcnt[e] >= 20000:
            self._new_psem(e)
        self.pcnt[e] += 1
        ins.then_inc(self.psem[e], 1)
        self.ninstr += 1
        self._commit((self.psem[e], self.pcnt[e], e), reads, writes)

    def dma(self, q, out, in_, reads=(), writes=()):
        reads = [b if isinstance(b, Buf) else self.buf(self._nm(b)) for b in reads]
        writes = [b if isinstance(b, Buf) else self.buf(self._nm(b)) for b in writes]
        self._sync(q, reads, writes)
        slot = self.rings[q][self.rpos[q] % self.RING]
        self.rpos[q] += 1
        sem, cnt = slot
        if cnt > 0:
            self._wait(q, (sem, cnt, "dma"))
        self.eng[q].dma_start(out=out, in_=in_).then_inc(sem, 16)
        slot[1] = cnt + 16
        self.ninstr += 1
        self._commit((sem, cnt + 16, f"dma_{q}{(self.rpos[q] - 1) % self.RING}"), reads, writes)

    @staticmethod
    def _nm(x):
        if isinstance(x, str):
            return x
        if isinstance(x, tuple):
            return KB._nm(x[0]) + x[1]
        if hasattr(x, "tensor"):
            return x.tensor.name.split("~")[0]
        return x.name.split("~")[0]

    def _names(self, xs):
        return [self._nm(x) for x in xs if not isinstance(x, (int, float)) and x is not None]

    def tt(self, e, out, a, b, op, rn=(), wn=()):
        self.op(e, lambda g: g.tensor_tensor(out, a, b, op), reads=self._names([a, b]) + list(rn), writes=self._names([out]) + list(wn))

    def ts(self, e, out, a, s1, op0, s2=None, op1=None, rn=(), wn=()):
        if op1 is None:
            self.op(e, lambda g: g.tensor_scalar(out, a, s1, None, op0), reads=self._names([a, s1]) + list(rn), writes=self._names([out]) + list(wn))
        else:
            self.op(e, lambda g: g.tensor_scalar(out, a, s1, s2, op0, op1), reads=self._names([a, s1, s2]) + list(rn), writes=self._names([out]) + list(wn))

    def stt(self, e, out, a, sc_, b, op0, op1, rn=(), wn=()):
        self.op(e, lambda g: g.scalar_tensor_tensor(out, a, sc_, b, op0, op1), reads=self._names([a, sc_, b]) + list(rn), writes=self._names([out]) + list(wn))

    def act(self, out, in_, func, bias=None, scale=1.0, rn=(), wn=()):
        kw = {}
        if bias is not None:
            kw["bias"] = bias
        self.op("act", lambda g: g.activation(out, in_, func, scale=scale, **kw), reads=self._names([in_, bias, scale]) + list(rn), writes=self._names([out]) + list(wn))

    def cp(self, e, out, in_, rn=(), wn=()):
        if e == "act":
            self.op(e, lambda g: g.copy(out, in_), reads=self._names([in_]) + list(rn), writes=self._names([out]) + list(wn))
        else:
            self.op(e, lambda g: g.tensor_copy(out, in_), reads=self._names([in_]) + list(rn), writes=self._names([out]) + list(wn))

    def mm(self, out, lhsT, rhs, start=True, stop=True, rn=(), wn=()):
        self.op("pe", lambda g: g.matmul(out, lhsT, rhs, start=start, stop=stop), reads=self._names([lhsT, rhs]) + list(rn), writes=self._names([out]) + list(wn))

    def tr(self, out, in_, ident, rn=(), wn=()):
        self.op("pe", lambda g: g.transpose(out, in_, ident), reads=self._names([in_, ident]) + list(rn), writes=self._names([out]) + list(wn))

    def scan(self, out, d0, d1, init, op0, op1, rn=(), wn=()):
        self.op("dve", lambda g: g.tensor_tensor_scan(out, d0, d1, init, op0, op1), reads=self._names([d0, d1, init]) + list(rn), writes=self._names([out]) + list(wn))

    def recip(self, out, in_):
        self.op("dve", lambda g: g.reciprocal(out, in_), reads=self._names([in_]), writes=self._names([out]))

    def memset(self, e, out, val):
        self.op(e, lambda g: g.memset(out, val), reads=[], writes=self._names([out]))

    def barrier(self):
        toks = [(self.psem[o], self.pcnt[o], o) for o in self.eng if self.pcnt[o] > 0]
        for q in self.rings:
            for sem, cnt in self.rings[q]:
                if cnt > 0:
                    toks.append((sem, cnt, "dma"))
        for e in self.eng:
            for t in toks:
                if t[2] != e:
                    self._wait(e, t)

    def idma(self, out, out_off, in_, in_off, reads=(), writes=(), bounds=None):
        q = "pool"
        reads = [b if isinstance(b, Buf) else self.buf(self._nm(b)) for b in reads]
        writes = [b if isinstance(b, Buf) else self.buf(self._nm(b)) for b in writes]
        self._sync(q, reads, writes)
        slot = self.rings[q][self.rpos[q] % self.RING]
        self.rpos[q] += 1
        sem, cnt = slot
        if cnt > 0:
            self._wait(q, (sem, cnt, "dma"))
        if bounds is None:
            self.eng[q].indirect_dma_start(out=out, out_offset=out_off, in_=in_, in_offset=in_off).then_inc(sem, 16)
        else:
            if not hasattr(self, "_breg") or self._breg[0] != bounds:
                self._breg = (bounds, self.eng[q].to_reg(bounds))
            self.eng[q].indirect_dma_start(out=out, out_offset=out_off, in_=in_, in_offset=in_off,
                                           bounds_check=self._breg[1], oob_is_err=False).then_inc(sem, 16)
        slot[1] = cnt + 16
        self.ninstr += 1
        self._commit((sem, cnt + 16, f"dma_{q}{(self.rpos[q] - 1) % self.RING}"), reads, writes)

    def finish(self, bufs):
        for b in bufs:
            b = self.buf(b) if isinstance(b, str) else b
            if b.w is not None:
                self._wait("sp", b.w)


def build_nc(stop_after=None, debug=False):
    nc = bass.Bass("TRN2", target_bir_lowering=False)
    okind = "ExternalOutput" if debug else "Internal"
    x_d = nc.dram_tensor("x", [NLAT, D], F32, kind="ExternalInput").ap()
    ctx_d = nc.dram_tensor("ctx", [NCTX, D], F32, kind="ExternalInput").ap()
    vecs_d = nc.dram_tensor("vecs", [NVBLK * 128, 128], F32, kind="ExternalInput").ap()
    consts_d = nc.dram_tensor("consts", [128, 256], F32, kind="ExternalInput").ap()
    modw_d = nc.dram_tensor("mod_w", [DEPTH, D, 6 * D], F32, kind="ExternalInput").ap()
    winx_d = nc.dram_tensor("w_in_x", [DEPTH, D, WINX], F32, kind="ExternalInput").ap()
    out_d = nc.dram_tensor("out", [NLAT, D], F32, kind="ExternalOutput").ap()
    zT_d = nc.dram_tensor("zT", [len(COLT) * 128, NT], F32, kind=okind).ap()
    ybT_d = nc.dram_tensor("ybT", [D, NT], F32, kind=okind).ap()
    s5B_d = nc.dram_tensor("s5B", [DEPTH, 2, 2, 8, 128, 128], F32, kind="ExternalInput").ap()
    s5C_d = nc.dram_tensor("s5C", [DEPTH, 2, 2, 8, 128, 128], F32, kind="ExternalInput").ap()
    gluw_d = nc.dram_tensor("s5_glu_w", [DEPTH, 256, 256], F32, kind="ExternalInput").ap()
    rope_d = nc.dram_tensor("rope", [128, 2, NLAT], F32, kind="ExternalInput").ap()
    wq_d = nc.dram_tensor("wq", [DEPTH, 256, 2, 768], F32, kind="ExternalInput").ap()
    wuk_d = nc.dram_tensor("mla_w_uk", [DEPTH, 128, 512], F32, kind="ExternalInput").ap()
    wuv_d = nc.dram_tensor("mla_w_uv", [DEPTH, 128, 512], F32, kind="ExternalInput").ap()
    lbl_d = nc.dram_tensor("hg_lb_logits", [1, DEPTH], F32, kind="ExternalInput").ap()
    wp_d = nc.dram_tensor("wp", [DEPTH, D, D], F32, kind="ExternalInput").ap()
    wo_d = nc.dram_tensor("w_out", [DEPTH, D, D], F32, kind="ExternalInput").ap()
    wr_d = nc.dram_tensor("wr", [DEPTH, D, 36], F32, kind="ExternalInput").ap()
    br_d = nc.dram_tensor("br", [DEPTH, 1, 36], F32, kind="ExternalInput").ap()
    w1_d = nc.dram_tensor("moe_w1", [DEPTH, 32 * 128, 8 * 512], F32, kind="ExternalInput").ap()
    w3_d = nc.dram_tensor("moe_w3", [DEPTH, 32 * 128, 8 * 512], F32, kind="ExternalInput").ap()
    w2_d = nc.dram_tensor("moe_w2", [DEPTH, 32 * 128, 4 * D], F32, kind="ExternalInput").ap()
    h2tok_d = nc.dram_tensor("h2tok", [NT, D], F32, kind=okind).ap()
    xs_d = nc.dram_tensor("xs", [NROWS, D], F32, kind=okind).ap()
    ys_d = nc.dram_tensor("ys", [NROWS, D], F32, kind=okind).ap()
    xdbg_d = nc.dram_tensor("xdbg", [128, 8 * NT], F32, kind=okind).ap()
    modT_d = nc.dram_tensor("modT", [DEPTH, 128, 96], F32, kind=okind).ap()

    with ExitStack() as es:
        kb = KB(nc, es)
        sb = lambda name, shape, dt=F32: es.enter_context(nc.sbuf_tensor(uq(name), shape, dt))

        xT = sb("xT", [128, 8, NT])
        cst = sb("cst", [128, 256])
        ident = cst[:, 0:128]
        onesr = sb("onesr", [128, 128], F32R)
        vecT = sb("vecT", [128, NVBLK * 128])
        modT = sb("modT_sb", [128, DEPTH, 48, 2])
        modA = sb("modA", [128, DEPTH, 2, 8, 2])
        sc = sb("sc", [128, 8, 2], F32R)

        def V(name, j=0):
            r0, n = VEC_ROWS[name]
            return vecT[:, r0 + j:r0 + j + 1]

        def Vn(name):
            r0, n = VEC_ROWS[name]
            return vecT[:, r0:r0 + n]

        kb.dma("sp", cst[:], consts_d, writes=["cst"])
        kb.dma("pool", onesr[:], consts_d[:, 128:256], writes=["onesr"])

        with ExitStack() as ph:
            psb = lambda name, shape, dt=F32: ph.enter_context(nc.sbuf_tensor(uq(name), shape, dt))
            pps = lambda name, shape, dt=F32: ph.enter_context(nc.psum_tensor(uq(name), shape, dt))
            stg = [psb(f"xstg{i}", [128, D]) for i in range(3)]
            tps = [pps(f"tps{i}", [128, 4, 128]) for i in range(4)]
            for blk in range(NVBLK):
                s = stg[blk % 3]
                kb.dma("sp", s[:, 0:128], vecs_d[blk * 128:(blk + 1) * 128, :], writes=[f"xstg{blk % 3}"])
                kb.op("pe", lambda e: e.transpose(tps[blk % 4][:, 0, :], s[:, 0:128], ident),
                      reads=[f"xstg{blk % 3}", "cst"], writes=[f"tps{blk % 4}"])
                kb.op("dve", lambda e: e.tensor_copy(vecT[:, blk * 128:(blk + 1) * 128], tps[blk % 4][:, 0, :]),
                      reads=[f"tps{blk % 4}"], writes=["vecT"])
            n_tt = NT // 128
            for tt in range(n_tt):
                s = stg[tt % 3]
                src = ctx_d[tt * 128:(tt + 1) * 128, :] if tt < 2 else x_d[(tt - 2) * 128:(tt - 1) * 128, :]
                kb.dma("sp", s[:], src, writes=[f"xstg{tt % 3}"])
                for half in range(2):
                    pi = (2 * tt + half) % 4
                    for j in range(4):
                        k = half * 4 + j
                        kb.op("pe", lambda e: e.transpose(tps[pi][:, j, :], s[:, k * 128:(k + 1) * 128], ident),
                              reads=[f"xstg{tt % 3}", "cst"], writes=[f"tps{pi}"])
                    eng = "dve" if half == 0 else "act"
                    dst = xT[:, half * 4:half * 4 + 4, tt * 128:(tt + 1) * 128]
                    if eng == "dve":
                        kb.op("dve", lambda e: e.tensor_copy(dst, tps[pi][:]), reads=[f"tps{pi}"], writes=["xT"])
                    else:
                        kb.op("act", lambda e: e.copy(dst, tps[pi][:]), reads=[f"tps{pi}"], writes=["xT"])
            kb.barrier()

        kb.op("act", lambda e: e.activation(sc[:, :, 0], Vn("c"), AF.Silu), reads=["vecT"], writes=["sc"])
        kb.op("act", lambda e: e.activation(sc[:, :, 1], Vn("c_ctx"), AF.Silu), reads=["vecT"], writes=["sc"])

        xTd_d = nc.dram_tensor("xTd", [128, 8 * NT], F32, kind=okind).ap()
        if debug and stop_after == "p0":
            kb.dma("sp", xTd_d, xT[:].rearrange("p a b -> p (a b)"), reads=["xT"], writes=["xTd"])
        lbv = sb("lbv", [128, 2, DEPTH])
        lbl = sb("lbl", [128, DEPTH])
        kb.dma("sp", lbl[:], lbl_d.to_broadcast([128, DEPTH]), writes=["lbl"])
        kb.memset("dve", lbv[:, 0, :], 0.0)
        kb.tt("dve", lbv[:, 0, 1:2], lbl[:, 1:2], lbl[:, 0:1], ALU.subtract)
        kb.act(lbv[:, 0, 1:2], lbv[:, 0, 1:2], AF.Sigmoid)
        kb.ts("dve", lbv[:, 1, :], lbv[:, 0, :], -1.0, ALU.mult, 1.0, ALU.add)
        for layer in range(DEPTH if stop_after != "p0" else 0):
            L = layer
            with ExitStack() as ph:
                psb = lambda name, shape, dt=F32: ph.enter_context(nc.sbuf_tensor(uq(name), shape, dt))
                pps = lambda name, shape, dt=F32: ph.enter_context(nc.psum_tensor(uq(name), shape, dt))
                mw = [psb(f"mw{i}", [128, 8, 128], F32R) for i in range(3)]
                mps = pps("mps", [128, 48, 2])
                for j in range(48):
                    w = mw[j % 3]
                    kb.dma("pool", w[:], modw_d[L, :, j * 128:(j + 1) * 128].rearrange("(k p) c -> p k c", p=128),
                           writes=[f"mw{j % 3}"])
                    for k in range(8):
                        kb.op("pe", lambda e: e.matmul(mps[:, j, :], w[:, k, :], sc[:, k, :], start=(k == 0), stop=(k == 7)),
                              reads=[f"mw{j % 3}", "sc"], writes=["mps"])
                for s in range(2):
                    kb.op("dve", lambda e: e.tensor_tensor(modT[:, L, :, s], mps[:, :, s], Vn(f"mod_b{L}"), ALU.add),
                          reads=["mps", "vecT"], writes=["modT"])
                for ni, (gname, t0) in enumerate(((f"norm1_g{L}", 8), (f"norm2_g{L}", 32))):
                    for s in range(2):
                        kb.op("dve", lambda e: e.scalar_tensor_tensor(
                            modA[:, L, ni, :, s], modT[:, L, t0:t0 + 8, s], 1.0, Vn(gname), ALU.add, ALU.mult),
                            reads=["modT", "vecT"], writes=["modA"])
                if debug:
                    kb.dma("sp", modT_d[L], modT[:, L].rearrange("p a b -> p (a b)"), reads=["modT"], writes=["modT_d"])
                kb.barrier()
            if stop_after == f"mod{L}":
                break

            with ExitStack() as ph:
                psb = lambda name, shape, dt=F32: ph.enter_context(nc.sbuf_tensor(uq(name), shape, dt))
                pps = lambda name, shape, dt=F32: ph.enter_context(nc.psum_tensor(uq(name), shape, dt))
                hT = psb("hT", [128, 8, NT], F32R)
                emit_norm(nc, kb, xT, hT, [(t0, n, t0) for (t0, n) in CHUNKS],
                          lambda k, s_: modA[:, L, 0, k, s_:s_ + 1], lambda k, s_: modT[:, L, k, s_:s_ + 1], onesr, D)
                wb = [psb(f"wb{i}", [128, 8, 128], F32R) for i in range(3)]
                zs = [psb(f"zs{i}", [128, 512]) for i in range(4)]
                zp = [pps(f"zp{i}", [128, 512]) for i in range(4)]
                cnt = 0
                for ti, (name, c0, wd) in enumerate(COLT):
                    w = wb[ti % 3]
                    kb.dma("pool", w[:, :, 0:wd], winx_d[L, :, c0:c0 + wd].rearrange("(k p) c -> p k c", p=128),
                           writes=[f"wb{ti % 3}"])
                    for (t0, n) in CHUNKS:
                        pi = cnt % 4
                        cnt += 1
                        for k in range(8):
                            kb.op("pe", lambda e: e.matmul(zp[pi][0:wd, 0:n], w[:, k, 0:wd], hT[:, k, t0:t0 + n],
                                                           start=(k == 0), stop=(k == 7)),
                                  reads=[f"wb{ti % 3}", "hT"], writes=[f"zp{pi}"])
                        if cnt % 2 == 0:
                            kb.op("dve", lambda e: e.tensor_copy(zs[pi][0:wd, 0:n], zp[pi][0:wd, 0:n]),
                                  reads=[f"zp{pi}"], writes=[f"zs{pi}"])
                        else:
                            kb.op("act", lambda e: e.copy(zs[pi][0:wd, 0:n], zp[pi][0:wd, 0:n]),
                                  reads=[f"zp{pi}"], writes=[f"zs{pi}"])
                        kb.dma("sp", zT_d[ti * 128:ti * 128 + wd, t0:t0 + n], zs[pi][0:wd, 0:n],
                               reads=[f"zs{pi}"], writes=[f"zT_{ti}"])
                kb.barrier()
            if stop_after == f"A{L}":
                break
            G = dict(nc=nc, kb=kb, L=L, xT=xT, cst=cst, ident=ident, onesr=onesr, vecT=vecT, modT=modT, modA=modA,
                     V=V, Vn=Vn, zT_d=zT_d, ybT_d=ybT_d, s5B_d=s5B_d, s5C_d=s5C_d, gluw_d=gluw_d, debug=debug,
                     rope_d=rope_d, wq_d=wq_d, wuk_d=wuk_d, wuv_d=wuv_d)
            if not (debug and stop_after in (f"C{L}", f"D{L}")):
                phase_s5(G)
            if stop_after == f"B{L}":
                break
            G["lbv"] = lbv
            if not (debug and stop_after in (f"D{L}",)):
                phase_mla(G)
            if stop_after == f"C{L}":
                break
            G.update(wp_d=wp_d, wo_d=wo_d, wr_d=wr_d, br_d=br_d, w1_d=w1_d, w3_d=w3_d, w2_d=w2_d, out_d=out_d,
                     h2tok_d=h2tok_d, xs_d=xs_d, ys_d=ys_d)
            phase_hg(G)
            if stop_after == f"D{L}":
                break
            phase_merge(G)
            if stop_after == f"E{L}":
                kb.dma("sp", xdbg_d, xT[:].rearrange("p a b -> p (a b)"), reads=["xT"], writes=["xdbg"])
                break
            phase_moe_sparse(G)
            if stop_after == f"F{L}":
                kb.dma("sp", xdbg_d, xT[:].rearrange("p a b -> p (a b)"), reads=["xT"], writes=["xdbg"])
                break
        else:
            phase_final(G)

        kb.finish(list(kb.bufs.values()))
        print("instructions:", kb.ninstr, "sems:", kb.nsem, "sbuf left:", nc.sbuf_bytes_remaining)
    return nc


def emit_norm(nc, kb, xT, dst, chunks, A, Sh, onesr, dmodel):
    with ExitStack() as ns:
        psb = lambda name, shape, dt=F32: ns.enter_context(nc.sbuf_tensor(uq(name), shape, dt))
        pps = lambda name, shape, dt=F32: ns.enter_context(nc.psum_tensor(uq(name), shape, dt))
        sq = [psb(f"nsq{i}", [128, 8, 512], F32R) for i in range(2)]
        ms = [pps(f"nms{i}", [128, 512]) for i in range(2)]
        rs = [psb(f"nrs{i}", [128, 512]) for i in range(2)]
        tmp = [psb(f"ntmp{i}", [128, 512]) for i in range(2)]
        for ci, (t0, n, d0) in enumerate(chunks):
            s = 1 if t0 < NCTX else 0
            b = ci % 2
            for k in range(8):
                kb.act(sq[b][:, k, 0:n], xT[:, k, t0:t0 + n], AF.Square)
            for k in range(8):
                kb.mm(ms[b][:, 0:n], onesr[:], sq[b][:, k, 0:n], start=(k == 0), stop=(k == 7))
            kb.act(rs[b][:, 0:n], ms[b][:, 0:n], AF.Sqrt, scale=1.0 / dmodel, bias=EPS)
            kb.recip(rs[b][:, 0:n], rs[b][:, 0:n])
            for k in range(8):
                tb = k % 2
                sh = Sh(k, s) if Sh is not None else None
                if sh is None:
                    kb.stt("dve", dst[:, k, d0:d0 + n], xT[:, k, t0:t0 + n], A(k, s), rs[b][:, 0:n], ALU.mult, ALU.mult)
                else:
                    kb.stt("dve", tmp[tb][:, 0:n], xT[:, k, t0:t0 + n], A(k, s), rs[b][:, 0:n], ALU.mult, ALU.mult)
                    kb.act(dst[:, k, d0:d0 + n], tmp[tb][:, 0:n], AF.Identity, bias=sh, scale=1.0)
        kb.barrier()


TWO_PI = 2.0 * math.pi


def range_reduce(kb, r, x, tM, tI):
    kb.ts("dve", tM, x, 1.0 / TWO_PI, ALU.mult)
    kb.cp("dve", tI, tM)
    kb.cp("dve", tM, tI)
    kb.stt("dve", r, tM, -TWO_PI, x, ALU.mult, ALU.add)
    kb.ts("dve", tM, r, math.pi, ALU.is_gt)
    kb.stt("dve", r, tM, -TWO_PI, r, ALU.mult, ALU.add)
    kb.ts("dve", tM, r, -math.pi, ALU.is_lt)
    kb.stt("dve", r, tM, TWO_PI, r, ALU.mult, ALU.add)
    kb.ts("dve", r, r, 3.1415925, ALU.min, -3.1415925, ALU.max)


def sincos(kb, sn, cs, x, r, tM, tI):
    range_reduce(kb, r, x, tM, tI)
    kb.act(sn, r, AF.Sin)
    kb.ts("dve", tM, x, math.pi / 2, ALU.add)
    range_reduce(kb, r, tM, tM, tI) if False else None
    return


def phase_s5(G):
    nc, kb, L = G["nc"], G["kb"], G["L"]
    Vn = G["Vn"]
    zT_d, ybT_d = G["zT_d"], G["ybT_d"]
    T = 256
    NCH = NT // T
    with ExitStack() as ph:
        psb = lambda name, shape, dt=F32: ph.enter_context(nc.sbuf_tensor(uq(name), shape, dt))
        pps = lambda name, shape, dt=F32: ph.enter_context(nc.psum_tensor(uq(name), shape, dt))
        uT = psb("s5_uT", [128, 2, NT], F32R)
        yacc = psb("s5_yacc", [128, 2, NT])
        for t in range(2):
            ti = COLIDX[f"s5u{t}"]
            kb.dma("pool", uT[:, t, :], zT_d[ti * 128:(ti + 1) * 128, :], reads=[f"zT_{ti}"], writes=["s5_uT"])
        Bw = psb("s5_Bw", [128, 2, 8, 128], F32R)
        Cw = psb("s5_Cw", [128, 2, 8, 128], F32R)
        iota_i = psb("s5_iota_i", [128, T + 1], I32)
        iota_f = psb("s5_iota_f", [128, T + 1])
        kb.op("pool", lambda g: g.iota(iota_i[:], [[1, T + 1]], base=0, channel_multiplier=0), writes=["s5_iota_i"])
        kb.cp("dve", iota_f[:], iota_i[:])
        COS = psb("s5_COS", [128, 8, T + 1])
        SIN = psb("s5_SIN", [128, 8, T + 1])
        ang = psb("s5_ang", [128, 8, T + 1])
        rr = psb("s5_rr", [128, 8, T + 1])
        ERE = ang[:, :, 0:T]
        EIM = rr[:, :, 0:T]
        tM = psb("s5_tM", [128, 8, T + 1])
        tI = psb("s5_tI", [128, 8, T + 1], I32)
        sm = psb("s5_sm", [128, 24, 8])
        gin = psb("s5_gin", [128, 8, 2])
        tmp = [[psb(f"s5_t{b}_{i}", [128, T]) for i in range(6)] for b in range(3)]
        tmpB = [[psb(f"s5_g{b}_{i}", [128, T]) for i in range(2)] for b in range(2)]
        hh = [[psb(f"s5_h{b}_{i}", [128, T], F32R) for i in range(2)] for b in range(2)]
        Pp = [pps(f"s5_P{i}", [128, 2, T]) for i in range(3)]
        Yp = [[pps(f"s5_Y{b}_{ct}", [128, 512]) for ct in range(2)] for b in range(2)]
        flat = lambda t: t[:].rearrange("p a b -> p (a b)")
        for d in range(2):
            kb.dma("pool", Bw[:].rearrange("c r s q -> c (r s) q"),
                   G["s5B_d"][L, d].rearrange("r s c q -> c (r s) q"), writes=["s5_Bw"])
            kb.dma("pool", Cw[:].rearrange("c r s q -> c (r s) q"),
                   G["s5C_d"][L, d].rearrange("r s c q -> c (r s) q"), writes=["s5_Cw"])
            kb.ts("pool", Cw[:, 1], Cw[:, 1].bitcast(F32), -1.0, ALU.mult)
            lre, lim, lst = Vn(f"lam_re{L}{d}"), Vn(f"lam_im{L}{d}"), Vn(f"lstep{L}{d}")
            c_ = lambda i: sm[:, i, :]
            DT, MAG, TH, SN, CS, LBR, LBI, DEN, FR, FI, X1, X2, X3, MI = (c_(i) for i in range(14))
            kb.act(DT, lst, AF.Exp)
            kb.tt("dve", X1, lre, DT, ALU.mult)
            kb.act(MAG, X1, AF.Exp)
            kb.tt("dve", TH, lim, DT, ALU.mult)
            smI = tI[:, 0, 0:8]
            range_reduce(kb, X2, TH, X3, smI)
            kb.act(SN, X2, AF.Sin)
            kb.ts("dve", X1, TH, math.pi / 2, ALU.add)
            range_reduce(kb, X2, X1, X3, smI)
            kb.act(CS, X2, AF.Sin)
            kb.tt("dve", LBR, MAG, CS, ALU.mult)
            kb.tt("dve", LBI, MAG, SN, ALU.mult)
            kb.tt("dve", X1, lre, lre, ALU.mult)
            kb.tt("dve", X2, lim, lim, ALU.mult)
            kb.tt("dve", DEN, X1, X2, ALU.add)
            kb.recip(DEN, DEN)
            kb.ts("dve", X3, LBR, -1.0, ALU.add)
            kb.tt("dve", X1, X3, lre, ALU.mult)
            kb.tt("dve", X2, LBI, lim, ALU.mult)
            kb.tt("dve", X1, X1, X2, ALU.add)
            kb.tt("dve", FR, X1, DEN, ALU.mult)
            kb.tt("dve", X1, LBI, lre, ALU.mult)
            kb.tt("dve", X2, X3, lim, ALU.mult)
            kb.tt("dve", X1, X1, X2, ALU.subtract)
            kb.tt("dve", FI, X1, DEN, ALU.mult)
            kb.tt("dve", ang[:], TH.unsqueeze(2).to_broadcast([128, 8, T + 1]),
                  iota_f[:].unsqueeze(1).to_broadcast([128, 8, T + 1]), ALU.mult)
            range_reduce(kb, flat(rr), flat(ang), flat(tM), flat(tI))
            kb.act(flat(SIN), flat(rr), AF.Sin)
            kb.ts("dve", flat(ang), flat(ang), math.pi / 2, ALU.add)
            range_reduce(kb, flat(rr), flat(ang), flat(tM), flat(tI))
            kb.act(flat(COS), flat(rr), AF.Sin)
            frb = FR.unsqueeze(2).to_broadcast([128, 8, T])
            fib = FI.unsqueeze(2).to_broadcast([128, 8, T])
            tF = tI[:].bitcast(F32)
            kb.tt("dve", tM[:, :, 0:T], COS[:, :, 0:T], frb, ALU.mult)
            kb.tt("dve", tF[:, :, 0:T], SIN[:, :, 0:T], fib, ALU.mult)
            kb.tt("dve", ERE, tM[:, :, 0:T], tF[:, :, 0:T], ALU.add)
            kb.tt("dve", tM[:, :, 0:T], COS[:, :, 0:T], fib, ALU.mult)
            kb.tt("dve", tF[:, :, 0:T], SIN[:, :, 0:T], frb, ALU.mult)
            kb.tt("dve", EIM, tM[:, :, 0:T], tF[:, :, 0:T], ALU.subtract)
            kb.memset("dve", gin[:], 0.0)
            NST = sm[:, 18, :]
            kb.ts("dve", NST, SIN[:, :, T], -1.0, ALU.mult)
            order = list(range(NCH)) if d == 0 else [0] + list(range(NCH - 1, 0, -1))
            units = [(oi, ci, s_) for oi, ci in enumerate(order) for s_ in range(8)]

            def stageA(u):
                oi, ci, s_ = units[u]
                t0 = ci * T
                ct = s_ // 4
                tq = tmp[u % 3]
                P = Pp[u % 3]
                kb.mm(P[:, 0, :], Bw[:, 0, s_, :], uT[:, ct, t0:t0 + T])
                kb.mm(P[:, 1, :], Bw[:, 1, s_, :], uT[:, ct, t0:t0 + T])
                Pre = P[:, 0, ::-1] if d == 1 else P[:, 0, :]
                Pim = P[:, 1, ::-1] if d == 1 else P[:, 1, :]
                kb.tt("dve", tq[0][:], ERE[:, s_, :], Pre, ALU.mult)
                kb.tt("dve", tq[1][:], EIM[:, s_, :], Pim, ALU.mult)
                kb.tt("pool", tq[4][:], tq[0][:], tq[1][:], ALU.subtract)
                kb.tt("dve", tq[2][:], ERE[:, s_, :], Pim, ALU.mult)
                kb.tt("dve", tq[3][:], EIM[:, s_, :], Pre, ALU.mult)
                kb.tt("pool", tq[5][:], tq[2][:], tq[3][:], ALU.add)

            def stageB(u):
                oi, ci, s_ = units[u]
                t0 = ci * T
                ct = s_ // 4
                yb_ = oi % 2
                tq = tmp[u % 3]
                gq = tmpB[u % 2]
                hb = hh[u % 2]
                rb = MAG[:, s_:s_ + 1].to_broadcast([128, T])
                kb.scan(gq[0][:], rb, tq[4][:], gin[:, s_, 0:1], ALU.mult, ALU.add)
                kb.scan(gq[1][:], rb, tq[5][:], gin[:, s_, 1:2], ALU.mult, ALU.add)
                cT, sT = COS[:, s_, T:T + 1], SIN[:, s_, T:T + 1]
                lr, li = gq[0][:, T - 1:T], gq[1][:, T - 1:T]
                xa, xb = sm[:, 14 + (u % 2) * 2, 0:1], sm[:, 15 + (u % 2) * 2, 0:1]
                kb.act(xa, li, AF.Identity, scale=NST[:, s_:s_ + 1])
                kb.act(xb, li, AF.Identity, scale=cT)
                kb.act(gin[:, s_, 0:1], lr, AF.Identity, bias=xa, scale=cT)
                kb.act(gin[:, s_, 1:2], lr, AF.Identity, bias=xb, scale=sT)
                kb.tt("pool", tq[0][:], COS[:, s_, 0:T], gq[0][:], ALU.mult)
                kb.tt("pool", tq[1][:], SIN[:, s_, 0:T], gq[1][:], ALU.mult)
                kb.tt("pool", hb[0][:], tq[0][:], tq[1][:], ALU.subtract)
                kb.tt("dve", tq[2][:], SIN[:, s_, 0:T], gq[0][:], ALU.mult)
                kb.tt("dve", tq[3][:], COS[:, s_, 0:T], gq[1][:], ALU.mult)
                kb.tt("pool", hb[1][:], tq[2][:], tq[3][:], ALU.add)
                Y = Yp[yb_][ct]
                kb.mm(Y[:, 0:T], Cw[:, 0, s_, :], hb[0][:], start=(s_ % 4 == 0), stop=False)
                kb.mm(Y[:, 0:T], Cw[:, 1, s_, :], hb[1][:], start=False, stop=(s_ % 4 == 3))
                if s_ == 7:
                    for ct2 in range(2):
                        Y2 = Yp[yb_][ct2]
                        if d == 0:
                            kb.cp("act", yacc[:, ct2, t0:t0 + T], Y2[:, 0:T])
                        else:
                            rv = slice(t0 + T - 1, (t0 - 1 if t0 > 0 else None), -1)
                            kb.tt("dve", yacc[:, ct2, rv], yacc[:, ct2, rv], Y2[:, 0:T], ALU.add)

            stageA(0)
            stageA(1)
            for u in range(len(units)):
                if u + 2 < len(units):
                    stageA(u + 2)
                stageB(u)
        gw = psb("s5_gw", [128, 2, 256], F32R)
        kb.dma("pool", gw[:], G["gluw_d"][L].rearrange("(k p) c -> p k c", p=128), writes=["s5_gw"])
        y1t = [hh[0][0], hh[0][1]]
        for ci in range(NCH):
            sl = slice(ci * T, (ci + 1) * T)
            for ct in range(2):
                a, b2, c2 = tmp[ct][0], tmp[ct][1], tmp[ct][2]
                kb.stt("dve", a[:], uT[:, ct, sl].bitcast(F32), Vn(f"s5_d{L}")[:, ct:ct + 1], yacc[:, ct, sl], ALU.mult, ALU.add)
                kb.tt("dve", b2[:], a[:], a[:], ALU.mult)
                kb.ts("dve", b2[:], b2[:], 0.044715, ALU.mult, 1.0, ALU.add)
                kb.tt("dve", b2[:], b2[:], a[:], ALU.mult)
                kb.act(c2[:], b2[:], AF.Sigmoid, scale=1.5957691216057308)
                kb.tt("dve", y1t[ct][:], a[:], c2[:], ALU.mult)
            for ct in range(2):
                Y = Yp[ci % 2][ct]
                for k in range(2):
                    kb.mm(Y[:, 0:T], gw[:, k, ct * 128:(ct + 1) * 128], y1t[k][:], start=(k == 0), stop=(k == 1))
                sg = tmp[ct][3]
                o = tmp[ct][4]
                kb.act(sg[:], Y[:, 0:T], AF.Sigmoid, bias=Vn(f"glu_b{L}")[:, ct:ct + 1])
                kb.tt("dve", o[:], y1t[ct][:].bitcast(F32), sg[:], ALU.mult)
                kb.dma("sp", ybT_d[ct * 128:(ct + 1) * 128, sl], o[:], reads=[o], writes=[f"ybT_{ct}"])
        kb.barrier()


MLA_SCALE = 1.0 / math.sqrt(96.0)


def phase_mla(G):
    nc, kb, L = G["nc"], G["kb"], G["L"]
    Vn, cst, onesr = G["Vn"], G["cst"], G["onesr"]
    zT_d, ybT_d = G["zT_d"], G["ybT_d"]
    need_ctx = L < DEPTH - 1
    with ExitStack() as ph:
        psb = lambda name, shape, dt=F32: ph.enter_context(nc.sbuf_tensor(uq(name), shape, dt))
        pps = lambda name, shape, dt=F32: ph.enter_context(nc.psum_tensor(uq(name), shape, dt))
        cqn = psb("ml_cqn", [128, 2, NT], F32R)
        ckvn = psb("ml_ckvn", [128, NT], F32R)
        KPE = psb("ml_KPE", [128, NT])
        ROPE = psb("ml_rope", [128, 2, NLAT])
        wq = psb("ml_wq", [128, 2, 2, 768], F32R)
        wuk = psb("ml_wuk", [128, 512], F32R)
        wuv = psb("ml_wuv", [128, 512], F32R)
        kb.dma("sp", ROPE[:], G["rope_d"], writes=["ml_rope"])
        kb.dma("pool", wq[:].rearrange("p k v c -> p k (v c)"),
               G["wq_d"][L].rearrange("(k p) v c -> p k (v c)", p=128), writes=["ml_wq"])
        kb.dma("pool", wuk[:], G["wuk_d"][L], writes=["ml_wuk"])
        kb.dma("pool", wuv[:], G["wuv_d"][L], writes=["ml_wuv"])
        ps = [pps(f"ml_ps{i}", [128, 512]) for i in range(8)]
        with ExitStack() as p1:
            qsb = lambda name, shape, dt=F32: p1.enter_context(nc.sbuf_tensor(uq(name), shape, dt))
            cqT = qsb("ml_cqT", [128, 2, NT])
            ckvT = qsb("ml_ckvT", [128, NT])
            kA = qsb("ml_kA", [128, NT])
            kB = qsb("ml_kB", [128, NT])
            for t in range(2):
                ti = COLIDX[f"cq{t}"]
                kb.dma("sp", cqT[:, t, :], zT_d[ti * 128:(ti + 1) * 128, :], reads=[f"zT_{ti}"], writes=["ml_cqT"])
            ti = COLIDX["ckv"]
            kb.dma("sp", ckvT[:], zT_d[ti * 128:(ti + 1) * 128, :], reads=[f"zT_{ti}"], writes=["ml_ckvT"])
            for nm_, tl in (("kpeA", kA), ("kpeB", kB)):
                ti = COLIDX[nm_]
                kb.dma("sp", tl[0:96, :], zT_d[ti * 128:ti * 128 + 96, :], reads=[f"zT_{ti}"], writes=[tl])
            sq = qsb("ml_sq", [128, 3, 512], F32R)
            rs = [qsb(f"ml_rs{i}", [128, 512]) for i in range(2)]
            tt1 = qsb("ml_tt1", [128, 512])
            tt2 = qsb("ml_tt2", [128, 512])
            for (t0, n) in CHUNKS:
                for t in range(2):
                    kb.act(sq[:, t, 0:n], cqT[:, t, t0:t0 + n], AF.Square)
                kb.act(sq[:, 2, 0:n], ckvT[:, t0:t0 + n], AF.Square)
                for t in range(2):
                    kb.mm(ps[0][:, 0:n], onesr[:], sq[:, t, 0:n], start=(t == 0), stop=(t == 1))
                kb.mm(ps[1][:, 0:n], onesr[:], sq[:, 2, 0:n])
                kb.act(rs[0][:, 0:n], ps[0][:, 0:n], AF.Sqrt, scale=1.0 / 256, bias=EPS)
                kb.recip(rs[0][:, 0:n], rs[0][:, 0:n])
                kb.act(rs[1][:, 0:n], ps[1][:, 0:n], AF.Sqrt, scale=1.0 / 128, bias=EPS)
                kb.recip(rs[1][:, 0:n], rs[1][:, 0:n])
                for t in range(2):
                    kb.stt("dve", cqn[:, t, t0:t0 + n], cqT[:, t, t0:t0 + n], Vn(f"qa_g{L}")[:, t:t + 1], rs[0][:, 0:n], ALU.mult, ALU.mult)
                kb.stt("dve", ckvn[:, t0:t0 + n], ckvT[:, t0:t0 + n], Vn(f"kva_g{L}")[:, 0:1], rs[1][:, 0:n], ALU.mult, ALU.mult)
                if t0 < NCTX:
                    kb.cp("dve", KPE[64:96, t0:t0 + n], kA[64:96, t0:t0 + n])
                else:
                    l0 = t0 - NCTX
                    kb.tt("dve", tt1[64:96, 0:n], kA[64:96, t0:t0 + n], ROPE[64:96, 0, l0:l0 + n], ALU.mult)
                    kb.tt("dve", tt2[64:96, 0:n], kB[64:96, t0:t0 + n], ROPE[64:96, 1, l0:l0 + n], ALU.mult)
                    kb.tt("dve", KPE[64:96, t0:t0 + n], tt1[64:96, 0:n], tt2[64:96, 0:n], ALU.add)
            kb.barrier()
        KT = psb("ml_KT", [128, NT], F32R)
        QT = psb("ml_QT", [128, NT], F32R)
        Vh = psb("ml_Vh", [128, 18, 65], F32R)
        PT = [psb(f"ml_PT{i}", [128, 512], F32R) for i in range(4)]
        Osb = [psb(f"ml_Osb{i}", [128, 512]) for i in range(2)]
        ys = [psb(f"ml_ys{i}", [128, 512]) for i in range(2)]
        u1 = psb("ml_u1", [128, 512])
        u2 = psb("ml_u2", [128, 512])
        onesf = cst[:, 128:256]
        kb.cp("dve", Vh[:, :, 64:65], onesf[:, 0:18].unsqueeze(2))
        cnt = 0
        for h in range(8):
            for (t0, n) in CHUNKS:
                kb.mm(ps[0][0:64, 0:n], wuk[:, h * 64:(h + 1) * 64], ckvn[:, t0:t0 + n])
                kb.cp("act", KT[0:64, t0:t0 + n], ps[0][0:64, 0:n])
            kb.cp("dve", KT[64:96, :], KPE[64:96, :])
            for g0 in range(0, 18, 8):
                gn = min(8, 18 - g0)
                for j in range(gn):
                    kt = g0 + j
                    kb.mm(ps[1][:, j * 64:(j + 1) * 64], ckvn[:, kt * 128:(kt + 1) * 128], wuv[:, h * 64:(h + 1) * 64])
                kb.cp("dve", Vh[:, g0:g0 + gn, 0:64], ps[1][:, 0:gn * 64].rearrange("p (a b) -> p a b", b=64))
            for (t0, n) in CHUNKS:
                lat = t0 >= NCTX
                if not lat and not need_ctx:
                    continue
                for k in range(2):
                    kb.mm(ps[0][0:96, 0:n], wq[:, k, 0, h * 96:(h + 1) * 96], cqn[:, k, t0:t0 + n], start=(k == 0), stop=(k == 1))
                if lat:
                    for k in range(2):
                        kb.mm(ps[1][0:96, 0:n], wq[:, k, 1, h * 96:(h + 1) * 96], cqn[:, k, t0:t0 + n], start=(k == 0), stop=(k == 1))
                kb.cp("act", QT[0:64, t0:t0 + n], ps[0][0:64, 0:n])
                if not lat:
                    kb.cp("act", QT[64:96, t0:t0 + n], ps[0][64:96, 0:n])
                else:
                    l0 = t0 - NCTX
                    kb.tt("dve", u1[64:96, 0:n], ps[0][64:96, 0:n], ROPE[64:96, 0, l0:l0 + n], ALU.mult)
                    kb.tt("dve", u2[64:96, 0:n], ps[1][64:96, 0:n], ROPE[64:96, 1, l0:l0 + n], ALU.mult)
                    kb.tt("dve", QT[64:96, t0:t0 + n], u1[64:96, 0:n], u2[64:96, 0:n], ALU.add)
            groups = ([[CHUNKS[0]]] if need_ctx else []) + [CHUNKS[1:3], CHUNKS[3:5]]
            for grp in groups:
                lat = grp[0][0] >= NCTX
                kts = list(range(18)) if lat else [0, 1]
                Sb = lambda a, i: ps[2 + 2 * a + i % 2]
                Pb = lambda a, i: PT[2 * a + i % 2]

                def emitS(i):
                    kt = kts[i]
                    for a, (t0, n) in enumerate(grp):
                        kb.mm(Sb(a, i)[:, 0:n], KT[0:96, kt * 128:(kt + 1) * 128], QT[0:96, t0:t0 + n])
                emitS(0)
                for i, kt in enumerate(kts):
                    for a, (t0, n) in enumerate(grp):
                        kb.act(Pb(a, i)[:, 0:n], Sb(a, i)[:, 0:n], AF.Exp, scale=MLA_SCALE)
                    if i + 1 < len(kts):
                        emitS(i + 1)
                    for a, (t0, n) in enumerate(grp):
                        kb.mm(ps[6 + a][0:65, 0:n], Vh[:, kt, :], Pb(a, i)[:, 0:n], start=(i == 0), stop=(i == len(kts) - 1))
                for a, (t0, n) in enumerate(grp):
                    ob = Osb[a]
                    yo = ys[a]
                    bcp = ps[a]
                    kb.cp("act", ob[0:65, 0:n], ps[6 + a][0:65, 0:n])
                    kb.recip(ob[64:65, 0:n], ob[64:65, 0:n])
                    kb.mm(bcp[0:64, 0:n], onesf[64:65, 0:64], ob[64:65, 0:n])
                    kb.tt("dve", yo[0:64, 0:n], ob[0:64, 0:n], bcp[0:64, 0:n], ALU.mult)
                    kb.dma("sp", ybT_d[256 + h * 64:256 + (h + 1) * 64, t0:t0 + n], yo[0:64, 0:n], reads=[yo], writes=[f"ybT_m{h}"])
        kb.barrier()


def phase_hg(G):
    nc, kb, L = G["nc"], G["kb"], G["L"]
    Vn, cst, onesr, ident, lbv = G["Vn"], G["cst"], G["onesr"], G["ident"], G["lbv"]
    zT_d, ybT_d = G["zT_d"], G["ybT_d"]
    CH = 64
    NC_ = NT // CH
    onesf = cst[:, 128:256]
    with ExitStack() as ph:
        psb = lambda name, shape, dt=F32: ph.enter_context(nc.sbuf_tensor(uq(name), shape, dt))
        pps = lambda name, shape, dt=F32: ph.enter_context(nc.psum_tensor(uq(name), shape, dt))
        A = psb("hg_A", [128, NT])
        KK = psb("hg_KK", [128, NT], F32R)
        Bt = psb("hg_Bt", [128, NT])
        E1 = psb("hg_E1", [128, NT], F32R)
        qT = psb("hg_qT", [128, NT])
        ig = psb("hg_ig", [128, NT])
        itok = psb("hg_itok", [128, NC_, 64], F32R)
        oacc = psb("hg_oacc", [128, NT])
        U = psb("hg_U", [128, NC_, 64])
        PTall = psb("hg_PT", [128, NC_, 64], F32R)
        Sst = psb("hg_Sst", [128, NC_, 64], F32R)
        ktok = [psb(f"hg_ktok{i}", [128, 4, 128], F32R) for i in range(2)]
        sct = [psb(f"hg_sct{i}", [128, 512]) for i in range(2)]
        small = psb("hg_small", [128, 4, NC_])
        S = psb("hg_S", [128, 64])
        tU = psb("hg_tU", [128, 64])
        fin = [psb(f"hg_fin{i}", [128, 512]) for i in range(3)]
        ps = [pps(f"hg_ps{i}", [128, 512]) for i in range(7)]
        MK = psb("hg_MK", [128, NT + 1])
        kb.memset("dve", MK[:], 1.0)
        kb.memset("dve", MK[:, 0:NT].rearrange("p (n c) -> p n c", c=CH)[:, :, 0:1], 0.0)
        pcnt = [0]

        def nps():
            pcnt[0] += 1
            return ps[pcnt[0] % 7]

        b3 = lambda t: t[:].rearrange("p (n c) -> p n c", c=CH)
        for h in range(4):
            tq, ti_, tg = COLIDX[f"hq{h}"], COLIDX[f"hi{h}"], COLIDX[f"hg{h}"]
            kb.dma("sp", qT[:], zT_d[tq * 128:(tq + 1) * 128, :], reads=[f"zT_{tq}"], writes=[qT])
            kb.dma("sp", ig[0:64, :], zT_d[ti_ * 128:ti_ * 128 + 64, :], reads=[f"zT_{ti_}"], writes=[ig])
            for c0 in range(0, NC_, 8):
                gn = min(8, NC_ - c0)
                p = nps()
                for j in range(gn):
                    c = c0 + j
                    kb.tr(p[0:64, j * 64:(j + 1) * 64], ig[0:64, c * CH:(c + 1) * CH], ident[0:64, 0:64])
                kb.cp("act", itok[0:64, c0:c0 + gn, :], p[0:64, 0:gn * 64].rearrange("p (a b) -> p a b", b=64))
            for d in range(2):
                tf = COLIDX[f"hf{h}" if d == 0 else f"hb{h}"]
                kb.dma("sp", A[:], zT_d[tf * 128:(tf + 1) * 128, :], reads=[f"zT_{tf}"], writes=[A])
                kb.act(A[:], A[:], AF.Sigmoid)
                kb.ts("dve", A[:], A[:], lbv[:, 1, L:L + 1], ALU.mult, lbv[:, 0, L:L + 1], ALU.add)
                kb.ts("dve", KK[:], A[:], -1.0, ALU.mult, 1.0, ALU.add)
                kb.act(A[:], A[:], AF.Ln)
                if d == 0:
                    kb.scan(Bt[:, :], MK[:, 0:NT], A[:, :], 0.0, ALU.mult, ALU.add)
                else:
                    kb.scan(Bt[:, ::-1], MK[:, 1:NT + 1][:, ::-1], A[:, ::-1], 0.0, ALU.mult, ALU.add)
                refpos = 31 if d == 0 else 32
                lastpos = 63 if d == 0 else 0
                refc, alpha, gamma, beta = (small[:, i, :] for i in range(4))
                kb.cp("dve", refc, b3(Bt)[:, :, refpos])
                kb.act(alpha, b3(Bt)[:, :, lastpos], AF.Exp)
                kb.act(gamma, refc, AF.Exp)
                kb.tt("dve", b3(Bt), b3(Bt), refc.unsqueeze(2).to_broadcast([128, NC_, CH]), ALU.subtract)
                kb.act(E1[:], Bt[:], AF.Exp)
                kb.cp("dve", beta, b3(E1)[:, :, lastpos].bitcast(F32))
                kb.act(Bt[:], Bt[:], AF.Exp, scale=-1.0)
                kb.tt("dve", E1[:], qT[:], E1[:].bitcast(F32), ALU.mult)
                kb.tt("dve", KK[:], KK[:].bitcast(F32), Bt[:], ALU.mult)
                for c0 in range(0, NC_, 4):
                    p = nps()
                    kt_ = ktok[(c0 // 4) % 2]
                    for j in range(4):
                        c = c0 + j
                        kb.tr(p[0:64, j * 128:(j + 1) * 128], KK[:, c * CH:(c + 1) * CH].bitcast(F32), ident)
                    kb.cp("act", kt_[0:64, :, :], p[0:64, :].rearrange("p (a b) -> p a b", b=128))
                    p2 = nps()
                    for j in range(4):
                        c = c0 + j
                        kb.mm(p2[:, j * 64:(j + 1) * 64], kt_[0:64, j, :], itok[0:64, c, :])
                    kb.cp("dve", U[:, c0:c0 + 4, :], p2[:, 0:256].rearrange("p (a b) -> p a b", b=64))
                for c0 in range(0, NC_, 8):
                    gn = min(8, NC_ - c0)
                    p = nps()
                    for j in range(gn):
                        c = c0 + j
                        kb.mm(p[0:64, j * 64:(j + 1) * 64], KK[:, c * CH:(c + 1) * CH], E1[:, c * CH:(c + 1) * CH])
                    st = sct[(c0 // 8) % 2]
                    kb.cp("act", st[0:64, 0:gn * 64], p[0:64, 0:gn * 64])
                    if d == 0:
                        kb.op("pool", lambda g: g.affine_select(PTall[0:64, c0:c0 + gn, :], st[0:64, 0:gn * 64].rearrange("p (a b) -> p a b", b=64),
                                                                [[0, gn], [1, 64]], ALU.is_ge, 0.0, base=0, channel_multiplier=-1),
                              reads=[st], writes=[PTall])
                    else:
                        kb.op("pool", lambda g: g.affine_select(PTall[0:64, c0:c0 + gn, :], st[0:64, 0:gn * 64].rearrange("p (a b) -> p a b", b=64),
                                                                [[0, gn], [-1, 64]], ALU.is_ge, 0.0, base=0, channel_multiplier=1),
                              reads=[st], writes=[PTall])
                AR = A[:].rearrange("p (v o) -> p v o", o=NC_)
                U2 = Bt[:].rearrange("p (v o) -> p v o", o=NC_)
                S2 = U[:].rearrange("p c v -> p (c v)").rearrange("p (v o) -> p v o", o=NC_)
                if d == 0:
                    segs = [(0, NC_, False)]
                    cof = lambda o: o
                else:
                    segs = [(0, 4, True), (4, NC_ - 4, True)]
                    cof = lambda o: (3 - o) if o < 4 else (NC_ + 3 - o)
                for (o0, n_, _) in segs:
                    c_hi, c_lo = cof(o0), cof(o0 + n_ - 1)
                    if d == 0:
                        csl = slice(c_hi, c_lo + 1)
                    else:
                        csl = slice(c_hi, (c_lo - 1) if c_lo > 0 else None, -1)
                    kb.tt("dve", U2[:, :, o0:o0 + n_].rearrange("p v o -> p o v"), U[:, csl, :],
                          beta[:, csl].unsqueeze(2).to_broadcast([128, n_, 64]), ALU.mult)
                    kb.cp("dve", AR[:, :, o0:o0 + n_].rearrange("p v o -> p o v"), alpha[:, csl].unsqueeze(2).to_broadcast([128, n_, 64]))
                kb.memset("dve", AR[:, :, 0:1], 0.0)
                kb.scan(U[:].rearrange("p c v -> p (c v)"), A[:, :], Bt[:, :], 0.0, ALU.mult, ALU.add)
                first_c = cof(0)
                kb.memset("dve", Sst[:, first_c, :].bitcast(F32), 0.0)
                for (o0, n_, _) in segs:
                    oa = max(o0, 1)
                    nn = o0 + n_ - oa
                    c_hi, c_lo = cof(oa), cof(oa + nn - 1)
                    if d == 0:
                        csl = slice(c_hi, c_lo + 1)
                    else:
                        csl = slice(c_hi, (c_lo - 1) if c_lo > 0 else None, -1)
                    kb.tt("dve", Sst[:, csl, :], S2[:, :, oa - 1:oa - 1 + nn].rearrange("p v o -> p o v"),
                          gamma[:, csl].unsqueeze(2).to_broadcast([128, nn, 64]), ALU.mult)
                for c0 in range(0, NC_, 8):
                    gn = min(8, NC_ - c0)
                    p = nps()
                    for j in range(gn):
                        c = c0 + j
                        kb.mm(p[0:64, j * 64:(j + 1) * 64], itok[0:64, c, :], PTall[0:64, c, :], start=True, stop=False)
                        kb.mm(p[0:64, j * 64:(j + 1) * 64], Sst[:, c, :], E1[:, c * CH:(c + 1) * CH], start=False, stop=True)
                    if d == 0:
                        kb.cp("act", oacc[0:64, c0 * CH:(c0 + gn) * CH], p[0:64, 0:gn * 64])
                    else:
                        kb.tt("dve", oacc[0:64, c0 * CH:(c0 + gn) * CH], oacc[0:64, c0 * CH:(c0 + gn) * CH], p[0:64, 0:gn * 64], ALU.add)
            kb.dma("sp", ig[0:64, :], zT_d[tg * 128:tg * 128 + 64, :], reads=[f"zT_{tg}"], writes=[ig])
            kb.act(KK[0:64, :], oacc[0:64, :], AF.Square)
            kb.act(ig[0:64, :], ig[0:64, :], AF.Silu)
            for (t0, n) in CHUNKS:
                p = nps()
                kb.mm(p[0:64, 0:n], onesr[0:64, 0:64], KK[0:64, t0:t0 + n])
                kb.act(fin[0][0:64, 0:n], p[0:64, 0:n], AF.Sqrt, scale=1.0 / 64, bias=EPS)
                kb.recip(fin[0][0:64, 0:n], fin[0][0:64, 0:n])
                kb.stt("dve", fin[1][0:64, 0:n], oacc[0:64, t0:t0 + n], Vn(f"hgn_g{L}")[0:64, h:h + 1], fin[0][0:64, 0:n], ALU.mult, ALU.mult)
                kb.tt("dve", fin[2][0:64, 0:n], fin[1][0:64, 0:n], ig[0:64, t0:t0 + n], ALU.mult)
                kb.dma("sp", ybT_d[768 + h * 64:768 + (h + 1) * 64, t0:t0 + n], fin[2][0:64, 0:n], reads=[fin[2]], writes=[f"ybT_h{h}"])
        kb.barrier()


def phase_merge(G):
    nc, kb, L = G["nc"], G["kb"], G["L"]
    xT, modT = G["xT"], G["modT"]
    zT_d, ybT_d = G["zT_d"], G["ybT_d"]
    need_ctx = L < DEPTH - 1
    chs = CHUNKS if need_ctx else CHUNKS[1:]
    yb_names = ["ybT_0", "ybT_1"] + [f"ybT_m{h}" for h in range(8)] + [f"ybT_h{h}" for h in range(4)]
    with ExitStack() as ph:
        psb = lambda name, shape, dt=F32: ph.enter_context(nc.sbuf_tensor(uq(name), shape, dt))
        pps = lambda name, shape, dt=F32: ph.enter_context(nc.psum_tensor(uq(name), shape, dt))
        wp = psb("mg_wp", [128, 8, D], F32R)
        wo = psb("mg_wo", [128, 8, D], F32R)
        kb.dma("pool", wp[:], G["wp_d"][L].rearrange("(k p) c -> p k c", p=128), writes=[wp])
        kb.dma("pool", wo[:], G["wo_d"][L].rearrange("(k p) c -> p k c", p=128), writes=[wo])
        yb = psb("mg_yb", [128, 8, 512], F32R)
        mT = psb("mg_mT", [128, 8, 512], F32R)
        gt = [psb(f"mg_gt{i}", [128, 3, 512]) for i in range(2)]
        t3 = [psb(f"mg_t{i}", [128, 512]) for i in range(3)]
        ps = [pps(f"mg_ps{i}", [128, 512]) for i in range(8)]
        branches = ((0, 2), (2, 6), (6, 8))
        for (t0, n) in chs:
            s_ = 1 if t0 < NCTX else 0
            kb.dma("pool", yb[:, :, 0:n], ybT_d[:, t0:t0 + n].rearrange("(k p) t -> p k t", p=128), reads=yb_names, writes=[yb])
            for f in range(8):
                g = gt[f % 2]
                for b in range(3):
                    ti = COLIDX[f"gate{b}_{f}"]
                    kb.dma("sp", g[:, b, 0:n], zT_d[ti * 128:(ti + 1) * 128, t0:t0 + n], reads=[f"zT_{ti}"], writes=[g])
                kb.act(g[:, :, 0:n], g[:, :, 0:n], AF.Sigmoid)
                for b, (k0, k1) in enumerate(branches):
                    p = ps[(f % 2) * 3 + b]
                    for k in range(k0, k1):
                        kb.mm(p[:, 0:n], wp[:, k, f * 128:(f + 1) * 128], yb[:, k, 0:n], start=(k == k0), stop=(k == k1 - 1))
                    kb.tt("dve", t3[b][:, 0:n], p[:, 0:n], g[:, b, 0:n], ALU.mult)
                kb.tt("pool", t3[0][:, 0:n], t3[0][:, 0:n], t3[1][:, 0:n], ALU.add)
                kb.tt("pool", mT[:, f, 0:n], t3[0][:, 0:n], t3[2][:, 0:n], ALU.add)
            for f in range(8):
                p = ps[6 + f % 2]
                for k in range(8):
                    kb.mm(p[:, 0:n], wo[:, k, f * 128:(f + 1) * 128], mT[:, k, 0:n], start=(k == 0), stop=(k == 7))
                kb.stt("dve", xT[:, f, t0:t0 + n], p[:, 0:n], modT[:, L, 16 + f, s_:s_ + 1], xT[:, f, t0:t0 + n], ALU.mult, ALU.add)
        kb.barrier()


def phase_moe(G):
    nc, kb, L = G["nc"], G["kb"], G["L"]
    xT, modT, modA, onesr, ident, cst = G["xT"], G["modT"], G["modA"], G["onesr"], G["ident"], G["cst"]
    need_ctx = L < DEPTH - 1
    chs = CHUNKS if need_ctx else CHUNKS[1:]
    groups = [chs[:3], chs[3:]] if need_ctx else [chs[:2], chs[2:]]
    onesf = cst[:, 128:256]
    for grp in groups:
        GN = sum(n for _, n in grp)
        gch = []
        o = 0
        for (t0, n) in grp:
            gch.append((t0, n, o))
            o += n
        with ExitStack() as ph:
            psb = lambda name, shape, dt=F32: ph.enter_context(nc.sbuf_tensor(uq(name), shape, dt))
            pps = lambda name, shape, dt=F32: ph.enter_context(nc.psum_tensor(uq(name), shape, dt))
            h2T = psb("me_h2T", [128, 8, GN], F32R)
            gateT = psb("me_gateT", [128, GN], F32R)
            emit_norm(nc, kb, xT, h2T, gch, lambda k, s_: modA[:, L, 1, k, s_:s_ + 1],
                      lambda k, s_: modT[:, L, 24 + k, s_:s_ + 1], onesr, D)
            with ExitStack() as rp:
                rsb = lambda name, shape, dt=F32: rp.enter_context(nc.sbuf_tensor(uq(name), shape, dt))
                rps = lambda name, shape, dt=F32: rp.enter_context(nc.psum_tensor(uq(name), shape, dt))
                wr = rsb("me_wr", [128, 8, 36])
                br = rsb("me_br", [128, 36])
                kb.dma("sp", wr[:], G["wr_d"][L].rearrange("(k p) c -> p k c", p=128), writes=[wr])
                kb.dma("sp", br[:], G["br_d"][L].to_broadcast([128, 36]), writes=[br])
                lp = [rps(f"me_lp{i}", [128, 512]) for i in range(2)]
                gp = [rps(f"me_gp{i}", [128, 512]) for i in range(2)]
                R = [[rsb(f"me_r{b}_{i}", [128, 40]) for i in range(12)] for b in range(2)]
                for tt in range(GN // 128):
                    b = tt % 2
                    r = R[b]
                    for k in range(8):
                        kb.mm(lp[b][:, 0:36], h2T[:, k, tt * 128:(tt + 1) * 128].bitcast(F32), wr[:, k, :], start=(k == 0), stop=(k == 7))
                    lg = r[0]
                    kb.tt("dve", lg[:, 0:36], lp[b][:, 0:36], br[:], ALU.add)
                    gmax, ngmax, gsum, gw = r[1][:, 0:1], r[1][:, 1:2], r[1][:, 2:3], r[1][:, 3:4]
                    kb.op("dve", lambda g: g.tensor_reduce(gmax, lg[:, 0:4], AX.X, ALU.max), reads=[lg], writes=[r[1]])
                    kb.ts("dve", ngmax, gmax, -1.0, ALU.mult)
                    kb.act(r[2][:, 0:4], lg[:, 0:4], AF.Exp, bias=ngmax)
                    kb.op("dve", lambda g: g.tensor_reduce(gsum, r[2][:, 0:4], AX.X, ALU.add), reads=[r[2]], writes=[r[1]])
                    kb.recip(gw, gsum)
                    kb.ts("dve", r[3][:, 0:4], lg[:, 0:4], gmax, ALU.is_equal)
                    kb.ts("dve", r[3][:, 0:4], r[3][:, 0:4], -1.0, ALU.add, 1e30, ALU.mult)
                    kb.tt("dve", r[4][:, 0:32].rearrange("p (a b) -> p a b", b=8), lg[:, 4:36].rearrange("p (a b) -> p a b", b=8),
                          r[3][:, 0:4].unsqueeze(2).to_broadcast([128, 4, 8]), ALU.add)
                    kb.op("dve", lambda g: g.max(r[5][:, 0:8], r[4][:, 0:32]), reads=[r[4]], writes=[r[5]])
                    m1, m2 = r[5][:, 0:1], r[5][:, 1:2]
                    kb.ts("dve", r[6][:, 0:32], r[4][:, 0:32], m1, ALU.is_equal)
                    kb.ts("dve", r[7][:, 0:32], r[4][:, 0:32], m2, ALU.is_equal)
                    dm, ee, p1, p2 = r[8][:, 0:1], r[8][:, 1:2], r[8][:, 2:3], r[8][:, 3:4]
                    kb.tt("dve", dm, m2, m1, ALU.subtract)
                    kb.act(ee, dm, AF.Exp)
                    kb.ts("dve", p1, ee, 1.0, ALU.add)
                    kb.recip(p1, p1)
                    kb.tt("dve", p2, ee, p1, ALU.mult)
                    kb.tt("dve", p1, p1, gw, ALU.mult)
                    kb.tt("dve", p2, p2, gw, ALU.mult)
                    kb.ts("dve", r[9][:, 0:32], r[6][:, 0:32], p1, ALU.mult)
                    kb.stt("dve", r[9][:, 0:32], r[7][:, 0:32], p2, r[9][:, 0:32], ALU.mult, ALU.add)
                    kb.tr(gp[b][0:32, 0:128], r[9][:, 0:32], ident)
                    kb.cp("act", gateT[0:32, tt * 128:(tt + 1) * 128], gp[b][0:32, 0:128])
                kb.barrier()
            Gall = psb("me_G", [128, 4, GN], F32R)
            w13 = [[psb(f"me_w{a}_{i}", [128, 8, 128], F32R) for i in range(2)] for a in (1, 3)]
            w2 = [psb(f"me_w2_{i}", [128, 4, D], F32R) for i in range(2)]
            sel = [psb(f"me_sel{i}", [128, 128], F32R) for i in range(2)]
            sil = [psb(f"me_sil{i}", [128, 512]) for i in range(2)]
            hp = [pps(f"me_hp{i}", [128, 512]) for i in range(4)]
            bp = [pps(f"me_bp{i}", [128, 512]) for i in range(2)]
            yp = [pps(f"me_yp{i}", [128, 512]) for i in range(2)]
            cnt = 0
            for e in range(32):
                se = sel[e % 2]
                kb.op("pool", lambda g: g.affine_select(se[0:32, :], onesf[0:32, :], [[0, 128]], ALU.is_equal, 0.0,
                                                        base=-e, channel_multiplier=1), reads=[cst], writes=[se])
                kb.dma("pool", w2[e % 2][:], G["w2_d"][L, e].rearrange("(j p) c -> p j c", p=128), writes=[w2[e % 2]])
                for j in range(4):
                    wa, wb_ = w13[0][(e * 4 + j) % 2], w13[1][(e * 4 + j) % 2]
                    kb.dma("pool", wa[:], G["w1_d"][L, e, :, j * 128:(j + 1) * 128].rearrange("(k p) c -> p k c", p=128), writes=[wa])
                    kb.dma("pool", wb_[:], G["w3_d"][L, e, :, j * 128:(j + 1) * 128].rearrange("(k p) c -> p k c", p=128), writes=[wb_])
                    for (t0, n, o) in gch:
                        b = cnt % 2
                        cnt += 1
                        for k in range(8):
                            kb.mm(hp[b][:, 0:n], wa[:, k, :], h2T[:, k, o:o + n], start=(k == 0), stop=(k == 7))
                        for k in range(8):
                            kb.mm(hp[2 + b][:, 0:n], wb_[:, k, :], h2T[:, k, o:o + n], start=(k == 0), stop=(k == 7))
                        kb.mm(bp[b][:, 0:n], se[0:32, :], gateT[0:32, o:o + n])
                        kb.act(sil[b][:, 0:n], hp[b][:, 0:n], AF.Silu)
                        kb.tt("dve", sil[b][:, 0:n], sil[b][:, 0:n], hp[2 + b][:, 0:n], ALU.mult)
                        kb.tt("dve", Gall[:, j, o:o + n], sil[b][:, 0:n], bp[b][:, 0:n], ALU.mult)
                for (t0, n, o) in gch:
                    s_ = 1 if t0 < NCTX else 0
                    for f in range(8):
                        p = yp[f % 2]
                        for j in range(4):
                            kb.mm(p[:, 0:n], w2[e % 2][:, j, f * 128:(f + 1) * 128], Gall[:, j, o:o + n], start=(j == 0), stop=(j == 3))
                        kb.stt("dve", xT[:, f, t0:t0 + n], p[:, 0:n], modT[:, L, 40 + f, s_:s_ + 1], xT[:, f, t0:t0 + n], ALU.mult, ALU.add)
            kb.barrier()


def phase_moe_sparse(G):
    nc, kb, L = G["nc"], G["kb"], G["L"]
    xT, modT, modA, onesr, ident, cst = G["xT"], G["modT"], G["modA"], G["onesr"], G["ident"], G["cst"]
    h2tok_d, xs_d, ys_d = G["h2tok_d"], G["xs_d"], G["ys_d"]
    need_ctx = L < DEPTH - 1
    chs = CHUNKS if need_ctx else CHUNKS[1:]
    tiles = [t0 // 128 + j for (t0, n) in chs for j in range(n // 128)]
    NTL = len(tiles)
    onesf = cst[:, 128:256]
    with ExitStack() as ph:
        psb = lambda name, shape, dt=F32: ph.enter_context(nc.sbuf_tensor(uq(name), shape, dt))
        pps = lambda name, shape, dt=F32: ph.enter_context(nc.psum_tensor(uq(name), shape, dt))
        pr = ExitStack()
        rsb_ = lambda name, shape, dt=F32: pr.enter_context(nc.sbuf_tensor(uq(name), shape, dt))
        GA = psb("ms_GA", [128, 18])
        GB = psb("ms_GB", [128, 18])
        D1f = psb("ms_D1f", [128, 18])
        D2f = psb("ms_D2f", [128, 18])
        D1i = psb("ms_D1i", [128, 18], I32)
        D2i = psb("ms_D2i", [128, 18], I32)
        idxW = psb("ms_idxW", [128, 128], I32)
        p0 = ExitStack()
        h2T = p0.enter_context(nc.sbuf_tensor(uq("ms_h2T"), [128, 8, NT], F32R))
        OH1 = rsb_("ms_OH1", [128, 18, 32])
        OH2 = rsb_("ms_OH2", [128, 18, 32])
        AA = rsb_("ms_AA", [128, 18, 32])
        with ExitStack() as p1:
            qsb = lambda name, shape, dt=F32: p1.enter_context(nc.sbuf_tensor(uq(name), shape, dt))
            qps = lambda name, shape, dt=F32: p1.enter_context(nc.psum_tensor(uq(name), shape, dt))
            emit_norm(nc, kb, xT, h2T, [(t0, n, t0) for (t0, n) in chs], lambda k, s_: modA[:, L, 1, k, s_:s_ + 1],
                      lambda k, s_: modT[:, L, 24 + k, s_:s_ + 1], onesr, D)
            wr = qsb("ms_wr", [128, 8, 36])
            br = qsb("ms_br", [128, 36])
            kb.dma("sp", wr[:], G["wr_d"][L].rearrange("(k p) c -> p k c", p=128), writes=[wr])
            kb.dma("sp", br[:], G["br_d"][L].to_broadcast([128, 36]), writes=[br])
            lp = [qps(f"ms_lp{i}", [128, 512]) for i in range(2)]
            T_ = NTL
            LG = qsb("ms_LG", [128, 18, 36])
            for i, tt in enumerate(tiles):
                b = i % 2
                tsl = slice(tt * 128, (tt + 1) * 128)
                for k in range(8):
                    kb.mm(lp[b][:, 0:36], h2T[:, k, tsl].bitcast(F32), wr[:, k, :], start=(k == 0), stop=(k == 7))
                kb.tt("dve", LG[:, i, :], lp[b][:, 0:36], br[:], ALU.add)
            B4 = lambda t: t[:, 0:T_, :]
            g4 = qsb("ms_g4", [128, 18, 4])
            oh4 = qsb("ms_oh4", [128, 18, 4])
            ls = qsb("ms_ls", [128, 18, 32])
            l2 = qsb("ms_l2", [128, 18, 32])
            sm_ = qsb("ms_sm", [128, 8, 18])
            gmax, gsum, gw, m1, m2, ee, p1_, p2_ = (sm_[:, j, 0:T_] for j in range(8))
            bc4 = lambda v: v.unsqueeze(2).to_broadcast([128, T_, 4])
            bc32 = lambda v: v.unsqueeze(2).to_broadcast([128, T_, 32])
            kb.op("dve", lambda g: g.tensor_reduce(gmax, LG[:, 0:T_, 0:4], AX.X, ALU.max), reads=[LG], writes=[sm_])
            kb.tt("dve", g4[:, 0:T_, :], LG[:, 0:T_, 0:4], bc4(gmax), ALU.subtract)
            kb.tt("dve", oh4[:, 0:T_, :], LG[:, 0:T_, 0:4], bc4(gmax), ALU.is_equal)
            kb.act(g4[:, 0:T_, :], g4[:, 0:T_, :], AF.Exp)
            kb.op("dve", lambda g: g.tensor_reduce(gsum, g4[:, 0:T_, :], AX.X, ALU.add), reads=[g4], writes=[sm_])
            kb.recip(gw, gsum)
            kb.ts("dve", oh4[:, 0:T_, :], oh4[:, 0:T_, :], -1.0, ALU.add, 1e30, ALU.mult)
            kb.tt("dve", ls[:, 0:T_, :].rearrange("p t (a b) -> p t a b", b=8), LG[:, 0:T_, 4:36].rearrange("p t (a b) -> p t a b", b=8),
                  oh4[:, 0:T_, :].unsqueeze(3).to_broadcast([128, T_, 4, 8]), ALU.add)
            kb.op("dve", lambda g: g.tensor_reduce(m1, ls[:, 0:T_, :], AX.X, ALU.max), reads=[ls], writes=[sm_])
            kb.tt("dve", OH1[:, 0:T_, :], ls[:, 0:T_, :], bc32(m1), ALU.is_equal)
            kb.stt("dve", l2[:, 0:T_, :], OH1[:, 0:T_, :], -1e30, ls[:, 0:T_, :], ALU.mult, ALU.add)
            kb.op("dve", lambda g: g.tensor_reduce(m2, l2[:, 0:T_, :], AX.X, ALU.max), reads=[l2], writes=[sm_])
            kb.tt("dve", OH2[:, 0:T_, :], l2[:, 0:T_, :], bc32(m2), ALU.is_equal)
            kb.tt("dve", AA[:, 0:T_, :], OH1[:, 0:T_, :], OH2[:, 0:T_, :], ALU.add)
            kb.tt("dve", ee, m2, m1, ALU.subtract)
            kb.act(ee, ee, AF.Exp)
            kb.ts("dve", p1_, ee, 1.0, ALU.add)
            kb.recip(p1_, p1_)
            kb.tt("dve", p2_, ee, p1_, ALU.mult)
            kb.tt("dve", GA[:, 0:T_], p1_, gw, ALU.mult)
            kb.tt("dve", GB[:, 0:T_], p2_, gw, ALU.mult)
            kb.barrier()
        with ExitStack() as p2:
            qsb = lambda name, shape, dt=F32: p2.enter_context(nc.sbuf_tensor(uq(name), shape, dt))
            qps = lambda name, shape, dt=F32: p2.enter_context(nc.psum_tensor(uq(name), shape, dt))
            ltri = qsb("ms_ltri", [128, 128])
            kb.op("pool", lambda g: g.affine_select(ltri[:], onesf, [[1, 128]], ALU.is_gt, 0.0, base=0, channel_multiplier=-1),
                  reads=[cst], writes=[ltri])
            Rp = [qps(f"ms_Rp{i}", [128, 512]) for i in range(2)]
            Cp = qps("ms_Cp", [128, 512])
            for i in range(NTL):
                out = Rp[i // 16][:, (i % 16) * 32:(i % 16 + 1) * 32]
                kb.mm(out, ltri[:], AA[:, i, :], start=True, stop=(i == 0))
                for i2 in range(i):
                    kb.mm(out, onesf, AA[:, i2, :], start=False, stop=(i2 == i - 1))
            for i in range(NTL):
                kb.mm(Cp[:, 0:32], onesf, AA[:, i, :], start=(i == 0), stop=(i == NTL - 1))
            w_ = [qsb(f"ms_w{i}", [128, 32]) for i in range(8)]
            cnt, x_, kf, msk, nb, pend, pstart, tmp = w_
            kb.cp("dve", cnt[:], Cp[:, 0:32])
            kb.ts("dve", x_[:], cnt[:], -float(SLOT), ALU.add, 0.0, ALU.max)
            kb.ts("dve", x_[:], x_[:], 127.0, ALU.add, 1.0 / 128, ALU.mult)
            ki = qsb("ms_ki", [128, 32], I32)
            kb.cp("dve", ki[:], x_[:])
            kb.cp("dve", kf[:], ki[:])
            kb.tt("dve", msk[:], kf[:], x_[:], ALU.is_gt)
            kb.tt("dve", kf[:], kf[:], msk[:], ALU.subtract)
            kb.ts("dve", tmp[:], kf[:], 1.0, ALU.add)
            kb.tt("dve", msk[:], tmp[:], x_[:], ALU.is_le)
            kb.tt("dve", nb[:], kf[:], msk[:], ALU.add)
            kb.scan(pend[:], onesf[:, 0:32], nb[:], 0.0, ALU.mult, ALU.add)
            kb.tt("dve", pstart[:], pend[:], nb[:], ALU.subtract)
            base1_i = qsb("ms_b1i", [128, 32], I32)
            base1 = qsb("ms_b1", [128, 32])
            kb.op("pool", lambda g: g.iota(base1_i[:], [[SLOT, 32]], base=0, channel_multiplier=0), writes=[base1_i])
            kb.cp("dve", base1[:], base1_i[:])
            kb.ts("dve", pstart[:], pstart[:], 128.0, ALU.mult, float(32 * SLOT - SLOT), ALU.add)
            kb.tt("dve", pstart[:], pstart[:], base1[:], ALU.subtract)
            RR = qsb("ms_RR", [128, 18, 32])
            T3 = qsb("ms_T3", [128, 18, 32])
            T4 = qsb("ms_T4", [128, 18, 32])
            n0 = min(NTL, 16)
            kb.cp("dve", RR[:, 0:n0, :], Rp[0][:, 0:n0 * 32].rearrange("p (a b) -> p a b", b=32))
            if NTL > 16:
                kb.cp("dve", RR[:, 16:NTL, :], Rp[1][:, 0:(NTL - 16) * 32].rearrange("p (a b) -> p a b", b=32))
            bcT = lambda v: v.unsqueeze(1).to_broadcast([128, NTL, 32])
            kb.ts("dve", T4[:, 0:NTL, :], RR[:, 0:NTL, :], float(SLOT), ALU.is_ge)
            kb.tt("dve", T4[:, 0:NTL, :], T4[:, 0:NTL, :], bcT(pstart[:]), ALU.mult)
            kb.tt("dve", T3[:, 0:NTL, :], RR[:, 0:NTL, :], bcT(base1[:]), ALU.add)
            kb.tt("dve", T3[:, 0:NTL, :], T3[:, 0:NTL, :], T4[:, 0:NTL, :], ALU.add)
            kb.tt("dve", T4[:, 0:NTL, :], T3[:, 0:NTL, :], OH1[:, 0:NTL, :], ALU.mult)
            kb.op("dve", lambda g: g.tensor_reduce(D1f[:, 0:NTL], T4[:, 0:NTL, :], AX.X, ALU.add), reads=[T4], writes=[D1f])
            kb.tt("dve", T4[:, 0:NTL, :], T3[:, 0:NTL, :], OH2[:, 0:NTL, :], ALU.mult)
            kb.op("dve", lambda g: g.tensor_reduce(D2f[:, 0:NTL], T4[:, 0:NTL, :], AX.X, ALU.add), reads=[T4], writes=[D2f])
            kb.cp("dve", D1i[:, 0:NTL], D1f[:, 0:NTL])
            kb.cp("dve", D2i[:, 0:NTL], D2f[:, 0:NTL])
            pidx_i = qsb("ms_pidx_i", [128, 1], I32)
            pidx = qsb("ms_pidx", [128, 1])
            kb.op("pool", lambda g: g.iota(pidx_i[:], [[0, 1]], base=0, channel_multiplier=1), writes=[pidx_i])
            kb.cp("dve", pidx[:], pidx_i[:])
            be = qsb("ms_be", [128, 1])
            kb.ts("dve", tmp[:], pend[:], pidx[:, 0:1], ALU.is_le)
            kb.op("dve", lambda g: g.tensor_reduce(be[:], tmp[:], AX.X, ALU.add), reads=[tmp], writes=[be])
            kb.ts("dve", be[:], be[:], 31.0, ALU.min)
            vb = qsb("ms_vb", [128, 1])
            kb.ts("dve", vb[:], pidx[:], pend[:, 31:32], ALU.is_lt)
            kb.tt("dve", be[:], be[:], vb[:], ALU.mult)
            kb.ts("dve", vb[:], vb[:], -1.0, ALU.add, -1000.0, ALU.mult)
            kb.tt("dve", be[:], be[:], vb[:], ALU.add)
            dg = qsb("ms_dg", [128, 128])
            kb.ts("dve", dg[:], ident, be[:, 0:1], ALU.mult)
            kb.mm(Cp[:, 128:256], onesf, dg[:])
            bef = qsb("ms_bef", [128, 128])
            kb.ts("dve", bef[:], Cp[:, 128:256], 128.0, ALU.mult, pidx[:, 0:1], ALU.add)
            if L > 0:
                kb.ts("dve", bef[:], bef[:], float(L * 32 * 128), ALU.add)
            kb.cp("dve", idxW[:], bef[:])
            kb.barrier()
        pr.close()
        with ExitStack() as p3:
            qsb = lambda name, shape, dt=F32: p3.enter_context(nc.sbuf_tensor(uq(name), shape, dt))
            qps = lambda name, shape, dt=F32: p3.enter_context(nc.psum_tensor(uq(name), shape, dt))
            hk = [qsb(f"ms_hs{i}", [128, D]) for i in range(3)]
            tp = [qps(f"ms_tp{i}", [128, 512]) for i in range(4)]
            for i, tt in enumerate(tiles):
                h = hk[i % 3]
                tsl = slice(tt * 128, (tt + 1) * 128)
                for half in range(2):
                    p = tp[(2 * i + half) % 4]
                    for j in range(4):
                        k = half * 4 + j
                        kb.tr(p[:, j * 128:(j + 1) * 128], h2T[:, k, tsl].bitcast(F32), ident)
                    kb.cp("act" if half == 0 else "dve", h[:, half * 512:(half + 1) * 512], p[:])
                kb.idma(xs_d, bass.IndirectOffsetOnAxis(ap=D1i[:, i:i + 1], axis=0), h[:], None, reads=[h, D1i], writes=["xs"])
                kb.idma(xs_d, bass.IndirectOffsetOnAxis(ap=D2i[:, i:i + 1], axis=0), h[:], None, reads=[h, D2i], writes=["xs"])
            kb.barrier()
        p0.close()
        with ExitStack() as p4:
            qsb = lambda name, shape, dt=F32: p4.enter_context(nc.sbuf_tensor(uq(name), shape, dt))
            qps = lambda name, shape, dt=F32: p4.enter_context(nc.psum_tensor(uq(name), shape, dt))
            W1 = [qsb(f"ms_W1_{i}", [128, 8 * 512], F32R) for i in range(2)]
            W3 = [qsb(f"ms_W3_{i}", [128, 8 * 512], F32R) for i in range(2)]
            W2 = [qsb(f"ms_W2_{i}", [128, 4 * D], F32R) for i in range(2)]
            Xb = [qsb(f"ms_Xb{i}", [128, D]) for i in range(3)]
            XT = [qsb(f"ms_XT{i}", [128, 8, 128], F32R) for i in range(2)]
            SL = [qsb(f"ms_SL{i}", [128, 512]) for i in range(2)]
            Gt = [qsb(f"ms_Gt{i}", [128, 4, 128], F32R) for i in range(2)]
            Ys = [qsb("ms_Ys0", [128, D])] * 2
            tpp = [qps(f"ms_tq{i}", [128, 512]) for i in range(2)]
            hp1 = [qps("ms_h1", [128, 512])] * 2
            hp3 = [qps("ms_h3", [128, 512])] * 2
            ypp = [qps(f"ms_yp{i}", [128, 512]) for i in range(2)]
            xtp = [qps(f"ms_xq{i}", [128, 512]) for i in range(2)]
            def xload(i):
                if i < len(subs):
                    kb.dma("sp", Xb[i % 3][:], xs_d[subs[i][0]:subs[i][0] + 128, :], reads=["xs"], writes=[Xb[i % 3]])

            def stageA(row0, W1t, W3t, xq, xb3):
                for half in range(2):
                    p = xtp[half]
                    for j in range(4):
                        k = half * 4 + j
                        kb.tr(p[:, j * 128:(j + 1) * 128], Xb[xb3][:, k * 128:(k + 1) * 128], ident)
                    kb.cp("act" if half == 0 else "dve", XT[xq][:, half * 4:half * 4 + 4, :], p[:].rearrange("p (a b) -> p a b", b=128))
                w1v = W1t[:].rearrange("p (k c) -> p k c", c=512)
                w3v = W3t[:].rearrange("p (k c) -> p k c", c=512)
                for k in range(8):
                    kb.mm(hp1[xq][:], XT[xq][:, k, :], w1v[:, k, :], start=(k == 0), stop=(k == 7))
                    kb.mm(hp3[xq][:], XT[xq][:, k, :], w3v[:, k, :], start=(k == 0), stop=(k == 7))
                kb.act(SL[xq][:], hp1[xq][:], AF.Silu)
                kb.tt("dve", SL[xq][:], SL[xq][:], hp3[xq][:], ALU.mult)

            def stageB(row0, W2t, xq):
                w2v = W2t[:].rearrange("p (j c) -> p j c", c=D)
                gp_ = tpp[xq]
                for j in range(4):
                    kb.tr(gp_[:, j * 128:(j + 1) * 128], SL[xq][:, j * 128:(j + 1) * 128], ident)
                kb.cp("act", Gt[xq][:].rearrange("p a b -> p (a b)"), gp_[:])
                for half in range(2):
                    yp = ypp[half]
                    for j in range(4):
                        kb.mm(yp[:], Gt[xq][:, j, :], w2v[:, j, half * 512:(half + 1) * 512], start=(j == 0), stop=(j == 3))
                    kb.cp("act" if half == 0 else "dve", Ys[xq][:, half * 512:(half + 1) * 512], yp[:])
                kb.dma("sp", ys_d[row0:row0 + 128, :], Ys[xq][:], reads=[Ys[xq]], writes=["ys"])

            subs = []
            wcnt = 0
            for e in range(32):
                pb = wcnt % 2
                wcnt += 1

                def ld(e=e, pb=pb):
                    kb.dma("pool", W1[pb][:], G["w1_d"][L, e * 128:(e + 1) * 128, :], writes=[W1[pb]])
                    kb.dma("pool", W3[pb][:], G["w3_d"][L, e * 128:(e + 1) * 128, :], writes=[W3[pb]])
                    kb.dma("pool", W2[pb][:], G["w2_d"][L, e * 128:(e + 1) * 128, :], writes=[W2[pb]])
                for j in range(SLOT // 128):
                    subs.append((e * SLOT + j * 128, pb, ld if j == 0 else None))
            for b in range(NOV):
                pb = wcnt % 2
                wcnt += 1

                def ld(b=b, pb=pb):
                    off = bass.IndirectOffsetOnAxis(ap=idxW[:, b:b + 1], axis=0)
                    bnd = (L + 1) * 32 * 128 - 1
                    kb.idma(W1[pb][:], None, G["w1_d"].rearrange("l r c -> (l r) c"), off, reads=[idxW], writes=[W1[pb]], bounds=bnd)
                    kb.idma(W3[pb][:], None, G["w3_d"].rearrange("l r c -> (l r) c"), off, reads=[idxW], writes=[W3[pb]], bounds=bnd)
                    kb.idma(W2[pb][:], None, G["w2_d"].rearrange("l r c -> (l r) c"), off, reads=[idxW], writes=[W2[pb]], bounds=bnd)
                subs.append((32 * SLOT + b * 128, pb, ld))
            xload(0)
            xload(1)
            for i, (row0, pb, ld) in enumerate(subs):
                if ld is not None:
                    ld()
                xload(i + 2)
                stageA(row0, W1[pb], W3[pb], i % 2, i % 3)
                if i >= 1:
                    r1, pb1, _ = subs[i - 1]
                    stageB(r1, W2[pb1], (i - 1) % 2)
            r1, pb1, _ = subs[-1]
            stageB(r1, W2[pb1], (len(subs) - 1) % 2)
            kb.barrier()
        with ExitStack() as p5:
            qsb = lambda name, shape, dt=F32: p5.enter_context(nc.sbuf_tensor(uq(name), shape, dt))
            qps = lambda name, shape, dt=F32: p5.enter_context(nc.psum_tensor(uq(name), shape, dt))
            y1 = [qsb(f"ms_y1_{i}", [128, D]) for i in range(2)]
            y2 = [qsb(f"ms_y2_{i}", [128, D]) for i in range(2)]
            tq = [qps(f"ms_cq{i}", [128, 512]) for i in range(4)]
            for i, tt in enumerate(tiles):
                pb = i % 2
                s_ = 1 if tt * 128 < NCTX else 0
                kb.idma(y1[pb][:], None, ys_d, bass.IndirectOffsetOnAxis(ap=D1i[:, i:i + 1], axis=0), reads=["ys", D1i], writes=[y1[pb]])
                kb.idma(y2[pb][:], None, ys_d, bass.IndirectOffsetOnAxis(ap=D2i[:, i:i + 1], axis=0), reads=["ys", D2i], writes=[y2[pb]])
                kb.ts("dve", y1[pb][:], y1[pb][:], GA[:, i:i + 1], ALU.mult)
                kb.stt("dve", y1[pb][:], y2[pb][:], GB[:, i:i + 1], y1[pb][:], ALU.mult, ALU.add)
                for half in range(2):
                    p = tq[(2 * i + half) % 4]
                    for j in range(4):
                        k = half * 4 + j
                        kb.tr(p[:, j * 128:(j + 1) * 128], y1[pb][:, k * 128:(k + 1) * 128], ident)
                    for j in range(4):
                        k = half * 4 + j
                        kb.stt("dve", xT[:, k, tt * 128:(tt + 1) * 128], p[:, j * 128:(j + 1) * 128], modT[:, L, 40 + k, s_:s_ + 1],
                               xT[:, k, tt * 128:(tt + 1) * 128], ALU.mult, ALU.add)
            kb.barrier()


def phase_final(G):
    nc, kb = G["nc"], G["kb"]
    xT, onesr, ident, Vn = G["xT"], G["onesr"], G["ident"], G["Vn"]
    out_d = G["out_d"]
    with ExitStack() as ph:
        psb = lambda name, shape, dt=F32: ph.enter_context(nc.sbuf_tensor(uq(name), shape, dt))
        pps = lambda name, shape, dt=F32: ph.enter_context(nc.psum_tensor(uq(name), shape, dt))
        fo = psb("fn_fo", [128, 8, NLAT])
        emit_norm(nc, kb, xT, fo, [(t0, n, t0 - NCTX) for (t0, n) in CHUNKS[1:]],
                  lambda k, s_: Vn("final_g")[:, k:k + 1], None, onesr, D)
        ost = [psb(f"fn_ost{i}", [128, D]) for i in range(2)]
        tp = [pps(f"fn_tp{i}", [128, 512]) for i in range(4)]
        for tt in range(NLAT // 128):
            o = ost[tt % 2]
            for half in range(2):
                p = tp[(2 * tt + half) % 4]
                for j in range(4):
                    k = half * 4 + j
                    kb.tr(p[:, j * 128:(j + 1) * 128], fo[:, k, tt * 128:(tt + 1) * 128], ident)
                if half == 0:
                    kb.cp("dve", o[:, 0:512], p[:])
                else:
                    kb.cp("act", o[:, 512:1024], p[:])
            kb.dma("sp", out_d[tt * 128:(tt + 1) * 128, :], o[:], reads=[o], writes=["out"])
        kb.barrier()


def host_prep(inputs):
    f = lambda a: np.ascontiguousarray(np.asarray(a, dtype=np.float32))
    w_in = f(inputs["w_in"])
    perm = np.concatenate([np.arange(8, 16), np.arange(0, 8), np.arange(24, 32), np.arange(16, 24)]) + 640
    w_in_x = np.concatenate([w_in, w_in[:, :, 576:672], w_in[:, :, 576:640], w_in[:, :, perm]], axis=2)
    consts = np.concatenate([np.eye(128, dtype=np.float32), np.ones((128, 128), np.float32)], axis=1)
    shared = {"consts": consts, "mod_w": f(inputs["mod_w"]), "w_in_x": np.ascontiguousarray(w_in_x)}
    s5B = np.zeros((DEPTH, 2, 2, 8, 128, 128), np.float32)
    s5C = np.zeros((DEPTH, 2, 2, 8, 128, 128), np.float32)
    for ri, (bn, cn) in enumerate((("s5_b_re", "s5_c_re"), ("s5_b_im", "s5_c_im"))):
        bb = f(inputs[bn])
        cc = f(inputs[cn])
        for g in range(16):
            s_ = g // 2
            ct = s_ // 4
            q0 = (g - 2 * s_) * 64
            c0 = (g - 8 * ct) * 16
            s5B[:, :, ri, s_, c0:c0 + 16, q0:q0 + 64] = bb[:, :, g].transpose(0, 1, 3, 2)
            s5C[:, :, ri, s_, q0:q0 + 64, c0:c0 + 16] = cc[:, :, g].transpose(0, 1, 3, 2)
    shared["s5B"] = s5B
    shared["s5C"] = s5C
    shared["s5_glu_w"] = f(inputs["s5_glu_w"])
    rows = NLAT // 64
    row = np.repeat(np.arange(rows, dtype=np.float32), 64)
    col = np.tile(np.arange(64, dtype=np.float32), rows)
    inv = (np.float32(10000.0) ** (-np.arange(8, dtype=np.float32) / np.float32(8))).astype(np.float32)
    rope = np.zeros((128, 2, NLAT), np.float32)
    for r in range(32):
        i, axis, half = r % 8, r // 16, (r // 8) % 2
        ang = ((row if axis == 0 else col) * inv[i]).astype(np.float32)
        rope[64 + r, 0] = np.cos(ang)
        rope[64 + r, 1] = np.sin(ang) * (-1.0 if half == 0 else 1.0)
    shared["rope"] = rope
    wuq = f(inputs["mla_w_uq"]).reshape(DEPTH, 256, 8, 96)
    pperm = np.concatenate([np.arange(64), 64 + np.concatenate([np.arange(8, 16), np.arange(0, 8), np.arange(24, 32), np.arange(16, 24)])])
    shared["wq"] = np.ascontiguousarray(np.stack([wuq, wuq[:, :, :, pperm]], axis=2).reshape(DEPTH, 256, 2, 768))
    shared["mla_w_uk"] = f(inputs["mla_w_uk"])
    shared["hg_lb_logits"] = f(inputs["hg_lb_logits"]).reshape(1, DEPTH)
    shared["wp"] = np.ascontiguousarray(np.concatenate([f(inputs["w_pa"]), f(inputs["w_pb"]), f(inputs["w_pc"])], axis=1))
    shared["w_out"] = f(inputs["w_out"])
    shared["wr"] = np.ascontiguousarray(np.concatenate([f(inputs["moe_w_group"]), f(inputs["moe_w_expert"])], axis=2))
    shared["br"] = np.ascontiguousarray(np.concatenate([f(inputs["moe_b_group"]), f(inputs["moe_b_expert"])], axis=1).reshape(DEPTH, 1, 36))
    for nm_, kt in (("moe_w1", 8), ("moe_w3", 8), ("moe_w2", 4)):
        w_ = f(inputs[nm_])
        cols = w_.shape[-1]
        shared[nm_] = np.ascontiguousarray(w_.reshape(DEPTH, 32, kt, 128, cols).transpose(0, 1, 3, 2, 4)).reshape(DEPTH, 32 * 128, kt * cols)
    shared["mla_w_uv"] = f(inputs["mla_w_uv"])
    in_maps = []
    for b in range(8):
        vecs = np.zeros((NVBLK * 128, 128), np.float32)

        def put(name, arr):
            r0, n = VEC_ROWS[name]
            vecs[r0:r0 + n, :] = np.asarray(arr, np.float32).reshape(n, 128)
        put("c", inputs["c"][b]); put("c_ctx", inputs["c_ctx"]); put("final_g", inputs["final_norm_g"])
        for l in range(DEPTH):
            put(f"norm1_g{l}", inputs["norm1_g"][l]); put(f"norm2_g{l}", inputs["norm2_g"][l])
            put(f"mod_b{l}", inputs["mod_b"][l]); put(f"s5_d{l}", inputs["s5_d"][l])
            put(f"glu_b{l}", inputs["s5_glu_b"][l]); put(f"qa_g{l}", inputs["mla_qa_g"][l])
            put(f"kva_g{l}", inputs["mla_kva_g"][l])
            hg = np.zeros((4, 128), np.float32)
            hg[:, 0:64] = np.asarray(inputs["hg_norm_g"][l], np.float32).reshape(4, 64)
            put(f"hgn_g{l}", hg)
            for d in range(2):
                put(f"lam_re{l}{d}", inputs["s5_lam_re"][l, d]); put(f"lam_im{l}{d}", inputs["s5_lam_im"][l, d])
                put(f"lstep{l}{d}", np.repeat(np.asarray(inputs["s5_log_step"][l, d], np.float32), 64))
        m = dict(shared)
        m["x"] = f(inputs["x"][b]); m["ctx"] = f(inputs["ctx"][b]); m["vecs"] = vecs
        in_maps.append(m)
    return in_maps


def kernel(**inputs):
    in_maps = host_prep(inputs)
    nc = build_nc()
    res = run_bass_kernel_spmd(nc, in_maps, core_ids=list(range(8)))
    return np.stack([r["out"] for r in res.results], axis=0)
```

```python
import math
from contextlib import ExitStack

import numpy as np
import concourse.bass as bass
import concourse.mybir as mybir
from concourse.bass_utils import run_bass_kernel_spmd

F32 = mybir.dt.float32
F32R = mybir.dt.float32r
I32 = mybir.dt.int32
AF = mybir.ActivationFunctionType
ALU = mybir.AluOpType
AX = mybir.AxisListType

D = 1024
NCTX = 256
NLAT = 2048
NT = NCTX + NLAT
DEPTH = 2
EPS = 1e-6
CHUNKS = [(0, 256)] + [(256 + 512 * i, 512) for i in range(4)]
SLOT = 256
NOV = 35
NROWS = 32 * SLOT + NOV * 128

COLT = []
def _ct(name, start, width):
    COLT.append((name, start, width))
for i in range(2): _ct(f"s5u{i}", 0 + 128 * i, 128)
for i in range(2): _ct(f"cq{i}", 256 + 128 * i, 128)
_ct("ckv", 512, 128)
_ct("kpeA", 5792, 96)
_ct("kpeB", 5792 + 96, 96)
for i in range(4): _ct(f"hq{i}", 672 + 128 * i, 128)
for i in range(4): _ct(f"hf{i}", 1184 + 128 * i, 128)
for i in range(4): _ct(f"hb{i}", 1696 + 128 * i, 128)
for i in range(4): _ct(f"hi{i}", 2208 + 64 * i, 64)
for i in range(4): _ct(f"hg{i}", 2464 + 64 * i, 64)
N_NONGATE = len(COLT)
for b in range(3):
    for i in range(8): _ct(f"gate{b}_{i}", 2720 + 1024 * b + 128 * i, 128)
COLIDX = {n: i for i, (n, _, _) in enumerate(COLT)}
WINX = 5792 + 192

VEC_ROWS = {}
def _vr(name, n):
    VEC_ROWS[name] = (sum(v[1] for v in VEC_ROWS.values()), n)
_vr("c", 8); _vr("c_ctx", 8); _vr("final_g", 8)
for l in range(DEPTH):
    _vr(f"norm1_g{l}", 8); _vr(f"norm2_g{l}", 8); _vr(f"mod_b{l}", 48)
    _vr(f"s5_d{l}", 2); _vr(f"glu_b{l}", 2); _vr(f"qa_g{l}", 2); _vr(f"kva_g{l}", 1)
    _vr(f"hgn_g{l}", 4)
    for d in range(2):
        _vr(f"lam_re{l}{d}", 8); _vr(f"lam_im{l}{d}", 8); _vr(f"lstep{l}{d}", 8)
NVROWS = sum(v[1] for v in VEC_ROWS.values())
NVBLK = (NVROWS + 127) // 128


_UQ = [0]


def uq(name):
    _UQ[0] += 1
    return f"{name}~{_UQ[0]}"


class Buf:
    __slots__ = ("name", "w", "r")

    def __init__(self, name):
        self.name = name
        self.w = None
        self.r = {}


class KB:
    RING = 12

    def __init__(self, nc, es):
        self.nc, self.es = nc, es
        self.eng = dict(pe=nc.tensor, dve=nc.vector, act=nc.scalar, pool=nc.gpsimd, sp=nc.sync)
        self.psem, self.pcnt, self.nsem = {}, {}, 0
        for e in self.eng:
            self._new_psem(e)
        self.waited = {}
        self.rings = {}
        self.rpos = {}
        for q in ("sp", "pool", "act"):
            self.rings[q] = [[self._sem(f"dq_{q}{i}"), 0] for i in range(self.RING)]
            self.rpos[q] = 0
        self.bufs = {}
        self.ninstr = 0

    def _sem(self, name):
        self.nsem += 1
        return self.es.enter_context(self.nc.semaphore(name))

    def _new_psem(self, e):
        self.psem[e] = self._sem(f"p_{e}_{self.nsem}")
        self.pcnt[e] = 0

    def buf(self, name):
        b = self.bufs.get(name)
        if b is None:
            b = self.bufs[name] = Buf(name)
        return b

    def _wait(self, e, tok):
        sem, val, src = tok
        key = (e, id(sem))
        if self.waited.get(key, 0) >= val:
            return
        self.eng[e].wait_ge(sem, val)
        self.waited[key] = val

    def _sync(self, e, reads, writes):
        for b in reads:
            if b.w is not None:
                self._dep(e, b.w)
        for b in writes:
            if b.w is not None:
                self._dep(e, b.w)
            for t in b.r.values():
                self._dep(e, t)

    def _dep(self, e, tok):
        if e == "pe" and tok[2] == "pe":
            return
        self._wait(e, tok)

    def _commit(self, tok, reads, writes):
        for b in writes:
            b.w = tok
            b.r = {}
        for b in reads:
            b.r[tok[2]] = tok

    def op(self, e, fn, reads=(), writes=()):
        reads = [b if isinstance(b, Buf) else self.buf(self._nm(b)) for b in reads]
        writes = [b if isinstance(b, Buf) else self.buf(self._nm(b)) for b in writes]
        self._sync(e, reads, writes)
        ins = fn(self.eng[e])
        if self.pcnt[e] >= 20000:
            self._new_psem(e)
        self.pcnt[e] += 1
        ins.then_inc(self.psem[e], 1)
        self.ninstr += 1
        self._commit((self.psem[e], self.pcnt[e], e), reads, writes)

    def dma(self, q, out, in_, reads=(), writes=()):
        reads = [b if isinstance(b, Buf) else self.buf(self._nm(b)) for b in reads]
        writes = [b if isinstance(b, Buf) else self.buf(self._nm(b)) for b in writes]
        self._sync(q, reads, writes)
        slot = self.rings[q][self.rpos[q] % self.RING]
        self.rpos[q] += 1
        sem, cnt = slot
        if cnt > 0:
            self._wait(q, (sem, cnt, "dma"))
        self.eng[q].dma_start(out=out, in_=in_).then_inc(sem, 16)
        slot[1] = cnt + 16
        self.ninstr += 1
        self._commit((sem, cnt + 16, f"dma_{q}{(self.rpos[q] - 1) % self.RING}"), reads, writes)

    @staticmethod
    def _nm(x):
        if isinstance(x, str):
            return x
        if isinstance(x, tuple):
            return KB._nm(x[0]) + x[1]
        if hasattr(x, "tensor"):
            return x.tensor.name.split("~")[0]
        return x.name.split("~")[0]

    def _names(self, xs):
        return [self._nm(x) for x in xs if not isinstance(x, (int, float)) and x is not None]

    def tt(self, e, out, a, b, op, rn=(), wn=()):
        self.op(e, lambda g: g.tensor_tensor(out, a, b, op), reads=self._names([a, b]) + list(rn), writes=self._names([out]) + list(wn))

    def ts(self, e, out, a, s1, op0, s2=None, op1=None, rn=(), wn=()):
        if op1 is None:
            self.op(e, lambda g: g.tensor_scalar(out, a, s1, None, op0), reads=self._names([a, s1]) + list(rn), writes=self._names([out]) + list(wn))
        else:
            self.op(e, lambda g: g.tensor_scalar(out, a, s1, s2, op0, op1), reads=self._names([a, s1, s2]) + list(rn), writes=self._names([out]) + list(wn))

    def stt(self, e, out, a, sc_, b, op0, op1, rn=(), wn=()):
        self.op(e, lambda g: g.scalar_tensor_tensor(out, a, sc_, b, op0, op1), reads=self._names([a, sc_, b]) + list(rn), writes=self._names([out]) + list(wn))

    def act(self, out, in_, func, bias=None, scale=1.0, rn=(), wn=()):
        kw = {}
        if bias is not None:
            kw["bias"] = bias
        self.op("act", lambda g: g.activation(out, in_, func, scale=scale, **kw), reads=self._names([in_, bias, scale]) + list(rn), writes=self._names([out]) + list(wn))

    def cp(self, e, out, in_, rn=(), wn=()):
        if e == "act":
            self.op(e, lambda g: g.copy(out, in_), reads=self._names([in_]) + list(rn), writes=self._names([out]) + list(wn))
        else:
            self.op(e, lambda g: g.tensor_copy(out, in_), reads=self._names([in_]) + list(rn), writes=self._names([out]) + list(wn))

    def mm(self, out, lhsT, rhs, start=True, stop=True, rn=(), wn=()):
        self.op("pe", lambda g: g.matmul(out, lhsT, rhs, start=start, stop=stop), reads=self._names([lhsT, rhs]) + list(rn), writes=self._names([out]) + list(wn))

    def tr(self, out, in_, ident, rn=(), wn=()):
        self.op("pe", lambda g: g.transpose(out, in_, ident), reads=self._names([in_, ident]) + list(rn), writes=self._names([out]) + list(wn))

    def scan(self, out, d0, d1, init, op0, op1, rn=(), wn=()):
        self.op("dve", lambda g: g.tensor_tensor_scan(out, d0, d1, init, op0, op1), reads=self._names([d0, d1, init]) + list(rn), writes=self._names([out]) + list(wn))

    def recip(self, out, in_):
        self.op("dve", lambda g: g.reciprocal(out, in_), reads=self._names([in_]), writes=self._names([out]))

    def memset(self, e, out, val):
        self.op(e, lambda g: g.memset(out, val), reads=[], writes=self._names([out]))

    def barrier(self):
        toks = [(self.psem[o], self.pcnt[o], o) for o in self.eng if self.pcnt[o] > 0]
        for q in self.rings:
            for sem, cnt in self.rings[q]:
                if cnt > 0:
                    toks.append((sem, cnt, "dma"))
        for e in self.eng:
            for t in toks:
                if t[2] != e:
                    self._wait(e, t)

    def idma(self, out, out_off, in_, in_off, reads=(), writes=(), bounds=None):
        q = "pool"
        reads = [b if isinstance(b, Buf) else self.buf(self._nm(b)) for b in reads]
        writes = [b if isinstance(b, Buf) else self.buf(self._nm(b)) for b in writes]
        self._sync(q, reads, writes)
        slot = self.rings[q][self.rpos[q] % self.RING]
        self.rpos[q] += 1
        sem, cnt = slot
        if cnt > 0:
            self._wait(q, (sem, cnt, "dma"))
        if bounds is None:
            self.eng[q].indirect_dma_start(out=out, out_offset=out_off, in_=in_, in_offset=in_off).then_inc(sem, 16)
        else:
            if not hasattr(self, "_breg") or self._breg[0] != bounds:
                self._breg = (bounds, self.eng[q].to_reg(bounds))
            self.eng[q].indirect_dma_start(out=out, out_offset=out_off, in_=in_, in_offset=in_off,
                                           bounds_check=self._breg[1], oob_is_err=False).then_inc(sem, 16)
        slot[1] = cnt + 16
        self.ninstr += 1
        self._commit((sem, cnt + 16, f"dma_{q}{(self.rpos[q] - 1) % self.RING}"), reads, writes)

    def finish(self, bufs):
        for b in bufs:
            b = self.buf(b) if isinstance(b, str) else b
            if b.w is not None:
                self._wait("sp", b.w)


def build_nc(stop_after=None, debug=False):
    nc = bass.Bass("TRN2", target_bir_lowering=False)
    okind = "ExternalOutput" if debug else "Internal"
    x_d = nc.dram_tensor("x", [NLAT, D], F32, kind="ExternalInput").ap()
    ctx_d = nc.dram_tensor("ctx", [NCTX, D], F32, kind="ExternalInput").ap()
    vecs_d = nc.dram_tensor("vecs", [NVBLK * 128, 128], F32, kind="ExternalInput").ap()
    consts_d = nc.dram_tensor("consts", [128, 256], F32, kind="ExternalInput").ap()
    modw_d = nc.dram_tensor("mod_w", [DEPTH, D, 6 * D], F32, kind="ExternalInput").ap()
    winx_d = nc.dram_tensor("w_in_x", [DEPTH, D, WINX], F32, kind="ExternalInput").ap()
    out_d = nc.dram_tensor("out", [NLAT, D], F32, kind="ExternalOutput").ap()
    zT_d = nc.dram_tensor("zT", [len(COLT) * 128, NT], F32, kind=okind).ap()
    ybT_d = nc.dram_tensor("ybT", [D, NT], F32, kind=okind).ap()
    s5B_d = nc.dram_tensor("s5B", [DEPTH, 2, 2, 8, 128, 128], F32, kind="ExternalInput").ap()
    s5C_d = nc.dram_tensor("s5C", [DEPTH, 2, 2, 8, 128, 128], F32, kind="ExternalInput").ap()
    gluw_d = nc.dram_tensor("s5_glu_w", [DEPTH, 256, 256], F32, kind="ExternalInput").ap()
    rope_d = nc.dram_tensor("rope", [128, 2, NLAT], F32, kind="ExternalInput").ap()
    wq_d = nc.dram_tensor("wq", [DEPTH, 256, 2, 768], F32, kind="ExternalInput").ap()
    wuk_d = nc.dram_tensor("mla_w_uk", [DEPTH, 128, 512], F32, kind="ExternalInput").ap()
    wuv_d = nc.dram_tensor("mla_w_uv", [DEPTH, 128, 512], F32, kind="ExternalInput").ap()
    lbl_d = nc.dram_tensor("hg_lb_logits", [1, DEPTH], F32, kind="ExternalInput").ap()
    wp_d = nc.dram_tensor("wp", [DEPTH, D, D], F32, kind="ExternalInput").ap()
    wo_d = nc.dram_tensor("w_out", [DEPTH, D, D], F32, kind="ExternalInput").ap()
    wr_d = nc.dram_tensor("wr", [DEPTH, D, 36], F32, kind="ExternalInput").ap()
    br_d = nc.dram_tensor("br", [DEPTH, 1, 36], F32, kind="ExternalInput").ap()
    w1_d = nc.dram_tensor("moe_w1", [DEPTH, 32 * 128, 8 * 512], F32, kind="ExternalInput").ap()
    w3_d = nc.dram_tensor("moe_w3", [DEPTH, 32 * 128, 8 * 512], F32, kind="ExternalInput").ap()
    w2_d = nc.dram_tensor("moe_w2", [DEPTH, 32 * 128, 4 * D], F32, kind="ExternalInput").ap()
    h2tok_d = nc.dram_tensor("h2tok", [NT, D], F32, kind=okind).ap()
    xs_d = nc.dram_tensor("xs", [NROWS, D], F32, kind=okind).ap()
    ys_d = nc.dram_tensor("ys", [NROWS, D], F32, kind=okind).ap()
    xdbg_d = nc.dram_tensor("xdbg", [128, 8 * NT], F32, kind=okind).ap()
    modT_d = nc.dram_tensor("modT", [DEPTH, 128, 96], F32, kind=okind).ap()

    with ExitStack() as es:
        kb = KB(nc, es)
        sb = lambda name, shape, dt=F32: es.enter_context(nc.sbuf_tensor(uq(name), shape, dt))

        xT = sb("xT", [128, 8, NT])
        cst = sb("cst", [128, 256])
        ident = cst[:, 0:128]
        onesr = sb("onesr", [128, 128], F32R)
        vecT = sb("vecT", [128, NVBLK * 128])
        modT = sb("modT_sb", [128, DEPTH, 48, 2])
        modA = sb("modA", [128, DEPTH, 2, 8, 2])
        sc = sb("sc", [128, 8, 2], F32R)

        def V(name, j=0):
            r0, n = VEC_ROWS[name]
            return vecT[:, r0 + j:r0 + j + 1]

        def Vn(name):
            r0, n = VEC_ROWS[name]
            return vecT[:, r0:r0 + n]

        kb.dma("sp", cst[:], consts_d, writes=["cst"])
        kb.dma("pool", onesr[:], consts_d[:, 128:256], writes=["onesr"])

        with ExitStack() as ph:
            psb = lambda name, shape, dt=F32: ph.enter_context(nc.sbuf_tensor(uq(name), shape, dt))
            pps = lambda name, shape, dt=F32: ph.enter_context(nc.psum_tensor(uq(name), shape, dt))
            stg = [psb(f"xstg{i}", [128, D]) for i in range(3)]
            tps = [pps(f"tps{i}", [128, 4, 128]) for i in range(4)]
            for blk in range(NVBLK):
                s = stg[blk % 3]
                kb.dma("sp", s[:, 0:128], vecs_d[blk * 128:(blk + 1) * 128, :], writes=[f"xstg{blk % 3}"])
                kb.op("pe", lambda e: e.transpose(tps[blk % 4][:, 0, :], s[:, 0:128], ident),
                      reads=[f"xstg{blk % 3}", "cst"], writes=[f"tps{blk % 4}"])
                kb.op("dve", lambda e: e.tensor_copy(vecT[:, blk * 128:(blk + 1) * 128], tps[blk % 4][:, 0, :]),
                      reads=[f"tps{blk % 4}"], writes=["vecT"])
            n_tt = NT // 128
            for tt in range(n_tt):
                s = stg[tt % 3]
                src = ctx_d[tt * 128:(tt + 1) * 128, :] if tt < 2 else x_d[(tt - 2) * 128:(tt - 1) * 128, :]
                kb.dma("sp", s[:], src, writes=[f"xstg{tt % 3}"])
                for half in range(2):
                    pi = (2 * tt + half) % 4
                    for j in range(4):
                        k = half * 4 + j
                        kb.op("pe", lambda e: e.transpose(tps[pi][:, j, :], s[:, k * 128:(k + 1) * 128], ident),
                              reads=[f"xstg{tt % 3}", "cst"], writes=[f"tps{pi}"])
                    eng = "dve" if half == 0 else "act"
                    dst = xT[:, half * 4:half * 4 + 4, tt * 128:(tt + 1) * 128]
                    if eng == "dve":
                        kb.op("dve", lambda e: e.tensor_copy(dst, tps[pi][:]), reads=[f"tps{pi}"], writes=["xT"])
                    else:
                        kb.op("act", lambda e: e.copy(dst, tps[pi][:]), reads=[f"tps{pi}"], writes=["xT"])
            kb.barrier()

        kb.op("act", lambda e: e.activation(sc[:, :, 0], Vn("c"), AF.Silu), reads=["vecT"], writes=["sc"])
        kb.op("act", lambda e: e.activation(sc[:, :, 1], Vn("c_ctx"), AF.Silu), reads=["vecT"], writes=["sc"])

        xTd_d = nc.dram_tensor("xTd", [128, 8 * NT], F32, kind=okind).ap()
        if debug and stop_after == "p0":
            kb.dma("sp", xTd_d, xT[:].rearrange("p a b -> p (a b)"), reads=["xT"], writes=["xTd"])
        lbv = sb("lbv", [128, 2, DEPTH])
        lbl = sb("lbl", [128, DEPTH])
        kb.dma("sp", lbl[:], lbl_d.to_broadcast([128, DEPTH]), writes=["lbl"])
        kb.memset("dve", lbv[:, 0, :], 0.0)
        kb.tt("dve", lbv[:, 0, 1:2], lbl[:, 1:2], lbl[:, 0:1], ALU.subtract)
        kb.act(lbv[:, 0, 1:2], lbv[:, 0, 1:2], AF.Sigmoid)
        kb.ts("dve", lbv[:, 1, :], lbv[:, 0, :], -1.0, ALU.mult, 1.0, ALU.add)
        for layer in range(DEPTH if stop_after != "p0" else 0):
            L = layer
            with ExitStack() as ph:
                psb = lambda name, shape, dt=F32: ph.enter_context(nc.sbuf_tensor(uq(name), shape, dt))
                pps = lambda name, shape, dt=F32: ph.enter_context(nc.psum_tensor(uq(name), shape, dt))
                mw = [psb(f"mw{i}", [128, 8, 128], F32R) for i in range(3)]
                mps = pps("mps", [128, 48, 2])
                for j in range(48):
                    w = mw[j % 3]
                    kb.dma("pool", w[:], modw_d[L, :, j * 128:(j + 1) * 128].rearrange("(k p) c -> p k c", p=128),
                           writes=[f"mw{j % 3}"])
                    for k in range(8):
                        kb.op("pe", lambda e: e.matmul(mps[:, j, :], w[:, k, :], sc[:, k, :], start=(k == 0), stop=(k == 7)),
                              reads=[f"mw{j % 3}", "sc"], writes=["mps"])
                for s in range(2):
                    kb.op("dve", lambda e: e.tensor_tensor(modT[:, L, :, s], mps[:, :, s], Vn(f"mod_b{L}"), ALU.add),
                          reads=["mps", "vecT"], writes=["modT"])
                for ni, (gname, t0) in enumerate(((f"norm1_g{L}", 8), (f"norm2_g{L}", 32))):
                    for s in range(2):
                        kb.op("dve", lambda e: e.scalar_tensor_tensor(
                            modA[:, L, ni, :, s], modT[:, L, t0:t0 + 8, s], 1.0, Vn(gname), ALU.add, ALU.mult),
                            reads=["modT", "vecT"], writes=["modA"])
                if debug:
                    kb.dma("sp", modT_d[L], modT[:, L].rearrange("p a b -> p (a b)"), reads=["modT"], writes=["modT_d"])
                kb.barrier()
            if stop_after == f"mod{L}":
                break

            with ExitStack() as ph:
                psb = lambda name, shape, dt=F32: ph.enter_context(nc.sbuf_tensor(uq(name), shape, dt))
                pps = lambda name, shape, dt=F32: ph.enter_context(nc.psum_tensor(uq(name), shape, dt))
                hT = psb("hT", [128, 8, NT], F32R)
                emit_norm(nc, kb, xT, hT, [(t0, n, t0) for (t0, n) in CHUNKS],
                          lambda k, s_: modA[:, L, 0, k, s_:s_ + 1], lambda k, s_: modT[:, L, k, s_:s_ + 1], onesr, D)
                wb = [psb(f"wb{i}", [128, 8, 128], F32R) for i in range(3)]
                zs = [psb(f"zs{i}", [128, 512]) for i in range(4)]
                zp = [pps(f"zp{i}", [128, 512]) for i in range(4)]
                cnt = 0
                for ti, (name, c0, wd) in enumerate(COLT):
                    w = wb[ti % 3]
                    kb.dma("pool", w[:, :, 0:wd], winx_d[L, :, c0:c0 + wd].rearrange("(k p) c -> p k c", p=128),
                           writes=[f"wb{ti % 3}"])
                    for (t0, n) in CHUNKS:
                        pi = cnt % 4
                        cnt += 1
                        for k in range(8):
                            kb.op("pe", lambda e: e.matmul(zp[pi][0:wd, 0:n], w[:, k, 0:wd], hT[:, k, t0:t0 + n],
                                                           start=(k == 0), stop=(k == 7)),
                                  reads=[f"wb{ti % 3}", "hT"], writes=[f"zp{pi}"])
                        if cnt % 2 == 0:
                            kb.op("dve", lambda e: e.tensor_copy(zs[pi][0:wd, 0:n], zp[pi][0:wd, 0:n]),
                                  reads=[f"zp{pi}"], writes=[f"zs{pi}"])
                        else:
                            kb.op("act", lambda e: e.copy(zs[pi][0:wd, 0:n], zp[pi][0:wd, 0:n]),
                                  reads=[f"zp{pi}"], writes=[f"zs{pi}"])
                        kb.dma("sp", zT_d[ti * 128:ti * 128 + wd, t0:t0 + n], zs[pi][0:wd, 0:n],
                               reads=[f"zs{pi}"], writes=[f"zT_{ti}"])
                kb.barrier()
            if stop_after == f"A{L}":
                break
            G = dict(nc=nc, kb=kb, L=L, xT=xT, cst=cst, ident=ident, onesr=onesr, vecT=vecT, modT=modT, modA=modA,
                     V=V, Vn=Vn, zT_d=zT_d, ybT_d=ybT_d, s5B_d=s5B_d, s5C_d=s5C_d, gluw_d=gluw_d, debug=debug,
                     rope_d=rope_d, wq_d=wq_d, wuk_d=wuk_d, wuv_d=wuv_d)
            if not (debug and stop_after in (f"C{L}", f"D{L}")):
                phase_s5(G)
            if stop_after == f"B{L}":
                break
            G["lbv"] = lbv
            if not (debug and stop_after in (f"D{L}",)):
                phase_mla(G)
            if stop_after == f"C{L}":
                break
            G.update(wp_d=wp_d, wo_d=wo_d, wr_d=wr_d, br_d=br_d, w1_d=w1_d, w3_d=w3_d, w2_d=w2_d, out_d=out_d,
                     h2tok_d=h2tok_d, xs_d=xs_d, ys_d=ys_d)
            phase_hg(G)
            if stop_after == f"D{L}":
                break
            phase_merge(G)
            if stop_after == f"E{L}":
                kb.dma("sp", xdbg_d, xT[:].rearrange("p a b -> p (a b)"), reads=["xT"], writes=["xdbg"])
                break
            phase_moe_sparse(G)
            if stop_after == f"F{L}":
                kb.dma("sp", xdbg_d, xT[:].rearrange("p a b -> p (a b)"), reads=["xT"], writes=["xdbg"])
                break
        else:
            phase_final(G)

        kb.finish(list(kb.bufs.values()))
        print("instructions:", kb.ninstr, "sems:", kb.nsem, "sbuf left:", nc.sbuf_bytes_remaining)
    return nc


def emit_norm(nc, kb, xT, dst, chunks, A, Sh, onesr, dmodel):
    with ExitStack() as ns:
        psb = lambda name, shape, dt=F32: ns.enter_context(nc.sbuf_tensor(uq(name), shape, dt))
        pps = lambda name, shape, dt=F32: ns.enter_context(nc.psum_tensor(uq(name), shape, dt))
        sq = [psb(f"nsq{i}", [128, 8, 512], F32R) for i in range(2)]
        ms = [pps(f"nms{i}", [128, 512]) for i in range(2)]
        rs = [psb(f"nrs{i}", [128, 512]) for i in range(2)]
        tmp = [psb(f"ntmp{i}", [128, 512]) for i in range(2)]
        for ci, (t0, n, d0) in enumerate(chunks):
            s = 1 if t0 < NCTX else 0
            b = ci % 2
            for k in range(8):
                kb.act(sq[b][:, k, 0:n], xT[:, k, t0:t0 + n], AF.Square)
            for k in range(8):
                kb.mm(ms[b][:, 0:n], onesr[:], sq[b][:, k, 0:n], start=(k == 0), stop=(k == 7))
            kb.act(rs[b][:, 0:n], ms[b][:, 0:n], AF.Sqrt, scale=1.0 / dmodel, bias=EPS)
            kb.recip(rs[b][:, 0:n], rs[b][:, 0:n])
            for k in range(8):
                tb = k % 2
                sh = Sh(k, s) if Sh is not None else None
                if sh is None:
                    kb.stt("dve", dst[:, k, d0:d0 + n], xT[:, k, t0:t0 + n], A(k, s), rs[b][:, 0:n], ALU.mult, ALU.mult)
                else:
                    kb.stt("dve", tmp[tb][:, 0:n], xT[:, k, t0:t0 + n], A(k, s), rs[b][:, 0:n], ALU.mult, ALU.mult)
                    kb.act(dst[:, k, d0:d0 + n], tmp[tb][:, 0:n], AF.Identity, bias=sh, scale=1.0)
        kb.barrier()


TWO_PI = 2.0 * math.pi


def range_reduce(kb, r, x, tM, tI):
    kb.ts("dve", tM, x, 1.0 / TWO_PI, ALU.mult)
    kb.cp("dve", tI, tM)
    kb.cp("dve", tM, tI)
    kb.stt("dve", r, tM, -TWO_PI, x, ALU.mult, ALU.add)
    kb.ts("dve", tM, r, math.pi, ALU.is_gt)
    kb.stt("dve", r, tM, -TWO_PI, r, ALU.mult, ALU.add)
    kb.ts("dve", tM, r, -math.pi, ALU.is_lt)
    kb.stt("dve", r, tM, TWO_PI, r, ALU.mult, ALU.add)
    kb.ts("dve", r, r, 3.1415925, ALU.min, -3.1415925, ALU.max)


def sincos(kb, sn, cs, x, r, tM, tI):
    range_reduce(kb, r, x, tM, tI)
    kb.act(sn, r, AF.Sin)
    kb.ts("dve", tM, x, math.pi / 2, ALU.add)
    range_reduce(kb, r, tM, tM, tI) if False else None
    return


def phase_s5(G):
    nc, kb, L = G["nc"], G["kb"], G["L"]
    Vn = G["Vn"]
    zT_d, ybT_d = G["zT_d"], G["ybT_d"]
    T = 256
    NCH = NT // T
    with ExitStack() as ph:
        psb = lambda name, shape, dt=F32: ph.enter_context(nc.sbuf_tensor(uq(name), shape, dt))
        pps = lambda name, shape, dt=F32: ph.enter_context(nc.psum_tensor(uq(name), shape, dt))
        uT = psb("s5_uT", [128, 2, NT], F32R)
        yacc = psb("s5_yacc", [128, 2, NT])
        for t in range(2):
            ti = COLIDX[f"s5u{t}"]
            kb.dma("pool", uT[:, t, :], zT_d[ti * 128:(ti + 1) * 128, :], reads=[f"zT_{ti}"], writes=["s5_uT"])
        Bw = psb("s5_Bw", [128, 2, 8, 128], F32R)
        Cw = psb("s5_Cw", [128, 2, 8, 128], F32R)
        iota_i = psb("s5_iota_i", [128, T + 1], I32)
        iota_f = psb("s5_iota_f", [128, T + 1])
        kb.op("pool", lambda g: g.iota(iota_i[:], [[1, T + 1]], base=0, channel_multiplier=0), writes=["s5_iota_i"])
        kb.cp("dve", iota_f[:], iota_i[:])
        COS = psb("s5_COS", [128, 8, T + 1])
        SIN = psb("s5_SIN", [128, 8, T + 1])
        ang = psb("s5_ang", [128, 8, T + 1])
        rr = psb("s5_rr", [128, 8, T + 1])
        ERE = ang[:, :, 0:T]
        EIM = rr[:, :, 0:T]
        tM = psb("s5_tM", [128, 8, T + 1])
        tI = psb("s5_tI", [128, 8, T + 1], I32)
        sm = psb("s5_sm", [128, 24, 8])
        gin = psb("s5_gin", [128, 8, 2])
        tmp = [[psb(f"s5_t{b}_{i}", [128, T]) for i in range(6)] for b in range(3)]
        tmpB = [[psb(f"s5_g{b}_{i}", [128, T]) for i in range(2)] for b in range(2)]
        hh = [[psb(f"s5_h{b}_{i}", [128, T], F32R) for i in range(2)] for b in range(2)]
        Pp = [pps(f"s5_P{i}", [128, 2, T]) for i in range(3)]
        Yp = [[pps(f"s5_Y{b}_{ct}", [128, 512]) for ct in range(2)] for b in range(2)]
        flat = lambda t: t[:].rearrange("p a b -> p (a b)")
        for d in range(2):
            kb.dma("pool", Bw[:].rearrange("c r s q -> c (r s) q"),
                   G["s5B_d"][L, d].rearrange("r s c q -> c (r s) q"), writes=["s5_Bw"])
            kb.dma("pool", Cw[:].rearrange("c r s q -> c (r s) q"),
                   G["s5C_d"][L, d].rearrange("r s c q -> c (r s) q"), writes=["s5_Cw"])
            kb.ts("pool", Cw[:, 1], Cw[:, 1].bitcast(F32), -1.0, ALU.mult)
            lre, lim, lst = Vn(f"lam_re{L}{d}"), Vn(f"lam_im{L}{d}"), Vn(f"lstep{L}{d}")
            c_ = lambda i: sm[:, i, :]
            DT, MAG, TH, SN, CS, LBR, LBI, DEN, FR, FI, X1, X2, X3, MI = (c_(i) for i in range(14))
            kb.act(DT, lst, AF.Exp)
            kb.tt("dve", X1, lre, DT, ALU.mult)
            kb.act(MAG, X1, AF.Exp)
            kb.tt("dve", TH, lim, DT, ALU.mult)
            smI = tI[:, 0, 0:8]
            range_reduce(kb, X2, TH, X3, smI)
            kb.act(SN, X2, AF.Sin)
            kb.ts("dve", X1, TH, math.pi / 2, ALU.add)
            range_reduce(kb, X2, X1, X3, smI)
            kb.act(CS, X2, AF.Sin)
            kb.tt("dve", LBR, MAG, CS, ALU.mult)
            kb.tt("dve", LBI, MAG, SN, ALU.mult)
            kb.tt("dve", X1, lre, lre, ALU.mult)
            kb.tt("dve", X2, lim, lim, ALU.mult)
            kb.tt("dve", DEN, X1, X2, ALU.add)
            kb.recip(DEN, DEN)
            kb.ts("dve", X3, LBR, -1.0, ALU.add)
            kb.tt("dve", X1, X3, lre, ALU.mult)
            kb.tt("dve", X2, LBI, lim, ALU.mult)
            kb.tt("dve", X1, X1, X2, ALU.add)
            kb.tt("dve", FR, X1, DEN, ALU.mult)
            kb.tt("dve", X1, LBI, lre, ALU.mult)
            kb.tt("dve", X2, X3, lim, ALU.mult)
            kb.tt("dve", X1, X1, X2, ALU.subtract)
            kb.tt("dve", FI, X1, DEN, ALU.mult)
            kb.tt("dve", ang[:], TH.unsqueeze(2).to_broadcast([128, 8, T + 1]),
                  iota_f[:].unsqueeze(1).to_broadcast([128, 8, T + 1]), ALU.mult)
            range_reduce(kb, flat(rr), flat(ang), flat(tM), flat(tI))
            kb.act(flat(SIN), flat(rr), AF.Sin)
            kb.ts("dve", flat(ang), flat(ang), math.pi / 2, ALU.add)
            range_reduce(kb, flat(rr), flat(ang), flat(tM), flat(tI))
            kb.act(flat(COS), flat(rr), AF.Sin)
            frb = FR.unsqueeze(2).to_broadcast([128, 8, T])
            fib = FI.unsqueeze(2).to_broadcast([128, 8, T])
            tF = tI[:].bitcast(F32)
            kb.tt("dve", tM[:, :, 0:T], COS[:, :, 0:T], frb, ALU.mult)
            kb.tt("dve", tF[:, :, 0:T], SIN[:, :, 0:T], fib, ALU.mult)
            kb.tt("dve", ERE, tM[:, :, 0:T], tF[:, :, 0:T], ALU.add)
            kb.tt("dve", tM[:, :, 0:T], COS[:, :, 0:T], fib, ALU.mult)
            kb.tt("dve", tF[:, :, 0:T], SIN[:, :, 0:T], frb, ALU.mult)
            kb.tt("dve", EIM, tM[:, :, 0:T], tF[:, :, 0:T], ALU.subtract)
            kb.memset("dve", gin[:], 0.0)
            NST = sm[:, 18, :]
            kb.ts("dve", NST, SIN[:, :, T], -1.0, ALU.mult)
            order = list(range(NCH)) if d == 0 else [0] + list(range(NCH - 1, 0, -1))
            units = [(oi, ci, s_) for oi, ci in enumerate(order) for s_ in range(8)]

            def stageA(u):
                oi, ci, s_ = units[u]
                t0 = ci * T
                ct = s_ // 4
                tq = tmp[u % 3]
                P = Pp[u % 3]
                kb.mm(P[:, 0, :], Bw[:, 0, s_, :], uT[:, ct, t0:t0 + T])
                kb.mm(P[:, 1, :], Bw[:, 1, s_, :], uT[:, ct, t0:t0 + T])
                Pre = P[:, 0, ::-1] if d == 1 else P[:, 0, :]
                Pim = P[:, 1, ::-1] if d == 1 else P[:, 1, :]
                kb.tt("dve", tq[0][:], ERE[:, s_, :], Pre, ALU.mult)
                kb.tt("dve", tq[1][:], EIM[:, s_, :], Pim, ALU.mult)
                kb.tt("pool", tq[4][:], tq[0][:], tq[1][:], ALU.subtract)
                kb.tt("dve", tq[2][:], ERE[:, s_, :], Pim, ALU.mult)
                kb.tt("dve", tq[3][:], EIM[:, s_, :], Pre, ALU.mult)
                kb.tt("pool", tq[5][:], tq[2][:], tq[3][:], ALU.add)

            def stageB(u):
                oi, ci, s_ = units[u]
                t0 = ci * T
                ct = s_ // 4
                yb_ = oi % 2
                tq = tmp[u % 3]
                gq = tmpB[u % 2]
                hb = hh[u % 2]
                rb = MAG[:, s_:s_ + 1].to_broadcast([128, T])
                kb.scan(gq[0][:], rb, tq[4][:], gin[:, s_, 0:1], ALU.mult, ALU.add)
                kb.scan(gq[1][:], rb, tq[5][:], gin[:, s_, 1:2], ALU.mult, ALU.add)
                cT, sT = COS[:, s_, T:T + 1], SIN[:, s_, T:T + 1]
                lr, li = gq[0][:, T - 1:T], gq[1][:, T - 1:T]
                xa, xb = sm[:, 14 + (u % 2) * 2, 0:1], sm[:, 15 + (u % 2) * 2, 0:1]
                kb.act(xa, li, AF.Identity, scale=NST[:, s_:s_ + 1])
                kb.act(xb, li, AF.Identity, scale=cT)
                kb.act(gin[:, s_, 0:1], lr, AF.Identity, bias=xa, scale=cT)
                kb.act(gin[:, s_, 1:2], lr, AF.Identity, bias=xb, scale=sT)
                kb.tt("pool", tq[0][:], COS[:, s_, 0:T], gq[0][:], ALU.mult)
                kb.tt("pool", tq[1][:], SIN[:, s_, 0:T], gq[1][:], ALU.mult)
                kb.tt("pool", hb[0][:], tq[0][:], tq[1][:], ALU.subtract)
                kb.tt("dve", tq[2][:], SIN[:, s_, 0:T], gq[0][:], ALU.mult)
                kb.tt("dve", tq[3][:], COS[:, s_, 0:T], gq[1][:], ALU.mult)
                kb.tt("pool", hb[1][:], tq[2][:], tq[3][:], ALU.add)
                Y = Yp[yb_][ct]
                kb.mm(Y[:, 0:T], Cw[:, 0, s_, :], hb[0][:], start=(s_ % 4 == 0), stop=False)
                kb.mm(Y[:, 0:T], Cw[:, 1, s_, :], hb[1][:], start=False, stop=(s_ % 4 == 3))
                if s_ == 7:
                    for ct2 in range(2):
                        Y2 = Yp[yb_][ct2]
                        if d == 0:
                            kb.cp("act", yacc[:, ct2, t0:t0 + T], Y2[:, 0:T])
                        else:
                            rv = slice(t0 + T - 1, (t0 - 1 if t0 > 0 else None), -1)
                            kb.tt("dve", yacc[:, ct2, rv], yacc[:, ct2, rv], Y2[:, 0:T], ALU.add)

            stageA(0)
            stageA(1)
            for u in range(len(units)):
                if u + 2 < len(units):
                    stageA(u + 2)
                stageB(u)
        gw = psb("s5_gw", [128, 2, 256], F32R)
        kb.dma("pool", gw[:], G["gluw_d"][L].rearrange("(k p) c -> p k c", p=128), writes=["s5_gw"])
        y1t = [hh[0][0], hh[0][1]]
        for ci in range(NCH):
            sl = slice(ci * T, (ci + 1) * T)
            for ct in range(2):
                a, b2, c2 = tmp[ct][0], tmp[ct][1], tmp[ct][2]
                kb.stt("dve", a[:], uT[:, ct, sl].bitcast(F32), Vn(f"s5_d{L}")[:, ct:ct + 1], yacc[:, ct, sl], ALU.mult, ALU.add)
                kb.tt("dve", b2[:], a[:], a[:], ALU.mult)
                kb.ts("dve", b2[:], b2[:], 0.044715, ALU.mult, 1.0, ALU.add)
                kb.tt("dve", b2[:], b2[:], a[:], ALU.mult)
                kb.act(c2[:], b2[:], AF.Sigmoid, scale=1.5957691216057308)
                kb.tt("dve", y1t[ct][:], a[:], c2[:], ALU.mult)
            for ct in range(2):
                Y = Yp[ci % 2][ct]
                for k in range(2):
                    kb.mm(Y[:, 0:T], gw[:, k, ct * 128:(ct + 1) * 128], y1t[k][:], start=(k == 0), stop=(k == 1))
                sg = tmp[ct][3]
                o = tmp[ct][4]
                kb.act(sg[:], Y[:, 0:T], AF.Sigmoid, bias=Vn(f"glu_b{L}")[:, ct:ct + 1])
                kb.tt("dve", o[:], y1t[ct][:].bitcast(F32), sg[:], ALU.mult)
                kb.dma("sp", ybT_d[ct * 128:(ct + 1) * 128, sl], o[:], reads=[o], writes=[f"ybT_{ct}"])
        kb.barrier()


MLA_SCALE = 1.0 / math.sqrt(96.0)


def phase_mla(G):
    nc, kb, L = G["nc"], G["kb"], G["L"]
    Vn, cst, onesr = G["Vn"], G["cst"], G["onesr"]
    zT_d, ybT_d = G["zT_d"], G["ybT_d"]
    need_ctx = L < DEPTH - 1
    with ExitStack() as ph:
        psb = lambda name, shape, dt=F32: ph.enter_context(nc.sbuf_tensor(uq(name), shape, dt))
        pps = lambda name, shape, dt=F32: ph.enter_context(nc.psum_tensor(uq(name), shape, dt))
        cqn = psb("ml_cqn", [128, 2, NT], F32R)
        ckvn = psb("ml_ckvn", [128, NT], F32R)
        KPE = psb("ml_KPE", [128, NT])
        ROPE = psb("ml_rope", [128, 2, NLAT])
        wq = psb("ml_wq", [128, 2, 2, 768], F32R)
        wuk = psb("ml_wuk", [128, 512], F32R)
        wuv = psb("ml_wuv", [128, 512], F32R)
        kb.dma("sp", ROPE[:], G["rope_d"], writes=["ml_rope"])
        kb.dma("pool", wq[:].rearrange("p k v c -> p k (v c)"),
               G["wq_d"][L].rearrange("(k p) v c -> p k (v c)", p=128), writes=["ml_wq"])
        kb.dma("pool", wuk[:], G["wuk_d"][L], writes=["ml_wuk"])
        kb.dma("pool", wuv[:], G["wuv_d"][L], writes=["ml_wuv"])
        ps = [pps(f"ml_ps{i}", [128, 512]) for i in range(8)]
        with ExitStack() as p1:
            qsb = lambda name, shape, dt=F32: p1.enter_context(nc.sbuf_tensor(uq(name), shape, dt))
            cqT = qsb("ml_cqT", [128, 2, NT])
            ckvT = qsb("ml_ckvT", [128, NT])
            kA = qsb("ml_kA", [128, NT])
            kB = qsb("ml_kB", [128, NT])
            for t in range(2):
                ti = COLIDX[f"cq{t}"]
                kb.dma("sp", cqT[:, t, :], zT_d[ti * 128:(ti + 1) * 128, :], reads=[f"zT_{ti}"], writes=["ml_cqT"])
            ti = COLIDX["ckv"]
            kb.dma("sp", ckvT[:], zT_d[ti * 128:(ti + 1) * 128, :], reads=[f"zT_{ti}"], writes=["ml_ckvT"])
            for nm_, tl in (("kpeA", kA), ("kpeB", kB)):
                ti = COLIDX[nm_]
                kb.dma("sp", tl[0:96, :], zT_d[ti * 128:ti * 128 + 96, :], reads=[f"zT_{ti}"], writes=[tl])
            sq = qsb("ml_sq", [128, 3, 512], F32R)
            rs = [qsb(f"ml_rs{i}", [128, 512]) for i in range(2)]
            tt1 = qsb("ml_tt1", [128, 512])
            tt2 = qsb("ml_tt2", [128, 512])
            for (t0, n) in CHUNKS:
                for t in range(2):
                    kb.act(sq[:, t, 0:n], cqT[:, t, t0:t0 + n], AF.Square)
                kb.act(sq[:, 2, 0:n], ckvT[:, t0:t0 + n], AF.Square)
                for t in range(2):
                    kb.mm(ps[0][:, 0:n], onesr[:], sq[:, t, 0:n], start=(t == 0), stop=(t == 1))
                kb.mm(ps[1][:, 0:n], onesr[:], sq[:, 2, 0:n])
                kb.act(rs[0][:, 0:n], ps[0][:, 0:n], AF.Sqrt, scale=1.0 / 256, bias=EPS)
                kb.recip(rs[0][:, 0:n], rs[0][:, 0:n])
                kb.act(rs[1][:, 0:n], ps[1][:, 0:n], AF.Sqrt, scale=1.0 / 128, bias=EPS)
                kb.recip(rs[1][:, 0:n], rs[1][:, 0:n])
                for t in range(2):
                    kb.stt("dve", cqn[:, t, t0:t0 + n], cqT[:, t, t0:t0 + n], Vn(f"qa_g{L}")[:, t:t + 1], rs[0][:, 0:n], ALU.mult, ALU.mult)
                kb.stt("dve", ckvn[:, t0:t0 + n], ckvT[:, t0:t0 + n], Vn(f"kva_g{L}")[:, 0:1], rs[1][:, 0:n], ALU.mult, ALU.mult)
                if t0 < NCTX:
                    kb.cp("dve", KPE[64:96, t0:t0 + n], kA[64:96, t0:t0 + n])
                else:
                    l0 = t0 - NCTX
                    kb.tt("dve", tt1[64:96, 0:n], kA[64:96, t0:t0 + n], ROPE[64:96, 0, l0:l0 + n], ALU.mult)
                    kb.tt("dve", tt2[64:96, 0:n], kB[64:96, t0:t0 + n], ROPE[64:96, 1, l0:l0 + n], ALU.mult)
                    kb.tt("dve", KPE[64:96, t0:t0 + n], tt1[64:96, 0:n], tt2[64:96, 0:n], ALU.add)
            kb.barrier()
        KT = psb("ml_KT", [128, NT], F32R)
        QT = psb("ml_QT", [128, NT], F32R)
        Vh = psb("ml_Vh", [128, 18, 65], F32R)
        PT = [psb(f"ml_PT{i}", [128, 512], F32R) for i in range(4)]
        Osb = [psb(f"ml_Osb{i}", [128, 512]) for i in range(2)]
        ys = [psb(f"ml_ys{i}", [128, 512]) for i in range(2)]
        u1 = psb("ml_u1", [128, 512])
        u2 = psb("ml_u2", [128, 512])
        onesf = cst[:, 128:256]
        kb.cp("dve", Vh[:, :, 64:65], onesf[:, 0:18].unsqueeze(2))
        cnt = 0
        for h in range(8):
            for (t0, n) in CHUNKS:
                kb.mm(ps[0][0:64, 0:n], wuk[:, h * 64:(h + 1) * 64], ckvn[:, t0:t0 + n])
                kb.cp("act", KT[0:64, t0:t0 + n], ps[0][0:64, 0:n])
            kb.cp("dve", KT[64:96, :], KPE[64:96, :])
            for g0 in range(0, 18, 8):
                gn = min(8, 18 - g0)
                for j in range(gn):
                    kt = g0 + j
                    kb.mm(ps[1][:, j * 64:(j + 1) * 64], ckvn[:, kt * 128:(kt + 1) * 128], wuv[:, h * 64:(h + 1) * 64])
                kb.cp("dve", Vh[:, g0:g0 + gn, 0:64], ps[1][:, 0:gn * 64].rearrange("p (a b) -> p a b", b=64))
            for (t0, n) in CHUNKS:
                lat = t0 >= NCTX
                if not lat and not need_ctx:
                    continue
                for k in range(2):
                    kb.mm(ps[0][0:96, 0:n], wq[:, k, 0, h * 96:(h + 1) * 96], cqn[:, k, t0:t0 + n], start=(k == 0), stop=(k == 1))
                if lat:
                    for k in range(2):
                        kb.mm(ps[1][0:96, 0:n], wq[:, k, 1, h * 96:(h + 1) * 96], cqn[:, k, t0:t0 + n], start=(k == 0), stop=(k == 1))
                kb.cp("act", QT[0:64, t0:t0 + n], ps[0][0:64, 0:n])
                if not lat:
                    kb.cp("act", QT[64:96, t0:t0 + n], ps[0][64:96, 0:n])
                else:
                    l0 = t0 - NCTX
                    kb.tt("dve", u1[64:96, 0:n], ps[0][64:96, 0:n], ROPE[64:96, 0, l0:l0 + n], ALU.mult)
                    kb.tt("dve", u2[64:96, 0:n], ps[1][64:96, 0:n], ROPE[64:96, 1, l0:l0 + n], ALU.mult)
                    kb.tt("dve", QT[64:96, t0:t0 + n], u1[64:96, 0:n], u2[64:96, 0:n], ALU.add)
            groups = ([[CHUNKS[0]]] if need_ctx else []) + [CHUNKS[1:3], CHUNKS[3:5]]
            for grp in groups:
                lat = grp[0][0] >= NCTX
                kts = list(range(18)) if lat else [0, 1]
                Sb = lambda a, i: ps[2 + 2 * a + i % 2]
                Pb = lambda a, i: PT[2 * a + i % 2]

                def emitS(i):
                    kt = kts[i]
                    for a, (t0, n) in enumerate(grp):
                        kb.mm(Sb(a, i)[:, 0:n], KT[0:96, kt * 128:(kt + 1) * 128], QT[0:96, t0:t0 + n])
                emitS(0)
                for i, kt in enumerate(kts):
                    for a, (t0, n) in enumerate(grp):
                        kb.act(Pb(a, i)[:, 0:n], Sb(a, i)[:, 0:n], AF.Exp, scale=MLA_SCALE)
                    if i + 1 < len(kts):
                        emitS(i + 1)
                    for a, (t0, n) in enumerate(grp):
                        kb.mm(ps[6 + a][0:65, 0:n], Vh[:, kt, :], Pb(a, i)[:, 0:n], start=(i == 0), stop=(i == len(kts) - 1))
                for a, (t0, n) in enumerate(grp):
                    ob = Osb[a]
                    yo = ys[a]
                    bcp = ps[a]
                    kb.cp("act", ob[0:65, 0:n], ps[6 + a][0:65, 0:n])
                    kb.recip(ob[64:65, 0:n], ob[64:65, 0:n])
                    kb.mm(bcp[0:64, 0:n], onesf[64:65, 0:64], ob[64:65, 0:n])
                    kb.tt("dve", yo[0:64, 0:n], ob[0:64, 0:n], bcp[0:64, 0:n], ALU.mult)
                    kb.dma("sp", ybT_d[256 + h * 64:256 + (h + 1) * 64, t0:t0 + n], yo[0:64, 0:n], reads=[yo], writes=[f"ybT_m{h}"])
        kb.barrier()


def phase_hg(G):
    nc, kb, L = G["nc"], G["kb"], G["L"]
    Vn, cst, onesr, ident, lbv = G["Vn"], G["cst"], G["onesr"], G["ident"], G["lbv"]
    zT_d, ybT_d = G["zT_d"], G["ybT_d"]
    CH = 64
    NC_ = NT // CH
    onesf = cst[:, 128:256]
    with ExitStack() as ph:
        psb = lambda name, shape, dt=F32: ph.enter_context(nc.sbuf_tensor(uq(name), shape, dt))
        pps = lambda name, shape, dt=F32: ph.enter_context(nc.psum_tensor(uq(name), shape, dt))
        A = psb("hg_A", [128, NT])
        KK = psb("hg_KK", [128, NT], F32R)
        Bt = psb("hg_Bt", [128, NT])
        E1 = psb("hg_E1", [128, NT], F32R)
        qT = psb("hg_qT", [128, NT])
        ig = psb("hg_ig", [128, NT])
        itok = psb("hg_itok", [128, NC_, 64], F32R)
        oacc = psb("hg_oacc", [128, NT])
        U = psb("hg_U", [128, NC_, 64])
        PTall = psb("hg_PT", [128, NC_, 64], F32R)
        Sst = psb("hg_Sst", [128, NC_, 64], F32R)
        ktok = [psb(f"hg_ktok{i}", [128, 4, 128], F32R) for i in range(2)]
        sct = [psb(f"hg_sct{i}", [128, 512]) for i in range(2)]
        small = psb("hg_small", [128, 4, NC_])
        S = psb("hg_S", [128, 64])
        tU = psb("hg_tU", [128, 64])
        fin = [psb(f"hg_fin{i}", [128, 512]) for i in range(3)]
        ps = [pps(f"hg_ps{i}", [128, 512]) for i in range(7)]
        MK = psb("hg_MK", [128, NT + 1])
        kb.memset("dve", MK[:], 1.0)
        kb.memset("dve", MK[:, 0:NT].rearrange("p (n c) -> p n c", c=CH)[:, :, 0:1], 0.0)
        pcnt = [0]

        def nps():
            pcnt[0] += 1
            return ps[pcnt[0] % 7]

        b3 = lambda t: t[:].rearrange("p (n c) -> p n c", c=CH)
        for h in range(4):
            tq, ti_, tg = COLIDX[f"hq{h}"], COLIDX[f"hi{h}"], COLIDX[f"hg{h}"]
            kb.dma("sp", qT[:], zT_d[tq * 128:(tq + 1) * 128, :], reads=[f"zT_{tq}"], writes=[qT])
            kb.dma("sp", ig[0:64, :], zT_d[ti_ * 128:ti_ * 128 + 64, :], reads=[f"zT_{ti_}"], writes=[ig])
            for c0 in range(0, NC_, 8):
                gn = min(8, NC_ - c0)
                p = nps()
                for j in range(gn):
                    c = c0 + j
                    kb.tr(p[0:64, j * 64:(j + 1) * 64], ig[0:64, c * CH:(c + 1) * CH], ident[0:64, 0:64])
                kb.cp("act", itok[0:64, c0:c0 + gn, :], p[0:64, 0:gn * 64].rearrange("p (a b) -> p a b", b=64))
            for d in range(2):
                tf = COLIDX[f"hf{h}" if d == 0 else f"hb{h}"]
                kb.dma("sp", A[:], zT_d[tf * 128:(tf + 1) * 128, :], reads=[f"zT_{tf}"], writes=[A])
                kb.act(A[:], A[:], AF.Sigmoid)
                kb.ts("dve", A[:], A[:], lbv[:, 1, L:L + 1], ALU.mult, lbv[:, 0, L:L + 1], ALU.add)
                kb.ts("dve", KK[:], A[:], -1.0, ALU.mult, 1.0, ALU.add)
                kb.act(A[:], A[:], AF.Ln)
                if d == 0:
                    kb.scan(Bt[:, :], MK[:, 0:NT], A[:, :], 0.0, ALU.mult, ALU.add)
                else:
                    kb.scan(Bt[:, ::-1], MK[:, 1:NT + 1][:, ::-1], A[:, ::-1], 0.0, ALU.mult, ALU.add)
                refpos = 31 if d == 0 else 32
                lastpos = 63 if d == 0 else 0
                refc, alpha, gamma, beta = (small[:, i, :] for i in range(4))
                kb.cp("dve", refc, b3(Bt)[:, :, refpos])
                kb.act(alpha, b3(Bt)[:, :, lastpos], AF.Exp)
                kb.act(gamma, refc, AF.Exp)
                kb.tt("dve", b3(Bt), b3(Bt), refc.unsqueeze(2).to_broadcast([128, NC_, CH]), ALU.subtract)
                kb.act(E1[:], Bt[:], AF.Exp)
                kb.cp("dve", beta, b3(E1)[:, :, lastpos].bitcast(F32))
                kb.act(Bt[:], Bt[:], AF.Exp, scale=-1.0)
                kb.tt("dve", E1[:], qT[:], E1[:].bitcast(F32), ALU.mult)
                kb.tt("dve", KK[:], KK[:].bitcast(F32), Bt[:], ALU.mult)
                for c0 in range(0, NC_, 4):
                    p = nps()
                    kt_ = ktok[(c0 // 4) % 2]
                    for j in range(4):
                        c = c0 + j
                        kb.tr(p[0:64, j * 128:(j + 1) * 128], KK[:, c * CH:(c + 1) * CH].bitcast(F32), ident)
                    kb.cp("act", kt_[0:64, :, :], p[0:64, :].rearrange("p (a b) -> p a b", b=128))
                    p2 = nps()
                    for j in range(4):
                        c = c0 + j
                        kb.mm(p2[:, j * 64:(j + 1) * 64], kt_[0:64, j, :], itok[0:64, c, :])
                    kb.cp("dve", U[:, c0:c0 + 4, :], p2[:, 0:256].rearrange("p (a b) -> p a b", b=64))
                for c0 in range(0, NC_, 8):
                    gn = min(8, NC_ - c0)
                    p = nps()
                    for j in range(gn):
                        c = c0 + j
                        kb.mm(p[0:64, j * 64:(j + 1) * 64], KK[:, c * CH:(c + 1) * CH], E1[:, c * CH:(c + 1) * CH])
                    st = sct[(c0 // 8) % 2]
                    kb.cp("act", st[0:64, 0:gn * 64], p[0:64, 0:gn * 64])
                    if d == 0:
                        kb.op("pool", lambda g: g.affine_select(PTall[0:64, c0:c0 + gn, :], st[0:64, 0:gn * 64].rearrange("p (a b) -> p a b", b=64),
                                                                [[0, gn], [1, 64]], ALU.is_ge, 0.0, base=0, channel_multiplier=-1),
                              reads=[st], writes=[PTall])
                    else:
                        kb.op("pool", lambda g: g.affine_select(PTall[0:64, c0:c0 + gn, :], st[0:64, 0:gn * 64].rearrange("p (a b) -> p a b", b=64),
                                                                [[0, gn], [-1, 64]], ALU.is_ge, 0.0, base=0, channel_multiplier=1),
                              reads=[st], writes=[PTall])
                AR = A[:].rearrange("p (v o) -> p v o", o=NC_)
                U2 = Bt[:].rearrange("p (v o) -> p v o", o=NC_)
                S2 = U[:].rearrange("p c v -> p (c v)").rearrange("p (v o) -> p v o", o=NC_)
                if d == 0:
                    segs = [(0, NC_, False)]
                    cof = lambda o: o
                else:
                    segs = [(0, 4, True), (4, NC_ - 4, True)]
                    cof = lambda o: (3 - o) if o < 4 else (NC_ + 3 - o)
                for (o0, n_, _) in segs:
                    c_hi, c_lo = cof(o0), cof(o0 + n_ - 1)
                    if d == 0:
                        csl = slice(c_hi, c_lo + 1)
                    else:
                        csl = slice(c_hi, (c_lo - 1) if c_lo > 0 else None, -1)
                    kb.tt("dve", U2[:, :, o0:o0 + n_].rearrange("p v o -> p o v"), U[:, csl, :],
                          beta[:, csl].unsqueeze(2).to_broadcast([128, n_, 64]), ALU.mult)
                    kb.cp("dve", AR[:, :, o0:o0 + n_].rearrange("p v o -> p o v"), alpha[:, csl].unsqueeze(2).to_broadcast([128, n_, 64]))
                kb.memset("dve", AR[:, :, 0:1], 0.0)
                kb.scan(U[:].rearrange("p c v -> p (c v)"), A[:, :], Bt[:, :], 0.0, ALU.mult, ALU.add)
                first_c = cof(0)
                kb.memset("dve", Sst[:, first_c, :].bitcast(F32), 0.0)
                for (o0, n_, _) in segs:
                    oa = max(o0, 1)
                    nn = o0 + n_ - oa
                    c_hi, c_lo = cof(oa), cof(oa + nn - 1)
                    if d == 0:
                        csl = slice(c_hi, c_lo + 1)
                    else:
                        csl = slice(c_hi, (c_lo - 1) if c_lo > 0 else None, -1)
                    kb.tt("dve", Sst[:, csl, :], S2[:, :, oa - 1:oa - 1 + nn].rearrange("p v o -> p o v"),
                          gamma[:, csl].unsqueeze(2).to_broadcast([128, nn, 64]), ALU.mult)
                for c0 in range(0, NC_, 8):
                    gn = min(8, NC_ - c0)
                    p = nps()
                    for j in range(gn):
                        c = c0 + j
                        kb.mm(p[0:64, j * 64:(j + 1) * 64], itok[0:64, c, :], PTall[0:64, c, :], start=True, stop=False)
                        kb.mm(p[0:64, j * 64:(j + 1) * 64], Sst[:, c, :], E1[:, c * CH:(c + 1) * CH], start=False, stop=True)
                    if d == 0:
                        kb.cp("act", oacc[0:64, c0 * CH:(c0 + gn) * CH], p[0:64, 0:gn * 64])
                    else:
                        kb.tt("dve", oacc[0:64, c0 * CH:(c0 + gn) * CH], oacc[0:64, c0 * CH:(c0 + gn) * CH], p[0:64, 0:gn * 64], ALU.add)
            kb.dma("sp", ig[0:64, :], zT_d[tg * 128:tg * 128 + 64, :], reads=[f"zT_{tg}"], writes=[ig])
            kb.act(KK[0:64, :], oacc[0:64, :], AF.Square)
            kb.act(ig[0:64, :], ig[0:64, :], AF.Silu)
            for (t0, n) in CHUNKS:
                p = nps()
                kb.mm(p[0:64, 0:n], onesr[0:64, 0:64], KK[0:64, t0:t0 + n])
                kb.act(fin[0][0:64, 0:n], p[0:64, 0:n], AF.Sqrt, scale=1.0 / 64, bias=EPS)
                kb.recip(fin[0][0:64, 0:n], fin[0][0:64, 0:n])
                kb.stt("dve", fin[1][0:64, 0:n], oacc[0:64, t0:t0 + n], Vn(f"hgn_g{L}")[0:64, h:h + 1], fin[0][0:64, 0:n], ALU.mult, ALU.mult)
                kb.tt("dve", fin[2][0:64, 0:n], fin[1][0:64, 0:n], ig[0:64, t0:t0 + n], ALU.mult)
                kb.dma("sp", ybT_d[768 + h * 64:768 + (h + 1) * 64, t0:t0 + n], fin[2][0:64, 0:n], reads=[fin[2]], writes=[f"ybT_h{h}"])
        kb.barrier()


def phase_merge(G):
    nc, kb, L = G["nc"], G["kb"], G["L"]
    xT, modT = G["xT"], G["modT"]
    zT_d, ybT_d = G["zT_d"], G["ybT_d"]
    need_ctx = L < DEPTH - 1
    chs = CHUNKS if need_ctx else CHUNKS[1:]
    yb_names = ["ybT_0", "ybT_1"] + [f"ybT_m{h}" for h in range(8)] + [f"ybT_h{h}" for h in range(4)]
    with ExitStack() as ph:
        psb = lambda name, shape, dt=F32: ph.enter_context(nc.sbuf_tensor(uq(name), shape, dt))
        pps = lambda name, shape, dt=F32: ph.enter_context(nc.psum_tensor(uq(name), shape, dt))
        wp = psb("mg_wp", [128, 8, D], F32R)
        wo = psb("mg_wo", [128, 8, D], F32R)
        kb.dma("pool", wp[:], G["wp_d"][L].rearrange("(k p) c -> p k c", p=128), writes=[wp])
        kb.dma("pool", wo[:], G["wo_d"][L].rearrange("(k p) c -> p k c", p=128), writes=[wo])
        yb = psb("mg_yb", [128, 8, 512], F32R)
        mT = psb("mg_mT", [128, 8, 512], F32R)
        gt = [psb(f"mg_gt{i}", [128, 3, 512]) for i in range(2)]
        t3 = [psb(f"mg_t{i}", [128, 512]) for i in range(3)]
        ps = [pps(f"mg_ps{i}", [128, 512]) for i in range(8)]
        branches = ((0, 2), (2, 6), (6, 8))
        for (t0, n) in chs:
            s_ = 1 if t0 < NCTX else 0
            kb.dma("pool", yb[:, :, 0:n], ybT_d[:, t0:t0 + n].rearrange("(k p) t -> p k t", p=128), reads=yb_names, writes=[yb])
            for f in range(8):
                g = gt[f % 2]
                for b in range(3):
                    ti = COLIDX[f"gate{b}_{f}"]
                    kb.dma("sp", g[:, b, 0:n], zT_d[ti * 128:(ti + 1) * 128, t0:t0 + n], reads=[f"zT_{ti}"], writes=[g])
                kb.act(g[:, :, 0:n], g[:, :, 0:n], AF.Sigmoid)
                for b, (k0, k1) in enumerate(branches):
                    p = ps[(f % 2) * 3 + b]
                    for k in range(k0, k1):
                        kb.mm(p[:, 0:n], wp[:, k, f * 128:(f + 1) * 128], yb[:, k, 0:n], start=(k == k0), stop=(k == k1 - 1))
                    kb.tt("dve", t3[b][:, 0:n], p[:, 0:n], g[:, b, 0:n], ALU.mult)
                kb.tt("pool", t3[0][:, 0:n], t3[0][:, 0:n], t3[1][:, 0:n], ALU.add)
                kb.tt("pool", mT[:, f, 0:n], t3[0][:, 0:n], t3[2][:, 0:n], ALU.add)
            for f in range(8):
                p = ps[6 + f % 2]
                for k in range(8):
                    kb.mm(p[:, 0:n], wo[:, k, f * 128:(f + 1) * 128], mT[:, k, 0:n], start=(k == 0), stop=(k == 7))
                kb.stt("dve", xT[:, f, t0:t0 + n], p[:, 0:n], modT[:, L, 16 + f, s_:s_ + 1], xT[:, f, t0:t0 + n], ALU.mult, ALU.add)
        kb.barrier()


def phase_moe(G):
    nc, kb, L = G["nc"], G["kb"], G["L"]
    xT, modT, modA, onesr, ident, cst = G["xT"], G["modT"], G["modA"], G["onesr"], G["ident"], G["cst"]
    need_ctx = L < DEPTH - 1
    chs = CHUNKS if need_ctx else CHUNKS[1:]
    groups = [chs[:3], chs[3:]] if need_ctx else [chs[:2], chs[2:]]
    onesf = cst[:, 128:256]
    for grp in groups:
        GN = sum(n for _, n in grp)
        gch = []
        o = 0
        for (t0, n) in grp:
            gch.append((t0, n, o))
            o += n
        with ExitStack() as ph:
            psb = lambda name, shape, dt=F32: ph.enter_context(nc.sbuf_tensor(uq(name), shape, dt))
            pps = lambda name, shape, dt=F32: ph.enter_context(nc.psum_tensor(uq(name), shape, dt))
            h2T = psb("me_h2T", [128, 8, GN], F32R)
            gateT = psb("me_gateT", [128, GN], F32R)
            emit_norm(nc, kb, xT, h2T, gch, lambda k, s_: modA[:, L, 1, k, s_:s_ + 1],
                      lambda k, s_: modT[:, L, 24 + k, s_:s_ + 1], onesr, D)
            with ExitStack() as rp:
                rsb = lambda name, shape, dt=F32: rp.enter_context(nc.sbuf_tensor(uq(name), shape, dt))
                rps = lambda name, shape, dt=F32: rp.enter_context(nc.psum_tensor(uq(name), shape, dt))
                wr = rsb("me_wr", [128, 8, 36])
                br = rsb("me_br", [128, 36])
                kb.dma("sp", wr[:], G["wr_d"][L].rearrange("(k p) c -> p k c", p=128), writes=[wr])
                kb.dma("sp", br[:], G["br_d"][L].to_broadcast([128, 36]), writes=[br])
                lp = [rps(f"me_lp{i}", [128, 512]) for i in range(2)]
                gp = [rps(f"me_gp{i}", [128, 512]) for i in range(2)]
                R = [[rsb(f"me_r{b}_{i}", [128, 40]) for i in range(12)] for b in range(2)]
                for tt in range(GN // 128):
                    b = tt % 2
                    r = R[b]
                    for k in range(8):
                        kb.mm(lp[b][:, 0:36], h2T[:, k, tt * 128:(tt + 1) * 128].bitcast(F32), wr[:, k, :], start=(k == 0), stop=(k == 7))
                    lg = r[0]
                    kb.tt("dve", lg[:, 0:36], lp[b][:, 0:36], br[:], ALU.add)
                    gmax, ngmax, gsum, gw = r[1][:, 0:1], r[1][:, 1:2], r[1][:, 2:3], r[1][:, 3:4]
                    kb.op("dve", lambda g: g.tensor_reduce(gmax, lg[:, 0:4], AX.X, ALU.max), reads=[lg], writes=[r[1]])
                    kb.ts("dve", ngmax, gmax, -1.0, ALU.mult)
                    kb.act(r[2][:, 0:4], lg[:, 0:4], AF.Exp, bias=ngmax)
                    kb.op("dve", lambda g: g.tensor_reduce(gsum, r[2][:, 0:4], AX.X, ALU.add), reads=[r[2]], writes=[r[1]])
                    kb.recip(gw, gsum)
                    kb.ts("dve", r[3][:, 0:4], lg[:, 0:4], gmax, ALU.is_equal)
                    kb.ts("dve", r[3][:, 0:4], r[3][:, 0:4], -1.0, ALU.add, 1e30, ALU.mult)
                    kb.tt("dve", r[4][:, 0:32].rearrange("p (a b) -> p a b", b=8), lg[:, 4:36].rearrange("p (a b) -> p a b", b=8),
                          r[3][:, 0:4].unsqueeze(2).to_broadcast([128, 4, 8]), ALU.add)
                    kb.op("dve", lambda g: g.max(r[5][:, 0:8], r[4][:, 0:32]), reads=[r[4]], writes=[r[5]])
                    m1, m2 = r[5][:, 0:1], r[5][:, 1:2]
                    kb.ts("dve", r[6][:, 0:32], r[4][:, 0:32], m1, ALU.is_equal)
                    kb.ts("dve", r[7][:, 0:32], r[4][:, 0:32], m2, ALU.is_equal)
                    dm, ee, p1, p2 = r[8][:, 0:1], r[8][:, 1:2], r[8][:, 2:3], r[8][:, 3:4]
                    kb.tt("dve", dm, m2, m1, ALU.subtract)
                    kb.act(ee, dm, AF.Exp)
                    kb.ts("dve", p1, ee, 1.0, ALU.add)
                    kb.recip(p1, p1)
                    kb.tt("dve", p2, ee, p1, ALU.mult)
                    kb.tt("dve", p1, p1, gw, ALU.mult)
                    kb.tt("dve", p2, p2, gw, ALU.mult)
                    kb.ts("dve", r[9][:, 0:32], r[6][:, 0:32], p1, ALU.mult)
                    kb.stt("dve", r[9][:, 0:32], r[7][:, 0:32], p2, r[9][:, 0:32], ALU.mult, ALU.add)
                    kb.tr(gp[b][0:32, 0:128], r[9][:, 0:32], ident)
                    kb.cp("act", gateT[0:32, tt * 128:(tt + 1) * 128], gp[b][0:32, 0:128])
                kb.barrier()
            Gall = psb("me_G", [128, 4, GN], F32R)
            w13 = [[psb(f"me_w{a}_{i}", [128, 8, 128], F32R) for i in range(2)] for a in (1, 3)]
            w2 = [psb(f"me_w2_{i}", [128, 4, D], F32R) for i in range(2)]
            sel = [psb(f"me_sel{i}", [128, 128], F32R) for i in range(2)]
            sil = [psb(f"me_sil{i}", [128, 512]) for i in range(2)]
            hp = [pps(f"me_hp{i}", [128, 512]) for i in range(4)]
            bp = [pps(f"me_bp{i}", [128, 512]) for i in range(2)]
            yp = [pps(f"me_yp{i}", [128, 512]) for i in range(2)]
            cnt = 0
            for e in range(32):
                se = sel[e % 2]
                kb.op("pool", lambda g: g.affine_select(se[0:32, :], onesf[0:32, :], [[0, 128]], ALU.is_equal, 0.0,
                                                        base=-e, channel_multiplier=1), reads=[cst], writes=[se])
                kb.dma("pool", w2[e % 2][:], G["w2_d"][L, e].rearrange("(j p) c -> p j c", p=128), writes=[w2[e % 2]])
                for j in range(4):
                    wa, wb_ = w13[0][(e * 4 + j) % 2], w13[1][(e * 4 + j) % 2]
                    kb.dma("pool", wa[:], G["w1_d"][L, e, :, j * 128:(j + 1) * 128].rearrange("(k p) c -> p k c", p=128), writes=[wa])
                    kb.dma("pool", wb_[:], G["w3_d"][L, e, :, j * 128:(j + 1) * 128].rearrange("(k p) c -> p k c", p=128), writes=[wb_])
                    for (t0, n, o) in gch:
                        b = cnt % 2
                        cnt += 1
                        for k in range(8):
                            kb.mm(hp[b][:, 0:n], wa[:, k, :], h2T[:, k, o:o + n], start=(k == 0), stop=(k == 7))
                        for k in range(8):
                            kb.mm(hp[2 + b][:, 0:n], wb_[:, k, :], h2T[:, k, o:o + n], start=(k == 0), stop=(k == 7))
                        kb.mm(bp[b][:, 0:n], se[0:32, :], gateT[0:32, o:o + n])
                        kb.act(sil[b][:, 0:n], hp[b][:, 0:n], AF.Silu)
                        kb.tt("dve", sil[b][:, 0:n], sil[b][:, 0:n], hp[2 + b][:, 0:n], ALU.mult)
                        kb.tt("dve", Gall[:, j, o:o + n], sil[b][:, 0:n], bp[b][:, 0:n], ALU.mult)
                for (t0, n, o) in gch:
                    s_ = 1 if t0 < NCTX else 0
                    for f in range(8):
                        p = yp[f % 2]
                        for j in range(4):
                            kb.mm(p[:, 0:n], w2[e % 2][:, j, f * 128:(f + 1) * 128], Gall[:, j, o:o + n], start=(j == 0), stop=(j == 3))
                        kb.stt("dve", xT[:, f, t0:t0 + n], p[:, 0:n], modT[:, L, 40 + f, s_:s_ + 1], xT[:, f, t0:t0 + n], ALU.mult, ALU.add)
            kb.barrier()


def phase_moe_sparse(G):
    nc, kb, L = G["nc"], G["kb"], G["L"]
    xT, modT, modA, onesr, ident, cst = G["xT"], G["modT"], G["modA"], G["onesr"], G["ident"], G["cst"]
    h2tok_d, xs_d, ys_d = G["h2tok_d"], G["xs_d"], G["ys_d"]
    need_ctx = L < DEPTH - 1
    chs = CHUNKS if need_ctx else CHUNKS[1:]
    tiles = [t0 // 128 + j for (t0, n) in chs for j in range(n // 128)]
    NTL = len(tiles)
    onesf = cst[:, 128:256]
    with ExitStack() as ph:
        psb = lambda name, shape, dt=F32: ph.enter_context(nc.sbuf_tensor(uq(name), shape, dt))
        pps = lambda name, shape, dt=F32: ph.enter_context(nc.psum_tensor(uq(name), shape, dt))
        pr = ExitStack()
        rsb_ = lambda name, shape, dt=F32: pr.enter_context(nc.sbuf_tensor(uq(name), shape, dt))
        GA = psb("ms_GA", [128, 18])
        GB = psb("ms_GB", [128, 18])
        D1f = psb("ms_D1f", [128, 18])
        D2f = psb("ms_D2f", [128, 18])
        D1i = psb("ms_D1i", [128, 18], I32)
        D2i = psb("ms_D2i", [128, 18], I32)
        idxW = psb("ms_idxW", [128, 128], I32)
        p0 = ExitStack()
        h2T = p0.enter_context(nc.sbuf_tensor(uq("ms_h2T"), [128, 8, NT], F32R))
        OH1 = rsb_("ms_OH1", [128, 18, 32])
        OH2 = rsb_("ms_OH2", [128, 18, 32])
        AA = rsb_("ms_AA", [128, 18, 32])
        with ExitStack() as p1:
            qsb = lambda name, shape, dt=F32: p1.enter_context(nc.sbuf_tensor(uq(name), shape, dt))
            qps = lambda name, shape, dt=F32: p1.enter_context(nc.psum_tensor(uq(name), shape, dt))
            emit_norm(nc, kb, xT, h2T, [(t0, n, t0) for (t0, n) in chs], lambda k, s_: modA[:, L, 1, k, s_:s_ + 1],
                      lambda k, s_: modT[:, L, 24 + k, s_:s_ + 1], onesr, D)
            wr = qsb("ms_wr", [128, 8, 36])
            br = qsb("ms_br", [128, 36])
            kb.dma("sp", wr[:], G["wr_d"][L].rearrange("(k p) c -> p k c", p=128), writes=[wr])
            kb.dma("sp", br[:], G["br_d"][L].to_broadcast([128, 36]), writes=[br])
            lp = [qps(f"ms_lp{i}", [128, 512]) for i in range(2)]
            T_ = NTL
            LG = qsb("ms_LG", [128, 18, 36])
            for i, tt in enumerate(tiles):
                b = i % 2
                tsl = slice(tt * 128, (tt + 1) * 128)
                for k in range(8):
                    kb.mm(lp[b][:, 0:36], h2T[:, k, tsl].bitcast(F32), wr[:, k, :], start=(k == 0), stop=(k == 7))
                kb.tt("dve", LG[:, i, :], lp[b][:, 0:36], br[:], ALU.add)
            B4 = lambda t: t[:, 0:T_, :]
            g4 = qsb("ms_g4", [128, 18, 4])
            oh4 = qsb("ms_oh4", [128, 18, 4])
            ls = qsb("ms_ls", [128, 18, 32])
            l2 = qsb("ms_l2", [128, 18, 32])
            sm_ = qsb("ms_sm", [128, 8, 18])
            gmax, gsum, gw, m1, m2, ee, p1_, p2_ = (sm_[:, j, 0:T_] for j in range(8))
            bc4 = lambda v: v.unsqueeze(2).to_broadcast([128, T_, 4])
            bc32 = lambda v: v.unsqueeze(2).to_broadcast([128, T_, 32])
            kb.op("dve", lambda g: g.tensor_reduce(gmax, LG[:, 0:T_, 0:4], AX.X, ALU.max), reads=[LG], writes=[sm_])
            kb.tt("dve", g4[:, 0:T_, :], LG[:, 0:T_, 0:4], bc4(gmax), ALU.subtract)
            kb.tt("dve", oh4[:, 0:T_, :], LG[:, 0:T_, 0:4], bc4(gmax), ALU.is_equal)
            kb.act(g4[:, 0:T_, :], g4[:, 0:T_, :], AF.Exp)
            kb.op("dve", lambda g: g.tensor_reduce(gsum, g4[:, 0:T_, :], AX.X, ALU.add), reads=[g4], writes=[sm_])
            kb.recip(gw, gsum)
            kb.ts("dve", oh4[:, 0:T_, :], oh4[:, 0:T_, :], -1.0, ALU.add, 1e30, ALU.mult)
            kb.tt("dve", ls[:, 0:T_, :].rearrange("p t (a b) -> p t a b", b=8), LG[:, 0:T_, 4:36].rearrange("p t (a b) -> p t a b", b=8),
                  oh4[:, 0:T_, :].unsqueeze(3).to_broadcast([128, T_, 4, 8]), ALU.add)
            kb.op("dve", lambda g: g.tensor_reduce(m1, ls[:, 0:T_, :], AX.X, ALU.max), reads=[ls], writes=[sm_])
            kb.tt("dve", OH1[:, 0:T_, :], ls[:, 0:T_, :], bc32(m1), ALU.is_equal)
            kb.stt("dve", l2[:, 0:T_, :], OH1[:, 0:T_, :], -1e30, ls[:, 0:T_, :], ALU.mult, ALU.add)
            kb.op("dve", lambda g: g.tensor_reduce(m2, l2[:, 0:T_, :], AX.X, ALU.max), reads=[l2], writes=[sm_])
            kb.tt("dve", OH2[:, 0:T_, :], l2[:, 0:T_, :], bc32(m2), ALU.is_equal)
            kb.tt("dve", AA[:, 0:T_, :], OH1[:, 0:T_, :], OH2[:, 0:T_, :], ALU.add)
            kb.tt("dve", ee, m2, m1, ALU.subtract)
            kb.act(ee, ee, AF.Exp)
            kb.ts("dve", p1_, ee, 1.0, ALU.add)
            kb.recip(p1_, p1_)
            kb.tt("dve", p2_, ee, p1_, ALU.mult)
            kb.tt("dve", GA[:, 0:T_], p1_, gw, ALU.mult)
            kb.tt("dve", GB[:, 0:T_], p2_, gw, ALU.mult)
            kb.barrier()
        with ExitStack() as p2:
            qsb = lambda name, shape, dt=F32: p2.enter_context(nc.sbuf_tensor(uq(name), shape, dt))
            qps = lambda name, shape, dt=F32: p2.enter_context(nc.psum_tensor(uq(name), shape, dt))
            ltri = qsb("ms_ltri", [128, 128])
            kb.op("pool", lambda g: g.affine_select(ltri[:], onesf, [[1, 128]], ALU.is_gt, 0.0, base=0, channel_multiplier=-1),
                  reads=[cst], writes=[ltri])
            Rp = [qps(f"ms_Rp{i}", [128, 512]) for i in range(2)]
            Cp = qps("ms_Cp", [128, 512])
            AAP = qsb("ms_AAP", [128, 18, 32])
            kb.memset("dve", AAP[:, 0, :], 0.0)
            for i in range(1, NTL):
                kb.tt("dve", AAP[:, i, :], AAP[:, i - 1, :], AA[:, i - 1, :], ALU.add)
            for i in range(NTL):
                out = Rp[i // 16][:, (i % 16) * 32:(i % 16 + 1) * 32]
                kb.mm(out, ltri[:], AA[:, i, :], start=True, stop=(i == 0))
                if i > 0:
                    kb.mm(out, onesf, AAP[:, i, :], start=False, stop=True)
            kb.mm(Cp[:, 0:32], onesf, AAP[:, NTL - 1, :], start=True, stop=False)
            kb.mm(Cp[:, 0:32], onesf, AA[:, NTL - 1, :], start=False, stop=True)
            w_ = [qsb(f"ms_w{i}", [128, 32]) for i in range(8)]
            cnt, x_, kf, msk, nb, pend, pstart, tmp = w_
            kb.cp("dve", cnt[:], Cp[:, 0:32])
            kb.ts("dve", x_[:], cnt[:], -float(SLOT), ALU.add, 0.0, ALU.max)
            kb.ts("dve", x_[:], x_[:], 127.0, ALU.add, 1.0 / 128, ALU.mult)
            ki = qsb("ms_ki", [128, 32], I32)
            kb.cp("dve", ki[:], x_[:])
            kb.cp("dve", kf[:], ki[:])
            kb.tt("dve", msk[:], kf[:], x_[:], ALU.is_gt)
            kb.tt("dve", kf[:], kf[:], msk[:], ALU.subtract)
            kb.ts("dve", tmp[:], kf[:], 1.0, ALU.add)
            kb.tt("dve", msk[:], tmp[:], x_[:], ALU.is_le)
            kb.tt("dve", nb[:], kf[:], msk[:], ALU.add)
            kb.scan(pend[:], onesf[:, 0:32], nb[:], 0.0, ALU.mult, ALU.add)
            kb.tt("dve", pstart[:], pend[:], nb[:], ALU.subtract)
            base1_i = qsb("ms_b1i", [128, 32], I32)
            base1 = qsb("ms_b1", [128, 32])
            kb.op("pool", lambda g: g.iota(base1_i[:], [[SLOT, 32]], base=0, channel_multiplier=0), writes=[base1_i])
            kb.cp("dve", base1[:], base1_i[:])
            kb.ts("dve", pstart[:], pstart[:], 128.0, ALU.mult, float(32 * SLOT - SLOT), ALU.add)
            kb.tt("dve", pstart[:], pstart[:], base1[:], ALU.subtract)
            RR = qsb("ms_RR", [128, 18, 32])
            T3 = qsb("ms_T3", [128, 18, 32])
            T4 = qsb("ms_T4", [128, 18, 32])
            n0 = min(NTL, 16)
            kb.cp("dve", RR[:, 0:n0, :], Rp[0][:, 0:n0 * 32].rearrange("p (a b) -> p a b", b=32))
            if NTL > 16:
                kb.cp("dve", RR[:, 16:NTL, :], Rp[1][:, 0:(NTL - 16) * 32].rearrange("p (a b) -> p a b", b=32))
            bcT = lambda v: v.unsqueeze(1).to_broadcast([128, NTL, 32])
            kb.ts("dve", T4[:, 0:NTL, :], RR[:, 0:NTL, :], float(SLOT), ALU.is_ge)
            kb.tt("dve", T4[:, 0:NTL, :], T4[:, 0:NTL, :], bcT(pstart[:]), ALU.mult)
            kb.tt("dve", T3[:, 0:NTL, :], RR[:, 0:NTL, :], bcT(base1[:]), ALU.add)
            kb.tt("dve", T3[:, 0:NTL, :], T3[:, 0:NTL, :], T4[:, 0:NTL, :], ALU.add)
            kb.tt("dve", T4[:, 0:NTL, :], T3[:, 0:NTL, :], OH1[:, 0:NTL, :], ALU.mult)
            kb.op("dve", lambda g: g.tensor_reduce(D1f[:, 0:NTL], T4[:, 0:NTL, :], AX.X, ALU.add), reads=[T4], writes=[D1f])
            kb.tt("dve", T4[:, 0:NTL, :], T3[:, 0:NTL, :], OH2[:, 0:NTL, :], ALU.mult)
            kb.op("dve", lambda g: g.tensor_reduce(D2f[:, 0:NTL], T4[:, 0:NTL, :], AX.X, ALU.add), reads=[T4], writes=[D2f])
            kb.cp("dve", D1i[:, 0:NTL], D1f[:, 0:NTL])
            kb.cp("dve", D2i[:, 0:NTL], D2f[:, 0:NTL])
            pidx_i = qsb("ms_pidx_i", [128, 1], I32)
            pidx = qsb("ms_pidx", [128, 1])
            kb.op("pool", lambda g: g.iota(pidx_i[:], [[0, 1]], base=0, channel_multiplier=1), writes=[pidx_i])
            kb.cp("dve", pidx[:], pidx_i[:])
            be = qsb("ms_be", [128, 1])
            kb.ts("dve", tmp[:], pend[:], pidx[:, 0:1], ALU.is_le)
            kb.op("dve", lambda g: g.tensor_reduce(be[:], tmp[:], AX.X, ALU.add), reads=[tmp], writes=[be])
            kb.ts("dve", be[:], be[:], 31.0, ALU.min)
            vb = qsb("ms_vb", [128, 1])
            kb.ts("dve", vb[:], pidx[:], pend[:, 31:32], ALU.is_lt)
            kb.tt("dve", be[:], be[:], vb[:], ALU.mult)
            kb.ts("dve", vb[:], vb[:], -1.0, ALU.add, -1000.0, ALU.mult)
            kb.tt("dve", be[:], be[:], vb[:], ALU.add)
            dg = qsb("ms_dg", [128, 128])
            kb.ts("dve", dg[:], ident, be[:, 0:1], ALU.mult)
            kb.mm(Cp[:, 128:256], onesf, dg[:])
            bef = qsb("ms_bef", [128, 128])
            kb.ts("dve", bef[:], Cp[:, 128:256], 128.0, ALU.mult, pidx[:, 0:1], ALU.add)
            if L > 0:
                kb.ts("dve", bef[:], bef[:], float(L * 32 * 128), ALU.add)
            kb.cp("dve", idxW[:], bef[:])
            kb.barrier()
        pr.close()
        with ExitStack() as p3:
            qsb = lambda name, shape, dt=F32: p3.enter_context(nc.sbuf_tensor(uq(name), shape, dt))
            qps = lambda name, shape, dt=F32: p3.enter_context(nc.psum_tensor(uq(name), shape, dt))
            hk = [qsb(f"ms_hs{i}", [128, D]) for i in range(3)]
            tp = [qps(f"ms_tp{i}", [128, 512]) for i in range(4)]
            for i, tt in enumerate(tiles):
                h = hk[i % 3]
                tsl = slice(tt * 128, (tt + 1) * 128)
                for half in range(2):
                    p = tp[(2 * i + half) % 4]
                    for j in range(4):
                        k = half * 4 + j
                        kb.tr(p[:, j * 128:(j + 1) * 128], h2T[:, k, tsl].bitcast(F32), ident)
                    kb.cp("act" if half == 0 else "dve", h[:, half * 512:(half + 1) * 512], p[:])
                kb.idma(xs_d, bass.IndirectOffsetOnAxis(ap=D1i[:, i:i + 1], axis=0), h[:], None, reads=[h, D1i], writes=["xs"])
                kb.idma(xs_d, bass.IndirectOffsetOnAxis(ap=D2i[:, i:i + 1], axis=0), h[:], None, reads=[h, D2i], writes=["xs"])
            kb.barrier()
        p0.close()
        with ExitStack() as p4:
            qsb = lambda name, shape, dt=F32: p4.enter_context(nc.sbuf_tensor(uq(name), shape, dt))
            qps = lambda name, shape, dt=F32: p4.enter_context(nc.psum_tensor(uq(name), shape, dt))
            W1 = [qsb(f"ms_W1_{i}", [128, 8 * 512], F32R) for i in range(2)]
            W3 = [qsb(f"ms_W3_{i}", [128, 8 * 512], F32R) for i in range(2)]
            W2 = [qsb(f"ms_W2_{i}", [128, 4 * D], F32R) for i in range(2)]
            Xb = [qsb(f"ms_Xb{i}", [128, D]) for i in range(3)]
            XT = [qsb(f"ms_XT{i}", [128, 8, 128], F32R) for i in range(2)]
            SL = [qsb(f"ms_SL{i}", [128, 512]) for i in range(2)]
            Gt = [qsb(f"ms_Gt{i}", [128, 4, 128], F32R) for i in range(2)]
            Ys = [qsb("ms_Ys0", [128, D])] * 2
            tpp = [qps(f"ms_tq{i}", [128, 512]) for i in range(2)]
            hp1 = [qps("ms_h1", [128, 512])] * 2
            hp3 = [qps("ms_h3", [128, 512])] * 2
            ypp = [qps(f"ms_yp{i}", [128, 512]) for i in range(2)]
            xtp = [qps(f"ms_xq{i}", [128, 512]) for i in range(2)]
            def xload(i):
                if i < len(subs):
                    kb.dma("sp", Xb[i % 3][:], xs_d[subs[i][0]:subs[i][0] + 128, :], reads=["xs"], writes=[Xb[i % 3]])

            def stageA(row0, W1t, W3t, xq, xb3):
                for half in range(2):
                    p = xtp[half]
                    for j in range(4):
                        k = half * 4 + j
                        kb.tr(p[:, j * 128:(j + 1) * 128], Xb[xb3][:, k * 128:(k + 1) * 128], ident)
                    kb.cp("act" if half == 0 else "dve", XT[xq][:, half * 4:half * 4 + 4, :], p[:].rearrange("p (a b) -> p a b", b=128))
                w1v = W1t[:].rearrange("p (k c) -> p k c", c=512)
                w3v = W3t[:].rearrange("p (k c) -> p k c", c=512)
                for k in range(8):
                    kb.mm(hp1[xq][:], XT[xq][:, k, :], w1v[:, k, :], start=(k == 0), stop=(k == 7))
                    kb.mm(hp3[xq][:], XT[xq][:, k, :], w3v[:, k, :], start=(k == 0), stop=(k == 7))
                kb.act(SL[xq][:], hp1[xq][:], AF.Silu)
                kb.tt("dve", SL[xq][:], SL[xq][:], hp3[xq][:], ALU.mult)

            def stageB(row0, W2t, xq):
                w2v = W2t[:].rearrange("p (j c) -> p j c", c=D)
                gp_ = tpp[xq]
                for j in range(4):
                    kb.tr(gp_[:, j * 128:(j + 1) * 128], SL[xq][:, j * 128:(j + 1) * 128], ident)
                kb.cp("act", Gt[xq][:].rearrange("p a b -> p (a b)"), gp_[:])
                for half in range(2):
                    yp = ypp[half]
                    for j in range(4):
                        kb.mm(yp[:], Gt[xq][:, j, :], w2v[:, j, half * 512:(half + 1) * 512], start=(j == 0), stop=(j == 3))
                    kb.cp("act" if half == 0 else "dve", Ys[xq][:, half * 512:(half + 1) * 512], yp[:])
                kb.dma("sp", ys_d[row0:row0 + 128, :], Ys[xq][:], reads=[Ys[xq]], writes=["ys"])

            subs = []
            wcnt = 0
            for e in range(32):
                pb = wcnt % 2
                wcnt += 1

                def ld(e=e, pb=pb):
                    kb.dma("pool", W1[pb][:], G["w1_d"][L, e * 128:(e + 1) * 128, :], writes=[W1[pb]])
                    kb.dma("pool", W3[pb][:], G["w3_d"][L, e * 128:(e + 1) * 128, :], writes=[W3[pb]])
                    kb.dma("pool", W2[pb][:], G["w2_d"][L, e * 128:(e + 1) * 128, :], writes=[W2[pb]])
                for j in range(SLOT // 128):
                    subs.append((e * SLOT + j * 128, pb, ld if j == 0 else None))
            nov = 2 * NTL - 1
            for b in range(nov):
                pb = wcnt % 2
                wcnt += 1

                def ld(b=b, pb=pb):
                    off = bass.IndirectOffsetOnAxis(ap=idxW[:, b:b + 1], axis=0)
                    bnd = (L + 1) * 32 * 128 - 1
                    kb.idma(W1[pb][:], None, G["w1_d"].rearrange("l r c -> (l r) c"), off, reads=[idxW], writes=[W1[pb]], bounds=bnd)
                    kb.idma(W3[pb][:], None, G["w3_d"].rearrange("l r c -> (l r) c"), off, reads=[idxW], writes=[W3[pb]], bounds=bnd)
                    kb.idma(W2[pb][:], None, G["w2_d"].rearrange("l r c -> (l r) c"), off, reads=[idxW], writes=[W2[pb]], bounds=bnd)
                subs.append((32 * SLOT + b * 128, pb, ld))
            xload(0)
            xload(1)
            for i, (row0, pb, ld) in enumerate(subs):
                if ld is not None:
                    ld()
                xload(i + 2)
                stageA(row0, W1[pb], W3[pb], i % 2, i % 3)
                if i >= 1:
                    r1, pb1, _ = subs[i - 1]
                    stageB(r1, W2[pb1], (i - 1) % 2)
            r1, pb1, _ = subs[-1]
            stageB(r1, W2[pb1], (len(subs) - 1) % 2)
            kb.barrier()
        with ExitStack() as p5:
            qsb = lambda name, shape, dt=F32: p5.enter_context(nc.sbuf_tensor(uq(name), shape, dt))
            qps = lambda name, shape, dt=F32: p5.enter_context(nc.psum_tensor(uq(name), shape, dt))
            y1 = [qsb(f"ms_y1_{i}", [128, D]) for i in range(2)]
            y2 = [qsb(f"ms_y2_{i}", [128, D]) for i in range(2)]
            tq = [qps(f"ms_cq{i}", [128, 512]) for i in range(4)]
            for i, tt in enumerate(tiles):
                pb = i % 2
                s_ = 1 if tt * 128 < NCTX else 0
                kb.idma(y1[pb][:], None, ys_d, bass.IndirectOffsetOnAxis(ap=D1i[:, i:i + 1], axis=0), reads=["ys", D1i], writes=[y1[pb]])
                kb.idma(y2[pb][:], None, ys_d, bass.IndirectOffsetOnAxis(ap=D2i[:, i:i + 1], axis=0), reads=["ys", D2i], writes=[y2[pb]])
                kb.act(y1[pb][:], y1[pb][:], AF.Identity, scale=GA[:, i:i + 1])
                kb.stt("dve", y1[pb][:], y2[pb][:], GB[:, i:i + 1], y1[pb][:], ALU.mult, ALU.add)
                for half in range(2):
                    p = tq[(2 * i + half) % 4]
                    for j in range(4):
                        k = half * 4 + j
                        kb.tr(p[:, j * 128:(j + 1) * 128], y1[pb][:, k * 128:(k + 1) * 128], ident)
                    for j in range(4):
                        k = half * 4 + j
                        kb.stt("dve", xT[:, k, tt * 128:(tt + 1) * 128], p[:, j * 128:(j + 1) * 128], modT[:, L, 40 + k, s_:s_ + 1],
                               xT[:, k, tt * 128:(tt + 1) * 128], ALU.mult, ALU.add)
            kb.barrier()


def phase_final(G):
    nc, kb = G["nc"], G["kb"]
    xT, onesr, ident, Vn = G["xT"], G["onesr"], G["ident"], G["Vn"]
    out_d = G["out_d"]
    with ExitStack() as ph:
        psb = lambda name, shape, dt=F32: ph.enter_context(nc.sbuf_tensor(uq(name), shape, dt))
        pps = lambda name, shape, dt=F32: ph.enter_context(nc.psum_tensor(uq(name), shape, dt))
        fo = psb("fn_fo", [128, 8, NLAT])
        emit_norm(nc, kb, xT, fo, [(t0, n, t0 - NCTX) for (t0, n) in CHUNKS[1:]],
                  lambda k, s_: Vn("final_g")[:, k:k + 1], None, onesr, D)
        ost = [psb(f"fn_ost{i}", [128, D]) for i in range(2)]
        tp = [pps(f"fn_tp{i}", [128, 512]) for i in range(4)]
        for tt in range(NLAT // 128):
            o = ost[tt % 2]
            for half in range(2):
                p = tp[(2 * tt + half) % 4]
                for j in range(4):
                    k = half * 4 + j
                    kb.tr(p[:, j * 128:(j + 1) * 128], fo[:, k, tt * 128:(tt + 1) * 128], ident)
                if half == 0:
                    kb.cp("dve", o[:, 0:512], p[:])
                else:
                    kb.cp("act", o[:, 512:1024], p[:])
            kb.dma("sp", out_d[tt * 128:(tt + 1) * 128, :], o[:], reads=[o], writes=["out"])
        kb.barrier()


def host_prep(inputs):
    f = lambda a: np.ascontiguousarray(np.asarray(a, dtype=np.float32))
    w_in = f(inputs["w_in"])
    perm = np.concatenate([np.arange(8, 16), np.arange(0, 8), np.arange(24, 32), np.arange(16, 24)]) + 640
    w_in_x = np.concatenate([w_in, w_in[:, :, 576:672], w_in[:, :, 576:640], w_in[:, :, perm]], axis=2)
    consts = np.concatenate([np.eye(128, dtype=np.float32), np.ones((128, 128), np.float32)], axis=1)
    shared = {"consts": consts, "mod_w": f(inputs["mod_w"]), "w_in_x": np.ascontiguousarray(w_in_x)}
    s5B = np.zeros((DEPTH, 2, 2, 8, 128, 128), np.float32)
    s5C = np.zeros((DEPTH, 2, 2, 8, 128, 128), np.float32)
    for ri, (bn, cn) in enumerate((("s5_b_re", "s5_c_re"), ("s5_b_im", "s5_c_im"))):
        bb = f(inputs[bn])
        cc = f(inputs[cn])
        for g in range(16):
            s_ = g // 2
            ct = s_ // 4
            q0 = (g - 2 * s_) * 64
            c0 = (g - 8 * ct) * 16
            s5B[:, :, ri, s_, c0:c0 + 16, q0:q0 + 64] = bb[:, :, g].transpose(0, 1, 3, 2)
            s5C[:, :, ri, s_, q0:q0 + 64, c0:c0 + 16] = cc[:, :, g].transpose(0, 1, 3, 2)
    shared["s5B"] = s5B
    shared["s5C"] = s5C
    shared["s5_glu_w"] = f(inputs["s5_glu_w"])
    rows = NLAT // 64
    row = np.repeat(np.arange(rows, dtype=np.float32), 64)
    col = np.tile(np.arange(64, dtype=np.float32), rows)
    inv = (np.float32(10000.0) ** (-np.arange(8, dtype=np.float32) / np.float32(8))).astype(np.float32)
    rope = np.zeros((128, 2, NLAT), np.float32)
    for r in range(32):
        i, axis, half = r % 8, r // 16, (r // 8) % 2
        ang = ((row if axis == 0 else col) * inv[i]).astype(np.float32)
        rope[64 + r, 0] = np.cos(ang)
        rope[64 + r, 1] = np.sin(ang) * (-1.0 if half == 0 else 1.0)
    shared["rope"] = rope
    wuq = f(inputs["mla_w_uq"]).reshape(DEPTH, 256, 8, 96)
    pperm = np.concatenate([np.arange(64), 64 + np.concatenate([np.arange(8, 16), np.arange(0, 8), np.arange(24, 32), np.arange(16, 24)])])
    shared["wq"] = np.ascontiguousarray(np.stack([wuq, wuq[:, :, :, pperm]], axis=2).reshape(DEPTH, 256, 2, 768))
    shared["mla_w_uk"] = f(inputs["mla_w_uk"])
    shared["hg_lb_logits"] = f(inputs["hg_lb_logits"]).reshape(1, DEPTH)
    shared["wp"] = np.ascontiguousarray(np.concatenate([f(inputs["w_pa"]), f(inputs["w_pb"]), f(inputs["w_pc"])], axis=1))
    shared["w_out"] = f(inputs["w_out"])
    shared["wr"] = np.ascontiguousarray(np.concatenate([f(inputs["moe_w_group"]), f(inputs["moe_w_expert"])], axis=2))
    shared["br"] = np.ascontiguousarray(np.concatenate([f(inputs["moe_b_group"]), f(inputs["moe_b_expert"])], axis=1).reshape(DEPTH, 1, 36))
    for nm_, kt in (("moe_w1", 8), ("moe_w3", 8), ("moe_w2", 4)):
        w_ = f(inputs[nm_])
        cols = w_.shape[-1]
        shared[nm_] = np.ascontiguousarray(w_.reshape(DEPTH, 32, kt, 128, cols).transpose(0, 1, 3, 2, 4)).reshape(DEPTH, 32 * 128, kt * cols)
    shared["mla_w_uv"] = f(inputs["mla_w_uv"])
    in_maps = []
    for b in range(8):
        vecs = np.zeros((NVBLK * 128, 128), np.float32)

        def put(name, arr):
            r0, n = VEC_ROWS[name]
            vecs[r0:r0 + n, :] = np.asarray(arr, np.float32).reshape(n, 128)
        put("c", inputs["c"][b]); put("c_ctx", inputs["c_ctx"]); put("final_g", inputs["final_norm_g"])
        for l in range(DEPTH):
            put(f"norm1_g{l}", inputs["norm1_g"][l]); put(f"norm2_g{l}", inputs["norm2_g"][l])
            put(f"mod_b{l}", inputs["mod_b"][l]); put(f"s5_d{l}", inputs["s5_d"][l])
            put(f"glu_b{l}", inputs["s5_glu_b"][l]); put(f"qa_g{l}", inputs["mla_qa_g"][l])
            put(f"kva_g{l}", inputs["mla_kva_g"][l])
            hg = np.zeros((4, 128), np.float32)
            hg[:, 0:64] = np.asarray(inputs["hg_norm_g"][l], np.float32).reshape(4, 64)
            put(f"hgn_g{l}", hg)
            for d in range(2):
                put(f"lam_re{l}{d}", inputs["s5_lam_re"][l, d]); put(f"lam_im{l}{d}", inputs["s5_lam_im"][l, d])
                put(f"lstep{l}{d}", np.repeat(np.asarray(inputs["s5_log_step"][l, d], np.float32), 64))
        m = dict(shared)
        m["x"] = f(inputs["x"][b]); m["ctx"] = f(inputs["ctx"][b]); m["vecs"] = vecs
        in_maps.append(m)
    return in_maps


def kernel(**inputs):
    in_maps = host_prep(inputs)
    nc = build_nc()
    res = run_bass_kernel_spmd(nc, in_maps, core_ids=list(range(8)))
    return np.stack([r["out"] for r in res.results], axis=0)
```

```python
import math
from contextlib import ExitStack

import numpy as np
import concourse.bass as bass
import concourse.mybir as mybir
from concourse.bass_utils import run_bass_kernel_spmd

F32 = mybir.dt.float32
F32R = mybir.dt.float32r
I32 = mybir.dt.int32
AF = mybir.ActivationFunctionType
ALU = mybir.AluOpType
AX = mybir.AxisListType

D = 1024
NCTX = 256
NLAT = 2048
NT = NCTX + NLAT
DEPTH = 2
EPS = 1e-6
CHUNKS = [(0, 256)] + [(256 + 512 * i, 512) for i in range(4)]
SLOT = 256
NOV = 35
NROWS = 32 * SLOT + NOV * 128

COLT = []
def _ct(name, start, width):
    COLT.append((name, start, width))
for i in range(2): _ct(f"s5u{i}", 0 + 128 * i, 128)
for i in range(2): _ct(f"cq{i}", 256 + 128 * i, 128)
_ct("ckv", 512, 128)
_ct("kpeA", 5792, 96)
_ct("kpeB", 5792 + 96, 96)
for i in range(4): _ct(f"hq{i}", 672 + 128 * i, 128)
for i in range(4): _ct(f"hf{i}", 1184 + 128 * i, 128)
for i in range(4): _ct(f"hb{i}", 1696 + 128 * i, 128)
for i in range(4): _ct(f"hi{i}", 2208 + 64 * i, 64)
for i in range(4): _ct(f"hg{i}", 2464 + 64 * i, 64)
N_NONGATE = len(COLT)
for b in range(3):
    for i in range(8): _ct(f"gate{b}_{i}", 2720 + 1024 * b + 128 * i, 128)
COLIDX = {n: i for i, (n, _, _) in enumerate(COLT)}
WINX = 5792 + 192

VEC_ROWS = {}
def _vr(name, n):
    VEC_ROWS[name] = (sum(v[1] for v in VEC_ROWS.values()), n)
_vr("c", 8); _vr("c_ctx", 8); _vr("final_g", 8)
for l in range(DEPTH):
    _vr(f"norm1_g{l}", 8); _vr(f"norm2_g{l}", 8); _vr(f"mod_b{l}", 48)
    _vr(f"s5_d{l}", 2); _vr(f"glu_b{l}", 2); _vr(f"qa_g{l}", 2); _vr(f"kva_g{l}", 1)
    _vr(f"hgn_g{l}", 4)
    for d in range(2):
        _vr(f"lam_re{l}{d}", 8); _vr(f"lam_im{l}{d}", 8); _vr(f"lstep{l}{d}", 8)
NVROWS = sum(v[1] for v in VEC_ROWS.values())
NVBLK = (NVROWS + 127) // 128


_UQ = [0]


def uq(name):
    _UQ[0] += 1
    return f"{name}~{_UQ[0]}"


class Buf:
    __slots__ = ("name", "w", "r")

    def __init__(self, name):
        self.name = name
        self.w = None
        self.r = {}


class KB:
    RING = 12

    def __init__(self, nc, es):
        self.nc, self.es = nc, es
        self.eng = dict(pe=nc.tensor, dve=nc.vector, act=nc.scalar, pool=nc.gpsimd, sp=nc.sync)
        self.psem, self.pcnt, self.nsem = {}, {}, 0
        for e in self.eng:
            self._new_psem(e)
        self.waited = {}
        self.rings = {}
        self.rpos = {}
        for q in ("sp", "pool", "act"):
            self.rings[q] = [[self._sem(f"dq_{q}{i}"), 0] for i in range(self.RING)]
            self.rpos[q] = 0
        self.bufs = {}
        self.ninstr = 0

    def _sem(self, name):
        self.nsem += 1
        return self.es.enter_context(self.nc.semaphore(name))

    def _new_psem(self, e):
        self.psem[e] = self._sem(f"p_{e}_{self.nsem}")
        self.pcnt[e] = 0

    def buf(self, name):
        b = self.bufs.get(name)
        if b is None:
            b = self.bufs[name] = Buf(name)
        return b

    def _wait(self, e, tok):
        sem, val, src = tok
        key = (e, id(sem))
        if self.waited.get(key, 0) >= val:
            return
        self.eng[e].wait_ge(sem, val)
        self.waited[key] = val

    def _sync(self, e, reads, writes):
        for b in reads:
            if b.w is not None:
                self._dep(e, b.w)
        for b in writes:
            if b.w is not None:
                self._dep(e, b.w)
            for t in b.r.values():
                self._dep(e, t)

    def _dep(self, e, tok):
        if e == "pe" and tok[2] == "pe":
            return
        self._wait(e, tok)

    def _commit(self, tok, reads, writes):
        for b in writes:
            b.w = tok
            b.r = {}
        for b in reads:
            b.r[tok[2]] = tok

    def op(self, e, fn, reads=(), writes=()):
        reads = [b if isinstance(b, Buf) else self.buf(self._nm(b)) for b in reads]
        writes = [b if isinstance(b, Buf) else self.buf(self._nm(b)) for b in writes]
        self._sync(e, reads, writes)
        ins = fn(self.eng[e])
        if self.pcnt[e] >= 20000:
            self._new_psem(e)
        self.pcnt[e] += 1
        ins.then_inc(self.psem[e], 1)
        self.ninstr += 1
        self._commit((self.psem[e], self.pcnt[e], e), reads, writes)

    def dma(self, q, out, in_, reads=(), writes=()):
        reads = [b if isinstance(b, Buf) else self.buf(self._nm(b)) for b in reads]
        writes = [b if isinstance(b, Buf) else self.buf(self._nm(b)) for b in writes]
        self._sync(q, reads, writes)
        slot = self.rings[q][self.rpos[q] % self.RING]
        self.rpos[q] += 1
        sem, cnt = slot
        if cnt > 0:
            self._wait(q, (sem, cnt, "dma"))
        self.eng[q].dma_start(out=out, in_=in_).then_inc(sem, 16)
        slot[1] = cnt + 16
        self.ninstr += 1
        self._commit((sem, cnt + 16, f"dma_{q}{(self.rpos[q] - 1) % self.RING}"), reads, writes)

    @staticmethod
    def _nm(x):
        if isinstance(x, str):
            return x
        if isinstance(x, tuple):
            return KB._nm(x[0]) + x[1]
        if hasattr(x, "tensor"):
            return x.tensor.name.split("~")[0]
        return x.name.split("~")[0]

    def _names(self, xs):
        return [self._nm(x) for x in xs if not isinstance(x, (int, float)) and x is not None]

    def tt(self, e, out, a, b, op, rn=(), wn=()):
        self.op(e, lambda g: g.tensor_tensor(out, a, b, op), reads=self._names([a, b]) + list(rn), writes=self._names([out]) + list(wn))

    def ts(self, e, out, a, s1, op0, s2=None, op1=None, rn=(), wn=()):
        if op1 is None:
            self.op(e, lambda g: g.tensor_scalar(out, a, s1, None, op0), reads=self._names([a, s1]) + list(rn), writes=self._names([out]) + list(wn))
        else:
            self.op(e, lambda g: g.tensor_scalar(out, a, s1, s2, op0, op1), reads=self._names([a, s1, s2]) + list(rn), writes=self._names([out]) + list(wn))

    def stt(self, e, out, a, sc_, b, op0, op1, rn=(), wn=()):
        self.op(e, lambda g: g.scalar_tensor_tensor(out, a, sc_, b, op0, op1), reads=self._names([a, sc_, b]) + list(rn), writes=self._names([out]) + list(wn))

    def act(self, out, in_, func, bias=None, scale=1.0, rn=(), wn=()):
        kw = {}
        if bias is not None:
            kw["bias"] = bias
        self.op("act", lambda g: g.activation(out, in_, func, scale=scale, **kw), reads=self._names([in_, bias, scale]) + list(rn), writes=self._names([out]) + list(wn))

    def cp(self, e, out, in_, rn=(), wn=()):
        if e == "act":
            self.op(e, lambda g: g.copy(out, in_), reads=self._names([in_]) + list(rn), writes=self._names([out]) + list(wn))
        else:
            self.op(e, lambda g: g.tensor_copy(out, in_), reads=self._names([in_]) + list(rn), writes=self._names([out]) + list(wn))

    def mm(self, out, lhsT, rhs, start=True, stop=True, rn=(), wn=()):
        self.op("pe", lambda g: g.matmul(out, lhsT, rhs, start=start, stop=stop), reads=self._names([lhsT, rhs]) + list(rn), writes=self._names([out]) + list(wn))

    def tr(self, out, in_, ident, rn=(), wn=()):
        self.op("pe", lambda g: g.transpose(out, in_, ident), reads=self._names([in_, ident]) + list(rn), writes=self._names([out]) + list(wn))

    def scan(self, out, d0, d1, init, op0, op1, rn=(), wn=()):
        self.op("dve", lambda g: g.tensor_tensor_scan(out, d0, d1, init, op0, op1), reads=self._names([d0, d1, init]) + list(rn), writes=self._names([out]) + list(wn))

    def recip(self, out, in_):
        self.op("dve", lambda g: g.reciprocal(out, in_), reads=self._names([in_]), writes=self._names([out]))

    def memset(self, e, out, val):
        self.op(e, lambda g: g.memset(out, val), reads=[], writes=self._names([out]))

    def barrier(self):
        toks = [(self.psem[o], self.pcnt[o], o) for o in self.eng if self.pcnt[o] > 0]
        for q in self.rings:
            for sem, cnt in self.rings[q]:
                if cnt > 0:
                    toks.append((sem, cnt, "dma"))
        for e in self.eng:
            for t in toks:
                if t[2] != e:
                    self._wait(e, t)

    def idma(self, out, out_off, in_, in_off, reads=(), writes=(), bounds=None):
        q = "pool"
        reads = [b if isinstance(b, Buf) else self.buf(self._nm(b)) for b in reads]
        writes = [b if isinstance(b, Buf) else self.buf(self._nm(b)) for b in writes]
        self._sync(q, reads, writes)
        slot = self.rings[q][self.rpos[q] % self.RING]
        self.rpos[q] += 1
        sem, cnt = slot
        if cnt > 0:
            self._wait(q, (sem, cnt, "dma"))
        if bounds is None:
            self.eng[q].indirect_dma_start(out=out, out_offset=out_off, in_=in_, in_offset=in_off).then_inc(sem, 16)
        else:
            if not hasattr(self, "_breg") or self._breg[0] != bounds:
                self._breg = (bounds, self.eng[q].to_reg(bounds))
            self.eng[q].indirect_dma_start(out=out, out_offset=out_off, in_=in_, in_offset=in_off,
                                           bounds_check=self._breg[1], oob_is_err=False).then_inc(sem, 16)
        slot[1] = cnt + 16
        self.ninstr += 1
        self._commit((sem, cnt + 16, f"dma_{q}{(self.rpos[q] - 1) % self.RING}"), reads, writes)

    def finish(self, bufs):
        for b in bufs:
            b = self.buf(b) if isinstance(b, str) else b
            if b.w is not None:
                self._wait("sp", b.w)


def build_nc(stop_after=None, debug=False):
    nc = bass.Bass("TRN2", target_bir_lowering=False)
    okind = "ExternalOutput" if debug else "Internal"
    x_d = nc.dram_tensor("x", [NLAT, D], F32, kind="ExternalInput").ap()
    ctx_d = nc.dram_tensor("ctx", [NCTX, D], F32, kind="ExternalInput").ap()
    vecs_d = nc.dram_tensor("vecs", [NVBLK * 128, 128], F32, kind="ExternalInput").ap()
    consts_d = nc.dram_tensor("consts", [128, 256], F32, kind="ExternalInput").ap()
    modw_d = nc.dram_tensor("mod_w", [DEPTH, D, 6 * D], F32, kind="ExternalInput").ap()
    winx_d = nc.dram_tensor("w_in_x", [DEPTH, D, WINX], F32, kind="ExternalInput").ap()
    out_d = nc.dram_tensor("out", [NLAT, D], F32, kind="ExternalOutput").ap()
    zT_d = nc.dram_tensor("zT", [len(COLT) * 128, NT], F32, kind=okind).ap()
    ybT_d = nc.dram_tensor("ybT", [D, NT], F32, kind=okind).ap()
    s5B_d = nc.dram_tensor("s5B", [DEPTH, 2, 2, 8, 128, 128], F32, kind="ExternalInput").ap()
    s5C_d = nc.dram_tensor("s5C", [DEPTH, 2, 2, 8, 128, 128], F32, kind="ExternalInput").ap()
    gluw_d = nc.dram_tensor("s5_glu_w", [DEPTH, 256, 256], F32, kind="ExternalInput").ap()
    rope_d = nc.dram_tensor("rope", [128, 2, NLAT], F32, kind="ExternalInput").ap()
    wq_d = nc.dram_tensor("wq", [DEPTH, 256, 2, 768], F32, kind="ExternalInput").ap()
    wuk_d = nc.dram_tensor("mla_w_uk", [DEPTH, 128, 512], F32, kind="ExternalInput").ap()
    wuv_d = nc.dram_tensor("mla_w_uv", [DEPTH, 128, 512], F32, kind="ExternalInput").ap()
    lbl_d = nc.dram_tensor("hg_lb_logits", [1, DEPTH], F32, kind="ExternalInput").ap()
    wp_d = nc.dram_tensor("wp", [DEPTH, D, D], F32, kind="ExternalInput").ap()
    wo_d = nc.dram_tensor("w_out", [DEPTH, D, D], F32, kind="ExternalInput").ap()
    wr_d = nc.dram_tensor("wr", [DEPTH, D, 36], F32, kind="ExternalInput").ap()
    br_d = nc.dram_tensor("br", [DEPTH, 1, 36], F32, kind="ExternalInput").ap()
    w1_d = nc.dram_tensor("moe_w1", [DEPTH, 32 * 128, 8 * 512], F32, kind="ExternalInput").ap()
    w3_d = nc.dram_tensor("moe_w3", [DEPTH, 32 * 128, 8 * 512], F32, kind="ExternalInput").ap()
    w2_d = nc.dram_tensor("moe_w2", [DEPTH, 32 * 128, 4 * D], F32, kind="ExternalInput").ap()
    h2tok_d = nc.dram_tensor("h2tok", [NT, D], F32, kind=okind).ap()
    xs_d = nc.dram_tensor("xs", [NROWS, D], F32, kind=okind).ap()
    ys_d = nc.dram_tensor("ys", [NROWS, D], F32, kind=okind).ap()
    xdbg_d = nc.dram_tensor("xdbg", [128, 8 * NT], F32, kind=okind).ap()
    modT_d = nc.dram_tensor("modT", [DEPTH, 128, 96], F32, kind=okind).ap()

    with ExitStack() as es:
        kb = KB(nc, es)
        sb = lambda name, shape, dt=F32: es.enter_context(nc.sbuf_tensor(uq(name), shape, dt))

        xT = sb("xT", [128, 8, NT])
        cst = sb("cst", [128, 256])
        ident = cst[:, 0:128]
        onesr = sb("onesr", [128, 128], F32R)
        vecT = sb("vecT", [128, NVBLK * 128])
        modT = sb("modT_sb", [128, DEPTH, 48, 2])
        modA = sb("modA", [128, DEPTH, 2, 8, 2])
        sc = sb("sc", [128, 8, 2], F32R)

        def V(name, j=0):
            r0, n = VEC_ROWS[name]
            return vecT[:, r0 + j:r0 + j + 1]

        def Vn(name):
            r0, n = VEC_ROWS[name]
            return vecT[:, r0:r0 + n]

        kb.dma("sp", cst[:], consts_d, writes=["cst"])
        kb.dma("pool", onesr[:], consts_d[:, 128:256], writes=["onesr"])

        with ExitStack() as ph:
            psb = lambda name, shape, dt=F32: ph.enter_context(nc.sbuf_tensor(uq(name), shape, dt))
            pps = lambda name, shape, dt=F32: ph.enter_context(nc.psum_tensor(uq(name), shape, dt))
            stg = [psb(f"xstg{i}", [128, D]) for i in range(3)]
            tps = [pps(f"tps{i}", [128, 4, 128]) for i in range(4)]
            for blk in range(NVBLK):
                s = stg[blk % 3]
                kb.dma("sp", s[:, 0:128], vecs_d[blk * 128:(blk + 1) * 128, :], writes=[f"xstg{blk % 3}"])
                kb.op("pe", lambda e: e.transpose(tps[blk % 4][:, 0, :], s[:, 0:128], ident),
                      reads=[f"xstg{blk % 3}", "cst"], writes=[f"tps{blk % 4}"])
                kb.op("dve", lambda e: e.tensor_copy(vecT[:, blk * 128:(blk + 1) * 128], tps[blk % 4][:, 0, :]),
                      reads=[f"tps{blk % 4}"], writes=["vecT"])
            n_tt = NT // 128
            for tt in range(n_tt):
                s = stg[tt % 3]
                src = ctx_d[tt * 128:(tt + 1) * 128, :] if tt < 2 else x_d[(tt - 2) * 128:(tt - 1) * 128, :]
                kb.dma("sp", s[:], src, writes=[f"xstg{tt % 3}"])
                for half in range(2):
                    pi = (2 * tt + half) % 4
                    for j in range(4):
                        k = half * 4 + j
                        kb.op("pe", lambda e: e.transpose(tps[pi][:, j, :], s[:, k * 128:(k + 1) * 128], ident),
                              reads=[f"xstg{tt % 3}", "cst"], writes=[f"tps{pi}"])
                    eng = "dve" if half == 0 else "act"
                    dst = xT[:, half * 4:half * 4 + 4, tt * 128:(tt + 1) * 128]
                    if eng == "dve":
                        kb.op("dve", lambda e: e.tensor_copy(dst, tps[pi][:]), reads=[f"tps{pi}"], writes=["xT"])
                    else:
                        kb.op("act", lambda e: e.copy(dst, tps[pi][:]), reads=[f"tps{pi}"], writes=["xT"])
            kb.barrier()

        kb.op("act", lambda e: e.activation(sc[:, :, 0], Vn("c"), AF.Silu), reads=["vecT"], writes=["sc"])
        kb.op("act", lambda e: e.activation(sc[:, :, 1], Vn("c_ctx"), AF.Silu), reads=["vecT"], writes=["sc"])

        xTd_d = nc.dram_tensor("xTd", [128, 8 * NT], F32, kind=okind).ap()
        if debug and stop_after == "p0":
            kb.dma("sp", xTd_d, xT[:].rearrange("p a b -> p (a b)"), reads=["xT"], writes=["xTd"])
        lbv = sb("lbv", [128, 2, DEPTH])
        lbl = sb("lbl", [128, DEPTH])
        kb.dma("sp", lbl[:], lbl_d.to_broadcast([128, DEPTH]), writes=["lbl"])
        kb.memset("dve", lbv[:, 0, :], 0.0)
        kb.tt("dve", lbv[:, 0, 1:2], lbl[:, 1:2], lbl[:, 0:1], ALU.subtract)
        kb.act(lbv[:, 0, 1:2], lbv[:, 0, 1:2], AF.Sigmoid)
        kb.ts("dve", lbv[:, 1, :], lbv[:, 0, :], -1.0, ALU.mult, 1.0, ALU.add)
        for layer in range(DEPTH if stop_after != "p0" else 0):
            L = layer
            with ExitStack() as ph:
                psb = lambda name, shape, dt=F32: ph.enter_context(nc.sbuf_tensor(uq(name), shape, dt))
                pps = lambda name, shape, dt=F32: ph.enter_context(nc.psum_tensor(uq(name), shape, dt))
                mw = [psb(f"mw{i}", [128, 8, 128], F32R) for i in range(3)]
                mps = pps("mps", [128, 48, 2])
                for j in range(48):
                    w = mw[j % 3]
                    kb.dma("pool", w[:], modw_d[L, :, j * 128:(j + 1) * 128].rearrange("(k p) c -> p k c", p=128),
                           writes=[f"mw{j % 3}"])
                    for k in range(8):
                        kb.op("pe", lambda e: e.matmul(mps[:, j, :], w[:, k, :], sc[:, k, :], start=(k == 0), stop=(k == 7)),
                              reads=[f"mw{j % 3}", "sc"], writes=["mps"])
                for s in range(2):
                    kb.op("dve", lambda e: e.tensor_tensor(modT[:, L, :, s], mps[:, :, s], Vn(f"mod_b{L}"), ALU.add),
                          reads=["mps", "vecT"], writes=["modT"])
                for ni, (gname, t0) in enumerate(((f"norm1_g{L}", 8), (f"norm2_g{L}", 32))):
                    for s in range(2):
                        kb.op("dve", lambda e: e.scalar_tensor_tensor(
                            modA[:, L, ni, :, s], modT[:, L, t0:t0 + 8, s], 1.0, Vn(gname), ALU.add, ALU.mult),
                            reads=["modT", "vecT"], writes=["modA"])
                if debug:
                    kb.dma("sp", modT_d[L], modT[:, L].rearrange("p a b -> p (a b)"), reads=["modT"], writes=["modT_d"])
                kb.barrier()
            if stop_after == f"mod{L}":
                break

            with ExitStack() as ph:
                psb = lambda name, shape, dt=F32: ph.enter_context(nc.sbuf_tensor(uq(name), shape, dt))
                pps = lambda name, shape, dt=F32: ph.enter_context(nc.psum_tensor(uq(name), shape, dt))
                hT = psb("hT", [128, 8, NT], F32R)
                emit_norm(nc, kb, xT, hT, [(t0, n, t0) for (t0, n) in CHUNKS],
                          lambda k, s_: modA[:, L, 0, k, s_:s_ + 1], lambda k, s_: modT[:, L, k, s_:s_ + 1], onesr, D)
                wb = [psb(f"wb{i}", [128, 8, 128], F32R) for i in range(3)]
                zs = [psb(f"zs{i}", [128, 512]) for i in range(4)]
                zp = [pps(f"zp{i}", [128, 512]) for i in range(4)]
                cnt = 0
                for ti, (name, c0, wd) in enumerate(COLT):
                    w = wb[ti % 3]
                    kb.dma("pool", w[:, :, 0:wd], winx_d[L, :, c0:c0 + wd].rearrange("(k p) c -> p k c", p=128),
                           writes=[f"wb{ti % 3}"])
                    for (t0, n) in CHUNKS:
                        pi = cnt % 4
                        cnt += 1
                        for k in range(8):
                            kb.op("pe", lambda e: e.matmul(zp[pi][0:wd, 0:n], w[:, k, 0:wd], hT[:, k, t0:t0 + n],
                                                           start=(k == 0), stop=(k == 7)),
                                  reads=[f"wb{ti % 3}", "hT"], writes=[f"zp{pi}"])
                        if cnt % 2 == 0:
                            kb.op("dve", lambda e: e.tensor_copy(zs[pi][0:wd, 0:n], zp[pi][0:wd, 0:n]),
                                  reads=[f"zp{pi}"], writes=[f"zs{pi}"])
                        else:
                            kb.op("act", lambda e: e.copy(zs[pi][0:wd, 0:n], zp[pi][0:wd, 0:n]),
                                  reads=[f"zp{pi}"], writes=[f"zs{pi}"])
                        kb.dma("sp", zT_d[ti * 128:ti * 128 + wd, t0:t0 + n], zs[pi][0:wd, 0:n],
                               reads=[f"zs{pi}"], writes=[f"zT_{ti}"])
                kb.barrier()
            if stop_after == f"A{L}":
                break
            G = dict(nc=nc, kb=kb, L=L, xT=xT, cst=cst, ident=ident, onesr=onesr, vecT=vecT, modT=modT, modA=modA,
                     V=V, Vn=Vn, zT_d=zT_d, ybT_d=ybT_d, s5B_d=s5B_d, s5C_d=s5C_d, gluw_d=gluw_d, debug=debug,
                     rope_d=rope_d, wq_d=wq_d, wuk_d=wuk_d, wuv_d=wuv_d)
            if not (debug and stop_after in (f"C{L}", f"D{L}")):
                phase_s5(G)
            if stop_after == f"B{L}":
                break
            G["lbv"] = lbv
            if not (debug and stop_after in (f"D{L}",)):
                phase_mla(G)
            if stop_after == f"C{L}":
                break
            G.update(wp_d=wp_d, wo_d=wo_d, wr_d=wr_d, br_d=br_d, w1_d=w1_d, w3_d=w3_d, w2_d=w2_d, out_d=out_d,
                     h2tok_d=h2tok_d, xs_d=xs_d, ys_d=ys_d)
            phase_hg(G)
            if stop_after == f"D{L}":
                break
            phase_merge(G)
            if stop_after == f"E{L}":
                kb.dma("sp", xdbg_d, xT[:].rearrange("p a b -> p (a b)"), reads=["xT"], writes=["xdbg"])
                break
            phase_moe_sparse(G)
            if stop_after == f"F{L}":
                kb.dma("sp", xdbg_d, xT[:].rearrange("p a b -> p (a b)"), reads=["xT"], writes=["xdbg"])
                break
        else:
            phase_final(G)

        kb.finish(list(kb.bufs.values()))
        print("instructions:", kb.ninstr, "sems:", kb.nsem, "sbuf left:", nc.sbuf_bytes_remaining)
    return nc


def emit_norm(nc, kb, xT, dst, chunks, A, Sh, onesr, dmodel):
    with ExitStack() as ns:
        psb = lambda name, shape, dt=F32: ns.enter_context(nc.sbuf_tensor(uq(name), shape, dt))
        pps = lambda name, shape, dt=F32: ns.enter_context(nc.psum_tensor(uq(name), shape, dt))
        sq = [psb(f"nsq{i}", [128, 8, 512], F32R) for i in range(2)]
        ms = [pps(f"nms{i}", [128, 512]) for i in range(2)]
        rs = [psb(f"nrs{i}", [128, 512]) for i in range(2)]
        tmp = [psb(f"ntmp{i}", [128, 512]) for i in range(2)]
        for ci, (t0, n, d0) in enumerate(chunks):
            s = 1 if t0 < NCTX else 0
            b = ci % 2
            for k in range(8):
                kb.act(sq[b][:, k, 0:n], xT[:, k, t0:t0 + n], AF.Square)
            for k in range(8):
                kb.mm(ms[b][:, 0:n], onesr[:], sq[b][:, k, 0:n], start=(k == 0), stop=(k == 7))
            kb.act(rs[b][:, 0:n], ms[b][:, 0:n], AF.Sqrt, scale=1.0 / dmodel, bias=EPS)
            kb.recip(rs[b][:, 0:n], rs[b][:, 0:n])
            for k in range(8):
                tb = k % 2
                sh = Sh(k, s) if Sh is not None else None
                if sh is None:
                    kb.stt("dve", dst[:, k, d0:d0 + n], xT[:, k, t0:t0 + n], A(k, s), rs[b][:, 0:n], ALU.mult, ALU.mult)
                else:
                    kb.stt("dve", tmp[tb][:, 0:n], xT[:, k, t0:t0 + n], A(k, s), rs[b][:, 0:n], ALU.mult, ALU.mult)
                    kb.act(dst[:, k, d0:d0 + n], tmp[tb][:, 0:n], AF.Identity, bias=sh, scale=1.0)
        kb.barrier()


TWO_PI = 2.0 * math.pi


def range_reduce(kb, r, x, tM, tI):
    kb.ts("dve", tM, x, 1.0 / TWO_PI, ALU.mult)
    kb.cp("dve", tI, tM)
    kb.cp("dve", tM, tI)
    kb.stt("dve", r, tM, -TWO_PI, x, ALU.mult, ALU.add)
    kb.ts("dve", tM, r, math.pi, ALU.is_gt)
    kb.stt("dve", r, tM, -TWO_PI, r, ALU.mult, ALU.add)
    kb.ts("dve", tM, r, -math.pi, ALU.is_lt)
    kb.stt("dve", r, tM, TWO_PI, r, ALU.mult, ALU.add)
    kb.ts("dve", r, r, 3.1415925, ALU.min, -3.1415925, ALU.max)


def sincos(kb, sn, cs, x, r, tM, tI):
    range_reduce(kb, r, x, tM, tI)
    kb.act(sn, r, AF.Sin)
    kb.ts("dve", tM, x, math.pi / 2, ALU.add)
    range_reduce(kb, r, tM, tM, tI) if False else None
    return


def phase_s5(G):
    nc, kb, L = G["nc"], G["kb"], G["L"]
    Vn = G["Vn"]
    zT_d, ybT_d = G["zT_d"], G["ybT_d"]
    T = 256
    NCH = NT // T
    with ExitStack() as ph:
        psb = lambda name, shape, dt=F32: ph.enter_context(nc.sbuf_tensor(uq(name), shape, dt))
        pps = lambda name, shape, dt=F32: ph.enter_context(nc.psum_tensor(uq(name), shape, dt))
        uT = psb("s5_uT", [128, 2, NT], F32R)
        yacc = psb("s5_yacc", [128, 2, NT])
        for t in range(2):
            ti = COLIDX[f"s5u{t}"]
            kb.dma("pool", uT[:, t, :], zT_d[ti * 128:(ti + 1) * 128, :], reads=[f"zT_{ti}"], writes=["s5_uT"])
        Bw = psb("s5_Bw", [128, 2, 8, 128], F32R)
        Cw = psb("s5_Cw", [128, 2, 8, 128], F32R)
        iota_i = psb("s5_iota_i", [128, T + 1], I32)
        iota_f = psb("s5_iota_f", [128, T + 1])
        kb.op("pool", lambda g: g.iota(iota_i[:], [[1, T + 1]], base=0, channel_multiplier=0), writes=["s5_iota_i"])
        kb.cp("dve", iota_f[:], iota_i[:])
        COS = psb("s5_COS", [128, 8, T + 1])
        SIN = psb("s5_SIN", [128, 8, T + 1])
        ang = psb("s5_ang", [128, 8, T + 1])
        rr = psb("s5_rr", [128, 8, T + 1])
        ERE = ang[:, :, 0:T]
        EIM = rr[:, :, 0:T]
        tM = psb("s5_tM", [128, 8, T + 1])
        tI = psb("s5_tI", [128, 8, T + 1], I32)
        sm = psb("s5_sm", [128, 24, 8])
        gin = psb("s5_gin", [128, 8, 2])
        tmp = [[psb(f"s5_t{b}_{i}", [128, T]) for i in range(6)] for b in range(3)]
        tmpB = [[psb(f"s5_g{b}_{i}", [128, T]) for i in range(2)] for b in range(2)]
        hh = [[psb(f"s5_h{b}_{i}", [128, T], F32R) for i in range(2)] for b in range(2)]
        Pp = [pps(f"s5_P{i}", [128, 2, T]) for i in range(3)]
        Yp = [[pps(f"s5_Y{b}_{ct}", [128, 512]) for ct in range(2)] for b in range(2)]
        flat = lambda t: t[:].rearrange("p a b -> p (a b)")
        for d in range(2):
            kb.dma("pool", Bw[:].rearrange("c r s q -> c (r s) q"),
                   G["s5B_d"][L, d].rearrange("r s c q -> c (r s) q"), writes=["s5_Bw"])
            kb.dma("pool", Cw[:].rearrange("c r s q -> c (r s) q"),
                   G["s5C_d"][L, d].rearrange("r s c q -> c (r s) q"), writes=["s5_Cw"])
            kb.ts("pool", Cw[:, 1], Cw[:, 1].bitcast(F32), -1.0, ALU.mult)
            lre, lim, lst = Vn(f"lam_re{L}{d}"), Vn(f"lam_im{L}{d}"), Vn(f"lstep{L}{d}")
            c_ = lambda i: sm[:, i, :]
            DT, MAG, TH, SN, CS, LBR, LBI, DEN, FR, FI, X1, X2, X3, MI = (c_(i) for i in range(14))
            kb.act(DT, lst, AF.Exp)
            kb.tt("dve", X1, lre, DT, ALU.mult)
            kb.act(MAG, X1, AF.Exp)
            kb.tt("dve", TH, lim, DT, ALU.mult)
            smI = tI[:, 0, 0:8]
            range_reduce(kb, X2, TH, X3, smI)
            kb.act(SN, X2, AF.Sin)
            kb.ts("dve", X1, TH, math.pi / 2, ALU.add)
            range_reduce(kb, X2, X1, X3, smI)
            kb.act(CS, X2, AF.Sin)
            kb.tt("dve", LBR, MAG, CS, ALU.mult)
            kb.tt("dve", LBI, MAG, SN, ALU.mult)
            kb.tt("dve", X1, lre, lre, ALU.mult)
            kb.tt("dve", X2, lim, lim, ALU.mult)
            kb.tt("dve", DEN, X1, X2, ALU.add)
            kb.recip(DEN, DEN)
            kb.ts("dve", X3, LBR, -1.0, ALU.add)
            kb.tt("dve", X1, X3, lre, ALU.mult)
            kb.tt("dve", X2, LBI, lim, ALU.mult)
            kb.tt("dve", X1, X1, X2, ALU.add)
            kb.tt("dve", FR, X1, DEN, ALU.mult)
            kb.tt("dve", X1, LBI, lre, ALU.mult)
            kb.tt("dve", X2, X3, lim, ALU.mult)
            kb.tt("dve", X1, X1, X2, ALU.subtract)
            kb.tt("dve", FI, X1, DEN, ALU.mult)
            kb.tt("dve", ang[:], TH.unsqueeze(2).to_broadcast([128, 8, T + 1]),
                  iota_f[:].unsqueeze(1).to_broadcast([128, 8, T + 1]), ALU.mult)
            range_reduce(kb, flat(rr), flat(ang), flat(tM), flat(tI))
            kb.act(flat(SIN), flat(rr), AF.Sin)
            kb.ts("dve", flat(ang), flat(ang), math.pi / 2, ALU.add)
            range_reduce(kb, flat(rr), flat(ang), flat(tM), flat(tI))
            kb.act(flat(COS), flat(rr), AF.Sin)
            frb = FR.unsqueeze(2).to_broadcast([128, 8, T])
            fib = FI.unsqueeze(2).to_broadcast([128, 8, T])
            tF = tI[:].bitcast(F32)
            kb.tt("dve", tM[:, :, 0:T], COS[:, :, 0:T], frb, ALU.mult)
            kb.tt("dve", tF[:, :, 0:T], SIN[:, :, 0:T], fib, ALU.mult)
            kb.tt("dve", ERE, tM[:, :, 0:T], tF[:, :, 0:T], ALU.add)
            kb.tt("dve", tM[:, :, 0:T], COS[:, :, 0:T], fib, ALU.mult)
            kb.tt("dve", tF[:, :, 0:T], SIN[:, :, 0:T], frb, ALU.mult)
            kb.tt("dve", EIM, tM[:, :, 0:T], tF[:, :, 0:T], ALU.subtract)
            kb.memset("dve", gin[:], 0.0)
            NST = sm[:, 18, :]
            kb.ts("dve", NST, SIN[:, :, T], -1.0, ALU.mult)
            order = list(range(NCH)) if d == 0 else [0] + list(range(NCH - 1, 0, -1))
            units = [(oi, ci, s_) for oi, ci in enumerate(order) for s_ in range(8)]

            def stageA(u):
                oi, ci, s_ = units[u]
                t0 = ci * T
                ct = s_ // 4
                tq = tmp[u % 3]
                P = Pp[u % 3]
                kb.mm(P[:, 0, :], Bw[:, 0, s_, :], uT[:, ct, t0:t0 + T])
                kb.mm(P[:, 1, :], Bw[:, 1, s_, :], uT[:, ct, t0:t0 + T])
                Pre = P[:, 0, ::-1] if d == 1 else P[:, 0, :]
                Pim = P[:, 1, ::-1] if d == 1 else P[:, 1, :]
                kb.tt("dve", tq[0][:], ERE[:, s_, :], Pre, ALU.mult)
                kb.tt("dve", tq[1][:], EIM[:, s_, :], Pim, ALU.mult)
                kb.tt("pool", tq[4][:], tq[0][:], tq[1][:], ALU.subtract)
                kb.tt("dve", tq[2][:], ERE[:, s_, :], Pim, ALU.mult)
                kb.tt("dve", tq[3][:], EIM[:, s_, :], Pre, ALU.mult)
                kb.tt("pool", tq[5][:], tq[2][:], tq[3][:], ALU.add)

            def stageB(u):
                oi, ci, s_ = units[u]
                t0 = ci * T
                ct = s_ // 4
                yb_ = oi % 2
                tq = tmp[u % 3]
                gq = tmpB[u % 2]
                hb = hh[u % 2]
                rb = MAG[:, s_:s_ + 1].to_broadcast([128, T])
                kb.scan(gq[0][:], rb, tq[4][:], gin[:, s_, 0:1], ALU.mult, ALU.add)
                kb.scan(gq[1][:], rb, tq[5][:], gin[:, s_, 1:2], ALU.mult, ALU.add)
                cT, sT = COS[:, s_, T:T + 1], SIN[:, s_, T:T + 1]
                lr, li = gq[0][:, T - 1:T], gq[1][:, T - 1:T]
                xa, xb = sm[:, 14 + (u % 2) * 2, 0:1], sm[:, 15 + (u % 2) * 2, 0:1]
                kb.act(xa, li, AF.Identity, scale=NST[:, s_:s_ + 1])
                kb.act(xb, li, AF.Identity, scale=cT)
                kb.act(gin[:, s_, 0:1], lr, AF.Identity, bias=xa, scale=cT)
                kb.act(gin[:, s_, 1:2], lr, AF.Identity, bias=xb, scale=sT)
                kb.tt("pool", tq[0][:], COS[:, s_, 0:T], gq[0][:], ALU.mult)
                kb.tt("pool", tq[1][:], SIN[:, s_, 0:T], gq[1][:], ALU.mult)
                kb.tt("pool", hb[0][:], tq[0][:], tq[1][:], ALU.subtract)
                kb.tt("dve", tq[2][:], SIN[:, s_, 0:T], gq[0][:], ALU.mult)
                kb.tt("dve", tq[3][:], COS[:, s_, 0:T], gq[1][:], ALU.mult)
                kb.tt("pool", hb[1][:], tq[2][:], tq[3][:], ALU.add)
                Y = Yp[yb_][ct]
                kb.mm(Y[:, 0:T], Cw[:, 0, s_, :], hb[0][:], start=(s_ % 4 == 0), stop=False)
                kb.mm(Y[:, 0:T], Cw[:, 1, s_, :], hb[1][:], start=False, stop=(s_ % 4 == 3))
                if s_ == 7:
                    for ct2 in range(2):
                        Y2 = Yp[yb_][ct2]
                        if d == 0:
                            kb.cp("act", yacc[:, ct2, t0:t0 + T], Y2[:, 0:T])
                        else:
                            rv = slice(t0 + T - 1, (t0 - 1 if t0 > 0 else None), -1)
                            kb.tt("dve", yacc[:, ct2, rv], yacc[:, ct2, rv], Y2[:, 0:T], ALU.add)

            stageA(0)
            stageA(1)
            for u in range(len(units)):
                if u + 2 < len(units):
                    stageA(u + 2)
                stageB(u)
        gw = psb("s5_gw", [128, 2, 256], F32R)
        kb.dma("pool", gw[:], G["gluw_d"][L].rearrange("(k p) c -> p k c", p=128), writes=["s5_gw"])
        y1t = [hh[0][0], hh[0][1]]
        for ci in range(NCH):
            sl = slice(ci * T, (ci + 1) * T)
            for ct in range(2):
                a, b2, c2 = tmp[ct][0], tmp[ct][1], tmp[ct][2]
                kb.stt("dve", a[:], uT[:, ct, sl].bitcast(F32), Vn(f"s5_d{L}")[:, ct:ct + 1], yacc[:, ct, sl], ALU.mult, ALU.add)
                kb.tt("dve", b2[:], a[:], a[:], ALU.mult)
                kb.ts("dve", b2[:], b2[:], 0.044715, ALU.mult, 1.0, ALU.add)
                kb.tt("dve", b2[:], b2[:], a[:], ALU.mult)
                kb.act(c2[:], b2[:], AF.Sigmoid, scale=1.5957691216057308)
                kb.tt("dve", y1t[ct][:], a[:], c2[:], ALU.mult)
            for ct in range(2):
                Y = Yp[ci % 2][ct]
                for k in range(2):
                    kb.mm(Y[:, 0:T], gw[:, k, ct * 128:(ct + 1) * 128], y1t[k][:], start=(k == 0), stop=(k == 1))
                sg = tmp[ct][3]
                o = tmp[ct][4]
                kb.act(sg[:], Y[:, 0:T], AF.Sigmoid, bias=Vn(f"glu_b{L}")[:, ct:ct + 1])
                kb.tt("dve", o[:], y1t[ct][:].bitcast(F32), sg[:], ALU.mult)
                kb.dma("sp", ybT_d[ct * 128:(ct + 1) * 128, sl], o[:], reads=[o], writes=[f"ybT_{ct}"])
        kb.barrier()


MLA_SCALE = 1.0 / math.sqrt(96.0)


def phase_mla(G):
    nc, kb, L = G["nc"], G["kb"], G["L"]
    Vn, cst, onesr = G["Vn"], G["cst"], G["onesr"]
    zT_d, ybT_d = G["zT_d"], G["ybT_d"]
    need_ctx = L < DEPTH - 1
    with ExitStack() as ph:
        psb = lambda name, shape, dt=F32: ph.enter_context(nc.sbuf_tensor(uq(name), shape, dt))
        pps = lambda name, shape, dt=F32: ph.enter_context(nc.psum_tensor(uq(name), shape, dt))
        cqn = psb("ml_cqn", [128, 2, NT], F32R)
        ckvn = psb("ml_ckvn", [128, NT], F32R)
        KPE = psb("ml_KPE", [128, NT])
        ROPE = psb("ml_rope", [128, 2, NLAT])
        wq = psb("ml_wq", [128, 2, 2, 768], F32R)
        wuk = psb("ml_wuk", [128, 512], F32R)
        wuv = psb("ml_wuv", [128, 512], F32R)
        kb.dma("sp", ROPE[:], G["rope_d"], writes=["ml_rope"])
        kb.dma("pool", wq[:].rearrange("p k v c -> p k (v c)"),
               G["wq_d"][L].rearrange("(k p) v c -> p k (v c)", p=128), writes=["ml_wq"])
        kb.dma("pool", wuk[:], G["wuk_d"][L], writes=["ml_wuk"])
        kb.dma("pool", wuv[:], G["wuv_d"][L], writes=["ml_wuv"])
        ps = [pps(f"ml_ps{i}", [128, 512]) for i in range(8)]
        with ExitStack() as p1:
            qsb = lambda name, shape, dt=F32: p1.enter_context(nc.sbuf_tensor(uq(name), shape, dt))
            cqT = qsb("ml_cqT", [128, 2, NT])
            ckvT = qsb("ml_ckvT", [128, NT])
            kA = qsb("ml_kA", [128, NT])
            kB = qsb("ml_kB", [128, NT])
            for t in range(2):
                ti = COLIDX[f"cq{t}"]
                kb.dma("sp", cqT[:, t, :], zT_d[ti * 128:(ti + 1) * 128, :], reads=[f"zT_{ti}"], writes=["ml_cqT"])
            ti = COLIDX["ckv"]
            kb.dma("sp", ckvT[:], zT_d[ti * 128:(ti + 1) * 128, :], reads=[f"zT_{ti}"], writes=["ml_ckvT"])
            for nm_, tl in (("kpeA", kA), ("kpeB", kB)):
                ti = COLIDX[nm_]
                kb.dma("sp", tl[0:96, :], zT_d[ti * 128:ti * 128 + 96, :], reads=[f"zT_{ti}"], writes=[tl])
            sq = qsb("ml_sq", [128, 3, 512], F32R)
            rs = [qsb(f"ml_rs{i}", [128, 512]) for i in range(2)]
            tt1 = qsb("ml_tt1", [128, 512])
            tt2 = qsb("ml_tt2", [128, 512])
            for (t0, n) in CHUNKS:
                for t in range(2):
                    kb.act(sq[:, t, 0:n], cqT[:, t, t0:t0 + n], AF.Square)
                kb.act(sq[:, 2, 0:n], ckvT[:, t0:t0 + n], AF.Square)
                for t in range(2):
                    kb.mm(ps[0][:, 0:n], onesr[:], sq[:, t, 0:n], start=(t == 0), stop=(t == 1))
                kb.mm(ps[1][:, 0:n], onesr[:], sq[:, 2, 0:n])
                kb.act(rs[0][:, 0:n], ps[0][:, 0:n], AF.Sqrt, scale=1.0 / 256, bias=EPS)
                kb.recip(rs[0][:, 0:n], rs[0][:, 0:n])
                kb.act(rs[1][:, 0:n], ps[1][:, 0:n], AF.Sqrt, scale=1.0 / 128, bias=EPS)
                kb.recip(rs[1][:, 0:n], rs[1][:, 0:n])
                for t in range(2):
                    kb.stt("dve", cqn[:, t, t0:t0 + n], cqT[:, t, t0:t0 + n], Vn(f"qa_g{L}")[:, t:t + 1], rs[0][:, 0:n], ALU.mult, ALU.mult)
                kb.stt("dve", ckvn[:, t0:t0 + n], ckvT[:, t0:t0 + n], Vn(f"kva_g{L}")[:, 0:1], rs[1][:, 0:n], ALU.mult, ALU.mult)
                if t0 < NCTX:
                    kb.cp("dve", KPE[64:96, t0:t0 + n], kA[64:96, t0:t0 + n])
                else:
                    l0 = t0 - NCTX
                    kb.tt("dve", tt1[64:96, 0:n], kA[64:96, t0:t0 + n], ROPE[64:96, 0, l0:l0 + n], ALU.mult)
                    kb.tt("dve", tt2[64:96, 0:n], kB[64:96, t0:t0 + n], ROPE[64:96, 1, l0:l0 + n], ALU.mult)
                    kb.tt("dve", KPE[64:96, t0:t0 + n], tt1[64:96, 0:n], tt2[64:96, 0:n], ALU.add)
            kb.barrier()
        KT = psb("ml_KT", [128, NT], F32R)
        QT = psb("ml_QT", [128, NT], F32R)
        Vh = psb("ml_Vh", [128, 18, 65], F32R)
        PT = [psb(f"ml_PT{i}", [128, 512], F32R) for i in range(4)]
        Osb = [psb(f"ml_Osb{i}", [128, 512]) for i in range(2)]
        ys = [psb(f"ml_ys{i}", [128, 512]) for i in range(2)]
        u1 = psb("ml_u1", [128, 512])
        u2 = psb("ml_u2", [128, 512])
        onesf = cst[:, 128:256]
        kb.cp("dve", Vh[:, :, 64:65], onesf[:, 0:18].unsqueeze(2))
        cnt = 0
        for h in range(8):
            for (t0, n) in CHUNKS:
                kb.mm(ps[0][0:64, 0:n], wuk[:, h * 64:(h + 1) * 64], ckvn[:, t0:t0 + n])
                kb.cp("dve", KT[0:64, t0:t0 + n], ps[0][0:64, 0:n])
            kb.cp("dve", KT[64:96, :], KPE[64:96, :])
            for g0 in range(0, 18, 8):
                gn = min(8, 18 - g0)
                for j in range(gn):
                    kt = g0 + j
                    kb.mm(ps[1][:, j * 64:(j + 1) * 64], ckvn[:, kt * 128:(kt + 1) * 128], wuv[:, h * 64:(h + 1) * 64])
                kb.cp("dve", Vh[:, g0:g0 + gn, 0:64], ps[1][:, 0:gn * 64].rearrange("p (a b) -> p a b", b=64))
            for (t0, n) in CHUNKS:
                lat = t0 >= NCTX
                if not lat and not need_ctx:
                    continue
                for k in range(2):
                    kb.mm(ps[0][0:96, 0:n], wq[:, k, 0, h * 96:(h + 1) * 96], cqn[:, k, t0:t0 + n], start=(k == 0), stop=(k == 1))
                if lat:
                    for k in range(2):
                        kb.mm(ps[1][0:96, 0:n], wq[:, k, 1, h * 96:(h + 1) * 96], cqn[:, k, t0:t0 + n], start=(k == 0), stop=(k == 1))
                kb.cp("dve", QT[0:64, t0:t0 + n], ps[0][0:64, 0:n])
                if not lat:
                    kb.cp("act", QT[64:96, t0:t0 + n], ps[0][64:96, 0:n])
                else:
                    l0 = t0 - NCTX
                    kb.tt("dve", u1[64:96, 0:n], ps[0][64:96, 0:n], ROPE[64:96, 0, l0:l0 + n], ALU.mult)
                    kb.tt("dve", u2[64:96, 0:n], ps[1][64:96, 0:n], ROPE[64:96, 1, l0:l0 + n], ALU.mult)
                    kb.tt("dve", QT[64:96, t0:t0 + n], u1[64:96, 0:n], u2[64:96, 0:n], ALU.add)
            groups = ([[CHUNKS[0]]] if need_ctx else []) + [CHUNKS[1:3], CHUNKS[3:5]]
            for grp in groups:
                lat = grp[0][0] >= NCTX
                kts = list(range(18)) if lat else [0, 1]
                Sb = lambda a, i: ps[2 + 2 * a + i % 2]
                Pb = lambda a, i: PT[2 * a + i % 2]

                def emitS(i):
                    kt = kts[i]
                    for a, (t0, n) in enumerate(grp):
                        kb.mm(Sb(a, i)[:, 0:n], KT[0:96, kt * 128:(kt + 1) * 128], QT[0:96, t0:t0 + n])
                emitS(0)
                for i, kt in enumerate(kts):
                    for a, (t0, n) in enumerate(grp):
                        kb.act(Pb(a, i)[:, 0:n], Sb(a, i)[:, 0:n], AF.Exp, scale=MLA_SCALE)
                    if i + 1 < len(kts):
                        emitS(i + 1)
                    for a, (t0, n) in enumerate(grp):
                        kb.mm(ps[6 + a][0:65, 0:n], Vh[:, kt, :], Pb(a, i)[:, 0:n], start=(i == 0), stop=(i == len(kts) - 1))
                for a, (t0, n) in enumerate(grp):
                    ob = Osb[a]
                    yo = ys[a]
                    bcp = ps[a]
                    kb.cp("act", ob[0:65, 0:n], ps[6 + a][0:65, 0:n])
                    kb.recip(ob[64:65, 0:n], ob[64:65, 0:n])
                    kb.mm(bcp[0:64, 0:n], onesf[64:65, 0:64], ob[64:65, 0:n])
                    kb.tt("dve", yo[0:64, 0:n], ob[0:64, 0:n], bcp[0:64, 0:n], ALU.mult)
                    kb.dma("sp", ybT_d[256 + h * 64:256 + (h + 1) * 64, t0:t0 + n], yo[0:64, 0:n], reads=[yo], writes=[f"ybT_m{h}"])
        kb.barrier()


def phase_hg(G):
    nc, kb, L = G["nc"], G["kb"], G["L"]
    Vn, cst, onesr, ident, lbv = G["Vn"], G["cst"], G["onesr"], G["ident"], G["lbv"]
    zT_d, ybT_d = G["zT_d"], G["ybT_d"]
    CH = 64
    NC_ = NT // CH
    onesf = cst[:, 128:256]
    with ExitStack() as ph:
        psb = lambda name, shape, dt=F32: ph.enter_context(nc.sbuf_tensor(uq(name), shape, dt))
        pps = lambda name, shape, dt=F32: ph.enter_context(nc.psum_tensor(uq(name), shape, dt))
        A = psb("hg_A", [128, NT])
        KK = psb("hg_KK", [128, NT], F32R)
        Bt = psb("hg_Bt", [128, NT])
        E1 = psb("hg_E1", [128, NT], F32R)
        qT = psb("hg_qT", [128, NT])
        ig = psb("hg_ig", [128, NT])
        itok = psb("hg_itok", [128, NC_, 64], F32R)
        oacc = psb("hg_oacc", [128, NT])
        U = psb("hg_U", [128, NC_, 64])
        PTall = psb("hg_PT", [128, NC_, 64], F32R)
        Sst = psb("hg_Sst", [128, NC_, 64], F32R)
        ktok = [psb(f"hg_ktok{i}", [128, 4, 128], F32R) for i in range(2)]
        sct = [psb(f"hg_sct{i}", [128, 512]) for i in range(2)]
        small = psb("hg_small", [128, 4, NC_])
        S = psb("hg_S", [128, 64])
        tU = psb("hg_tU", [128, 64])
        fin = [psb(f"hg_fin{i}", [128, 512]) for i in range(3)]
        ps = [pps(f"hg_ps{i}", [128, 512]) for i in range(7)]
        MK = psb("hg_MK", [128, NT + 1])
        kb.memset("dve", MK[:], 1.0)
        kb.memset("dve", MK[:, 0:NT].rearrange("p (n c) -> p n c", c=CH)[:, :, 0:1], 0.0)
        pcnt = [0]

        def nps():
            pcnt[0] += 1
            return ps[pcnt[0] % 7]

        b3 = lambda t: t[:].rearrange("p (n c) -> p n c", c=CH)
        for h in range(4):
            tq, ti_, tg = COLIDX[f"hq{h}"], COLIDX[f"hi{h}"], COLIDX[f"hg{h}"]
            kb.dma("sp", qT[:], zT_d[tq * 128:(tq + 1) * 128, :], reads=[f"zT_{tq}"], writes=[qT])
            kb.dma("sp", ig[0:64, :], zT_d[ti_ * 128:ti_ * 128 + 64, :], reads=[f"zT_{ti_}"], writes=[ig])
            for c0 in range(0, NC_, 8):
                gn = min(8, NC_ - c0)
                p = nps()
                for j in range(gn):
                    c = c0 + j
                    kb.tr(p[0:64, j * 64:(j + 1) * 64], ig[0:64, c * CH:(c + 1) * CH], ident[0:64, 0:64])
                kb.cp("act", itok[0:64, c0:c0 + gn, :], p[0:64, 0:gn * 64].rearrange("p (a b) -> p a b", b=64))
            for d in range(2):
                tf = COLIDX[f"hf{h}" if d == 0 else f"hb{h}"]
                kb.dma("sp", A[:], zT_d[tf * 128:(tf + 1) * 128, :], reads=[f"zT_{tf}"], writes=[A])
                kb.act(A[:], A[:], AF.Sigmoid)
                kb.ts("dve", A[:], A[:], lbv[:, 1, L:L + 1], ALU.mult, lbv[:, 0, L:L + 1], ALU.add)
                kb.ts("dve", KK[:], A[:], -1.0, ALU.mult, 1.0, ALU.add)
                kb.act(A[:], A[:], AF.Ln)
                if d == 0:
                    kb.scan(Bt[:, :], MK[:, 0:NT], A[:, :], 0.0, ALU.mult, ALU.add)
                else:
                    kb.scan(Bt[:, ::-1], MK[:, 1:NT + 1][:, ::-1], A[:, ::-1], 0.0, ALU.mult, ALU.add)
                refpos = 31 if d == 0 else 32
                lastpos = 63 if d == 0 else 0
                refc, alpha, gamma, beta = (small[:, i, :] for i in range(4))
                kb.cp("dve", refc, b3(Bt)[:, :, refpos])
                kb.act(alpha, b3(Bt)[:, :, lastpos], AF.Exp)
                kb.act(gamma, refc, AF.Exp)
                kb.tt("dve", b3(Bt), b3(Bt), refc.unsqueeze(2).to_broadcast([128, NC_, CH]), ALU.subtract)
                kb.act(E1[:], Bt[:], AF.Exp)
                kb.cp("dve", beta, b3(E1)[:, :, lastpos].bitcast(F32))
                kb.act(Bt[:], Bt[:], AF.Exp, scale=-1.0)
                kb.tt("dve", E1[:], qT[:], E1[:].bitcast(F32), ALU.mult)
                kb.tt("dve", KK[:], KK[:].bitcast(F32), Bt[:], ALU.mult)
                for c0 in range(0, NC_, 4):
                    p = nps()
                    kt_ = ktok[(c0 // 4) % 2]
                    for j in range(4):
                        c = c0 + j
                        kb.tr(p[0:64, j * 128:(j + 1) * 128], KK[:, c * CH:(c + 1) * CH].bitcast(F32), ident)
                    kb.cp("act", kt_[0:64, :, :], p[0:64, :].rearrange("p (a b) -> p a b", b=128))
                    p2 = nps()
                    for j in range(4):
                        c = c0 + j
                        kb.mm(p2[:, j * 64:(j + 1) * 64], kt_[0:64, j, :], itok[0:64, c, :])
                    kb.cp("dve", U[:, c0:c0 + 4, :], p2[:, 0:256].rearrange("p (a b) -> p a b", b=64))
                for c0 in range(0, NC_, 8):
                    gn = min(8, NC_ - c0)
                    p = nps()
                    for j in range(gn):
                        c = c0 + j
                        kb.mm(p[0:64, j * 64:(j + 1) * 64], KK[:, c * CH:(c + 1) * CH], E1[:, c * CH:(c + 1) * CH])
                    st = sct[(c0 // 8) % 2]
                    kb.cp("act", st[0:64, 0:gn * 64], p[0:64, 0:gn * 64])
                    if d == 0:
                        kb.op("pool", lambda g: g.affine_select(PTall[0:64, c0:c0 + gn, :], st[0:64, 0:gn * 64].rearrange("p (a b) -> p a b", b=64),
                                                                [[0, gn], [1, 64]], ALU.is_ge, 0.0, base=0, channel_multiplier=-1),
                              reads=[st], writes=[PTall])
                    else:
                        kb.op("pool", lambda g: g.affine_select(PTall[0:64, c0:c0 + gn, :], st[0:64, 0:gn * 64].rearrange("p (a b) -> p a b", b=64),
                                                                [[0, gn], [-1, 64]], ALU.is_ge, 0.0, base=0, channel_multiplier=1),
                              reads=[st], writes=[PTall])
                AR = A[:].rearrange("p (v o) -> p v o", o=NC_)
                U2 = Bt[:].rearrange("p (v o) -> p v o", o=NC_)
                S2 = U[:].rearrange("p c v -> p (c v)").rearrange("p (v o) -> p v o", o=NC_)
                if d == 0:
                    segs = [(0, NC_, False)]
                    cof = lambda o: o
                else:
                    segs = [(0, 4, True), (4, NC_ - 4, True)]
                    cof = lambda o: (3 - o) if o < 4 else (NC_ + 3 - o)
                for (o0, n_, _) in segs:
                    c_hi, c_lo = cof(o0), cof(o0 + n_ - 1)
                    if d == 0:
                        csl = slice(c_hi, c_lo + 1)
                    else:
                        csl = slice(c_hi, (c_lo - 1) if c_lo > 0 else None, -1)
                    kb.tt("dve", U2[:, :, o0:o0 + n_].rearrange("p v o -> p o v"), U[:, csl, :],
                          beta[:, csl].unsqueeze(2).to_broadcast([128, n_, 64]), ALU.mult)
                    kb.cp("dve", AR[:, :, o0:o0 + n_].rearrange("p v o -> p o v"), alpha[:, csl].unsqueeze(2).to_broadcast([128, n_, 64]))
                kb.memset("dve", AR[:, :, 0:1], 0.0)
                kb.scan(U[:].rearrange("p c v -> p (c v)"), A[:, :], Bt[:, :], 0.0, ALU.mult, ALU.add)
                first_c = cof(0)
                kb.memset("dve", Sst[:, first_c, :].bitcast(F32), 0.0)
                for (o0, n_, _) in segs:
                    oa = max(o0, 1)
                    nn = o0 + n_ - oa
                    c_hi, c_lo = cof(oa), cof(oa + nn - 1)
                    if d == 0:
                        csl = slice(c_hi, c_lo + 1)
                    else:
                        csl = slice(c_hi, (c_lo - 1) if c_lo > 0 else None, -1)
                    kb.tt("dve", Sst[:, csl, :], S2[:, :, oa - 1:oa - 1 + nn].rearrange("p v o -> p o v"),
                          gamma[:, csl].unsqueeze(2).to_broadcast([128, nn, 64]), ALU.mult)
                for c0 in range(0, NC_, 8):
                    gn = min(8, NC_ - c0)
                    p = nps()
                    for j in range(gn):
                        c = c0 + j
                        kb.mm(p[0:64, j * 64:(j + 1) * 64], itok[0:64, c, :], PTall[0:64, c, :], start=True, stop=False)
                        kb.mm(p[0:64, j * 64:(j + 1) * 64], Sst[:, c, :], E1[:, c * CH:(c + 1) * CH], start=False, stop=True)
                    if d == 0:
                        kb.cp("act", oacc[0:64, c0 * CH:(c0 + gn) * CH], p[0:64, 0:gn * 64])
                    else:
                        kb.tt("dve", oacc[0:64, c0 * CH:(c0 + gn) * CH], oacc[0:64, c0 * CH:(c0 + gn) * CH], p[0:64, 0:gn * 64], ALU.add)
            kb.dma("sp", ig[0:64, :], zT_d[tg * 128:tg * 128 + 64, :], reads=[f"zT_{tg}"], writes=[ig])
            kb.act(KK[0:64, :], oacc[0:64, :], AF.Square)
            kb.act(ig[0:64, :], ig[0:64, :], AF.Silu)
            for (t0, n) in CHUNKS:
                p = nps()
                kb.mm(p[0:64, 0:n], onesr[0:64, 0:64], KK[0:64, t0:t0 + n])
                kb.act(fin[0][0:64, 0:n], p[0:64, 0:n], AF.Sqrt, scale=1.0 / 64, bias=EPS)
                kb.recip(fin[0][0:64, 0:n], fin[0][0:64, 0:n])
                kb.stt("dve", fin[1][0:64, 0:n], oacc[0:64, t0:t0 + n], Vn(f"hgn_g{L}")[0:64, h:h + 1], fin[0][0:64, 0:n], ALU.mult, ALU.mult)
                kb.tt("dve", fin[2][0:64, 0:n], fin[1][0:64, 0:n], ig[0:64, t0:t0 + n], ALU.mult)
                kb.dma("sp", ybT_d[768 + h * 64:768 + (h + 1) * 64, t0:t0 + n], fin[2][0:64, 0:n], reads=[fin[2]], writes=[f"ybT_h{h}"])
        kb.barrier()


def phase_merge(G):
    nc, kb, L = G["nc"], G["kb"], G["L"]
    xT, modT = G["xT"], G["modT"]
    zT_d, ybT_d = G["zT_d"], G["ybT_d"]
    need_ctx = L < DEPTH - 1
    chs = CHUNKS if need_ctx else CHUNKS[1:]
    yb_names = ["ybT_0", "ybT_1"] + [f"ybT_m{h}" for h in range(8)] + [f"ybT_h{h}" for h in range(4)]
    with ExitStack() as ph:
        psb = lambda name, shape, dt=F32: ph.enter_context(nc.sbuf_tensor(uq(name), shape, dt))
        pps = lambda name, shape, dt=F32: ph.enter_context(nc.psum_tensor(uq(name), shape, dt))
        wp = psb("mg_wp", [128, 8, D], F32R)
        wo = psb("mg_wo", [128, 8, D], F32R)
        kb.dma("pool", wp[:], G["wp_d"][L].rearrange("(k p) c -> p k c", p=128), writes=[wp])
        kb.dma("pool", wo[:], G["wo_d"][L].rearrange("(k p) c -> p k c", p=128), writes=[wo])
        yb = psb("mg_yb", [128, 8, 512], F32R)
        mT = psb("mg_mT", [128, 8, 512], F32R)
        gt = [psb(f"mg_gt{i}", [128, 3, 512]) for i in range(2)]
        t3 = [psb(f"mg_t{i}", [128, 512]) for i in range(3)]
        ps = [pps(f"mg_ps{i}", [128, 512]) for i in range(8)]
        branches = ((0, 2), (2, 6), (6, 8))
        for (t0, n) in chs:
            s_ = 1 if t0 < NCTX else 0
            kb.dma("pool", yb[:, :, 0:n], ybT_d[:, t0:t0 + n].rearrange("(k p) t -> p k t", p=128), reads=yb_names, writes=[yb])
            for f in range(8):
                g = gt[f % 2]
                for b in range(3):
                    ti = COLIDX[f"gate{b}_{f}"]
                    kb.dma("sp", g[:, b, 0:n], zT_d[ti * 128:(ti + 1) * 128, t0:t0 + n], reads=[f"zT_{ti}"], writes=[g])
                kb.act(g[:, :, 0:n], g[:, :, 0:n], AF.Sigmoid)
                for b, (k0, k1) in enumerate(branches):
                    p = ps[(f % 2) * 3 + b]
                    for k in range(k0, k1):
                        kb.mm(p[:, 0:n], wp[:, k, f * 128:(f + 1) * 128], yb[:, k, 0:n], start=(k == k0), stop=(k == k1 - 1))
                    kb.tt("dve", t3[b][:, 0:n], p[:, 0:n], g[:, b, 0:n], ALU.mult)
                kb.tt("pool", t3[0][:, 0:n], t3[0][:, 0:n], t3[1][:, 0:n], ALU.add)
                kb.tt("pool", mT[:, f, 0:n], t3[0][:, 0:n], t3[2][:, 0:n], ALU.add)
            for f in range(8):
                p = ps[6 + f % 2]
                for k in range(8):
                    kb.mm(p[:, 0:n], wo[:, k, f * 128:(f + 1) * 128], mT[:, k, 0:n], start=(k == 0), stop=(k == 7))
                kb.stt("dve", xT[:, f, t0:t0 + n], p[:, 0:n], modT[:, L, 16 + f, s_:s_ + 1], xT[:, f, t0:t0 + n], ALU.mult, ALU.add)
        kb.barrier()


def phase_moe(G):
    nc, kb, L = G["nc"], G["kb"], G["L"]
    xT, modT, modA, onesr, ident, cst = G["xT"], G["modT"], G["modA"], G["onesr"], G["ident"], G["cst"]
    need_ctx = L < DEPTH - 1
    chs = CHUNKS if need_ctx else CHUNKS[1:]
    groups = [chs[:3], chs[3:]] if need_ctx else [chs[:2], chs[2:]]
    onesf = cst[:, 128:256]
    for grp in groups:
        GN = sum(n for _, n in grp)
        gch = []
        o = 0
        for (t0, n) in grp:
            gch.append((t0, n, o))
            o += n
        with ExitStack() as ph:
            psb = lambda name, shape, dt=F32: ph.enter_context(nc.sbuf_tensor(uq(name), shape, dt))
            pps = lambda name, shape, dt=F32: ph.enter_context(nc.psum_tensor(uq(name), shape, dt))
            h2T = psb("me_h2T", [128, 8, GN], F32R)
            gateT = psb("me_gateT", [128, GN], F32R)
            emit_norm(nc, kb, xT, h2T, gch, lambda k, s_: modA[:, L, 1, k, s_:s_ + 1],
                      lambda k, s_: modT[:, L, 24 + k, s_:s_ + 1], onesr, D)
            with ExitStack() as rp:
                rsb = lambda name, shape, dt=F32: rp.enter_context(nc.sbuf_tensor(uq(name), shape, dt))
                rps = lambda name, shape, dt=F32: rp.enter_context(nc.psum_tensor(uq(name), shape, dt))
                wr = rsb("me_wr", [128, 8, 36])
                br = rsb("me_br", [128, 36])
                kb.dma("sp", wr[:], G["wr_d"][L].rearrange("(k p) c -> p k c", p=128), writes=[wr])
                kb.dma("sp", br[:], G["br_d"][L].to_broadcast([128, 36]), writes=[br])
                lp = [rps(f"me_lp{i}", [128, 512]) for i in range(2)]
                gp = [rps(f"me_gp{i}", [128, 512]) for i in range(2)]
                R = [[rsb(f"me_r{b}_{i}", [128, 40]) for i in range(12)] for b in range(2)]
                for tt in range(GN // 128):
                    b = tt % 2
                    r = R[b]
                    for k in range(8):
                        kb.mm(lp[b][:, 0:36], h2T[:, k, tt * 128:(tt + 1) * 128].bitcast(F32), wr[:, k, :], start=(k == 0), stop=(k == 7))
                    lg = r[0]
                    kb.tt("dve", lg[:, 0:36], lp[b][:, 0:36], br[:], ALU.add)
                    gmax, ngmax, gsum, gw = r[1][:, 0:1], r[1][:, 1:2], r[1][:, 2:3], r[1][:, 3:4]
                    kb.op("dve", lambda g: g.tensor_reduce(gmax, lg[:, 0:4], AX.X, ALU.max), reads=[lg], writes=[r[1]])
                    kb.ts("dve", ngmax, gmax, -1.0, ALU.mult)
                    kb.act(r[2][:, 0:4], lg[:, 0:4], AF.Exp, bias=ngmax)
                    kb.op("dve", lambda g: g.tensor_reduce(gsum, r[2][:, 0:4], AX.X, ALU.add), reads=[r[2]], writes=[r[1]])
                    kb.recip(gw, gsum)
                    kb.ts("dve", r[3][:, 0:4], lg[:, 0:4], gmax, ALU.is_equal)
                    kb.ts("dve", r[3][:, 0:4], r[3][:, 0:4], -1.0, ALU.add, 1e30, ALU.mult)
                    kb.tt("dve", r[4][:, 0:32].rearrange("p (a b) -> p a b", b=8), lg[:, 4:36].rearrange("p (a b) -> p a b", b=8),
                          r[3][:, 0:4].unsqueeze(2).to_broadcast([128, 4, 8]), ALU.add)
                    kb.op("dve", lambda g: g.max(r[5][:, 0:8], r[4][:, 0:32]), reads=[r[4]], writes=[r[5]])
                    m1, m2 = r[5][:, 0:1], r[5][:, 1:2]
                    kb.ts("dve", r[6][:, 0:32], r[4][:, 0:32], m1, ALU.is_equal)
                    kb.ts("dve", r[7][:, 0:32], r[4][:, 0:32], m2, ALU.is_equal)
                    dm, ee, p1, p2 = r[8][:, 0:1], r[8][:, 1:2], r[8][:, 2:3], r[8][:, 3:4]
                    kb.tt("dve", dm, m2, m1, ALU.subtract)
                    kb.act(ee, dm, AF.Exp)
                    kb.ts("dve", p1, ee, 1.0, ALU.add)
                    kb.recip(p1, p1)
                    kb.tt("dve", p2, ee, p1, ALU.mult)
                    kb.tt("dve", p1, p1, gw, ALU.mult)
                    kb.tt("dve", p2, p2, gw, ALU.mult)
                    kb.ts("dve", r[9][:, 0:32], r[6][:, 0:32], p1, ALU.mult)
                    kb.stt("dve", r[9][:, 0:32], r[7][:, 0:32], p2, r[9][:, 0:32], ALU.mult, ALU.add)
                    kb.tr(gp[b][0:32, 0:128], r[9][:, 0:32], ident)
                    kb.cp("act", gateT[0:32, tt * 128:(tt + 1) * 128], gp[b][0:32, 0:128])
                kb.barrier()
            Gall = psb("me_G", [128, 4, GN], F32R)
            w13 = [[psb(f"me_w{a}_{i}", [128, 8, 128], F32R) for i in range(2)] for a in (1, 3)]
            w2 = [psb(f"me_w2_{i}", [128, 4, D], F32R) for i in range(2)]
            sel = [psb(f"me_sel{i}", [128, 128], F32R) for i in range(2)]
            sil = [psb(f"me_sil{i}", [128, 512]) for i in range(2)]
            hp = [pps(f"me_hp{i}", [128, 512]) for i in range(4)]
            bp = [pps(f"me_bp{i}", [128, 512]) for i in range(2)]
            yp = [pps(f"me_yp{i}", [128, 512]) for i in range(2)]
            cnt = 0
            for e in range(32):
                se = sel[e % 2]
                kb.op("pool", lambda g: g.affine_select(se[0:32, :], onesf[0:32, :], [[0, 128]], ALU.is_equal, 0.0,
                                                        base=-e, channel_multiplier=1), reads=[cst], writes=[se])
                kb.dma("pool", w2[e % 2][:], G["w2_d"][L, e].rearrange("(j p) c -> p j c", p=128), writes=[w2[e % 2]])
                for j in range(4):
                    wa, wb_ = w13[0][(e * 4 + j) % 2], w13[1][(e * 4 + j) % 2]
                    kb.dma("pool", wa[:], G["w1_d"][L, e, :, j * 128:(j + 1) * 128].rearrange("(k p) c -> p k c", p=128), writes=[wa])
                    kb.dma("pool", wb_[:], G["w3_d"][L, e, :, j * 128:(j + 1) * 128].rearrange("(k p) c -> p k c", p=128), writes=[wb_])
                    for (t0, n, o) in gch:
                        b = cnt % 2
                        cnt += 1
                        for k in range(8):
                            kb.mm(hp[b][:, 0:n], wa[:, k, :], h2T[:, k, o:o + n], start=(k == 0), stop=(k == 7))
                        for k in range(8):
                            kb.mm(hp[2 + b][:, 0:n], wb_[:, k, :], h2T[:, k, o:o + n], start=(k == 0), stop=(k == 7))
                        kb.mm(bp[b][:, 0:n], se[0:32, :], gateT[0:32, o:o + n])
                        kb.act(sil[b][:, 0:n], hp[b][:, 0:n], AF.Silu)
                        kb.tt("dve", sil[b][:, 0:n], sil[b][:, 0:n], hp[2 + b][:, 0:n], ALU.mult)
                        kb.tt("dve", Gall[:, j, o:o + n], sil[b][:, 0:n], bp[b][:, 0:n], ALU.mult)
                for (t0, n, o) in gch:
                    s_ = 1 if t0 < NCTX else 0
                    for f in range(8):
                        p = yp[f % 2]
                        for j in range(4):
                            kb.mm(p[:, 0:n], w2[e % 2][:, j, f * 128:(f + 1) * 128], Gall[:, j, o:o + n], start=(j == 0), stop=(j == 3))
                        kb.stt("dve", xT[:, f, t0:t0 + n], p[:, 0:n], modT[:, L, 40 + f, s_:s_ + 1], xT[:, f, t0:t0 + n], ALU.mult, ALU.add)
            kb.barrier()


def phase_moe_sparse(G):
    nc, kb, L = G["nc"], G["kb"], G["L"]
    xT, modT, modA, onesr, ident, cst = G["xT"], G["modT"], G["modA"], G["onesr"], G["ident"], G["cst"]
    h2tok_d, xs_d, ys_d = G["h2tok_d"], G["xs_d"], G["ys_d"]
    need_ctx = L < DEPTH - 1
    chs = CHUNKS if need_ctx else CHUNKS[1:]
    tiles = [t0 // 128 + j for (t0, n) in chs for j in range(n // 128)]
    NTL = len(tiles)
    onesf = cst[:, 128:256]
    with ExitStack() as ph:
        psb = lambda name, shape, dt=F32: ph.enter_context(nc.sbuf_tensor(uq(name), shape, dt))
        pps = lambda name, shape, dt=F32: ph.enter_context(nc.psum_tensor(uq(name), shape, dt))
        pr = ExitStack()
        rsb_ = lambda name, shape, dt=F32: pr.enter_context(nc.sbuf_tensor(uq(name), shape, dt))
        GA = psb("ms_GA", [128, 18])
        GB = psb("ms_GB", [128, 18])
        D1f = psb("ms_D1f", [128, 18])
        D2f = psb("ms_D2f", [128, 18])
        D1i = psb("ms_D1i", [128, 18], I32)
        D2i = psb("ms_D2i", [128, 18], I32)
        idxW = psb("ms_idxW", [128, 128], I32)
        p0 = ExitStack()
        h2T = p0.enter_context(nc.sbuf_tensor(uq("ms_h2T"), [128, 8, NT], F32R))
        OH1 = rsb_("ms_OH1", [128, 18, 32])
        OH2 = rsb_("ms_OH2", [128, 18, 32])
        AA = rsb_("ms_AA", [128, 18, 32])
        with ExitStack() as p1:
            qsb = lambda name, shape, dt=F32: p1.enter_context(nc.sbuf_tensor(uq(name), shape, dt))
            qps = lambda name, shape, dt=F32: p1.enter_context(nc.psum_tensor(uq(name), shape, dt))
            emit_norm(nc, kb, xT, h2T, [(t0, n, t0) for (t0, n) in chs], lambda k, s_: modA[:, L, 1, k, s_:s_ + 1],
                      lambda k, s_: modT[:, L, 24 + k, s_:s_ + 1], onesr, D)
            wr = qsb("ms_wr", [128, 8, 36], F32R)
            br = qsb("ms_br", [128, 36])
            kb.dma("pool", wr[:], G["wr_d"][L].rearrange("(k p) c -> p k c", p=128), writes=[wr])
            kb.dma("sp", br[:], G["br_d"][L].to_broadcast([128, 36]), writes=[br])
            lp = [qps(f"ms_lp{i}", [128, 512]) for i in range(2)]
            T_ = NTL
            LG = qsb("ms_LG", [128, 18, 36])
            for i, tt in enumerate(tiles):
                b = i % 2
                tsl = slice(tt * 128, (tt + 1) * 128)
                for k in range(8):
                    kb.mm(lp[b][:, 0:36], h2T[:, k, tsl], wr[:, k, :], start=(k == 0), stop=(k == 7))
                kb.tt("dve", LG[:, i, :], lp[b][:, 0:36], br[:], ALU.add)
            B4 = lambda t: t[:, 0:T_, :]
            g4 = qsb("ms_g4", [128, 18, 4])
            oh4 = qsb("ms_oh4", [128, 18, 4])
            ls = qsb("ms_ls", [128, 18, 32])
            l2 = qsb("ms_l2", [128, 18, 32])
            sm_ = qsb("ms_sm", [128, 8, 18])
            gmax, gsum, gw, m1, m2, ee, p1_, p2_ = (sm_[:, j, 0:T_] for j in range(8))
            bc4 = lambda v: v.unsqueeze(2).to_broadcast([128, T_, 4])
            bc32 = lambda v: v.unsqueeze(2).to_broadcast([128, T_, 32])
            kb.op("dve", lambda g: g.tensor_reduce(gmax, LG[:, 0:T_, 0:4], AX.X, ALU.max), reads=[LG], writes=[sm_])
            kb.tt("dve", g4[:, 0:T_, :], LG[:, 0:T_, 0:4], bc4(gmax), ALU.subtract)
            kb.tt("dve", oh4[:, 0:T_, :], LG[:, 0:T_, 0:4], bc4(gmax), ALU.is_equal)
            kb.act(g4[:, 0:T_, :], g4[:, 0:T_, :], AF.Exp)
            kb.op("dve", lambda g: g.tensor_reduce(gsum, g4[:, 0:T_, :], AX.X, ALU.add), reads=[g4], writes=[sm_])
            kb.recip(gw, gsum)
            kb.ts("dve", oh4[:, 0:T_, :], oh4[:, 0:T_, :], -1.0, ALU.add, 1e30, ALU.mult)
            kb.tt("dve", ls[:, 0:T_, :].rearrange("p t (a b) -> p t a b", b=8), LG[:, 0:T_, 4:36].rearrange("p t (a b) -> p t a b", b=8),
                  oh4[:, 0:T_, :].unsqueeze(3).to_broadcast([128, T_, 4, 8]), ALU.add)
            kb.op("dve", lambda g: g.tensor_reduce(m1, ls[:, 0:T_, :], AX.X, ALU.max), reads=[ls], writes=[sm_])
            kb.tt("dve", OH1[:, 0:T_, :], ls[:, 0:T_, :], bc32(m1), ALU.is_equal)
            kb.stt("dve", l2[:, 0:T_, :], OH1[:, 0:T_, :], -1e30, ls[:, 0:T_, :], ALU.mult, ALU.add)
            kb.op("dve", lambda g: g.tensor_reduce(m2, l2[:, 0:T_, :], AX.X, ALU.max), reads=[l2], writes=[sm_])
            kb.tt("dve", OH2[:, 0:T_, :], l2[:, 0:T_, :], bc32(m2), ALU.is_equal)
            kb.tt("dve", AA[:, 0:T_, :], OH1[:, 0:T_, :], OH2[:, 0:T_, :], ALU.add)
            kb.tt("dve", ee, m2, m1, ALU.subtract)
            kb.act(ee, ee, AF.Exp)
            kb.ts("dve", p1_, ee, 1.0, ALU.add)
            kb.recip(p1_, p1_)
            kb.tt("dve", p2_, ee, p1_, ALU.mult)
            kb.tt("dve", GA[:, 0:T_], p1_, gw, ALU.mult)
            kb.tt("dve", GB[:, 0:T_], p2_, gw, ALU.mult)
            kb.barrier()
        with ExitStack() as p2:
            qsb = lambda name, shape, dt=F32: p2.enter_context(nc.sbuf_tensor(uq(name), shape, dt))
            qps = lambda name, shape, dt=F32: p2.enter_context(nc.psum_tensor(uq(name), shape, dt))
            ltri = qsb("ms_ltri", [128, 128])
            kb.op("pool", lambda g: g.affine_select(ltri[:], onesf, [[1, 128]], ALU.is_gt, 0.0, base=0, channel_multiplier=-1),
                  reads=[cst], writes=[ltri])
            Rp = [qps(f"ms_Rp{i}", [128, 512]) for i in range(2)]
            Cp = qps("ms_Cp", [128, 512])
            AAP = qsb("ms_AAP", [128, 18, 32])
            kb.memset("dve", AAP[:, 0, :], 0.0)
            for i in range(1, NTL):
                kb.tt("dve", AAP[:, i, :], AAP[:, i - 1, :], AA[:, i - 1, :], ALU.add)
            for i in range(NTL):
                out = Rp[i // 16][:, (i % 16) * 32:(i % 16 + 1) * 32]
                kb.mm(out, ltri[:], AA[:, i, :], start=True, stop=(i == 0))
                if i > 0:
                    kb.mm(out, onesf, AAP[:, i, :], start=False, stop=True)
            kb.mm(Cp[:, 0:32], onesf, AAP[:, NTL - 1, :], start=True, stop=False)
            kb.mm(Cp[:, 0:32], onesf, AA[:, NTL - 1, :], start=False, stop=True)
            w_ = [qsb(f"ms_w{i}", [128, 32]) for i in range(8)]
            cnt, x_, kf, msk, nb, pend, pstart, tmp = w_
            kb.cp("dve", cnt[:], Cp[:, 0:32])
            kb.ts("dve", x_[:], cnt[:], -float(SLOT), ALU.add, 0.0, ALU.max)
            kb.ts("dve", x_[:], x_[:], 127.0, ALU.add, 1.0 / 128, ALU.mult)
            ki = qsb("ms_ki", [128, 32], I32)
            kb.cp("dve", ki[:], x_[:])
            kb.cp("dve", kf[:], ki[:])
            kb.tt("dve", msk[:], kf[:], x_[:], ALU.is_gt)
            kb.tt("dve", kf[:], kf[:], msk[:], ALU.subtract)
            kb.ts("dve", tmp[:], kf[:], 1.0, ALU.add)
            kb.tt("dve", msk[:], tmp[:], x_[:], ALU.is_le)
            kb.tt("dve", nb[:], kf[:], msk[:], ALU.add)
            kb.scan(pend[:], onesf[:, 0:32], nb[:], 0.0, ALU.mult, ALU.add)
            kb.tt("dve", pstart[:], pend[:], nb[:], ALU.subtract)
            base1_i = qsb("ms_b1i", [128, 32], I32)
            base1 = qsb("ms_b1", [128, 32])
            kb.op("pool", lambda g: g.iota(base1_i[:], [[SLOT, 32]], base=0, channel_multiplier=0), writes=[base1_i])
            kb.cp("dve", base1[:], base1_i[:])
            kb.ts("dve", pstart[:], pstart[:], 128.0, ALU.mult, float(32 * SLOT - SLOT), ALU.add)
            kb.tt("dve", pstart[:], pstart[:], base1[:], ALU.subtract)
            RR = qsb("ms_RR", [128, 18, 32])
            T3 = qsb("ms_T3", [128, 18, 32])
            T4 = qsb("ms_T4", [128, 18, 32])
            n0 = min(NTL, 16)
            kb.cp("dve", RR[:, 0:n0, :], Rp[0][:, 0:n0 * 32].rearrange("p (a b) -> p a b", b=32))
            if NTL > 16:
                kb.cp("dve", RR[:, 16:NTL, :], Rp[1][:, 0:(NTL - 16) * 32].rearrange("p (a b) -> p a b", b=32))
            bcT = lambda v: v.unsqueeze(1).to_broadcast([128, NTL, 32])
            kb.ts("dve", T4[:, 0:NTL, :], RR[:, 0:NTL, :], float(SLOT), ALU.is_ge)
            kb.tt("dve", T4[:, 0:NTL, :], T4[:, 0:NTL, :], bcT(pstart[:]), ALU.mult)
            kb.tt("dve", T3[:, 0:NTL, :], RR[:, 0:NTL, :], bcT(base1[:]), ALU.add)
            kb.tt("dve", T3[:, 0:NTL, :], T3[:, 0:NTL, :], T4[:, 0:NTL, :], ALU.add)
            kb.tt("dve", T4[:, 0:NTL, :], T3[:, 0:NTL, :], OH1[:, 0:NTL, :], ALU.mult)
            kb.op("dve", lambda g: g.tensor_reduce(D1f[:, 0:NTL], T4[:, 0:NTL, :], AX.X, ALU.add), reads=[T4], writes=[D1f])
            kb.tt("dve", T4[:, 0:NTL, :], T3[:, 0:NTL, :], OH2[:, 0:NTL, :], ALU.mult)
            kb.op("dve", lambda g: g.tensor_reduce(D2f[:, 0:NTL], T4[:, 0:NTL, :], AX.X, ALU.add), reads=[T4], writes=[D2f])
            kb.cp("dve", D1i[:, 0:NTL], D1f[:, 0:NTL])
            kb.cp("dve", D2i[:, 0:NTL], D2f[:, 0:NTL])
            pidx_i = qsb("ms_pidx_i", [128, 1], I32)
            pidx = qsb("ms_pidx", [128, 1])
            kb.op("pool", lambda g: g.iota(pidx_i[:], [[0, 1]], base=0, channel_multiplier=1), writes=[pidx_i])
            kb.cp("dve", pidx[:], pidx_i[:])
            be = qsb("ms_be", [128, 1])
            kb.ts("dve", tmp[:], pend[:], pidx[:, 0:1], ALU.is_le)
            kb.op("dve", lambda g: g.tensor_reduce(be[:], tmp[:], AX.X, ALU.add), reads=[tmp], writes=[be])
            kb.ts("dve", be[:], be[:], 31.0, ALU.min)
            vb = qsb("ms_vb", [128, 1])
            kb.ts("dve", vb[:], pidx[:], pend[:, 31:32], ALU.is_lt)
            kb.tt("dve", be[:], be[:], vb[:], ALU.mult)
            kb.ts("dve", vb[:], vb[:], -1.0, ALU.add, -1000.0, ALU.mult)
            kb.tt("dve", be[:], be[:], vb[:], ALU.add)
            dg = qsb("ms_dg", [128, 128])
            kb.ts("dve", dg[:], ident, be[:, 0:1], ALU.mult)
            kb.mm(Cp[:, 128:256], onesf, dg[:])
            bef = qsb("ms_bef", [128, 128])
            kb.ts("dve", bef[:], Cp[:, 128:256], 128.0, ALU.mult, pidx[:, 0:1], ALU.add)
            if L > 0:
                kb.ts("dve", bef[:], bef[:], float(L * 32 * 128), ALU.add)
            kb.cp("dve", idxW[:], bef[:])
            kb.barrier()
        pr.close()
        with ExitStack() as p3:
            qsb = lambda name, shape, dt=F32: p3.enter_context(nc.sbuf_tensor(uq(name), shape, dt))
            qps = lambda name, shape, dt=F32: p3.enter_context(nc.psum_tensor(uq(name), shape, dt))
            hk = [qsb(f"ms_hs{i}", [128, D]) for i in range(3)]
            tp = [qps(f"ms_tp{i}", [128, 512]) for i in range(4)]
            for i, tt in enumerate(tiles):
                h = hk[i % 3]
                tsl = slice(tt * 128, (tt + 1) * 128)
                for half in range(2):
                    p = tp[(2 * i + half) % 4]
                    for j in range(4):
                        k = half * 4 + j
                        kb.tr(p[:, j * 128:(j + 1) * 128], h2T[:, k, tsl].bitcast(F32), ident)
                    kb.cp("act" if half == 0 else "dve", h[:, half * 512:(half + 1) * 512], p[:])
                kb.idma(xs_d, bass.IndirectOffsetOnAxis(ap=D1i[:, i:i + 1], axis=0), h[:], None, reads=[h, D1i], writes=["xs"])
                kb.idma(xs_d, bass.IndirectOffsetOnAxis(ap=D2i[:, i:i + 1], axis=0), h[:], None, reads=[h, D2i], writes=["xs"])
            kb.barrier()
        p0.close()
        with ExitStack() as p4:
            qsb = lambda name, shape, dt=F32: p4.enter_context(nc.sbuf_tensor(uq(name), shape, dt))
            qps = lambda name, shape, dt=F32: p4.enter_context(nc.psum_tensor(uq(name), shape, dt))
            W1 = [qsb(f"ms_W1_{i}", [128, 8 * 512], F32R) for i in range(2)]
            W3 = [qsb(f"ms_W3_{i}", [128, 8 * 512], F32R) for i in range(2)]
            W2 = [qsb(f"ms_W2_{i}", [128, 4 * D], F32R) for i in range(2)]
            Xb = [qsb(f"ms_Xb{i}", [128, D]) for i in range(3)]
            XT = [qsb(f"ms_XT{i}", [128, 8, 128], F32R) for i in range(2)]
            SL = [qsb(f"ms_SL{i}", [128, 512]) for i in range(2)]
            Gt = [qsb(f"ms_Gt{i}", [128, 4, 128], F32R) for i in range(2)]
            Ys = [qsb("ms_Ys0", [128, D])] * 2
            tpp = [qps(f"ms_tq{i}", [128, 512]) for i in range(2)]
            hp1 = [qps("ms_h1", [128, 512])] * 2
            hp3 = [qps("ms_h3", [128, 512])] * 2
            ypp = [qps(f"ms_yp{i}", [128, 512]) for i in range(2)]
            xtp = [qps(f"ms_xq{i}", [128, 512]) for i in range(2)]
            def xload(i):
                if i < len(subs):
                    kb.dma("sp", Xb[i % 3][:], xs_d[subs[i][0]:subs[i][0] + 128, :], reads=["xs"], writes=[Xb[i % 3]])

            def stageA(row0, W1t, W3t, xq, xb3):
                for half in range(2):
                    p = xtp[half]
                    for j in range(4):
                        k = half * 4 + j
                        kb.tr(p[:, j * 128:(j + 1) * 128], Xb[xb3][:, k * 128:(k + 1) * 128], ident)
                    kb.cp("act" if half == 0 else "dve", XT[xq][:, half * 4:half * 4 + 4, :], p[:].rearrange("p (a b) -> p a b", b=128))
                w1v = W1t[:].rearrange("p (k c) -> p k c", c=512)
                w3v = W3t[:].rearrange("p (k c) -> p k c", c=512)
                for k in range(8):
                    kb.mm(hp1[xq][:], XT[xq][:, k, :], w1v[:, k, :], start=(k == 0), stop=(k == 7))
                    kb.mm(hp3[xq][:], XT[xq][:, k, :], w3v[:, k, :], start=(k == 0), stop=(k == 7))
                kb.act(SL[xq][:], hp1[xq][:], AF.Silu)
                kb.tt("dve", SL[xq][:], SL[xq][:], hp3[xq][:], ALU.mult)

            def stageB(row0, W2t, xq):
                w2v = W2t[:].rearrange("p (j c) -> p j c", c=D)
                gp_ = tpp[xq]
                for j in range(4):
                    kb.tr(gp_[:, j * 128:(j + 1) * 128], SL[xq][:, j * 128:(j + 1) * 128], ident)
                kb.cp("act", Gt[xq][:].rearrange("p a b -> p (a b)"), gp_[:])
                for half in range(2):
                    yp = ypp[half]
                    for j in range(4):
                        kb.mm(yp[:], Gt[xq][:, j, :], w2v[:, j, half * 512:(half + 1) * 512], start=(j == 0), stop=(j == 3))
                    kb.cp("act" if half == 0 else "dve", Ys[xq][:, half * 512:(half + 1) * 512], yp[:])
                kb.dma("sp", ys_d[row0:row0 + 128, :], Ys[xq][:], reads=[Ys[xq]], writes=["ys"])

            subs = []
            wcnt = 0
            for e in range(32):
                pb = wcnt % 2
                wcnt += 1

                def ld(e=e, pb=pb):
                    kb.dma("pool", W1[pb][:], G["w1_d"][L, e * 128:(e + 1) * 128, :], writes=[W1[pb]])
                    kb.dma("pool", W3[pb][:], G["w3_d"][L, e * 128:(e + 1) * 128, :], writes=[W3[pb]])
                    kb.dma("pool", W2[pb][:], G["w2_d"][L, e * 128:(e + 1) * 128, :], writes=[W2[pb]])
                for j in range(SLOT // 128):
                    subs.append((e * SLOT + j * 128, pb, ld if j == 0 else None))
            nov = 2 * NTL - 1
            for b in range(nov):
                pb = wcnt % 2
                wcnt += 1

                def ld(b=b, pb=pb):
                    off = bass.IndirectOffsetOnAxis(ap=idxW[:, b:b + 1], axis=0)
                    bnd = (L + 1) * 32 * 128 - 1
                    kb.idma(W1[pb][:], None, G["w1_d"].rearrange("l r c -> (l r) c"), off, reads=[idxW], writes=[W1[pb]], bounds=bnd)
                    kb.idma(W3[pb][:], None, G["w3_d"].rearrange("l r c -> (l r) c"), off, reads=[idxW], writes=[W3[pb]], bounds=bnd)
                    kb.idma(W2[pb][:], None, G["w2_d"].rearrange("l r c -> (l r) c"), off, reads=[idxW], writes=[W2[pb]], bounds=bnd)
                subs.append((32 * SLOT + b * 128, pb, ld))
            xload(0)
            xload(1)
            for i, (row0, pb, ld) in enumerate(subs):
                if ld is not None:
                    ld()
                xload(i + 2)
                stageA(row0, W1[pb], W3[pb], i % 2, i % 3)
                if i >= 1:
                    r1, pb1, _ = subs[i - 1]
                    stageB(r1, W2[pb1], (i - 1) % 2)
            r1, pb1, _ = subs[-1]
            stageB(r1, W2[pb1], (len(subs) - 1) % 2)
            kb.barrier()
        with ExitStack() as p5:
            qsb = lambda name, shape, dt=F32: p5.enter_context(nc.sbuf_tensor(uq(name), shape, dt))
            qps = lambda name, shape, dt=F32: p5.enter_context(nc.psum_tensor(uq(name), shape, dt))
            y1 = [qsb(f"ms_y1_{i}", [128, D]) for i in range(2)]
            y2 = [qsb(f"ms_y2_{i}", [128, D]) for i in range(2)]
            tq = [qps(f"ms_cq{i}", [128, 512]) for i in range(4)]
            for i, tt in enumerate(tiles):
                pb = i % 2
                s_ = 1 if tt * 128 < NCTX else 0
                kb.idma(y1[pb][:], None, ys_d, bass.IndirectOffsetOnAxis(ap=D1i[:, i:i + 1], axis=0), reads=["ys", D1i], writes=[y1[pb]])
                kb.idma(y2[pb][:], None, ys_d, bass.IndirectOffsetOnAxis(ap=D2i[:, i:i + 1], axis=0), reads=["ys", D2i], writes=[y2[pb]])
                kb.act(y1[pb][:], y1[pb][:], AF.Identity, scale=GA[:, i:i + 1])
                kb.stt("dve", y1[pb][:], y2[pb][:], GB[:, i:i + 1], y1[pb][:], ALU.mult, ALU.add)
                for half in range(2):
                    p = tq[(2 * i + half) % 4]
                    for j in range(4):
                        k = half * 4 + j
                        kb.tr(p[:, j * 128:(j + 1) * 128], y1[pb][:, k * 128:(k + 1) * 128], ident)
                    for j in range(4):
                        k = half * 4 + j
                        kb.stt("dve", xT[:, k, tt * 128:(tt + 1) * 128], p[:, j * 128:(j + 1) * 128], modT[:, L, 40 + k, s_:s_ + 1],
                               xT[:, k, tt * 128:(tt + 1) * 128], ALU.mult, ALU.add)
            kb.barrier()


def phase_final(G):
    nc, kb = G["nc"], G["kb"]
    xT, onesr, ident, Vn = G["xT"], G["onesr"], G["ident"], G["Vn"]
    out_d = G["out_d"]
    with ExitStack() as ph:
        psb = lambda name, shape, dt=F32: ph.enter_context(nc.sbuf_tensor(uq(name), shape, dt))
        pps = lambda name, shape, dt=F32: ph.enter_context(nc.psum_tensor(uq(name), shape, dt))
        fo = psb("fn_fo", [128, 8, NLAT])
        emit_norm(nc, kb, xT, fo, [(t0, n, t0 - NCTX) for (t0, n) in CHUNKS[1:]],
                  lambda k, s_: Vn("final_g")[:, k:k + 1], None, onesr, D)
        ost = [psb(f"fn_ost{i}", [128, D]) for i in range(2)]
        tp = [pps(f"fn_tp{i}", [128, 512]) for i in range(4)]
        for tt in range(NLAT // 128):
            o = ost[tt % 2]
            for half in range(2):
                p = tp[(2 * tt + half) % 4]
                for j in range(4):
                    k = half * 4 + j
                    kb.tr(p[:, j * 128:(j + 1) * 128], fo[:, k, tt * 128:(tt + 1) * 128], ident)
                if half == 0:
                    kb.cp("dve", o[:, 0:512], p[:])
                else:
                    kb.cp("act", o[:, 512:1024], p[:])
            kb.dma("sp", out_d[tt * 128:(tt + 1) * 128, :], o[:], reads=[o], writes=["out"])
        kb.barrier()


def host_prep(inputs):
    f = lambda a: np.ascontiguousarray(np.asarray(a, dtype=np.float32))
    w_in = f(inputs["w_in"])
    perm = np.concatenate([np.arange(8, 16), np.arange(0, 8), np.arange(24, 32), np.arange(16, 24)]) + 640
    w_in_x = np.concatenate([w_in, w_in[:, :, 576:672], w_in[:, :, 576:640], w_in[:, :, perm]], axis=2)
    consts = np.concatenate([np.eye(128, dtype=np.float32), np.ones((128, 128), np.float32)], axis=1)
    shared = {"consts": consts, "mod_w": f(inputs["mod_w"]), "w_in_x": np.ascontiguousarray(w_in_x)}
    s5B = np.zeros((DEPTH, 2, 2, 8, 128, 128), np.float32)
    s5C = np.zeros((DEPTH, 2, 2, 8, 128, 128), np.float32)
    for ri, (bn, cn) in enumerate((("s5_b_re", "s5_c_re"), ("s5_b_im", "s5_c_im"))):
        bb = f(inputs[bn])
        cc = f(inputs[cn])
        for g in range(16):
            s_ = g // 2
            ct = s_ // 4
            q0 = (g - 2 * s_) * 64
            c0 = (g - 8 * ct) * 16
            s5B[:, :, ri, s_, c0:c0 + 16, q0:q0 + 64] = bb[:, :, g].transpose(0, 1, 3, 2)
            s5C[:, :, ri, s_, q0:q0 + 64, c0:c0 + 16] = cc[:, :, g].transpose(0, 1, 3, 2)
    shared["s5B"] = s5B
    shared["s5C"] = s5C
    shared["s5_glu_w"] = f(inputs["s5_glu_w"])
    rows = NLAT // 64
    row = np.repeat(np.arange(rows, dtype=np.float32), 64)
    col = np.tile(np.arange(64, dtype=np.float32), rows)
    inv = (np.float32(10000.0) ** (-np.arange(8, dtype=np.float32) / np.float32(8))).astype(np.float32)
    rope = np.zeros((128, 2, NLAT), np.float32)
    for r in range(32):
        i, axis, half = r % 8, r // 16, (r // 8) % 2
        ang = ((row if axis == 0 else col) * inv[i]).astype(np.float32)
        rope[64 + r, 0] = np.cos(ang)
        rope[64 + r, 1] = np.sin(ang) * (-1.0 if half == 0 else 1.0)
    shared["rope"] = rope
    wuq = f(inputs["mla_w_uq"]).reshape(DEPTH, 256, 8, 96)
    pperm = np.concatenate([np.arange(64), 64 + np.concatenate([np.arange(8, 16), np.arange(0, 8), np.arange(24, 32), np.arange(16, 24)])])
    shared["wq"] = np.ascontiguousarray(np.stack([wuq, wuq[:, :, :, pperm]], axis=2).reshape(DEPTH, 256, 2, 768))
    shared["mla_w_uk"] = f(inputs["mla_w_uk"])
    shared["hg_lb_logits"] = f(inputs["hg_lb_logits"]).reshape(1, DEPTH)
    shared["wp"] = np.ascontiguousarray(np.concatenate([f(inputs["w_pa"]), f(inputs["w_pb"]), f(inputs["w_pc"])], axis=1))
    shared["w_out"] = f(inputs["w_out"])
    shared["wr"] = np.ascontiguousarray(np.concatenate([f(inputs["moe_w_group"]), f(inputs["moe_w_expert"])], axis=2))
    shared["br"] = np.ascontiguousarray(np.concatenate([f(inputs["moe_b_group"]), f(inputs["moe_b_expert"])], axis=1).reshape(DEPTH, 1, 36))
    for nm_, kt in (("moe_w1", 8), ("moe_w3", 8), ("moe_w2", 4)):
        w_ = f(inputs[nm_])
        cols = w_.shape[-1]
        shared[nm_] = np.ascontiguousarray(w_.reshape(DEPTH, 32, kt, 128, cols).transpose(0, 1, 3, 2, 4)).reshape(DEPTH, 32 * 128, kt * cols)
    shared["mla_w_uv"] = f(inputs["mla_w_uv"])
    in_maps = []
    for b in range(8):
        vecs = np.zeros((NVBLK * 128, 128), np.float32)

        def put(name, arr):
            r0, n = VEC_ROWS[name]
            vecs[r0:r0 + n, :] = np.asarray(arr, np.float32).reshape(n, 128)
        put("c", inputs["c"][b]); put("c_ctx", inputs["c_ctx"]); put("final_g", inputs["final_norm_g"])
        for l in range(DEPTH):
            put(f"norm1_g{l}", inputs["norm1_g"][l]); put(f"norm2_g{l}", inputs["norm2_g"][l])
            put(f"mod_b{l}", inputs["mod_b"][l]); put(f"s5_d{l}", inputs["s5_d"][l])
            put(f"glu_b{l}", inputs["s5_glu_b"][l]); put(f"qa_g{l}", inputs["mla_qa_g"][l])
            put(f"kva_g{l}", inputs["mla_kva_g"][l])
            hg = np.zeros((4, 128), np.float32)
            hg[:, 0:64] = np.asarray(inputs["hg_norm_g"][l], np.float32).reshape(4, 64)
            put(f"hgn_g{l}", hg)
            for d in range(2):
                put(f"lam_re{l}{d}", inputs["s5_lam_re"][l, d]); put(f"lam_im{l}{d}", inputs["s5_lam_im"][l, d])
                put(f"lstep{l}{d}", np.repeat(np.asarray(inputs["s5_log_step"][l, d], np.float32), 64))
        m = dict(shared)
        m["x"] = f(inputs["x"][b]); m["ctx"] = f(inputs["ctx"][b]); m["vecs"] = vecs
        in_maps.append(m)
    return in_maps


def kernel(**inputs):
    in_maps = host_prep(inputs)
    nc = build_nc()
    res = run_bass_kernel_spmd(nc, in_maps, core_ids=list(range(8)))
    return np.stack([r["out"] for r in res.results], axis=0)
```
